# Optimizing a Trainium2 kernel written in Bass

```python
import jax
import jax.numpy as jnp
from jax import lax
import numpy as np

D_MODEL = 1024
BATCH = 8
SEQ = 4096
DEPTH = 2

GRID_W = 64
CTX_LEN = 256
F32 = jnp.float32
EPS = 1e-6
NEG_INF = -1e30
HEAD_DIM = 64
ROPE_THETA = 10000.0

RET_HEADS = 4
RET_DK = 64
RET_DV = 128
RET_CHUNK = 128
WIN_HEADS = 8
WIN_KV_HEADS = 2
WINDOW = 128
WIN_BLOCK = 128
NA_HEADS = 8
NA_KH = 8
NA_KW = 16
NA_QCOLS = 16
NA_BAND = NA_QCOLS + NA_KW
GDN_HEADS = 4
GDN_DK = 128
GDN_DV = 128
GDN_CHUNK = 64
SHORT_CONV = 3
N_BRANCH = 4
BRANCH_W = 512
N_EXPERTS = 32
N_GROUPS = 8
EXPERTS_PER_GROUP = N_EXPERTS // N_GROUPS
TOP_K = 2
D_EXPERT = 512
MOE_BLOCK = 128

IN_LAYOUT = (
    ('ret_q', RET_HEADS * RET_DK), ('ret_k', RET_HEADS * RET_DK),
    ('ret_v', RET_HEADS * RET_DV), ('ret_g', RET_HEADS * RET_DV),
    ('win_q', WIN_HEADS * HEAD_DIM), ('win_k', WIN_KV_HEADS * HEAD_DIM), ('win_v', WIN_KV_HEADS * HEAD_DIM),
    ('na_q', NA_HEADS * HEAD_DIM), ('na_k', NA_HEADS * HEAD_DIM), ('na_v', NA_HEADS * HEAD_DIM),
    ('gdn_q', GDN_HEADS * GDN_DK), ('gdn_k', GDN_HEADS * GDN_DK), ('gdn_v', GDN_HEADS * GDN_DV),
    ('gdn_g', GDN_HEADS * GDN_DV), ('gdn_a', 2 * GDN_HEADS), ('gdn_b', 2 * GDN_HEADS),
)
IN_NAMES = tuple(n for n, _ in IN_LAYOUT)
IN_OFFSETS = tuple(sum(w for _, w in IN_LAYOUT[:i + 1]) for i in range(len(IN_LAYOUT) - 1))
D_IN = sum(w for _, w in IN_LAYOUT)
GDN_QKV = 2 * GDN_HEADS * GDN_DK + GDN_HEADS * GDN_DV

kernel_name = 'hybrid_prefix_dit_retention_window_natten_gdn_moe'


def rmsnorm(x, gain):
    xf = x.astype(F32)
    y = xf * lax.rsqrt(jnp.mean(xf * xf, axis=-1, keepdims=True) + EPS)
    return (y * gain.astype(F32)).astype(x.dtype)


def l2norm(x):
    return x * lax.rsqrt(jnp.sum(x * x, axis=-1, keepdims=True) + EPS)


def modulate(x, gain, shift, scale):
    return rmsnorm(x, gain) * (1.0 + scale) + shift


def to_heads(z, n_heads):
    b, l, _ = z.shape
    return z.reshape(b, l, n_heads, -1).transpose(0, 2, 1, 3)


def from_heads(z):
    b, h, l, d = z.shape
    return z.transpose(0, 2, 1, 3).reshape(b, l, h * d)


def flip_seq(a):
    return jnp.flip(a, axis=2)


def split_projection(z):
    return dict(zip(IN_NAMES, jnp.split(z, IN_OFFSETS, axis=-1)))


def axial_rope_tables(n_tokens, dim):
    t = jnp.arange(n_tokens)
    row = (t // GRID_W).astype(F32)
    col = (t % GRID_W).astype(F32)
    n_freq = dim // 4
    inv = ROPE_THETA ** (-jnp.arange(n_freq, dtype=F32) / n_freq)
    ang = jnp.concatenate([row[:, None] * inv, col[:, None] * inv], axis=-1)
    return jnp.cos(ang), jnp.sin(ang)


def apply_rope(x, cos, sin):
    xf = x.astype(F32)
    x1, x2 = jnp.split(xf, 2, axis=-1)
    return jnp.concatenate([x1 * cos - x2 * sin, x1 * sin + x2 * cos], axis=-1).astype(x.dtype)


def sink_softmax(s, sink):
    m = jnp.maximum(jnp.max(s, axis=-1, keepdims=True), sink)
    p = jnp.exp(s - m)
    return p / (jnp.sum(p, axis=-1, keepdims=True) + jnp.exp(sink - m))


def context_attention(q, k, v, sink):
    b, hq, lc, d = q.shape
    g = k.shape[1]
    qg = q.reshape(b, g, hq // g, lc, d)
    s = jnp.einsum('bgrqd,bgkd->bgrqk', qg, k).astype(F32) * d ** -0.5
    if sink is None:
        p = jax.nn.softmax(s, axis=-1)
    else:
        p = sink_softmax(s, sink.astype(F32).reshape(g, -1)[None, :, :, None, None])
    o = jnp.einsum('bgrqk,bgkd->bgrqd', p.astype(v.dtype), v)
    return o.reshape(b, hq, lc, d)


def retention_chunked(q, k, v, log_gamma, s0):
    b, h, l, dk = q.shape
    dv = v.shape[-1]
    c = RET_CHUNK
    n = l // c
    qc = q.reshape(b, h, n, c, dk)
    kc = k.reshape(b, h, n, c, dk)
    vc = v.reshape(b, h, n, c, dv)
    pos = jnp.arange(c, dtype=F32)
    diff = pos[:, None] - pos[None, :]
    dmask = jnp.where(diff >= 0, jnp.exp(log_gamma[:, None, None] * jnp.maximum(diff, 0.0)), 0.0)
    q_decay = jnp.exp(log_gamma[:, None] * (pos + 1.0))
    k_decay = jnp.exp(log_gamma[:, None] * (c - 1.0 - pos))
    chunk_decay = jnp.exp(log_gamma * c)
    scores = jnp.einsum('bhncd,bhnsd->bhncs', qc, kc) * dmask[None, :, None]
    o_intra = jnp.einsum('bhncs,bhnse->bhnce', scores, vc)
    kv = jnp.einsum('bhncd,bhnce->bhnde', kc * k_decay[None, :, None, :, None], vc)

    def step(s, kv_n):
        return chunk_decay[None, :, None, None] * s + kv_n, s

    s_fin, s_prev = lax.scan(step, s0, jnp.moveaxis(kv, 2, 0))
    s_prev = jnp.moveaxis(s_prev, 0, 2)
    o_inter = jnp.einsum('bhncd,bhnde->bhnce', qc * q_decay[None, :, None, :, None], s_prev)
    return (o_intra + o_inter).reshape(b, h, l, dv), s_fin


def retention_state(k, v, log_gamma):
    l = k.shape[2]
    w = jnp.exp(log_gamma[:, None] * (l - 1.0 - jnp.arange(l, dtype=F32)))
    return jnp.einsum('bhld,bhle->bhde', k * w[None, :, :, None], v)


def retention_output(o, gate, gain):
    mu = jnp.mean(o, axis=-1, keepdims=True)
    var = jnp.mean(jnp.square(o - mu), axis=-1, keepdims=True)
    y = from_heads((o - mu) * lax.rsqrt(var + EPS)) * gain.astype(F32)
    return (y * jax.nn.silu(gate.astype(F32))).astype(gate.dtype)


def retention_branch(zl, zc, decay_logit, gn_gain, cos, sin, ctx_out):
    log_gamma = jax.nn.log_sigmoid(decay_logit.astype(F32))

    def qkv(z):
        q = to_heads(z['ret_q'], RET_HEADS).astype(F32)
        k = to_heads(z['ret_k'], RET_HEADS).astype(F32) * RET_DK ** -0.5
        v = to_heads(z['ret_v'], RET_HEADS).astype(F32)
        return q, k, v

    qc, kc, vc = qkv(zc)
    ql, kl, vl = qkv(zl)
    ql = apply_rope(ql, cos, sin)
    kl = apply_rope(kl, cos, sin)
    s0 = jnp.zeros((ql.shape[0], RET_HEADS, RET_DK, RET_DV), F32)
    yc = None
    if ctx_out:
        oc_f, sc_f = retention_chunked(qc, kc, vc, log_gamma[0], s0)
        oc_b, sc_b = retention_chunked(flip_seq(qc), flip_seq(kc), flip_seq(vc), log_gamma[1], s0)
        yc = retention_output(oc_f + flip_seq(oc_b), zc['ret_g'], gn_gain)
    else:
        sc_f = retention_state(kc, vc, log_gamma[0])
        sc_b = retention_state(flip_seq(kc), flip_seq(vc), log_gamma[1])
    ol_f, _ = retention_chunked(ql, kl, vl, log_gamma[0], sc_f)
    ol_b, _ = retention_chunked(flip_seq(ql), flip_seq(kl), flip_seq(vl), log_gamma[1], sc_b)
    yl = retention_output(ol_f + flip_seq(ol_b), zl['ret_g'], gn_gain)
    return yc, yl


def window_attention(q, k, v, kc, vc, sink):
    b, hq, l, d = q.shape
    g = k.shape[1]
    r = hq // g
    nb = l // WIN_BLOCK
    span = WIN_BLOCK + 2 * WINDOW
    qg = (q * d ** -0.5).reshape(b, g, r, l, d)
    kp = jnp.pad(k, ((0, 0), (0, 0), (WINDOW, WINDOW), (0, 0)))
    vp = jnp.pad(v, ((0, 0), (0, 0), (WINDOW, WINDOW), (0, 0)))
    sink_b = sink.astype(F32).reshape(g, r)[None, :, :, None, None]

    def block(n):
        s0 = n * WIN_BLOCK
        qb = lax.dynamic_slice_in_dim(qg, s0, WIN_BLOCK, axis=3)
        kb = lax.dynamic_slice_in_dim(kp, s0, span, axis=2)
        vb = lax.dynamic_slice_in_dim(vp, s0, span, axis=2)
        qpos = s0 + jnp.arange(WIN_BLOCK)
        kpos = s0 - WINDOW + jnp.arange(span)
        ok = (kpos[None, :] >= 0) & (kpos[None, :] < l) & (jnp.abs(qpos[:, None] - kpos[None, :]) <= WINDOW)
        s_loc = jnp.where(ok, jnp.einsum('bgrqd,bgkd->bgrqk', qb, kb).astype(F32), NEG_INF)
        s_ctx = jnp.einsum('bgrqd,bgkd->bgrqk', qb, kc).astype(F32)
        p = sink_softmax(jnp.concatenate([s_loc, s_ctx], axis=-1), sink_b).astype(v.dtype)
        return (jnp.einsum('bgrqk,bgkd->bgrqd', p[..., :span], vb)
                + jnp.einsum('bgrqk,bgkd->bgrqd', p[..., span:], vc))

    out = lax.map(block, jnp.arange(nb))
    return jnp.moveaxis(out, 0, 3).reshape(b, hq, l, d)


def window_branch(zl, zc, q_gain, k_gain, sink, cos, sin, ctx_out):
    def qkv(z):
        q = rmsnorm(to_heads(z['win_q'], WIN_HEADS), q_gain)
        k = rmsnorm(to_heads(z['win_k'], WIN_KV_HEADS), k_gain)
        v = to_heads(z['win_v'], WIN_KV_HEADS)
        return q, k, v

    qc, kc, vc = qkv(zc)
    ql, kl, vl = qkv(zl)
    ql = apply_rope(ql, cos, sin)
    kl = apply_rope(kl, cos, sin)
    yl = from_heads(window_attention(ql, kl, vl, kc, vc, sink))
    yc = from_heads(context_attention(qc, kc, vc, sink)) if ctx_out else None
    return yc, yl


def neighborhood_attention(q, k, v, kc, vc, rpb):
    b, h, l, d = q.shape
    rows = l // GRID_W
    kh = min(NA_KH, rows)
    n_cb = GRID_W // NA_QCOLS
    qcol = np.arange(GRID_W).reshape(n_cb, NA_QCOLS)
    band0 = np.clip(qcol[:, 0] - NA_KW // 2, 0, GRID_W - NA_BAND)
    kcol = band0[:, None] + np.arange(NA_BAND)
    cstart = np.clip(qcol - NA_KW // 2, 0, GRID_W - NA_KW)
    col_ok = (kcol[:, None, :] >= cstart[:, :, None]) & (kcol[:, None, :] < cstart[:, :, None] + NA_KW)
    col_off = np.clip(kcol[:, None, :] - qcol[:, :, None] + NA_KW - 1, 0, 2 * NA_KW - 2)
    rstart = np.clip(np.arange(rows) - kh // 2, 0, rows - kh)
    row_off = rstart[:, None] + np.arange(kh)[None, :] - np.arange(rows)[:, None] + NA_KH - 1
    bias = rpb.astype(F32)[:, row_off[:, None, None, :, None], col_off[None, :, :, None, :]]
    bias = jnp.where(col_ok[None, None, :, :, None, :], bias, NEG_INF)
    bias = jnp.moveaxis(bias, 1, 0).reshape(rows, h, n_cb, NA_QCOLS, kh * NA_BAND)
    qg = (q * d ** -0.5).reshape(b, h, rows, n_cb, NA_QCOLS, d)
    kg = k.reshape(b, h, rows, GRID_W, d)
    vg = v.reshape(b, h, rows, GRID_W, d)

    def row_step(args):
        r, rs, bias_r = args
        qr = lax.dynamic_index_in_dim(qg, r, axis=2, keepdims=False)
        kr = lax.dynamic_slice_in_dim(kg, rs, kh, axis=2)[:, :, :, kcol]
        vr = lax.dynamic_slice_in_dim(vg, rs, kh, axis=2)[:, :, :, kcol]
        s_loc = jnp.einsum('bhcqd,bhicjd->bhcqij', qr, kr).astype(F32)
        s_loc = s_loc.reshape(b, h, n_cb, NA_QCOLS, kh * NA_BAND) + bias_r
        s_ctx = jnp.einsum('bhcqd,bhkd->bhcqk', qr, kc).astype(F32)
        p = jax.nn.softmax(jnp.concatenate([s_loc, s_ctx], axis=-1), axis=-1).astype(v.dtype)
        p_loc = p[..., :kh * NA_BAND].reshape(b, h, n_cb, NA_QCOLS, kh, NA_BAND)
        o = (jnp.einsum('bhcqij,bhicjd->bhcqd', p_loc, vr)
             + jnp.einsum('bhcqk,bhkd->bhcqd', p[..., kh * NA_BAND:], vc))
        return o.reshape(b, h, GRID_W, d)

    out = lax.map(row_step, (jnp.arange(rows, dtype=jnp.int32), jnp.asarray(rstart, jnp.int32), bias))
    return jnp.moveaxis(out, 0, 2).reshape(b, h, l, d)


def neighborhood_branch(zl, zc, q_gain, k_gain, rpb, ctx_out):
    def qkv(z):
        q = rmsnorm(to_heads(z['na_q'], NA_HEADS), q_gain)
        k = rmsnorm(to_heads(z['na_k'], NA_HEADS), k_gain)
        v = to_heads(z['na_v'], NA_HEADS)
        return q, k, v

    qc, kc, vc = qkv(zc)
    ql, kl, vl = qkv(zl)
    yl = from_heads(neighborhood_attention(ql, kl, vl, kc, vc, rpb))
    yc = from_heads(context_attention(qc, kc, vc, None)) if ctx_out else None
    return yc, yl


def short_conv(z, w):
    taps, l = w.shape[0], z.shape[1]
    pad = taps // 2
    zp = jnp.pad(z, ((0, 0), (pad, pad), (0, 0)))
    return sum(zp[:, j:j + l] * w[j] for j in range(taps))


def gated_delta_chunked(q, k, v, g, beta, s0, with_out):
    b, h, l, dk = k.shape
    dv = v.shape[-1]
    c = GDN_CHUNK
    n = l // c
    kc = k.reshape(b, h, n, c, dk)
    vc = v.reshape(b, h, n, c, dv)
    gcum = jnp.cumsum(g.reshape(b, h, n, c), axis=-1)
    bc = beta.reshape(b, h, n, c)
    incl = jnp.asarray(np.tril(np.ones((c, c), bool)))
    strict = jnp.asarray(np.tril(np.ones((c, c), bool), -1))
    gdiff = gcum[..., :, None] - gcum[..., None, :]
    decay = jnp.where(incl, jnp.exp(jnp.where(incl, gdiff, 0.0)), 0.0)
    kb = kc * bc[..., None]
    a_low = jnp.where(strict, jnp.einsum('bhnid,bhnjd->bhnij', kb, kc) * decay, 0.0)
    rhs = jnp.concatenate([vc * bc[..., None], kb * jnp.exp(gcum)[..., None]], axis=-1)
    sol = lax.linalg.triangular_solve(a_low + jnp.eye(c, dtype=F32), rhs, left_side=True, lower=True)
    u, w = sol[..., :dv], sol[..., dv:]
    k_end = kc * jnp.exp(gcum[..., -1:] - gcum)[..., None]
    c_decay = jnp.exp(gcum[..., -1])

    def mv(a):
        return jnp.moveaxis(a, 2, 0)

    if with_out:
        qc = q.reshape(b, h, n, c, dk)
        q_dec = qc * jnp.exp(gcum)[..., None]
        a_qk = jnp.where(incl, jnp.einsum('bhnid,bhnjd->bhnij', qc, kc) * decay, 0.0)

        def step(s, xs):
            qd, ke, u_n, w_n, aq, cd = xs
            v_new = u_n - jnp.einsum('bhcd,bhde->bhce', w_n, s)
            o = jnp.einsum('bhcd,bhde->bhce', qd, s) + jnp.einsum('bhcs,bhse->bhce', aq, v_new)
            s = s * cd[..., None, None] + jnp.einsum('bhcd,bhce->bhde', ke, v_new)
            return s, o

        s_fin, o = lax.scan(step, s0, (mv(q_dec), mv(k_end), mv(u), mv(w), mv(a_qk), mv(c_decay)))
        return jnp.moveaxis(o, 0, 2).reshape(b, h, l, dv), s_fin

    def step_state(s, xs):
        ke, u_n, w_n, cd = xs
        v_new = u_n - jnp.einsum('bhcd,bhde->bhce', w_n, s)
        return s * cd[..., None, None] + jnp.einsum('bhcd,bhce->bhde', ke, v_new), None

    s_fin, _ = lax.scan(step_state, s0, (mv(k_end), mv(u), mv(w), mv(c_decay)))
    return None, s_fin


def gdn_output(o, gate, gain):
    y = o * lax.rsqrt(jnp.mean(o * o, axis=-1, keepdims=True) + EPS) * gain.astype(F32)
    return (from_heads(y) * jax.nn.silu(gate.astype(F32))).astype(gate.dtype)


def gdn_branch(zl, zc, conv_w, a_log, dt_bias, norm_gain, ctx_out):
    def prep(z):
        b, l, _ = z['gdn_q'].shape
        qkv = jnp.concatenate([z['gdn_q'], z['gdn_k'], z['gdn_v']], axis=-1)
        qkv = jax.nn.silu(short_conv(qkv, conv_w)).astype(F32)
        q, k, v = jnp.split(qkv, [GDN_HEADS * GDN_DK, 2 * GDN_HEADS * GDN_DK], axis=-1)
        q = l2norm(to_heads(q, GDN_HEADS)) * GDN_DK ** -0.5
        k = l2norm(to_heads(k, GDN_HEADS))
        v = to_heads(v, GDN_HEADS)
        a = z['gdn_a'].astype(F32).reshape(b, l, 2, GDN_HEADS)
        bb = z['gdn_b'].astype(F32).reshape(b, l, 2, GDN_HEADS)
        g = -jnp.exp(a_log.astype(F32)) * jax.nn.softplus(a + dt_bias.astype(F32))
        beta = jax.nn.sigmoid(bb)
        return q, k, v, jnp.transpose(g, (2, 0, 3, 1)), jnp.transpose(beta, (2, 0, 3, 1))

    qc, kc, vc, gc, bc = prep(zc)
    ql, kl, vl, gl, bl = prep(zl)
    s0 = jnp.zeros((ql.shape[0], GDN_HEADS, GDN_DK, GDN_DV), F32)
    oc_f, sc_f = gated_delta_chunked(qc, kc, vc, gc[0], bc[0], s0, ctx_out)
    oc_b, sc_b = gated_delta_chunked(flip_seq(qc), flip_seq(kc), flip_seq(vc),
                                     flip_seq(gc[1]), flip_seq(bc[1]), s0, ctx_out)
    ol_f, _ = gated_delta_chunked(ql, kl, vl, gl[0], bl[0], sc_f, True)
    ol_b, _ = gated_delta_chunked(flip_seq(ql), flip_seq(kl), flip_seq(vl),
                                  flip_seq(gl[1]), flip_seq(bl[1]), sc_b, True)
    yl = gdn_output(ol_f + flip_seq(ol_b), zl['gdn_g'], norm_gain)
    yc = gdn_output(oc_f + flip_seq(oc_b), zc['gdn_g'], norm_gain) if ctx_out else None
    return yc, yl


def merge_branches(h, ys, w_merge, w_branch, w_out):
    acc = 0.0
    for i, y in enumerate(ys):
        gate = jax.nn.sigmoid(h @ w_merge[:, i * D_MODEL:(i + 1) * D_MODEL])
        acc = acc + gate * (y @ w_branch[i])
    return acc @ w_out


def moe(h, w_router, router_bias, w_gate, w_up, w_down):
    t, d = h.shape
    scores = jax.nn.sigmoid((h @ w_router).astype(F32))
    sel = (scores + router_bias.astype(F32)).reshape(t, N_GROUPS, EXPERTS_PER_GROUP)
    grp_score = jnp.sum(lax.top_k(sel, 2)[0], axis=-1)
    g_idx = jnp.argmax(grp_score, axis=-1)
    in_grp = jnp.take_along_axis(sel, g_idx[:, None, None], axis=1)[:, 0]
    _, local = lax.top_k(in_grp, TOP_K)
    e_idx = g_idx[:, None] * EXPERTS_PER_GROUP + local
    wts = jnp.take_along_axis(scores, e_idx, axis=1)
    wts = wts / jnp.sum(wts, axis=-1, keepdims=True)
    a = t * TOP_K
    flat_e = e_idx.reshape(-1)
    flat_tok = jnp.repeat(jnp.arange(t, dtype=jnp.int32), TOP_K)
    flat_w = wts.reshape(-1).astype(h.dtype)
    order = jnp.argsort(flat_e)
    se, stok, sw = flat_e[order], flat_tok[order], flat_w[order]
    counts = jnp.bincount(flat_e, length=N_EXPERTS)
    starts = jnp.cumsum(counts) - counts
    padded = (counts + MOE_BLOCK - 1) // MOE_BLOCK * MOE_BLOCK
    ends = jnp.cumsum(padded)
    pstarts = ends - padded
    dest = pstarts[se] + jnp.arange(a) - starts[se]
    n_blocks = -(-a // MOE_BLOCK) + N_EXPERTS
    size = n_blocks * MOE_BLOCK
    tok_buf = jnp.full((size,), t, jnp.int32).at[dest].set(stok)
    w_buf = jnp.zeros((size,), h.dtype).at[dest].set(sw)
    block_e = jnp.minimum(jnp.searchsorted(ends, jnp.arange(n_blocks) * MOE_BLOCK, side='right'), N_EXPERTS - 1)
    h_pad = jnp.concatenate([h, jnp.zeros((1, d), h.dtype)], axis=0)
    xb = h_pad[tok_buf].reshape(n_blocks, MOE_BLOCK, d)

    def run(args):
        xe, e = args
        return (jax.nn.silu(xe @ w_gate[e]) * (xe @ w_up[e])) @ w_down[e]

    yb = lax.map(run, (xb, block_e)).reshape(size, d)
    out = jnp.zeros((t + 1, d), h.dtype).at[tok_buf].add(yb * w_buf[:, None])
    return out[:t]


def trunk_layer(xl, xc, c, c_ctx, p, w_router, router_bias, cos, sin, ctx_out):
    mod_l = (jax.nn.silu(c) @ p['w_mod'] + p['b_mod'])[:, None, :]
    mod_c = (jax.nn.silu(c_ctx) @ p['w_mod'] + p['b_mod'])[None, None, :]
    sh1l, sc1l, g1l, sh2l, sc2l, g2l = jnp.split(mod_l, 6, axis=-1)
    sh1c, sc1c, g1c, sh2c, sc2c, g2c = jnp.split(mod_c, 6, axis=-1)
    hl = modulate(xl, p['norm1'], sh1l, sc1l)
    hc = modulate(xc, p['norm1'], sh1c, sc1c)
    zl = split_projection(hl @ p['w_in'])
    zc = split_projection(hc @ p['w_in'])
    ret_c, ret_l = retention_branch(zl, zc, p['ret_decay'], p['ret_gn'], cos, sin, ctx_out)
    win_c, win_l = window_branch(zl, zc, p['win_qnorm'], p['win_knorm'], p['win_sink'], cos, sin, ctx_out)
    na_c, na_l = neighborhood_branch(zl, zc, p['na_qnorm'], p['na_knorm'], p['na_rpb'], ctx_out)
    gdn_c, gdn_l = gdn_branch(zl, zc, p['gdn_conv'], p['gdn_a_log'], p['gdn_dt_bias'], p['gdn_norm'], ctx_out)
    xl = xl + g1l * merge_branches(hl, (ret_l, win_l, na_l, gdn_l), p['w_merge'], p['w_branch'], p['w_out'])
    hl2 = modulate(xl, p['norm2'], sh2l, sc2l)
    b, l, d = xl.shape
    if not ctx_out:
        y = moe(hl2.reshape(b * l, d), w_router, router_bias, p['w_e_gate'], p['w_e_up'], p['w_e_down'])
        return xl + g2l * y.reshape(b, l, d), xc
    xc = xc + g1c * merge_branches(hc, (ret_c, win_c, na_c, gdn_c), p['w_merge'], p['w_branch'], p['w_out'])
    hc2 = modulate(xc, p['norm2'], sh2c, sc2c)
    lc = xc.shape[1]
    tokens = jnp.concatenate([hc2.reshape(b * lc, d), hl2.reshape(b * l, d)], axis=0)
    y = moe(tokens, w_router, router_bias, p['w_e_gate'], p['w_e_up'], p['w_e_down'])
    xc = xc + g2c * y[:b * lc].reshape(b, lc, d)
    xl = xl + g2l * y[b * lc:].reshape(b, l, d)
    return xl, xc


def setup_inputs(seed: int = 0) -> dict:
    key = jax.random.key(seed)
    ks = iter(jax.random.split(key, 40))
    D = D_MODEL

    def nrm(shape, scale):
        return scale * jax.random.normal(next(ks), shape, F32)

    def unif(shape, lo, hi):
        return jax.random.uniform(next(ks), shape, F32, lo, hi)

    gamma0 = 1.0 - 2.0 ** (-5.0 - jnp.arange(RET_HEADS, dtype=F32))
    dt = jnp.exp(unif((DEPTH, 2, GDN_HEADS), float(np.log(1e-3)), float(np.log(1e-1))))
    return {
        'x': nrm((BATCH, SEQ, D), 1.0),
        'c': nrm((BATCH, D), 1.0),
        'ctx': nrm((BATCH, CTX_LEN, D), 1.0),
        'c_ctx': nrm((D,), 1.0),
        'w_mod': nrm((DEPTH, D, 6 * D), 0.5 * D ** -0.5),
        'b_mod': nrm((DEPTH, 6 * D), 0.01),
        'norm1': 1.0 + nrm((DEPTH, D), 0.02),
        'norm2': 1.0 + nrm((DEPTH, D), 0.02),
        'w_in': nrm((DEPTH, D, D_IN), D ** -0.5),
        'ret_decay': jnp.log(gamma0 / (1.0 - gamma0))[None, None, :] + nrm((DEPTH, 2, RET_HEADS), 0.1),
        'ret_gn': 1.0 + nrm((DEPTH, RET_HEADS * RET_DV), 0.02),
        'win_qnorm': 1.0 + nrm((DEPTH, HEAD_DIM), 0.02),
        'win_knorm': 1.0 + nrm((DEPTH, HEAD_DIM), 0.02),
        'win_sink': nrm((DEPTH, WIN_HEADS), 0.5),
        'na_qnorm': 1.0 + nrm((DEPTH, HEAD_DIM), 0.02),
        'na_knorm': 1.0 + nrm((DEPTH, HEAD_DIM), 0.02),
        'na_rpb': nrm((DEPTH, NA_HEADS, 2 * NA_KH - 1, 2 * NA_KW - 1), 0.02),
        'gdn_conv': nrm((DEPTH, SHORT_CONV, GDN_QKV), SHORT_CONV ** -0.5),
        'gdn_a_log': jnp.log(unif((DEPTH, 2, GDN_HEADS), 1.0, 16.0)),
        'gdn_dt_bias': dt + jnp.log(-jnp.expm1(-dt)),
        'gdn_norm': 1.0 + nrm((DEPTH, GDN_DV), 0.02),
        'w_branch': nrm((DEPTH, N_BRANCH, BRANCH_W, D), BRANCH_W ** -0.5),
        'w_merge': nrm((DEPTH, D, N_BRANCH * D), D ** -0.5),
        'w_out': nrm((DEPTH, D, D), D ** -0.5),
        'w_router': nrm((D, N_EXPERTS), D ** -0.5),
        'router_bias': nrm((N_EXPERTS,), 0.01),
        'w_e_gate': nrm((DEPTH, N_EXPERTS, D, D_EXPERT), D ** -0.5),
        'w_e_up': nrm((DEPTH, N_EXPERTS, D, D_EXPERT), D ** -0.5),
        'w_e_down': nrm((DEPTH, N_EXPERTS, D_EXPERT, D), D_EXPERT ** -0.5),
    }


def reference(x, c, ctx, c_ctx, w_mod, b_mod, norm1, norm2, w_in, ret_decay, ret_gn, win_qnorm, win_knorm,
              win_sink, na_qnorm, na_knorm, na_rpb, gdn_conv, gdn_a_log, gdn_dt_bias, gdn_norm, w_branch,
              w_merge, w_out, w_router, router_bias, w_e_gate, w_e_up, w_e_down):
    cos, sin = axial_rope_tables(x.shape[1], HEAD_DIM)
    xl, xc = x, ctx
    for layer in range(DEPTH):
        p = {
            'w_mod': w_mod[layer], 'b_mod': b_mod[layer], 'norm1': norm1[layer], 'norm2': norm2[layer],
            'w_in': w_in[layer], 'ret_decay': ret_decay[layer], 'ret_gn': ret_gn[layer],
            'win_qnorm': win_qnorm[layer], 'win_knorm': win_knorm[layer], 'win_sink': win_sink[layer],
            'na_qnorm': na_qnorm[layer], 'na_knorm': na_knorm[layer], 'na_rpb': na_rpb[layer],
            'gdn_conv': gdn_conv[layer], 'gdn_a_log': gdn_a_log[layer], 'gdn_dt_bias': gdn_dt_bias[layer],
            'gdn_norm': gdn_norm[layer], 'w_branch': w_branch[layer], 'w_merge': w_merge[layer],
            'w_out': w_out[layer], 'w_e_gate': w_e_gate[layer], 'w_e_up': w_e_up[layer],
            'w_e_down': w_e_down[layer],
        }
        xl, xc = trunk_layer(xl, xc, c, c_ctx, p, w_router, router_bias, cos, sin, layer < DEPTH - 1)
    return xl
```

```python
from contextlib import ExitStack
import numpy as np
import concourse.bass as bass
import concourse.mybir as mybir

F32 = mybir.dt.float32
BF16 = mybir.dt.bfloat16
I32 = mybir.dt.int32
AF = mybir.ActivationFunctionType
ALU = mybir.AluOpType
AX = mybir.AxisListType

ENGS = ("pe", "dve", "act", "pool", "sp")
N_DSEM = 8
SEM_WRAP = 30000


class Buf:
    def __init__(self, t, name=""):
        self.t = t
        self.name = name
        self.w = {}
        self.r = {}

    v = None
    is_psum = False

    def __getitem__(self, idx):
        if self.v is not None:
            return self.v[idx]
        return self.t[idx]

    def ap(self):
        return self.t.ap() if hasattr(self.t, "ap") else self.t[:]


class Prog:
    def __init__(self, nc, same_engine_sync=True, direct=True):
        self.nc = nc
        self.es = ExitStack()
        self.ops = {e: [] for e in ENGS}
        self.cnt = {e: 0 for e in ENGS}
        self.seen = {e: {} for e in ENGS}
        self.sems = {}
        self.same = same_engine_sync
        self.dma_n = {e: 0 for e in ENGS}
        self.uid = 0
        self.stacks = [self.es]
        self.scope_bufs = [[]]
        self.free_deps = {}
        self.direct = direct
        self.nops = {}
        self.engobj = {"pe": nc.tensor, "dve": nc.vector, "act": nc.scalar, "pool": nc.gpsimd, "sp": nc.sync}

    def scope(self):
        prog = self

        class _S:
            def __enter__(s):
                st = ExitStack()
                prog.stacks.append(st)
                prog.scope_bufs.append([])
                return s

            def __exit__(s, *a):
                st = prog.stacks.pop()
                for b in prog.scope_bufs.pop():
                    for dd in (b.w, b.r):
                        for kk, v in dd.items():
                            prog.free_deps[kk] = max(prog.free_deps.get(kk, 0), v)
                st.close()
                return False
        return _S()

    def sem(self, key):
        if key not in self.sems:
            self.sems[key] = self.es.enter_context(self.nc.semaphore("s_%s_%s_%d" % key))
        return self.sems[key]

    def sbuf(self, shape, dt, name=None):
        self.uid += 1
        name = "%s_%d" % (name or "sb", self.uid)
        t = self.stacks[-1].enter_context(self.nc.sbuf_tensor(name, list(shape), dt))
        b = Buf(t, name)
        b.w = dict(self.free_deps)
        self.scope_bufs[-1].append(b)
        return b

    def psum(self, shape, dt=F32, name=None):
        self.uid += 1
        name = "%s_%d" % (name or "ps", self.uid)
        p, n = shape
        nb = (n * 4 + 2047) // 2048
        t = self.stacks[-1].enter_context(self.nc.psum_tensor(name, [128, nb * 512], F32))
        b = Buf(t, name)
        b.v = t[0:p, 0:n]
        b.is_psum = True
        b.w = dict(self.free_deps)
        self.scope_bufs[-1].append(b)
        return b

    def dram(self, name, shape, dt, kind="Internal"):
        t = self.nc.dram_tensor(name, list(shape), dt, kind=kind)
        return Buf(t, name)

    def _waits(self, eng, reads, writes):
        need = {}
        for b in reads:
            for k, v in b.w.items():
                need[k] = max(need.get(k, 0), v)
        for b in writes:
            for k, v in b.w.items():
                need[k] = max(need.get(k, 0), v)
            for k, v in b.r.items():
                need[k] = max(need.get(k, 0), v)
        out = []
        seen = self.seen[eng]
        for k, v in need.items():
            if k[0] == eng and k[1] != "d":
                if eng == "pe" or not self.same:
                    continue
            if seen.get(k, 0) >= v:
                continue
            seen[k] = v
            out.append((k, v))
        return out

    def op(self, eng, fn, reads=(), writes=()):
        pr = [b for b in reads if b.is_psum]
        if pr:
            writes = list(writes) + pr
        waits = self._waits(eng, reads, writes)
        self.cnt[eng] += 1
        n = self.cnt[eng]
        key = (eng, "c", (n - 1) // SEM_WRAP)
        val = (n - 1) % SEM_WRAP + 1
        self._put(eng, waits, fn, key, 1)
        for b in reads:
            b.r[key] = max(b.r.get(key, 0), val)
        for b in writes:
            b.w[key] = max(b.w.get(key, 0), val)

    def dma(self, q, out_ap, in_ap, reads=(), writes=(), **kw):
        waits = self._waits(q, reads, writes)
        n = self.dma_n[q]
        self.dma_n[q] += 1
        key = (q, "d", n % N_DSEM)
        val = 16 * (n // N_DSEM + 1)
        if val > 16 and self.seen[q].get(key, 0) < val - 16:
            self.seen[q][key] = val - 16
            waits.append((key, val - 16))

        def fn(e, out_ap=out_ap, in_ap=in_ap, kw=kw):
            return e.dma_start(out=out_ap, in_=in_ap, **kw)

        self._put(q, waits, fn, key, 16)
        for b in reads:
            b.r[key] = max(b.r.get(key, 0), val)
        for b in writes:
            b.w[key] = max(b.w.get(key, 0), val)

    def wait_all(self, eng, bufs):
        need = {}
        for b in bufs:
            for k, v in b.w.items():
                need[k] = max(need.get(k, 0), v)
        self._put(eng, list(need.items()), None, None, 0)

    def _put(self, eng, waits, fn, key, inc):
        if not self.direct:
            self.ops[eng].append((waits, fn, key, inc))
            return
        self.nops[eng] = self.nops.get(eng, 0) + 1
        e = self.engobj[eng]
        for k, v in waits:
            e.wait_ge(self.sem(k), v)
        if fn is not None:
            fn(e).then_inc(self.sem(key), inc)

    def emit(self):
        nc = self.nc
        if self.direct:
            self.es.close()
            return
        for e in ENGS:
            for waits, fn, key, inc in self.ops[e]:
                for k, v in waits:
                    self.sem(k)
                if key is not None:
                    self.sem(key)
        with nc.Block() as block:
            def run(engname):
                def body(eng):
                    for waits, fn, key, inc in self.ops[engname]:
                        for k, v in waits:
                            eng.wait_ge(self.sems[k], v)
                        if fn is not None:
                            fn(eng).then_inc(self.sems[key], inc)
                return body
            if self.ops["sp"]:
                block.sync(run("sp"))
            if self.ops["pe"]:
                block.tensor(run("pe"))
            if self.ops["dve"]:
                block.vector(run("dve"))
            if self.ops["act"]:
                block.scalar(run("act"))
            if self.ops["pool"]:
                block.gpsimd(run("pool"))
        self.es.close()


D = 1024
T = 4352
NT = T // 128
LC = 256
DIN = 5904
EPS = 1e-6


class K:
    def __init__(self, P):
        self.P = P
        self.rr = 0

    def evac_engine(self):
        self.rr += 1
        return "act" if self.rr % 2 else "dve"


def mk_consts():
    c = {}
    c["ident_f"] = np.eye(128, dtype=np.float32)
    cos, sin = rope_tables()
    c["cos"] = cos; c["sin"] = sin
    j = np.arange(128)[:, None]; i = np.arange(128)[None, :]
    c["maskPrev"] = (i <= j).astype(np.float32)
    c["maskNext"] = (j <= i).astype(np.float32)
    ret_consts(c)
    gdn_consts(c)
    return c


def load_consts(k, cd):
    P = k.P
    k.ident_f = P.sbuf([128, 128], F32, "ident_f")
    P.dma("sp", k.ident_f[:], cd["ident_f"].ap(), writes=[k.ident_f])
    k.ident_b = P.sbuf([128, 128], BF16, "ident_b")
    P.op("dve", lambda e: e.tensor_copy(k.ident_b[:], k.ident_f[:]), reads=[k.ident_f], writes=[k.ident_b])
    k.cos = P.sbuf([128, 32, 32], F32, "cos"); k.sin = P.sbuf([128, 32, 32], F32, "sin")
    P.dma("sp", k.cos[:], cd["cos"].ap().rearrange("(t p) f -> p t f", p=128), writes=[k.cos])
    P.dma("sp", k.sin[:], cd["sin"].ap().rearrange("(t p) f -> p t f", p=128), writes=[k.sin])
    k.maskPrev = P.sbuf([128, 128], BF16, "maskPrev"); k.maskNext = P.sbuf([128, 128], BF16, "maskNext")
    with P.scope():
        mst = P.sbuf([128, 2, 128], F32, "mst")
        P.dma("sp", mst[:, 0, :], cd["maskPrev"].ap(), writes=[mst])
        P.dma("sp", mst[:, 1, :], cd["maskNext"].ap(), writes=[mst])
        P.op("dve", lambda e: e.tensor_copy(k.maskPrev[:], mst[:, 0, :]), reads=[mst], writes=[k.maskPrev])
        P.op("dve", lambda e: e.tensor_copy(k.maskNext[:], mst[:, 1, :]), reads=[mst], writes=[k.maskNext])
    k.cd = cd
    k.one_col = P.sbuf([128, 1], F32, "one_col")
    P.op("dve", lambda e: e.memset(k.one_col[:], 1.0), writes=[k.one_col])
    k.eps_col = P.sbuf([128, 1], F32, "eps_col")
    P.op("dve", lambda e: e.memset(k.eps_col[:], EPS), writes=[k.eps_col])


def load_w_bf16(k, dst, dst_ap, src_ap, stage, stage_ap):
    P = k.P
    P.dma("sp", stage_ap, src_ap, writes=[stage])
    P.op("pool", lambda e: e.tensor_copy(dst_ap, stage_ap), reads=[stage], writes=[dst])


def cols_from_rows(k, dst_ap, rows_ap, R, tag):
    P = k.P
    with P.scope():
        st = P.sbuf([R, 128], F32, tag + "_rows")
        ps = P.psum([128, R], F32, tag + "_ps")
        P.dma("sp", st[:], rows_ap, writes=[st])
        P.op("pe", lambda e: e.transpose(ps[:], st[:], k.ident_f[0:R, 0:R]), reads=[st, k.ident_f], writes=[ps])
        return st, ps


def phase_mod(k, cc, w_mod, b_mod, modv, mT):
    P = k.P
    with P.scope():
        ccr = P.sbuf([16, 128], F32, "ccr")
        bcr = P.sbuf([48, 128], F32, "bcr")
        pst = P.psum([128, 64], F32, "mod_pst")
        P.dma("sp", ccr[:], cc.ap().rearrange("r (k p) -> (r k) p", p=128), writes=[ccr])
        P.dma("sp", bcr[:], b_mod.ap().rearrange("(c p) -> c p", p=128), writes=[bcr])
        P.op("pe", lambda e: e.transpose(pst[:, 0:16], ccr[:], k.ident_f[0:16, 0:16]), reads=[ccr, k.ident_f], writes=[pst])
        P.op("pe", lambda e: e.transpose(pst[:, 16:64], bcr[:], k.ident_f[0:48, 0:48]), reads=[bcr, k.ident_f], writes=[pst])
        sT = P.sbuf([128, 2, 8], F32, "sccT")
        bcol = P.sbuf([128, 48], F32, "bcol")
        P.op("act", lambda e: e.activation(sT[:].rearrange("p r k -> p (r k)"), pst[:, 0:16], AF.Silu), reads=[pst], writes=[sT])
        P.op("dve", lambda e: e.tensor_copy(bcol[:], pst[:, 16:64]), reads=[pst], writes=[bcol])
        wst = [P.sbuf([128, 8, 512], F32, "wmod_st%d" % i) for i in range(2)]
        ps = P.psum([128, 96], F32, "ps_mod")
        wv = w_mod.ap().rearrange("(k p) n -> p k n", p=128)
        for nb in range(12):
            w = wst[nb % 2]
            P.dma("sp", w[:], wv[:, :, nb * 512:(nb + 1) * 512], writes=[w])
            for cl in range(4):
                c = nb * 4 + cl
                for kk in range(8):
                    P.op("pe", lambda e, kk=kk, w=w, c=c, cl=cl: e.matmul(ps[:, 2 * c:2 * c + 2], w[:, kk, cl * 128:(cl + 1) * 128], sT[:, :, kk],
                                                                     start=(kk == 0), stop=(kk == 7)),
                         reads=[sT, w], writes=[ps])
        P.op("dve", lambda e: e.tensor_tensor(mT[:], ps[:].rearrange("p (c r) -> p c r", r=2), bcol[:].unsqueeze(2).to_broadcast([128, 48, 2]), ALU.add),
             reads=[ps, bcol], writes=[mT])
        for r in range(2):
            pr = P.psum([48, 128], F32, "mod_pr%d" % r)
            sr = P.sbuf([48, 128], F32, "mod_sr%d" % r)
            mr = P.sbuf([128, 48], F32, "mod_mr%d" % r)
            P.op("dve", lambda e, r=r, mr=mr: e.tensor_copy(mr[:], mT[:, :, r]), reads=[mT], writes=[mr])
            P.op("pe", lambda e, r=r, pr=pr, mr=mr: e.transpose(pr[:], mr[:], k.ident_f[:]), reads=[mr, k.ident_f], writes=[pr])
            P.op("act", lambda e, pr=pr, sr=sr: e.copy(sr[:], pr[:]), reads=[pr], writes=[sr])
            P.dma("sp", modv.ap()[r].rearrange("(c p) -> c p", p=128), sr[:], reads=[sr], writes=[modv])


def phase_norm(k, x, gain, mT, which, hT_sb, hT32_d=None, t_lo=0):
    P = k.P
    with P.scope():
        gr = P.sbuf([8, 128], F32, "gr%d" % which)
        gps = P.psum([128, 8], F32, "gps%d" % which)
        P.dma("sp", gr[:], gain.rearrange("(k p) -> k p", p=128), writes=[gr])
        P.op("pe", lambda e: e.transpose(gps[:], gr[:], k.ident_f[0:8, 0:8]), reads=[gr, k.ident_f], writes=[gps])
        G = P.sbuf([128, 2, 8], F32, "G%d" % which)
        gcol = P.sbuf([128, 8], F32, "gcol%d" % which)
        P.op("dve", lambda e: e.tensor_copy(gcol[:], gps[:]), reads=[gps], writes=[gcol])
        sh_i, sc_i = 3 * which, 3 * which + 1
        for r in range(2):
            P.op("dve", lambda e, r=r: e.scalar_tensor_tensor(G[:, r, :], mT[:, sc_i * 8:sc_i * 8 + 8, r], 1.0, gcol[:], ALU.add, ALU.mult),
                 reads=[mT, gcol], writes=[G])
        xt = [P.sbuf([128, 1024], F32, "nx%d_%d" % (which, i)) for i in range(3)]
        junk = P.sbuf([128, 1024], F32, "njunk%d" % which)
        xs = [P.sbuf([128, 1024], F32, "nxs%d_%d" % (which, i)) for i in range(5)]
        ss = [P.sbuf([128, 2], F32, "nss%d_%d" % (which, i)) for i in range(3)]
        pts = [P.psum([128, 512], F32, "npt%d_%d" % (which, i)) for i in range(2)]
        h32 = [P.sbuf([128, 512], F32, "nh32_%d_%d" % (which, i)) for i in range(2)] if hT32_d is not None else None
        groups = [[0, 1]] + [list(range(2 + 4 * g, 6 + 4 * g)) for g in range(8)]
        ti = 0
        ng = 0
        for grp in groups:
            if grp[0] < t_lo:
                continue
            r = 0 if grp[0] < 2 else 1
            tiles = []
            for t in grp:
                xb = xt[ti % 3]; sb = ss[ti % 3]; xsb = xs[ti % 5]
                ti += 1
                P.dma("sp", xb[:], x.ap()[t * 128:(t + 1) * 128, :], writes=[xb])
                P.op("act", lambda e, xb=xb, sb=sb: e.activation(junk[:], xb[:], AF.Square, accum_out=sb[:, 0:1]),
                     reads=[xb], writes=[junk, sb])
                P.op("act", lambda e, sb=sb: e.activation(sb[:, 1:2], sb[:, 0:1], AF.Sqrt, bias=k.eps_col[:], scale=1.0 / D),
                     reads=[sb, k.eps_col], writes=[sb])
                P.op("dve", lambda e, sb=sb: e.reciprocal(sb[:, 1:2], sb[:, 1:2]), reads=[sb], writes=[sb])
                P.op("dve", lambda e, xb=xb, sb=sb, xsb=xsb: e.tensor_scalar(xsb[:], xb[:], sb[:, 1:2], None, ALU.mult),
                     reads=[xb, sb], writes=[xsb])
                tiles.append(xsb)
            n = len(grp) * 128
            tok0 = grp[0] * 128
            for kk in range(8):
                pt = pts[ng % 2]
                for j, xsb in enumerate(tiles):
                    P.op("pe", lambda e, pt=pt, xsb=xsb, kk=kk, j=j: e.transpose(pt[:, j * 128:(j + 1) * 128], xsb[:, kk * 128:(kk + 1) * 128], k.ident_f[:]),
                         reads=[xsb, k.ident_f], writes=[pt])
                bias_ap = mT[:, sh_i * 8 + kk, r:r + 1]
                P.op("act", lambda e, pt=pt, kk=kk, r=r, tok0=tok0, n=n, bias_ap=bias_ap: e.activation(
                    hT_sb[:, kk, tok0:tok0 + n], pt[:, 0:n], AF.Identity, bias=bias_ap, scale=G[:, r, kk:kk + 1]),
                    reads=[pt, mT, G], writes=[hT_sb])
                if hT32_d is not None:
                    hb = h32[ng % 2]
                    P.op("dve", lambda e, pt=pt, kk=kk, r=r, n=n, hb=hb, bias_ap=bias_ap: e.tensor_scalar(
                        hb[:, 0:n], pt[:, 0:n], G[:, r, kk:kk + 1], bias_ap, ALU.mult, ALU.add),
                        reads=[pt, mT, G], writes=[hb])
                    P.dma("sp", hT32_d.ap()[kk * 128:(kk + 1) * 128, tok0:tok0 + n], hb[:, 0:n], reads=[hb], writes=[hT32_d])
                ng += 1


def phase_proj(k, hT_sb, w, z, N, tag="pj"):
    P = k.P
    with P.scope():
        _phase_proj(k, hT_sb, w, z, N, tag)


def _phase_proj(k, hT_sb, w, z, N, tag):
    P = k.P
    wst = [P.sbuf([128, 8, 512], F32, "%s_st%d" % (tag, i)) for i in range(2)]
    wb = [P.sbuf([128, 8, 512], BF16, "%s_wb%d" % (tag, i)) for i in range(2)]
    pss = [P.psum([128, 512], F32, "%s_ps%d" % (tag, i)) for i in range(3)]
    ob = [P.sbuf([128, 512], F32, "%s_ob%d" % (tag, i)) for i in range(4)]
    wv = w.rearrange("(k p) n -> p k n", p=128)
    nblk = (N + 511) // 512
    it = 0
    for nb in range(nblk):
        c0 = nb * 512
        cw = min(512, N - c0)
        st, wbb = wst[nb % 2], wb[nb % 2]
        load_w_bf16(k, wbb, wbb[:, :, 0:cw], wv[:, :, c0:c0 + cw], st, st[:, :, 0:cw])
        for t in range(NT):
            ps = pss[it % 3]; o = ob[it % 4]
            it += 1
            for kk in range(8):
                P.op("pe", lambda e, ps=ps, kk=kk, t=t, wbb=wbb, cw=cw: e.matmul(
                    ps[:, 0:cw], hT_sb[:, kk, t * 128:(t + 1) * 128], wbb[:, kk, 0:cw], start=(kk == 0), stop=(kk == 7)),
                    reads=[hT_sb, wbb], writes=[ps])
            if it % 2:
                P.op("act", lambda e, ps=ps, o=o, cw=cw: e.copy(o[:, 0:cw], ps[:, 0:cw]), reads=[ps], writes=[o])
            else:
                P.op("dve", lambda e, ps=ps, o=o, cw=cw: e.tensor_copy(o[:, 0:cw], ps[:, 0:cw]), reads=[ps], writes=[o])
            P.dma("sp", z.ap()[t * 128:(t + 1) * 128, c0:c0 + cw], o[:, 0:cw], reads=[o], writes=[z])


ZOFF = dict(ret_q=0, ret_k=256, ret_v=512, ret_g=1024, win_q=1536, win_k=2048, win_v=2176,
            na_q=2304, na_k=2816, na_v=3328, gdn_q=3840, gdn_k=4352, gdn_v=4864, gdn_g=5376, gdn_a=5888, gdn_b=5896)
NEG = -30000.0


def rope_tables():
    t = np.arange(4096)
    row = (t // 64).astype(np.float32); col = (t % 64).astype(np.float32)
    inv = (10000.0 ** (-np.arange(16, dtype=np.float32) / 16)).astype(np.float32)
    ang = np.concatenate([row[:, None] * inv, col[:, None] * inv], axis=-1).astype(np.float32)
    return np.cos(ang).astype(np.float32), np.sin(ang).astype(np.float32)


def na_classes():
    cls = [(10, 10 + dl) for dl in range(-2, 3)]
    for p in (0, 1):
        cls += [(p, b) for b in range(4)]
    for p in (30, 31):
        cls += [(p, b) for b in range(28, 32)]
    return cls


def na_plan(p):
    if 2 <= p <= 29:
        return [(p + dl, dl + 2) for dl in range(-2, 3)]
    e = {0: 0, 1: 1, 30: 2, 31: 3}[p]
    b0 = 0 if p < 2 else 28
    return [(b0 + b, 5 + e * 4 + b) for b in range(4)]


def na_bias_gather(rpb):
    cls = na_classes()
    out = np.full((8, 128, len(cls), 128), NEG, np.float32)
    kc = np.arange(64)[:, None]; qc = np.arange(64)[None, :]
    cst = np.clip(qc - 8, 0, 48)
    colok = (kc >= cst) & (kc < cst + 16)
    coff = np.clip(kc - qc + 15, 0, 30)
    for ci, (p, blk) in enumerate(cls):
        for a in range(2):
            krow = 2 * blk + a
            for b in range(2):
                qrow = 2 * p + b
                rs = min(max(qrow - 4, 0), 56)
                if not (rs <= krow < rs + 8):
                    continue
                ro = krow - qrow + 7
                vals = rpb[:, ro][:, coff]
                blkv = np.where(colok[None], vals, NEG)
                out[:, a * 64:(a + 1) * 64, ci, b * 64:(b + 1) * 64] = blkv
    return out


def attn_prep_qk(k, z, col, gain_bc, rope, outT, tag, cs=None):
    P = k.P
    with P.scope():
        x = P.sbuf([128, NT, 64], F32, tag + "x")
        zv = z.ap()[:, col:col + 64].rearrange("(t p) c -> p t c", p=128)
        P.dma("sp", x[:, 0:17, :], zv[:, 0:17, :], writes=[x])
        P.dma("sp", x[:, 17:34, :], zv[:, 17:34, :], writes=[x])
        sq = P.sbuf([128, NT, 64], F32, tag + "sq")
        P.op("pool", lambda e: e.tensor_tensor(sq[:], x[:], x[:], ALU.mult), reads=[x], writes=[sq])
        ss = P.sbuf([128, NT], F32, tag + "ss")
        P.op("dve", lambda e: e.tensor_reduce(ss[:], sq[:], AX.X, ALU.add), reads=[sq], writes=[ss])
        P.op("act", lambda e: e.activation(ss[:], ss[:], AF.Sqrt, bias=k.eps_col[:], scale=1.0 / 64), reads=[ss, k.eps_col], writes=[ss])
        P.op("dve", lambda e: e.reciprocal(ss[:], ss[:]), reads=[ss], writes=[ss])
        P.op("dve", lambda e: e.tensor_tensor(x[:], x[:], ss[:].unsqueeze(2).to_broadcast([128, NT, 64]), ALU.mult), reads=[x, ss], writes=[x])
        P.op("pool", lambda e: e.tensor_tensor(x[:], x[:], gain_bc[:].unsqueeze(1).to_broadcast([128, NT, 64]), ALU.mult), reads=[x, gain_bc], writes=[x])
        if rope:
            rope_apply(k, x, 1, tag)
        transpose_to(k, lambda t: x[:, t, :], 64, outT, tag, src_bufs=[x])


def rope_apply(k, x, H, tag):
    P = k.P
    with P.scope():
        _rope_apply(k, x, H, tag)


def _rope_apply(k, x, H, tag):
    P = k.P
    xl = x[:, 2:NT, :].rearrange("p t (h c) -> p t h c", c=64)
    x1 = xl[:, :, :, 0:32]; x2 = xl[:, :, :, 32:64]
    shp = [128, 32, H, 32]
    a = P.sbuf(shp, F32, tag + "ra"); b = P.sbuf(shp, F32, tag + "rb")
    cosb = k.cos[:].unsqueeze(2).to_broadcast(shp); sinb = k.sin[:].unsqueeze(2).to_broadcast(shp)
    P.op("dve", lambda e: e.tensor_tensor(a[:], x1, sinb, ALU.mult), reads=[x, k.sin], writes=[a])
    P.op("pool", lambda e: e.tensor_tensor(b[:], x2, sinb, ALU.mult), reads=[x, k.sin], writes=[b])
    P.op("dve", lambda e: e.tensor_tensor(x1, x1, cosb, ALU.mult), reads=[x, k.cos, a, b], writes=[x])
    P.op("dve", lambda e: e.tensor_tensor(x2, x2, cosb, ALU.mult), reads=[x, k.cos], writes=[x])
    P.op("dve", lambda e: e.tensor_tensor(x1, x1, b[:], ALU.subtract), reads=[x, b], writes=[x])
    P.op("dve", lambda e: e.tensor_tensor(x2, x2, a[:], ALU.add), reads=[x, a], writes=[x])


def transpose_to(k, src_fn, C, outT, tag, deps=None, t_list=None, src_bufs=None, out_off=0):
    P = k.P
    with P.scope():
        pss = [P.psum([128, 512], F32, tag + "tp%d" % i) for i in range(2)]
        tl = list(range(NT)) if t_list is None else t_list
        for g0 in range(0, len(tl), 4):
            grp = tl[g0:g0 + 4]
            ps = pss[(g0 // 4) % 2]
            for j, t in enumerate(grp):
                ap, bufs = src_fn(t), (src_bufs or [])
                P.op("pe", lambda e, ps=ps, j=j, ap=ap: e.transpose(ps[0:C, j * 128:(j + 1) * 128], ap, k.ident_f[:]),
                     reads=list(bufs) + [k.ident_f] + (deps or []), writes=[ps])
            n = len(grp) * 128
            o = outT[0:C, out_off + grp[0] * 128: out_off + grp[0] * 128 + n]
            if (g0 // 4) % 2:
                P.op("act", lambda e, ps=ps, o=o, n=n: e.copy(o, ps[0:C, 0:n]), reads=[ps], writes=[outT])
            else:
                P.op("dve", lambda e, ps=ps, o=o, n=n: e.tensor_copy(o, ps[0:C, 0:n]), reads=[ps], writes=[outT])


def attn_prep_v(k, z, col, Vaug, tag):
    P = k.P
    with P.scope():
        v = P.sbuf([128, NT, 64], F32, tag + "v")
        zv = z.ap()[:, col:col + 64].rearrange("(t p) c -> p t c", p=128)
        P.dma("sp", v[:, 0:17, :], zv[:, 0:17, :], writes=[v])
        P.dma("sp", v[:, 17:34, :], zv[:, 17:34, :], writes=[v])
        P.op("pool", lambda e: e.tensor_copy(Vaug[:, :, 0:64], v[:]), reads=[v], writes=[Vaug])
        P.op("pool", lambda e: e.memset(Vaug[:, :, 64:65], 1.0), writes=[Vaug])


def attn_core(k, qT, kT, Vaug, plan, sink_ap, sink_buf, yh, tag):
    P = k.P
    with P.scope():
        pss = [[P.psum([128, 512], F32, "%ss%d_%d" % (tag, i, j)) for j in range(2)] for i in range(2)]
        pos = [P.psum([128, 65], F32, "%so%d" % (tag, i)) for i in range(2)]
        pts = [P.sbuf([128, 8 * 128], BF16, "%spt%d" % (tag, i)) for i in range(2)]
        dens = [P.sbuf([128, 1], F32, "%sden%d" % (tag, i)) for i in range(2)]
        for qi, (qt, blocks) in enumerate(plan):
            sset = pss[qi % 2]; pt = pts[qi % 2]; po = pos[qi % 2]; den = dens[qi % 2]
            nb = len(blocks)
            for bi, (kt, m, mb) in enumerate(blocks):
                ps = sset[bi // 4]
                P.op("pe", lambda e, ps=ps, bi=bi, kt=kt, qt=qt: e.matmul(ps[:, (bi % 4) * 128:(bi % 4 + 1) * 128], kT[:, kt * 128:(kt + 1) * 128],
                                                                      qT[:, qt * 128:(qt + 1) * 128], start=True, stop=True),
                     reads=[kT, qT], writes=[ps])
            for bk in range((nb + 3) // 4):
                n = min(4, nb - bk * 4) * 128
                P.op("act", lambda e, pt=pt, bk=bk, n=n, sset=sset: e.activation(pt[:, bk * 512:bk * 512 + n], sset[bk][:, 0:n], AF.Exp, scale=0.125),
                     reads=[sset[bk]], writes=[pt])
            for bi, (kt, m, mb) in enumerate(blocks):
                if m is not None:
                    eng = "pool" if bi % 2 else "dve"
                    P.op(eng, lambda e, pt=pt, bi=bi, m=m: e.tensor_tensor(pt[:, bi * 128:(bi + 1) * 128], pt[:, bi * 128:(bi + 1) * 128], m, ALU.mult),
                         reads=[pt, mb], writes=[pt])
            for bi, (kt, m, mb) in enumerate(blocks):
                P.op("pe", lambda e, po=po, pt=pt, bi=bi, kt=kt, nb=nb: e.matmul(po[:], pt[:, bi * 128:(bi + 1) * 128], Vaug[:, kt, :],
                                                                             start=(bi == 0), stop=(bi == nb - 1)),
                     reads=[pt, Vaug], writes=[po])
            if sink_ap is not None:
                P.op("dve", lambda e, den=den, po=po: e.tensor_scalar(den[:], po[:, 64:65], sink_ap, None, ALU.add), reads=[po, sink_buf], writes=[den])
                P.op("dve", lambda e, den=den: e.reciprocal(den[:], den[:]), reads=[den], writes=[den])
            else:
                P.op("dve", lambda e, den=den, po=po: e.reciprocal(den[:], po[:, 64:65]), reads=[po], writes=[den])
            P.op("act", lambda e, den=den, po=po, qt=qt: e.activation(yh[:, qt, :], po[:, 0:64], AF.Copy, scale=den[:, 0:1]), reads=[po, den], writes=[yh])


def store_head(k, yh, ybr_ap, col, t_lo=0):
    P = k.P
    C = yh.t.shape[2]
    yv = ybr_ap[:, col:col + C].rearrange("(t p) c -> p t c", p=128)
    P.dma("sp", yv[:, t_lo:17, :], yh[:, t_lo:17, :], reads=[yh], writes=[k.ybr])
    P.dma("sp", yv[:, 17:34, :], yh[:, 17:34, :], reads=[yh], writes=[k.ybr])


def bc_load(k, vec_ap, n, name):
    P = k.P
    t = P.sbuf([128, n], F32, name)
    P.dma("sp", t[:], vec_ap.partition_broadcast(128), writes=[t])
    return t


def phase_window(k, z, qn, kn, sink, ctx_out):
    P = k.P
    with P.scope():
        qg = bc_load(k, qn, 64, "wqg"); kg = bc_load(k, kn, 64, "wkg")
        sk = bc_load(k, sink, 8, "wsk")
        P.op("act", lambda e: e.activation(sk[:], sk[:], AF.Exp), reads=[sk], writes=[sk])
        qT = P.sbuf([64, T], BF16, "wqT"); kT = P.sbuf([64, T], BF16, "wkT")
        Vaug = P.sbuf([128, NT, 65], BF16, "wV")
        yh = P.sbuf([128, NT, 64], F32, "wyh")
        plan = []
        if ctx_out:
            plan += [(qt, [(0, None, None), (1, None, None)]) for qt in range(2)]
        for n in range(32):
            bl = []
            if n > 0:
                bl.append((2 + n - 1, k.maskPrev[:], k.maskPrev))
            bl.append((2 + n, None, None))
            if n < 31:
                bl.append((2 + n + 1, k.maskNext[:], k.maskNext))
            bl += [(0, None, None), (1, None, None)]
            plan.append((2 + n, bl))
        for h in range(8):
            g = h // 4
            if h % 4 == 0:
                attn_prep_qk(k, z, ZOFF["win_k"] + g * 64, kg, True, kT, "wk")
                attn_prep_v(k, z, ZOFF["win_v"] + g * 64, Vaug, "wv")
            attn_prep_qk(k, z, ZOFF["win_q"] + h * 64, qg, True, qT, "wq")
            attn_core(k, qT, kT, Vaug, plan, sk[:, h:h + 1], sk, yh, "wa")
            store_head(k, yh, k.ybr.ap()[1], h * 64, 0 if ctx_out else 2)


def phase_na(k, z, qn, kn, nab, ctx_out):
    P = k.P
    with P.scope():
        qg = bc_load(k, qn, 64, "nqg"); kg = bc_load(k, kn, 64, "nkg")
        qT = P.sbuf([64, T], BF16, "nqT"); kT = P.sbuf([64, T], BF16, "nkT")
        Vaug = P.sbuf([128, NT, 65], BF16, "nV")
        yh = P.sbuf([128, NT, 64], F32, "nyh")
        bst = P.sbuf([128, 21, 128], F32, "nbst")
        eb = P.sbuf([128, 21, 128], BF16, "neb")
        for h in range(8):
            P.dma("sp", bst[:], nab[h], writes=[bst])
            P.op("act", lambda e: e.activation(eb[:], bst[:], AF.Exp), reads=[bst], writes=[eb])
            plan = []
            if ctx_out:
                plan += [(qt, [(0, None, None), (1, None, None)]) for qt in range(2)]
            for p in range(32):
                bl = [(2 + blk, eb[:, ci, :], eb) for blk, ci in na_plan(p)]
                bl += [(0, None, None), (1, None, None)]
                plan.append((2 + p, bl))
            attn_prep_qk(k, z, ZOFF["na_k"] + h * 64, kg, False, kT, "nk")
            attn_prep_v(k, z, ZOFF["na_v"] + h * 64, Vaug, "nv")
            attn_prep_qk(k, z, ZOFF["na_q"] + h * 64, qg, False, qT, "nq")
            attn_core(k, qT, kT, Vaug, plan, None, None, yh, "na")
            store_head(k, yh, k.ybr.ap()[2], h * 64, 0 if ctx_out else 2)


def ret_consts(c):
    j = np.arange(128)[:, None].astype(np.float32); i = np.arange(128)[None, :].astype(np.float32)
    c["dpos"] = np.stack([np.maximum(i - j, 0), np.maximum(j - i, 0)], 1).astype(np.float32)
    c["dmsk"] = np.stack([(i >= j), (j >= i)], 1).astype(np.float32)
    c["rowidx"] = np.stack([np.broadcast_to(i + 1, (128, 128)), np.broadcast_to(128 - i, (128, 128))], 1).astype(np.float32)
    c["colidx"] = np.concatenate([127 - j, j], 1).astype(np.float32)


def phase_ret(k, z, decay, gn, ctx_out):
    P = k.P
    with P.scope():
        dpos = P.sbuf([128, 2, 128], F32, "r_dpos"); dmsk = P.sbuf([128, 2, 128], F32, "r_dmsk")
        rowidx = P.sbuf([128, 2, 128], F32, "r_rowidx"); colidx = P.sbuf([128, 2], F32, "r_colidx")
        for t_, n_ in ((dpos, "dpos"), (dmsk, "dmsk"), (rowidx, "rowidx"), (colidx, "colidx")):
            P.dma("sp", t_[:], k.cd[n_].ap(), writes=[t_])
        lg = bc_load(k, decay, 8, "r_lg")
        P.op("act", lambda e: e.activation(lg[:], lg[:], AF.Exp, scale=-1.0), reads=[lg], writes=[lg])
        P.op("act", lambda e: e.activation(lg[:], lg[:], AF.Ln, bias=k.one_col[:]), reads=[lg, k.one_col], writes=[lg])
        P.op("dve", lambda e: e.tensor_scalar(lg[:], lg[:], -1.0, None, ALU.mult), reads=[lg], writes=[lg])
        dm = P.sbuf([128, 8, 128], F32, "r_dm"); qdT = P.sbuf([128, 8, 128], F32, "r_qdT")
        kdec = P.sbuf([128, 8], F32, "r_kdec"); cdec = P.sbuf([128, 8], F32, "r_cdec")
        for dr in range(2):
            for h in range(4):
                c = dr * 4 + h
                P.op("act", lambda e, dr=dr, c=c: e.activation(dm[:, c, :], dpos[:, dr, :], AF.Exp, scale=lg[:, c:c + 1]), reads=[dpos, lg], writes=[dm])
                P.op("dve", lambda e, dr=dr, c=c: e.tensor_tensor(dm[:, c, :], dm[:, c, :], dmsk[:, dr, :], ALU.mult), reads=[dm, dmsk], writes=[dm])
                P.op("act", lambda e, dr=dr, c=c: e.activation(qdT[:, c, :], rowidx[:, dr, :], AF.Exp, scale=lg[:, c:c + 1]), reads=[rowidx, lg], writes=[qdT])
                P.op("act", lambda e, dr=dr, c=c: e.activation(kdec[:, c:c + 1], colidx[:, dr:dr + 1], AF.Exp, scale=lg[:, c:c + 1]), reads=[colidx, lg], writes=[kdec])
        P.op("act", lambda e: e.activation(cdec[:], lg[:], AF.Exp, scale=128.0), reads=[lg], writes=[cdec])
        gnb = bc_load(k, gn, 512, "r_gn")
        q = P.sbuf([128, NT, 64], F32, "r_q"); kk_ = P.sbuf([128, NT, 64], F32, "r_k")
        v = P.sbuf([128, NT, 128], F32, "r_v"); gt = P.sbuf([128, NT, 128], F32, "r_g")
        qT = P.sbuf([64, T], F32, "r_qT"); kT = P.sbuf([64, T], F32, "r_kT")
        qTd = P.sbuf([64, T], F32, "r_qTd"); kd = P.sbuf([128, NT, 64], F32, "r_kd")
        of = P.sbuf([128, NT, 128], F32, "r_of"); ot = P.sbuf([128, NT, 128], F32, "r_ot")
        S = [P.sbuf([64, 128], F32, "r_S%d" % i) for i in range(2)]
        pss = [P.psum([128, 128], F32, "r_pss%d" % i) for i in range(2)]
        pso = [P.psum([128, 128], F32, "r_pso%d" % i) for i in range(2)]
        pkv = [P.psum([64, 128], F32, "r_pkv%d" % i) for i in range(2)]
        sm = [P.sbuf([128, 128], F32, "r_sm%d" % i) for i in range(2)]
        st = P.sbuf([128, NT, 4], F32, "r_st")

        def ld(dst, col, w):
            zv = z.ap()[:, col:col + w].rearrange("(t p) c -> p t c", p=128)
            P.dma("sp", dst[:, 0:17, :], zv[:, 0:17, :], writes=[dst])
            P.dma("sp", dst[:, 17:34, :], zv[:, 17:34, :], writes=[dst])

        for h in range(4):
            ld(q, ZOFF["ret_q"] + h * 64, 64); ld(kk_, ZOFF["ret_k"] + h * 64, 64)
            ld(v, ZOFF["ret_v"] + h * 128, 128); ld(gt, ZOFF["ret_g"] + h * 128, 128)
            rope_apply(k, q, 1, "r_rq"); rope_apply(k, kk_, 1, "r_rk")
            P.op("pool", lambda e: e.tensor_scalar(kk_[:], kk_[:], 0.125, None, ALU.mult), reads=[kk_], writes=[kk_])
            transpose_to(k, lambda t: q[:, t, :], 64, qT, "r_tq", src_bufs=[q])
            transpose_to(k, lambda t: kk_[:, t, :], 64, kT, "r_tk", src_bufs=[kk_])
            it = 0
            for dr in range(2):
                c = dr * 4 + h
                P.op("dve", lambda e, c=c: e.tensor_tensor(qTd[:].rearrange("p (t i) -> p t i", i=128), qT[:].rearrange("p (t i) -> p t i", i=128),
                                                           qdT[0:64, c, :].unsqueeze(1).to_broadcast([64, NT, 128]), ALU.mult), reads=[qT, qdT], writes=[qTd])
                P.op("pool", lambda e, c=c: e.tensor_scalar(kd[:], kk_[:], kdec[:, c:c + 1], None, ALU.mult), reads=[kk_, kdec], writes=[kd])
                order = list(range(NT)) if dr == 0 else [1, 0] + list(range(NT - 1, 1, -1))
                Scur = None
                for n in order:
                    ps, po, pk, smb = pss[it % 2], pso[it % 2], pkv[it % 2], sm[it % 2]
                    Snew = S[it % 2]
                    it += 1
                    sl = slice(n * 128, (n + 1) * 128)
                    P.op("pe", lambda e, ps=ps, sl=sl: e.matmul(ps[:], kT[:, sl], qT[:, sl], start=True, stop=True), reads=[kT, qT], writes=[ps])
                    P.op("dve", lambda e, ps=ps, smb=smb, c=c: e.tensor_tensor(smb[:], ps[:], dm[:, c, :], ALU.mult), reads=[ps, dm], writes=[smb])
                    P.op("pe", lambda e, po=po, smb=smb, n=n, last=(Scur is None): e.matmul(po[:], smb[:], v[:, n, :], start=True, stop=last), reads=[smb, v], writes=[po])
                    if Scur is not None:
                        P.op("pe", lambda e, po=po, sl=sl, Scur=Scur: e.matmul(po[:], qTd[:, sl], Scur[:], start=False, stop=True), reads=[qTd, Scur], writes=[po])
                    P.op("pe", lambda e, pk=pk, n=n: e.matmul(pk[:], kd[:, n, :], v[:, n, :], start=True, stop=True), reads=[kd, v], writes=[pk])
                    if Scur is None:
                        P.op("act", lambda e, pk=pk, Snew=Snew: e.copy(Snew[:], pk[:]), reads=[pk], writes=[Snew])
                    else:
                        P.op("dve", lambda e, pk=pk, Snew=Snew, Scur=Scur, c=c: e.scalar_tensor_tensor(Snew[:], Scur[:], cdec[0:64, c:c + 1], pk[:], ALU.mult, ALU.add),
                             reads=[pk, Scur, cdec], writes=[Snew])
                    Scur = Snew
                    if dr == 0:
                        P.op("act", lambda e, po=po, n=n: e.copy(of[:, n, :], po[:]), reads=[po], writes=[of])
                    else:
                        P.op("dve", lambda e, po=po, n=n: e.tensor_tensor(ot[:, n, :], po[:], of[:, n, :], ALU.add), reads=[po, of], writes=[ot])
            s1 = st[:, :, 0]; s2 = st[:, :, 1]; mu = st[:, :, 2]; rs = st[:, :, 3]
            bshape = [128, NT, 128]
            P.op("dve", lambda e: e.tensor_reduce(s1, ot[:], AX.X, ALU.add), reads=[ot], writes=[st])
            P.op("pool", lambda e: e.tensor_tensor(of[:], ot[:], ot[:], ALU.mult), reads=[ot], writes=[of])
            P.op("dve", lambda e: e.tensor_reduce(s2, of[:], AX.X, ALU.add), reads=[of], writes=[st])
            P.op("dve", lambda e: e.tensor_scalar(mu, s1, 1.0 / 128, None, ALU.mult), reads=[st], writes=[st])
            P.op("dve", lambda e: e.tensor_tensor(s1, mu, mu, ALU.mult), reads=[st], writes=[st])
            P.op("dve", lambda e: e.scalar_tensor_tensor(rs, s2, 1.0 / 128, s1, ALU.mult, ALU.subtract), reads=[st], writes=[st])
            P.op("act", lambda e: e.activation(rs, rs, AF.Sqrt, bias=k.eps_col[:]), reads=[st, k.eps_col], writes=[st])
            P.op("dve", lambda e: e.reciprocal(rs, rs), reads=[st], writes=[st])
            P.op("dve", lambda e: e.tensor_tensor(ot[:], ot[:], mu.unsqueeze(2).to_broadcast(bshape), ALU.subtract), reads=[ot, st], writes=[ot])
            P.op("dve", lambda e: e.tensor_tensor(ot[:], ot[:], rs.unsqueeze(2).to_broadcast(bshape), ALU.mult), reads=[ot, st], writes=[ot])
            P.op("pool", lambda e, h=h: e.tensor_tensor(ot[:], ot[:], gnb[:, h * 128:(h + 1) * 128].unsqueeze(1).to_broadcast(bshape), ALU.mult), reads=[ot, gnb], writes=[ot])
            P.op("act", lambda e: e.activation(gt[:], gt[:], AF.Silu), reads=[gt], writes=[gt])
            P.op("dve", lambda e: e.tensor_tensor(ot[:], ot[:], gt[:], ALU.mult), reads=[ot, gt], writes=[ot])
            store_head(k, ot, k.ybr.ap()[0], h * 128, 0 if ctx_out else 2)


TGROUPS = [[0, 1]] + [list(range(2 + 4 * g, 6 + 4 * g)) for g in range(8)]


def load_big_w(k, dst, dview_fn, src_fn, nblk, st):
    P = k.P
    for i in range(nblk):
        s = st[i % len(st)]
        sap = src_fn(i)
        shp = sap.shape
        sv = s[:, 0:shp[1], 0:shp[2]]
        P.dma("sp", sv, sap, writes=[s])
        P.op("pool", lambda e, i=i, sv=sv: e.tensor_copy(dview_fn(i), sv), reads=[s], writes=[dst])


def phase_merge(k, hT_d, x, xmid, w_merge, w_branch, w_out, modv, ctx_out):
    P = k.P
    with P.scope():
        wm = P.sbuf([128, 8, 4096], BF16, "m_wm"); wb = P.sbuf([128, 4, 4, 1024], BF16, "m_wb"); wo = P.sbuf([128, 8, 1024], BF16, "m_wo")
        with P.scope():
            st = [P.sbuf([128, 8, 512], F32, "m_st%d" % i) for i in range(2)]
            wmv = w_merge.rearrange("(k p) n -> p k n", p=128)
            load_big_w(k, wm, lambda i: wm[:, :, i * 512:(i + 1) * 512], lambda i: wmv[:, :, i * 512:(i + 1) * 512], 8, st)
            wbv = w_branch.rearrange("b (k p) n -> p b k n", p=128)
            load_big_w(k, wb, lambda i: wb[:, i // 2, :, (i % 2) * 512:(i % 2 + 1) * 512], lambda i: wbv[:, i // 2, :, (i % 2) * 512:(i % 2 + 1) * 512], 8, st)
            wov = w_out.rearrange("(k p) n -> p k n", p=128)
            load_big_w(k, wo, lambda i: wo[:, :, i * 512:(i + 1) * 512], lambda i: wov[:, :, i * 512:(i + 1) * 512], 2, st)
        g1bc = P.sbuf([128, 2, 1024], F32, "m_g1")
        for r in range(2):
            P.dma("sp", g1bc[:, r, :], modv.ap()[r, 2048:3072].partition_broadcast(128), writes=[g1bc])
        hT = [P.sbuf([128, 8, 512], BF16, "m_hT%d" % i) for i in range(2)]
        yin = [P.sbuf([128, 512], F32, "m_yin%d" % i) for i in range(3)]
        yT = P.sbuf([128, 4, 4, 512], BF16, "m_yT")
        accT = P.sbuf([128, 8, 512], BF16, "m_accT")
        acc = P.sbuf([128, 512], F32, "m_acc"); tmp = P.sbuf([128, 512], F32, "m_tmp"); sg = [P.sbuf([128, 512], F32, "m_sg%d" % i) for i in range(2)]
        xt = [P.sbuf([128, 1024], F32, "m_xt%d" % i) for i in range(2)]
        xo = [P.sbuf([128, 1024], F32, "m_xo%d" % i) for i in range(2)]
        pg = [P.psum([128, 512], F32, "m_pg%d" % i) for i in range(2)]
        pb = [P.psum([128, 512], F32, "m_pb%d" % i) for i in range(2)]
        po = [P.psum([128, 1024], F32, "m_po%d" % i) for i in range(2)]
        yi = 0; it = 0; oi = 0
        for gi, grp in enumerate(TGROUPS):
            if not ctx_out and grp[0] < 2:
                continue
            r = 0 if grp[0] < 2 else 1
            n = len(grp) * 128; tok0 = grp[0] * 128
            hb = hT[gi % 2]
            P.dma("sp", hb[:, :, 0:n], hT_d.ap().rearrange("(k p) t -> p k t", p=128)[:, :, tok0:tok0 + n], writes=[hb])
            for i in range(4):
                for j, t in enumerate(grp):
                    yb = yin[yi % 3]; yi += 1
                    P.dma("sp", yb[:], k.ybr.ap()[i, t * 128:(t + 1) * 128, :], reads=[k.ybr], writes=[yb])
                    pt = po[yi % 2]
                    for kk in range(4):
                        P.op("pe", lambda e, pt=pt, yb=yb, kk=kk: e.transpose(pt[:, kk * 128:(kk + 1) * 128], yb[:, kk * 128:(kk + 1) * 128], k.ident_f[:]),
                             reads=[yb, k.ident_f], writes=[pt])
                    eng = "act" if yi % 2 else "dve"
                    if eng == "act":
                        P.op("act", lambda e, pt=pt, i=i, j=j: e.copy(yT[:, i, :, j * 128:(j + 1) * 128], pt[:, 0:512].rearrange("p (k t) -> p k t", t=128)), reads=[pt], writes=[yT])
                    else:
                        P.op("dve", lambda e, pt=pt, i=i, j=j: e.tensor_copy(yT[:, i, :, j * 128:(j + 1) * 128], pt[:, 0:512].rearrange("p (k t) -> p k t", t=128)), reads=[pt], writes=[yT])
            for fc in range(8):
                for i in range(4):
                    g_, b_, s_ = pg[it % 2], pb[it % 2], sg[it % 2]; it += 1
                    for kk in range(8):
                        P.op("pe", lambda e, g_=g_, kk=kk, i=i, fc=fc, hb=hb, n=n: e.matmul(g_[:, 0:n], wm[:, kk, i * 1024 + fc * 128:i * 1024 + (fc + 1) * 128], hb[:, kk, 0:n],
                                                                                         start=(kk == 0), stop=(kk == 7)), reads=[wm, hb], writes=[g_])
                    for kk in range(4):
                        P.op("pe", lambda e, b_=b_, kk=kk, i=i, fc=fc, n=n: e.matmul(b_[:, 0:n], wb[:, i, kk, fc * 128:(fc + 1) * 128], yT[:, i, kk, 0:n],
                                                                                 start=(kk == 0), stop=(kk == 3)), reads=[wb, yT], writes=[b_])
                    P.op("act", lambda e, g_=g_, s_=s_, n=n: e.activation(s_[:, 0:n], g_[:, 0:n], AF.Sigmoid), reads=[g_], writes=[s_])
                    if i == 0:
                        P.op("dve", lambda e, s_=s_, b_=b_, n=n: e.tensor_tensor(acc[:, 0:n], s_[:, 0:n], b_[:, 0:n], ALU.mult), reads=[s_, b_], writes=[acc])
                    else:
                        P.op("dve", lambda e, s_=s_, b_=b_, n=n: e.tensor_tensor(tmp[:, 0:n], s_[:, 0:n], b_[:, 0:n], ALU.mult), reads=[s_, b_], writes=[tmp])
                        if i < 3:
                            P.op("pool", lambda e, n=n: e.tensor_tensor(acc[:, 0:n], acc[:, 0:n], tmp[:, 0:n], ALU.add), reads=[acc, tmp], writes=[acc])
                        else:
                            P.op("pool", lambda e, n=n, fc=fc: e.tensor_tensor(accT[:, fc, 0:n], acc[:, 0:n], tmp[:, 0:n], ALU.add), reads=[acc, tmp], writes=[accT])
            for j, t in enumerate(grp):
                o_ = po[oi % 2]; xb = xt[oi % 2]; xob = xo[oi % 2]; oi += 1
                P.dma("sp", xb[:], x.ap()[t * 128:(t + 1) * 128, :], reads=[x], writes=[xb])
                for half in range(2):
                    for fc in range(8):
                        P.op("pe", lambda e, o_=o_, half=half, fc=fc, j=j: e.matmul(o_[:, half * 512:(half + 1) * 512], accT[:, fc, j * 128:(j + 1) * 128], wo[:, fc, half * 512:(half + 1) * 512],
                                                                                start=(fc == 0), stop=(fc == 7)), reads=[accT, wo], writes=[o_])
                P.op("dve", lambda e, o_=o_, xob=xob, r=r: e.tensor_tensor(xob[:], o_[:], g1bc[:, r, :], ALU.mult), reads=[o_, g1bc], writes=[xob])
                P.op("pool", lambda e, xob=xob, xb=xb: e.tensor_tensor(xob[:], xob[:], xb[:], ALU.add), reads=[xob, xb], writes=[xob])
                P.dma("sp", xmid.ap()[t * 128:(t + 1) * 128, :], xob[:], reads=[xob], writes=[xmid])


def phase_router(k, h32_d, w_router, rbias, wgt, t_lo):
    P = k.P
    with P.scope():
        wr = P.sbuf([128, 8, 32], F32, "rt_w")
        P.dma("sp", wr[:], w_router.rearrange("(k p) n -> p k n", p=128), writes=[wr])
        rb = bc_load(k, rbias, 32, "rt_b")
        hs = [P.sbuf([128, 8, 128], F32, "rt_h%d" % i) for i in range(2)]
        ps = [P.psum([128, 32], F32, "rt_ps%d" % i) for i in range(2)]
        sc = P.sbuf([128, 32], F32, "rt_sc"); sel = P.sbuf([128, 32], F32, "rt_sel")
        w8 = P.sbuf([128, 10, 8], F32, "rt_w8"); msk = P.sbuf([128, 32], F32, "rt_msk"); c1 = P.sbuf([128, 2], F32, "rt_c1")
        hv = h32_d.ap().rearrange("(k p) t -> p k t", p=128)
        for t in range(t_lo, NT):
            hb = hs[t % 2]; p_ = ps[t % 2]
            P.dma("sp", hb[:], hv[:, :, t * 128:(t + 1) * 128], reads=[h32_d], writes=[hb])
            for kk in range(8):
                P.op("pe", lambda e, p_=p_, hb=hb, kk=kk: e.matmul(p_[:], hb[:, kk, :], wr[:, kk, :], start=(kk == 0), stop=(kk == 7)), reads=[hb, wr], writes=[p_])
            P.op("act", lambda e, p_=p_: e.activation(sc[:], p_[:], AF.Sigmoid), reads=[p_], writes=[sc])
            P.op("dve", lambda e: e.tensor_tensor(sel[:], sc[:], rb[:], ALU.add), reads=[sc, rb], writes=[sel])
            s4 = sel[:].rearrange("p (g e) -> p g e", e=4)
            a, b, c, d_ = s4[:, :, 0], s4[:, :, 1], s4[:, :, 2], s4[:, :, 3]
            W = lambda i: w8[:, i, :]
            seq = [(W(0), a, b, ALU.max), (W(1), a, b, ALU.min), (W(2), c, d_, ALU.max), (W(3), c, d_, ALU.min),
                   (W(4), W(0), W(2), ALU.max), (W(5), W(0), W(2), ALU.min), (W(6), W(1), W(3), ALU.max),
                   (W(7), W(5), W(6), ALU.max), (W(8), W(4), W(7), ALU.add)]
            for o, i0, i1, op in seq:
                P.op("dve", lambda e, o=o, i0=i0, i1=i1, op=op: e.tensor_tensor(o, i0, i1, op), reads=[sel, w8], writes=[w8])
            P.op("dve", lambda e: e.tensor_reduce(c1[:, 0:1], W(8), AX.X, ALU.max), reads=[w8], writes=[c1])
            P.op("dve", lambda e: e.tensor_scalar(W(9), W(8), c1[:, 0:1], None, ALU.is_ge), reads=[w8, c1], writes=[w8])
            m4 = msk[:].rearrange("p (g e) -> p g e", e=4)
            P.op("dve", lambda e: e.tensor_tensor(m4, s4, W(7).unsqueeze(2).to_broadcast([128, 8, 4]), ALU.is_ge), reads=[sel, w8], writes=[msk])
            P.op("dve", lambda e: e.tensor_tensor(m4, m4, W(9).unsqueeze(2).to_broadcast([128, 8, 4]), ALU.mult), reads=[msk, w8], writes=[msk])
            P.op("dve", lambda e: e.tensor_tensor(msk[:], msk[:], sc[:], ALU.mult), reads=[msk, sc], writes=[msk])
            P.op("dve", lambda e: e.tensor_reduce(c1[:, 1:2], msk[:], AX.X, ALU.add), reads=[msk], writes=[c1])
            P.op("dve", lambda e: e.reciprocal(c1[:, 1:2], c1[:, 1:2]), reads=[c1], writes=[c1])
            P.op("dve", lambda e, t=t: e.tensor_scalar(wgt[:, t, :], msk[:], c1[:, 1:2], None, ALU.mult), reads=[msk, c1], writes=[wgt])


def phase_moe(k, h2T_d, wgt, xmid, xout, weg, weu, wed, modv, t_lo, out_lat=None, nexp=32):
    P = k.P
    parts = [list(range(0, 12)), list(range(12, 23)), list(range(23, 34))]
    with P.scope():
        g2bc = P.sbuf([128, 2, 1024], F32, "e_g2")
        for r in range(2):
            P.dma("sp", g2bc[:, r, :], modv.ap()[r, 5120:6144].partition_broadcast(128), writes=[g2bc])
        acc = P.sbuf([128, 12, 1024], F32, "e_acc")
        hT = P.sbuf([128, 8, 12 * 128], BF16, "e_hT")
        stg = [P.sbuf([128, 8, 512], F32, "e_stg%d" % i) for i in range(2)]
        wg = [P.sbuf([128, 8, 512], BF16, "e_wg%d" % i) for i in range(2)]
        wu = [P.sbuf([128, 8, 512], BF16, "e_wu%d" % i) for i in range(2)]
        wd = [P.sbuf([128, 4, 1024], BF16, "e_wd%d" % i) for i in range(2)]
        hid = [P.sbuf([128, 4, 512], BF16, "e_hid%d" % i) for i in range(2)]
        sl = [P.sbuf([128, 512], F32, "e_sl%d" % i) for i in range(2)]
        pg = [P.psum([128, 512], F32, "e_pg%d" % i) for i in range(2)]
        pu = [P.psum([128, 512], F32, "e_pu%d" % i) for i in range(2)]
        po = [P.psum([128, 1024], F32, "e_po%d" % i) for i in range(2)]
        xt = [P.sbuf([128, 1024], F32, "e_xt%d" % i) for i in range(2)]
        hv = h2T_d.ap().rearrange("(k p) t -> p k t", p=128)
        si = 0; ci = 0; oi = 0; gi = 0
        for part in parts:
            tiles = [t for t in part if t >= t_lo]
            if not tiles:
                continue
            nt = len(tiles); tok0 = tiles[0] * 128; ntok = nt * 128
            P.dma("sp", hT[:, :, 0:ntok], hv[:, :, tok0:tok0 + ntok], reads=[h2T_d], writes=[hT])
            for e_ in range(nexp):
                wgb, wub, wdb = wg[e_ % 2], wu[e_ % 2], wd[e_ % 2]
                for dst, src in ((wgb, weg[e_].rearrange("(k p) n -> p k n", p=128)), (wub, weu[e_].rearrange("(k p) n -> p k n", p=128))):
                    s = stg[si % 2]; si += 1
                    P.dma("sp", s[:], src, writes=[s])
                    P.op("pool", lambda e, dst=dst, s=s: e.tensor_copy(dst[:], s[:]), reads=[s], writes=[dst])
                s = stg[si % 2]; si += 1
                sv = s[:].rearrange("p k n -> p (k n)").rearrange("p (k n) -> p k n", n=1024)
                P.dma("sp", sv, wed[e_].rearrange("(k p) n -> p k n", p=128), writes=[s])
                P.op("pool", lambda e, wdb=wdb, sv=sv: e.tensor_copy(wdb[:], sv), reads=[s], writes=[wdb])
                for g0 in range(0, nt, 4):
                    gt_ = tiles[g0:g0 + 4]; n = len(gt_) * 128; off = g0 * 128
                    hb = hid[gi % 2]; gi += 1
                    for c in range(4):
                        g_, u_, s_ = pg[ci % 2], pu[ci % 2], sl[ci % 2]; ci += 1
                        for kk in range(8):
                            P.op("pe", lambda e, g_=g_, kk=kk, c=c, wgb=wgb, off=off, n=n: e.matmul(g_[:, 0:n], wgb[:, kk, c * 128:(c + 1) * 128], hT[:, kk, off:off + n],
                                                                                                 start=(kk == 0), stop=(kk == 7)), reads=[wgb, hT], writes=[g_])
                        for kk in range(8):
                            P.op("pe", lambda e, u_=u_, kk=kk, c=c, wub=wub, off=off, n=n: e.matmul(u_[:, 0:n], wub[:, kk, c * 128:(c + 1) * 128], hT[:, kk, off:off + n],
                                                                                                 start=(kk == 0), stop=(kk == 7)), reads=[wub, hT], writes=[u_])
                        P.op("act", lambda e, g_=g_, s_=s_, n=n: e.activation(s_[:, 0:n], g_[:, 0:n], AF.Silu), reads=[g_], writes=[s_])
                        P.op("dve", lambda e, s_=s_, u_=u_, hb=hb, c=c, n=n: e.tensor_tensor(hb[:, c, 0:n], s_[:, 0:n], u_[:, 0:n], ALU.mult), reads=[s_, u_], writes=[hb])
                    for j, t in enumerate(gt_):
                        o_ = po[oi % 2]; oi += 1
                        for half in range(2):
                            for c in range(4):
                                P.op("pe", lambda e, o_=o_, half=half, c=c, j=j, hb=hb, wdb=wdb: e.matmul(o_[:, half * 512:(half + 1) * 512], hb[:, c, j * 128:(j + 1) * 128],
                                                                                                       wdb[:, c, half * 512:(half + 1) * 512], start=(c == 0), stop=(c == 3)),
                                     reads=[hb, wdb], writes=[o_])
                        ai = g0 + j
                        if e_ == 0:
                            P.op("dve", lambda e, o_=o_, ai=ai, t=t: e.tensor_scalar(acc[:, ai, :], o_[:], wgt[:, t, 0:1], None, ALU.mult), reads=[o_, wgt], writes=[acc])
                        else:
                            P.op("dve", lambda e, o_=o_, ai=ai, t=t, e_=e_: e.scalar_tensor_tensor(acc[:, ai, :], o_[:], wgt[:, t, e_:e_ + 1], acc[:, ai, :], ALU.mult, ALU.add),
                                 reads=[o_, wgt, acc], writes=[acc])
            for ai, t in enumerate(tiles):
                r = 0 if t < 2 else 1
                xb = xt[ai % 2]
                P.dma("sp", xb[:], xmid.ap()[t * 128:(t + 1) * 128, :], reads=[xmid], writes=[xb])
                P.op("pool", lambda e, ai=ai, r=r: e.tensor_tensor(acc[:, ai, :], acc[:, ai, :], g2bc[:, r, :], ALU.mult), reads=[acc, g2bc], writes=[acc])
                P.op("pool", lambda e, ai=ai, xb=xb: e.tensor_tensor(xb[:], xb[:], acc[:, ai, :], ALU.add), reads=[acc, xb], writes=[xb])
                if out_lat is not None:
                    if t >= 2:
                        P.dma("sp", out_lat.ap()[(t - 2) * 128:(t - 1) * 128, :], xb[:], reads=[xb], writes=[out_lat])
                else:
                    P.dma("sp", xout.ap()[t * 128:(t + 1) * 128, :], xb[:], reads=[xb], writes=[xout])


def gdn_consts(c):
    t = np.arange(128)[:, None]; i = np.arange(128)[None, :]
    sb = (t // 64) == (i // 64)
    c["g_tri"] = np.stack([(t <= i) & sb, (t >= i) & sb], 1).astype(np.float32)
    c["g_triS"] = np.stack([(t > i) & sb, (t < i) & sb], 1).astype(np.float32)
    c["g_mS"] = np.stack([(i > t) & sb, (i < t) & sb], 1).astype(np.float32)
    c["g_blk"] = np.concatenate([sb.astype(np.float32)[:, None, :],
                                 np.broadcast_to((t < 64), (128, 128)).astype(np.float32)[:, None, :],
                                 np.broadcast_to((t >= 64), (128, 128)).astype(np.float32)[:, None, :]], 1)
    c["zeros"] = np.zeros((1, 128), np.float32)


def phase_gdn(k, z, conv_w, a_log, dt_bias, gnorm, ctx_out):
    P = k.P
    with P.scope():
        tri = P.sbuf([128, 2, 128], F32, "g_tri"); triS = P.sbuf([128, 2, 128], F32, "g_triS")
        mS = P.sbuf([128, 2, 128], F32, "g_mS"); blk = P.sbuf([128, 3, 128], F32, "g_blk")
        for t_, n_ in ((tri, "g_tri"), (triS, "g_triS"), (mS, "g_mS"), (blk, "g_blk")):
            P.dma("sp", t_[:], k.cd[n_].ap(), writes=[t_])
        gnb = bc_load(k, gnorm, 128, "g_gn")
        gall = P.sbuf([128, NT, 8], F32, "g_gall"); ball = P.sbuf([128, NT, 8], F32, "g_ball")
        with P.scope():
            ab = P.sbuf([128, NT, 16], F32, "g_ab")
            zv = z.ap()[:, 5888:5904].rearrange("(t p) c -> p t c", p=128)
            P.dma("sp", ab[:, 0:17, :], zv[:, 0:17, :], writes=[ab]); P.dma("sp", ab[:, 17:34, :], zv[:, 17:34, :], writes=[ab])
            al = bc_load(k, a_log, 8, "g_al"); db = bc_load(k, dt_bias, 8, "g_db")
            P.op("act", lambda e: e.activation(al[:], al[:], AF.Exp), reads=[al], writes=[al])
            P.op("dve", lambda e: e.tensor_scalar(al[:], al[:], -1.0, None, ALU.mult), reads=[al], writes=[al])
            P.op("dve", lambda e: e.tensor_tensor(gall[:], ab[:, :, 0:8], db[:].unsqueeze(1).to_broadcast([128, NT, 8]), ALU.add), reads=[ab, db], writes=[gall])
            P.op("act", lambda e: e.activation(gall[:], gall[:], AF.Exp), reads=[gall], writes=[gall])
            P.op("act", lambda e: e.activation(gall[:], gall[:], AF.Ln, bias=k.one_col[:]), reads=[gall, k.one_col], writes=[gall])
            P.op("dve", lambda e: e.tensor_tensor(gall[:], gall[:], al[:].unsqueeze(1).to_broadcast([128, NT, 8]), ALU.mult), reads=[gall, al], writes=[gall])
            P.op("act", lambda e: e.activation(ball[:], ab[:, :, 8:16], AF.Sigmoid), reads=[ab], writes=[ball])
        q = P.sbuf([128, NT, 128], F32, "g_q"); kk_ = P.sbuf([128, NT, 128], F32, "g_k"); v = P.sbuf([128, NT, 128], F32, "g_v")
        kT = P.sbuf([128, T], F32, "g_kT"); qT = P.sbuf([128, T], F32, "g_qT")
        od = [P.sbuf([128, NT, 128], F32, "g_od%d" % i) for i in range(2)]
        zview = z.ap().rearrange("(t p) c -> p t c", p=128)
        for h in range(4):
            with P.scope():
                xp = P.sbuf([128, NT, 128], F32, "g_xp"); xn = P.sbuf([128, NT, 128], F32, "g_xn"); wcb = P.sbuf([128, 3, 128], F32, "g_wc")
                ss = P.sbuf([128, NT], F32, "g_ss")
                for name, dst in (("gdn_q", q), ("gdn_k", kk_), ("gdn_v", v)):
                    col = ZOFF[name] + h * 128
                    zc = zview[:, :, col:col + 128]
                    for lo, hi in ((0, 17), (17, 34)):
                        P.dma("sp", dst[:, lo:hi, :], zc[:, lo:hi, :], writes=[dst])
                        P.dma("sp", xp[1:128, lo:hi, :], zc[0:127, lo:hi, :], writes=[xp])
                        P.dma("sp", xn[0:127, lo:hi, :], zc[1:128, lo:hi, :], writes=[xn])
                    P.dma("sp", xp[0:1, 1:34, :], zc[127:128, 0:33, :], writes=[xp])
                    P.dma("sp", xn[127:128, 0:33, :], zc[0:1, 1:34, :], writes=[xn])
                    for tt in (0, 2):
                        P.dma("sp", xp[0:1, tt, :], k.cd["zeros"].ap(), writes=[xp])
                    for tt in (1, 33):
                        P.dma("sp", xn[127:128, tt, :], k.cd["zeros"].ap(), writes=[xn])
                    for j in range(3):
                        P.dma("sp", wcb[:, j, :], conv_w[j, col - ZOFF["gdn_q"]:col - ZOFF["gdn_q"] + 128].partition_broadcast(128), writes=[wcb])
                    bs = [128, NT, 128]
                    P.op("dve", lambda e, dst=dst: e.tensor_tensor(dst[:], dst[:], wcb[:, 1, :].unsqueeze(1).to_broadcast(bs), ALU.mult), reads=[dst, wcb], writes=[dst])
                    P.op("pool", lambda e: e.tensor_tensor(xp[:], xp[:], wcb[:, 0, :].unsqueeze(1).to_broadcast(bs), ALU.mult), reads=[xp, wcb], writes=[xp])
                    P.op("dve", lambda e: e.tensor_tensor(xn[:], xn[:], wcb[:, 2, :].unsqueeze(1).to_broadcast(bs), ALU.mult), reads=[xn, wcb], writes=[xn])
                    P.op("pool", lambda e, dst=dst: e.tensor_tensor(dst[:], dst[:], xp[:], ALU.add), reads=[dst, xp], writes=[dst])
                    P.op("dve", lambda e, dst=dst: e.tensor_tensor(dst[:], dst[:], xn[:], ALU.add), reads=[dst, xn], writes=[dst])
                    P.op("act", lambda e, dst=dst: e.activation(dst[:], dst[:], AF.Silu), reads=[dst], writes=[dst])
                    if name != "gdn_v":
                        P.op("pool", lambda e, dst=dst: e.tensor_tensor(xp[:], dst[:], dst[:], ALU.mult), reads=[dst], writes=[xp])
                        P.op("dve", lambda e: e.tensor_reduce(ss[:], xp[:], AX.X, ALU.add), reads=[xp], writes=[ss])
                        P.op("act", lambda e: e.activation(ss[:], ss[:], AF.Sqrt, bias=k.eps_col[:]), reads=[ss, k.eps_col], writes=[ss])
                        P.op("dve", lambda e: e.reciprocal(ss[:], ss[:]), reads=[ss], writes=[ss])
                        if name == "gdn_q":
                            P.op("dve", lambda e: e.tensor_scalar(ss[:], ss[:], 128.0 ** -0.5, None, ALU.mult), reads=[ss], writes=[ss])
                        P.op("dve", lambda e, dst=dst: e.tensor_tensor(dst[:], dst[:], ss[:].unsqueeze(2).to_broadcast(bs), ALU.mult), reads=[dst, ss], writes=[dst])
            transpose_to(k, lambda t: q[:, t, :], 128, qT, "g_tq", src_bufs=[q])
            transpose_to(k, lambda t: kk_[:, t, :], 128, kT, "g_tk", src_bufs=[kk_])
            with P.scope():
                psO = [P.psum([128, 512], F32, "g_psO%d" % i) for i in range(2)]
                psW = [P.psum([128, 512], F32, "g_psW%d" % i) for i in range(2)]
                psM = [P.psum([128, 512], F32, "g_psM%d" % i) for i in range(4)]
                gens = [gdn_dir(k, dr, h, q, kk_, v, qT, kT, gall, ball, tri, triS, mS, blk, od[dr], psO[dr], psW[dr], psM[2 * dr:2 * dr + 2]) for dr in range(2)]
                alive = [True, True]
                while any(alive):
                    for dr in range(2):
                        if alive[dr]:
                            try:
                                next(gens[dr])
                            except StopIteration:
                                alive[dr] = False
            with P.scope():
                gt = P.sbuf([128, NT, 128], F32, "g_gate"); ss = P.sbuf([128, NT], F32, "g_fss"); sq = P.sbuf([128, NT, 128], F32, "g_fsq")
                zc = zview[:, :, ZOFF["gdn_g"] + h * 128:ZOFF["gdn_g"] + (h + 1) * 128]
                P.dma("sp", gt[:, 0:17, :], zc[:, 0:17, :], writes=[gt]); P.dma("sp", gt[:, 17:34, :], zc[:, 17:34, :], writes=[gt])
                o = od[0]; bs = [128, NT, 128]
                P.op("dve", lambda e: e.tensor_tensor(o[:], o[:], od[1][:], ALU.add), reads=[o, od[1]], writes=[o])
                P.op("pool", lambda e: e.tensor_tensor(sq[:], o[:], o[:], ALU.mult), reads=[o], writes=[sq])
                P.op("dve", lambda e: e.tensor_reduce(ss[:], sq[:], AX.X, ALU.add), reads=[sq], writes=[ss])
                P.op("act", lambda e: e.activation(ss[:], ss[:], AF.Sqrt, bias=k.eps_col[:], scale=1.0 / 128), reads=[ss, k.eps_col], writes=[ss])
                P.op("dve", lambda e: e.reciprocal(ss[:], ss[:]), reads=[ss], writes=[ss])
                P.op("dve", lambda e: e.tensor_tensor(o[:], o[:], ss[:].unsqueeze(2).to_broadcast(bs), ALU.mult), reads=[o, ss], writes=[o])
                P.op("pool", lambda e: e.tensor_tensor(o[:], o[:], gnb[:].unsqueeze(1).to_broadcast(bs), ALU.mult), reads=[o, gnb], writes=[o])
                P.op("act", lambda e: e.activation(gt[:], gt[:], AF.Silu), reads=[gt], writes=[gt])
                P.op("dve", lambda e: e.tensor_tensor(o[:], o[:], gt[:], ALU.mult), reads=[o, gt], writes=[o])
                store_head(k, o, k.ybr.ap()[3], h * 128, 0 if ctx_out else 2)


def gdn_dir(k, dr, h, q, kk_, v, qT, kT, gall, ball, tri, triS, mS, blk, od, psO, psW, psM):
    P = k.P
    c = dr * 4 + h
    tg = "g%d_" % dr
    A = lambda nm, shape=(128, 128), n=2: [P.sbuf(list(shape), F32, tg + nm + str(i)) for i in range(n)]
    gcs = P.sbuf([128, 4, NT], F32, tg + "gcs")
    cdb = P.sbuf([128, 2, NT], F32, tg + "cdb")
    gc = gall[:, :, c]; bt = ball[:, :, c]
    ident = k.ident_f
    pm = psM[0]
    P.op("pe", lambda e: e.matmul(pm[:, 0:NT], tri[:, dr, :], gc, start=True, stop=True), reads=[tri, gall], writes=[pm]); yield
    P.op("pe", lambda e: e.matmul(pm[:, 64:64 + NT], blk[:, 0, :], gc, start=True, stop=True), reads=[blk, gall], writes=[pm]); yield
    P.op("pe", lambda e: e.matmul(pm[:, 128:128 + NT], blk[:, 1, :], gc, start=True, stop=True), reads=[blk, gall], writes=[pm]); yield
    P.op("pe", lambda e: e.matmul(pm[:, 192:192 + NT], blk[:, 2, :], gc, start=True, stop=True), reads=[blk, gall], writes=[pm]); yield
    P.op("dve", lambda e: e.tensor_copy(gcs[:, 0, :], pm[:, 0:NT]), reads=[pm], writes=[gcs]); yield
    P.op("act", lambda e: e.activation(gcs[:, 1, :], pm[:, 0:NT], AF.Exp), reads=[pm], writes=[gcs]); yield
    P.op("dve", lambda e: e.tensor_tensor(gcs[:, 2, :], pm[:, 64:64 + NT], gcs[:, 0, :], ALU.subtract), reads=[pm, gcs], writes=[gcs]); yield
    P.op("act", lambda e: e.activation(gcs[:, 2, :], gcs[:, 2, :], AF.Exp), reads=[gcs], writes=[gcs]); yield
    P.op("act", lambda e: e.activation(cdb[:, 0, :], pm[:, 128:128 + NT], AF.Exp), reads=[pm], writes=[cdb]); yield
    P.op("act", lambda e: e.activation(cdb[:, 1, :], pm[:, 192:192 + NT], AF.Exp), reads=[pm], writes=[cdb]); yield
    kb, kbg, vb, kend, qd, gtri = A("kb"), A("kbg"), A("vb"), A("kend"), A("qd"), A("gtri")
    EDs, EDi, kbT, AT, aqkT = A("EDs"), A("EDi"), A("kbT"), A("AT"), A("aqkT")
    Ap, Bp, Pm = A("Ap", n=3), A("Bp", n=3), A("Pm", n=3)
    u, wT, vnew = A("u"), A("wT"), A("vnew")
    qdTm = [P.sbuf([128, 2, 128], F32, tg + "qdTm%d" % i) for i in range(2)]
    for b_ in qdTm:
        P.op("pool", lambda e, b_=b_: e.memset(b_[:], 0.0), writes=[b_]); yield
    S = [P.sbuf([128, 128], F32, tg + "S%d" % i) for i in range(2)]
    P.op("pool", lambda e: e.memset(S[0][:], 0.0), writes=[S[0]]); yield
    Scur = S[0]; si = 0
    order = list(range(NT)) if dr == 0 else [1, 0] + list(range(NT - 1, 1, -1))
    slot = [0]

    def ms():
        s_ = slot[0]; slot[0] += 1
        bank = psM[(s_ // 4) % 2]; o_ = (s_ % 4) * 128
        return bank, bank[:, o_:o_ + 128]

    for it, n in enumerate(order):
        r2 = it % 2
        sl = slice(n * 128, (n + 1) * 128)
        kt, qt, vt = kk_[:, n, :], q[:, n, :], v[:, n, :]
        bcol = ball[:, n, c:c + 1]; gcol = gall[:, n, c:c + 1]
        eg = gcs[:, 1, n:n + 1]; ek = gcs[:, 2, n:n + 1]
        kb_, kbg_, vb_, kend_, qd_, gtri_ = kb[r2], kbg[r2], vb[r2], kend[r2], qd[r2], gtri[r2]
        P.op("dve", lambda e: e.tensor_scalar(kb_[:], kt, bcol, None, ALU.mult), reads=[kk_, ball], writes=[kb_]); yield
        P.op("pool", lambda e: e.tensor_scalar(kbg_[:], kb_[:], eg, None, ALU.mult), reads=[kb_, gcs], writes=[kbg_]); yield
        P.op("pool", lambda e: e.tensor_scalar(vb_[:], vt, bcol, None, ALU.mult), reads=[v, ball], writes=[vb_]); yield
        P.op("pool", lambda e: e.tensor_scalar(kend_[:], kt, ek, None, ALU.mult), reads=[kk_, gcs], writes=[kend_]); yield
        P.op("dve", lambda e: e.tensor_scalar(qd_[:], qt, eg, None, ALU.mult), reads=[q, gcs], writes=[qd_]); yield
        P.op("dve", lambda e: e.tensor_scalar(gtri_[:], tri[:, dr, :], gcol, None, ALU.mult), reads=[tri, gall], writes=[gtri_]); yield
        bk, p_ = ms()
        P.op("pe", lambda e: e.matmul(p_, triS[:, dr, :], gtri_[:], start=True, stop=True), reads=[triS, gtri_], writes=[bk]); yield
        EDs_, EDi_ = EDs[r2], EDi[r2]
        P.op("act", lambda e: e.activation(EDs_[:], p_, AF.Exp), reads=[bk], writes=[EDs_]); yield
        P.op("pool", lambda e: e.tensor_tensor(EDs_[:], EDs_[:], mS[:, dr, :], ALU.mult), reads=[EDs_, mS], writes=[EDs_]); yield
        P.op("pool", lambda e: e.tensor_tensor(EDi_[:], EDs_[:], ident[:], ALU.add), reads=[EDs_, ident], writes=[EDi_]); yield
        bk, p_ = ms(); kbT_ = kbT[r2]
        P.op("pe", lambda e: e.transpose(p_, kb_[:], ident[:]), reads=[kb_, ident], writes=[bk]); yield
        P.op("act", lambda e: e.copy(kbT_[:], p_), reads=[bk], writes=[kbT_]); yield
        bk, p_ = ms(); AT_ = AT[r2]
        P.op("pe", lambda e: e.matmul(p_, kT[:, sl], kbT_[:], start=True, stop=True), reads=[kT, kbT_], writes=[bk]); yield
        P.op("dve", lambda e: e.tensor_tensor(AT_[:], p_, EDs_[:], ALU.mult), reads=[bk, EDs_], writes=[AT_]); yield
        bk, p_ = ms(); aqkT_ = aqkT[r2]
        P.op("pe", lambda e: e.matmul(p_, kT[:, sl], qT[:, sl], start=True, stop=True), reads=[kT, qT], writes=[bk]); yield
        P.op("dve", lambda e: e.tensor_tensor(aqkT_[:], p_, EDi_[:], ALU.mult), reads=[bk, EDi_], writes=[aqkT_]); yield
        bk, p_ = ms(); pi = 0
        Ac, Bc, Pc = Ap[0], AT_, Pm[0]
        P.op("pe", lambda e: e.transpose(p_, AT_[:], ident[:]), reads=[AT_, ident], writes=[bk]); yield
        P.op("act", lambda e: e.copy(Ac[:], p_), reads=[bk], writes=[Ac]); yield
        P.op("dve", lambda e: e.tensor_tensor(Pc[:], ident[:], AT_[:], ALU.subtract), reads=[ident, AT_], writes=[Pc]); yield
        for lv in range(1, 6):
            An, Bn, Pn = Ap[lv % 3], Bp[lv % 3], Pm[lv % 3]
            bk, p_ = ms()
            P.op("pe", lambda e, p_=p_, Ac=Ac, Bc=Bc: e.matmul(p_, Bc[:], Ac[:], start=True, stop=True), reads=[Ac, Bc], writes=[bk]); yield
            P.op("act", lambda e, p_=p_, An=An: e.copy(An[:], p_), reads=[bk], writes=[An]); yield
            if lv < 5:
                bk2, p2 = ms()
                P.op("pe", lambda e, p2=p2, Ac=Ac, Bc=Bc: e.matmul(p2, Ac[:], Bc[:], start=True, stop=True), reads=[Ac, Bc], writes=[bk2]); yield
                P.op("dve", lambda e, p2=p2, Bn=Bn: e.tensor_copy(Bn[:], p2), reads=[bk2], writes=[Bn]); yield
            bk3, p3 = ms()
            P.op("pe", lambda e, p3=p3, Pc=Pc: e.matmul(p3, ident[:], Pc[:], start=True, stop=False), reads=[ident, Pc], writes=[bk3]); yield
            P.op("pe", lambda e, p3=p3, Pc=Pc, An=An: e.matmul(p3, An[:], Pc[:], start=False, stop=True), reads=[An, Pc], writes=[bk3]); yield
            P.op("dve", lambda e, p3=p3, Pn=Pn: e.tensor_copy(Pn[:], p3), reads=[bk3], writes=[Pn]); yield
            Ac, Bc, Pc = An, Bn, Pn
        MT = Pc
        u_, wT_, vnew_, qdTm_ = u[r2], wT[r2], vnew[r2], qdTm[r2]
        bk, p_ = ms()
        P.op("pe", lambda e: e.matmul(p_, MT[:], vb_[:], start=True, stop=True), reads=[MT, vb_], writes=[bk]); yield
        P.op("act", lambda e: e.copy(u_[:], p_), reads=[bk], writes=[u_]); yield
        bk, p_ = ms()
        P.op("pe", lambda e: e.matmul(p_, kbg_[:], MT[:], start=True, stop=True), reads=[MT, kbg_], writes=[bk]); yield
        P.op("dve", lambda e: e.tensor_copy(wT_[:], p_), reads=[bk], writes=[wT_]); yield
        bk, p_ = ms()
        P.op("pe", lambda e: e.transpose(p_, qd_[:], ident[:]), reads=[qd_, ident], writes=[bk]); yield
        P.op("act", lambda e: e.copy(qdTm_[:, 0, 0:64], p_[:, 0:64]), reads=[bk], writes=[qdTm_]); yield
        P.op("dve", lambda e: e.tensor_copy(qdTm_[:, 1, 64:128], p_[:, 64:128]), reads=[bk], writes=[qdTm_]); yield
        pO = psO[:, 0:128]
        for bi, b in enumerate((0, 1) if dr == 0 else (1, 0)):
            rb = slice(b * 64, (b + 1) * 64)
            pW = psW[:, 0:128]; pKV = psW[:, 128:256]
            Sn = S[(si + 1) % 2]
            P.op("pe", lambda e, Scur=Scur: e.matmul(pW, wT_[:], Scur[:], start=True, stop=True), reads=[wT_, Scur], writes=[psW]); yield
            P.op("dve", lambda e, rb=rb: e.tensor_tensor(vnew_[rb, :], u_[rb, :], pW[rb, :], ALU.subtract), reads=[u_, psW], writes=[vnew_]); yield
            P.op("pe", lambda e, b=b, bi=bi, Scur=Scur: e.matmul(pO, qdTm_[:, b, :], Scur[:], start=(bi == 0), stop=False), reads=[qdTm_, Scur], writes=[psO]); yield
            P.op("pe", lambda e, rb=rb, bi=bi: e.matmul(pO, aqkT_[rb, :], vnew_[rb, :], start=False, stop=(bi == 1)), reads=[aqkT_, vnew_], writes=[psO]); yield
            P.op("pe", lambda e, rb=rb: e.matmul(pKV, kend_[rb, :], vnew_[rb, :], start=True, stop=True), reads=[kend_, vnew_], writes=[psW]); yield
            P.op("dve", lambda e, b=b, Scur=Scur, Sn=Sn: e.scalar_tensor_tensor(Sn[:], Scur[:], cdb[:, b, n:n + 1], pKV, ALU.mult, ALU.add),
                 reads=[Scur, cdb, psW], writes=[Sn]); yield
            Scur = Sn; si += 1
        P.op("act", lambda e: e.copy(od[:, n, :], pO), reads=[psO], writes=[od]); yield


from concourse.bass_utils import run_bass_kernel_spmd

DEPTH = 2
W_NAMES = ["w_mod", "b_mod", "norm1", "norm2", "w_in", "ret_decay", "ret_gn", "win_qnorm", "win_knorm", "win_sink",
           "na_qnorm", "na_knorm", "nab", "gdn_conv", "gdn_a_log", "gdn_dt_bias", "gdn_norm", "w_branch", "w_merge", "w_out",
           "w_router", "router_bias", "w_e_gate", "w_e_up", "w_e_down"]
W_SHAPES = dict(w_mod=[2, D, 6144], b_mod=[2, 6144], norm1=[2, D], norm2=[2, D], w_in=[2, D, DIN], ret_decay=[2, 8], ret_gn=[2, 512],
                win_qnorm=[2, 64], win_knorm=[2, 64], win_sink=[2, 8], na_qnorm=[2, 64], na_knorm=[2, 64], nab=[2, 8, 128, 21, 128],
                gdn_conv=[2, 3, 1536], gdn_a_log=[2, 8], gdn_dt_bias=[2, 8], gdn_norm=[2, 128], w_branch=[2, 4, 512, D],
                w_merge=[2, D, 4096], w_out=[2, D, D], w_router=[D, 32], router_bias=[32], w_e_gate=[2, 32, D, 512],
                w_e_up=[2, 32, D, 512], w_e_down=[2, 32, 512, D])


LAYERED = [n for n in W_NAMES if n not in ("w_router", "router_bias")]


def build_program(fused=True):
    nc = bass.Bass("TRN2", target_bir_lowering=False)
    P = Prog(nc)
    k = K(P)
    nl = DEPTH if fused else 1
    cd = {n: P.dram("c_" + n, list(a.shape), F32, kind="ExternalInput") for n, a in mk_consts().items()}
    xin = P.dram("xin", [T, D], F32, kind="ExternalInput")
    cc = P.dram("cc", [2, D], F32, kind="ExternalInput")
    W = {n: P.dram(n, (W_SHAPES[n] if (fused or n not in LAYERED) else W_SHAPES[n][1:]), F32, kind="ExternalInput") for n in W_NAMES}
    if fused:
        out = P.dram("out", [4096, D], F32, kind="ExternalOutput")
        x1 = P.dram("x1", [T, D], F32)
    else:
        out = None
        x1 = P.dram("xo", [T, D], F32, kind="ExternalOutput")
    modv = [P.dram("modv%d" % l, [2, 6144], F32) for l in range(nl)]
    hT_d = P.dram("hT_d", [D, T], BF16)
    z = P.dram("z", [T, DIN], F32)
    k.ybr = P.dram("ybr", [4, T, 512], F32)
    xmid = P.dram("xmid", [T, D], F32)
    h2T_d = P.dram("h2T_d", [D, T], BF16)
    h32_d = P.dram("h32_d", [D, T], F32)
    load_consts(k, cd)
    mT = [P.sbuf([128, 48, 2], F32, "mT%d" % l) for l in range(nl)]

    def wsub(n, l):
        return _Sub(W[n], l) if (fused and n in LAYERED) else W[n]

    for l in range(nl):
        phase_mod(k, cc, wsub("w_mod", l), wsub("b_mod", l), modv[l], mT[l])
    xs = [xin, x1]
    for l in range(nl):
        ctx_out = (l < DEPTH - 1) if fused else True
        x = xs[l]
        w = lambda n: wsub(n, l).ap()
        with P.scope():
            hsb = P.sbuf([128, 8, T], BF16, "hsb")
            phase_norm(k, x, w("norm1"), mT[l], 0, hsb)
            for kk in range(8):
                P.dma("sp", hT_d.ap()[kk * 128:(kk + 1) * 128, :], hsb[:, kk, :], reads=[hsb], writes=[hT_d])
            phase_proj(k, hsb, w("w_in"), z, DIN)
        phase_ret(k, z, w("ret_decay"), w("ret_gn"), ctx_out)
        phase_window(k, z, w("win_qnorm"), w("win_knorm"), w("win_sink"), ctx_out)
        phase_na(k, z, w("na_qnorm"), w("na_knorm"), w("nab"), ctx_out)
        phase_gdn(k, z, w("gdn_conv"), w("gdn_a_log"), w("gdn_dt_bias"), w("gdn_norm"), ctx_out)
        phase_merge(k, hT_d, x, xmid, w("w_merge"), w("w_branch"), w("w_out"), modv[l], ctx_out)
        t_lo = 0 if ctx_out else 2
        with P.scope():
            h2 = P.sbuf([128, 8, T], BF16, "h2sb")
            phase_norm(k, xmid, w("norm2"), mT[l], 1, h2, hT32_d=h32_d, t_lo=t_lo)
            for kk in range(8):
                P.dma("sp", h2T_d.ap()[kk * 128:(kk + 1) * 128, t_lo * 128:], h2[:, kk, t_lo * 128:], reads=[h2], writes=[h2T_d])
        with P.scope():
            wgt = P.sbuf([128, NT, 32], F32, "wgt")
            phase_router(k, h32_d, W["w_router"].ap(), W["router_bias"].ap(), wgt, t_lo)
            phase_moe(k, h2T_d, wgt, xmid, x1, w("w_e_gate"), w("w_e_up"), w("w_e_down"), modv[l], t_lo,
                      out_lat=(out if (fused and l == DEPTH - 1) else None))
    P.wait_all("sp", [out] if fused else [x1])
    P.emit()
    return nc, P


class _Sub:
    def __init__(self, parent, l):
        self.p = parent; self.l = l
        self.w = parent.w; self.r = parent.r

    def ap(self):
        return self.p.ap()[self.l]


def _sub(buf, l):
    return _Sub(buf, l)


_CACHE = {}
FUSED = False


def host_inputs(inputs, layer=None, x_prev=None):
    f = lambda a: np.ascontiguousarray(np.asarray(a, dtype=np.float32))
    shared = {"c_" + n: a for n, a in mk_consts().items()}
    for n in W_NAMES:
        if n == "nab":
            full = np.stack([na_bias_gather(f(inputs["na_rpb"][l])) for l in range(DEPTH)])
        else:
            full = f(inputs[n]).reshape(W_SHAPES[n])
        shared[n] = full if (layer is None or n not in LAYERED) else np.ascontiguousarray(full[layer])
    maps = []
    for b in range(8):
        m = dict(shared)
        if x_prev is None:
            m["xin"] = np.concatenate([f(inputs["ctx"][b]), f(inputs["x"][b])], axis=0)
        else:
            m["xin"] = x_prev[b]
        m["cc"] = np.stack([f(inputs["c_ctx"]), f(inputs["c"][b])])
        maps.append(m)
    return maps


def kernel(**inputs):
    if "nc" not in _CACHE:
        _CACHE["nc"] = build_program(FUSED)[0]
    nc = _CACHE["nc"]
    cores = list(range(8))
    if FUSED:
        res = run_bass_kernel_spmd(nc, host_inputs(inputs), core_ids=cores)
        return np.stack([np.asarray(r["out"], dtype=np.float32) for r in res.results], axis=0)
    xp = None
    for l in range(DEPTH):
        res = run_bass_kernel_spmd(nc, host_inputs(inputs, layer=l, x_prev=xp), core_ids=cores)
        xp = [np.asarray(r["xo"], dtype=np.float32) for r in res.results]
    return np.stack([x[LC:] for x in xp], axis=0)
```

```python
from contextlib import ExitStack
import numpy as np
import concourse.bass as bass
import concourse.mybir as mybir

F32 = mybir.dt.float32
BF16 = mybir.dt.bfloat16
I32 = mybir.dt.int32
AF = mybir.ActivationFunctionType
ALU = mybir.AluOpType
AX = mybir.AxisListType

ENGS = ("pe", "dve", "act", "pool", "sp")
N_DSEM = 8
SEM_WRAP = 30000


class Buf:
    def __init__(self, t, name=""):
        self.t = t
        self.name = name
        self.w = {}
        self.r = {}

    v = None
    is_psum = False

    def __getitem__(self, idx):
        if self.v is not None:
            return self.v[idx]
        return self.t[idx]

    def ap(self):
        return self.t.ap() if hasattr(self.t, "ap") else self.t[:]


class Prog:
    def __init__(self, nc, same_engine_sync=True, direct=True):
        self.nc = nc
        self.es = ExitStack()
        self.ops = {e: [] for e in ENGS}
        self.cnt = {e: 0 for e in ENGS}
        self.seen = {e: {} for e in ENGS}
        self.sems = {}
        self.same = same_engine_sync
        self.dma_n = {e: 0 for e in ENGS}
        self.uid = 0
        self.stacks = [self.es]
        self.scope_bufs = [[]]
        self.free_deps = {}
        self.direct = direct
        self.nops = {}
        self.engobj = {"pe": nc.tensor, "dve": nc.vector, "act": nc.scalar, "pool": nc.gpsimd, "sp": nc.sync}

    def scope(self):
        prog = self

        class _S:
            def __enter__(s):
                st = ExitStack()
                prog.stacks.append(st)
                prog.scope_bufs.append([])
                return s

            def __exit__(s, *a):
                st = prog.stacks.pop()
                for b in prog.scope_bufs.pop():
                    for dd in (b.w, b.r):
                        for kk, v in dd.items():
                            prog.free_deps[kk] = max(prog.free_deps.get(kk, 0), v)
                st.close()
                return False
        return _S()

    def sem(self, key):
        if key not in self.sems:
            self.sems[key] = self.es.enter_context(self.nc.semaphore("s_%s_%s_%d" % key))
        return self.sems[key]

    def sbuf(self, shape, dt, name=None):
        self.uid += 1
        name = "%s_%d" % (name or "sb", self.uid)
        t = self.stacks[-1].enter_context(self.nc.sbuf_tensor(name, list(shape), dt))
        b = Buf(t, name)
        b.w = dict(self.free_deps)
        self.scope_bufs[-1].append(b)
        return b

    def psum(self, shape, dt=F32, name=None):
        self.uid += 1
        name = "%s_%d" % (name or "ps", self.uid)
        p, n = shape
        nb = (n * 4 + 2047) // 2048
        t = self.stacks[-1].enter_context(self.nc.psum_tensor(name, [128, nb * 512], F32))
        b = Buf(t, name)
        b.v = t[0:p, 0:n]
        b.is_psum = True
        b.w = dict(self.free_deps)
        self.scope_bufs[-1].append(b)
        return b

    def dram(self, name, shape, dt, kind="Internal"):
        t = self.nc.dram_tensor(name, list(shape), dt, kind=kind)
        return Buf(t, name)

    def _waits(self, eng, reads, writes):
        need = {}
        for b in reads:
            for k, v in b.w.items():
                need[k] = max(need.get(k, 0), v)
        for b in writes:
            for k, v in b.w.items():
                need[k] = max(need.get(k, 0), v)
            for k, v in b.r.items():
                need[k] = max(need.get(k, 0), v)
        out = []
        seen = self.seen[eng]
        for k, v in need.items():
            if k[0] == eng and k[1] != "d":
                if eng == "pe" or not self.same:
                    continue
            if seen.get(k, 0) >= v:
                continue
            seen[k] = v
            out.append((k, v))
        return out

    def op(self, eng, fn, reads=(), writes=()):
        pr = [b for b in reads if b.is_psum]
        if pr:
            writes = list(writes) + pr
        waits = self._waits(eng, reads, writes)
        self.cnt[eng] += 1
        n = self.cnt[eng]
        key = (eng, "c", (n - 1) // SEM_WRAP)
        val = (n - 1) % SEM_WRAP + 1
        self._put(eng, waits, fn, key, 1)
        for b in reads:
            b.r[key] = max(b.r.get(key, 0), val)
        for b in writes:
            b.w[key] = max(b.w.get(key, 0), val)

    def dma(self, q, out_ap, in_ap, reads=(), writes=(), **kw):
        waits = self._waits(q, reads, writes)
        n = self.dma_n[q]
        self.dma_n[q] += 1
        key = (q, "d", n % N_DSEM)
        val = 16 * (n // N_DSEM + 1)
        if val > 16 and self.seen[q].get(key, 0) < val - 16:
            self.seen[q][key] = val - 16
            waits.append((key, val - 16))

        def fn(e, out_ap=out_ap, in_ap=in_ap, kw=kw):
            return e.dma_start(out=out_ap, in_=in_ap, **kw)

        self._put(q, waits, fn, key, 16)
        for b in reads:
            b.r[key] = max(b.r.get(key, 0), val)
        for b in writes:
            b.w[key] = max(b.w.get(key, 0), val)

    def wait_all(self, eng, bufs):
        need = {}
        for b in bufs:
            for k, v in b.w.items():
                need[k] = max(need.get(k, 0), v)
        self._put(eng, list(need.items()), None, None, 0)

    def _put(self, eng, waits, fn, key, inc):
        if not self.direct:
            self.ops[eng].append((waits, fn, key, inc))
            return
        self.nops[eng] = self.nops.get(eng, 0) + 1
        e = self.engobj[eng]
        for k, v in waits:
            e.wait_ge(self.sem(k), v)
        if fn is not None:
            fn(e).then_inc(self.sem(key), inc)

    def emit(self):
        nc = self.nc
        if self.direct:
            self.es.close()
            return
        for e in ENGS:
            for waits, fn, key, inc in self.ops[e]:
                for k, v in waits:
                    self.sem(k)
                if key is not None:
                    self.sem(key)
        with nc.Block() as block:
            def run(engname):
                def body(eng):
                    for waits, fn, key, inc in self.ops[engname]:
                        for k, v in waits:
                            eng.wait_ge(self.sems[k], v)
                        if fn is not None:
                            fn(eng).then_inc(self.sems[key], inc)
                return body
            if self.ops["sp"]:
                block.sync(run("sp"))
            if self.ops["pe"]:
                block.tensor(run("pe"))
            if self.ops["dve"]:
                block.vector(run("dve"))
            if self.ops["act"]:
                block.scalar(run("act"))
            if self.ops["pool"]:
                block.gpsimd(run("pool"))
        self.es.close()


D = 1024
T = 4352
NT = T // 128
LC = 256
DIN = 5904
EPS = 1e-6


class K:
    def __init__(self, P):
        self.P = P
        self.rr = 0

    def evac_engine(self):
        self.rr += 1
        return "act" if self.rr % 2 else "dve"


def mk_consts():
    c = {}
    c["ident_f"] = np.eye(128, dtype=np.float32)
    cos, sin = rope_tables()
    c["cos"] = cos; c["sin"] = sin
    j = np.arange(128)[:, None]; i = np.arange(128)[None, :]
    c["maskPrev"] = (i <= j).astype(np.float32)
    c["maskNext"] = (j <= i).astype(np.float32)
    ret_consts(c)
    gdn_consts(c)
    return c


def load_consts(k, cd):
    P = k.P
    k.ident_f = P.sbuf([128, 128], F32, "ident_f")
    P.dma("sp", k.ident_f[:], cd["ident_f"].ap(), writes=[k.ident_f])
    k.ident_b = P.sbuf([128, 128], BF16, "ident_b")
    P.op("dve", lambda e: e.tensor_copy(k.ident_b[:], k.ident_f[:]), reads=[k.ident_f], writes=[k.ident_b])
    k.cos = P.sbuf([128, 32, 32], F32, "cos"); k.sin = P.sbuf([128, 32, 32], F32, "sin")
    P.dma("sp", k.cos[:], cd["cos"].ap().rearrange("(t p) f -> p t f", p=128), writes=[k.cos])
    P.dma("sp", k.sin[:], cd["sin"].ap().rearrange("(t p) f -> p t f", p=128), writes=[k.sin])
    k.maskPrev = P.sbuf([128, 128], BF16, "maskPrev"); k.maskNext = P.sbuf([128, 128], BF16, "maskNext")
    with P.scope():
        mst = P.sbuf([128, 2, 128], F32, "mst")
        P.dma("sp", mst[:, 0, :], cd["maskPrev"].ap(), writes=[mst])
        P.dma("sp", mst[:, 1, :], cd["maskNext"].ap(), writes=[mst])
        P.op("dve", lambda e: e.tensor_copy(k.maskPrev[:], mst[:, 0, :]), reads=[mst], writes=[k.maskPrev])
        P.op("dve", lambda e: e.tensor_copy(k.maskNext[:], mst[:, 1, :]), reads=[mst], writes=[k.maskNext])
    k.cd = cd
    k.one_col = P.sbuf([128, 1], F32, "one_col")
    P.op("dve", lambda e: e.memset(k.one_col[:], 1.0), writes=[k.one_col])
    k.eps_col = P.sbuf([128, 1], F32, "eps_col")
    P.op("dve", lambda e: e.memset(k.eps_col[:], EPS), writes=[k.eps_col])


def load_w_bf16(k, dst, dst_ap, src_ap, stage, stage_ap):
    P = k.P
    P.dma("sp", stage_ap, src_ap, writes=[stage])
    P.op("pool", lambda e: e.tensor_copy(dst_ap, stage_ap), reads=[stage], writes=[dst])


def cols_from_rows(k, dst_ap, rows_ap, R, tag):
    P = k.P
    with P.scope():
        st = P.sbuf([R, 128], F32, tag + "_rows")
        ps = P.psum([128, R], F32, tag + "_ps")
        P.dma("sp", st[:], rows_ap, writes=[st])
        P.op("pe", lambda e: e.transpose(ps[:], st[:], k.ident_f[0:R, 0:R]), reads=[st, k.ident_f], writes=[ps])
        return st, ps


def phase_mod(k, cc, w_mod, b_mod, modv, mT):
    P = k.P
    with P.scope():
        ccr = P.sbuf([16, 128], F32, "ccr")
        bcr = P.sbuf([48, 128], F32, "bcr")
        pst = P.psum([128, 64], F32, "mod_pst")
        P.dma("sp", ccr[:], cc.ap().rearrange("r (k p) -> (r k) p", p=128), writes=[ccr])
        P.dma("sp", bcr[:], b_mod.ap().rearrange("(c p) -> c p", p=128), writes=[bcr])
        P.op("pe", lambda e: e.transpose(pst[:, 0:16], ccr[:], k.ident_f[0:16, 0:16]), reads=[ccr, k.ident_f], writes=[pst])
        P.op("pe", lambda e: e.transpose(pst[:, 16:64], bcr[:], k.ident_f[0:48, 0:48]), reads=[bcr, k.ident_f], writes=[pst])
        sT = P.sbuf([128, 2, 8], F32, "sccT")
        bcol = P.sbuf([128, 48], F32, "bcol")
        P.op("act", lambda e: e.activation(sT[:].rearrange("p r k -> p (r k)"), pst[:, 0:16], AF.Silu), reads=[pst], writes=[sT])
        P.op("dve", lambda e: e.tensor_copy(bcol[:], pst[:, 16:64]), reads=[pst], writes=[bcol])
        wst = [P.sbuf([128, 8, 512], F32, "wmod_st%d" % i) for i in range(2)]
        ps = P.psum([128, 96], F32, "ps_mod")
        wv = w_mod.ap().rearrange("(k p) n -> p k n", p=128)
        for nb in range(12):
            w = wst[nb % 2]
            P.dma("sp", w[:], wv[:, :, nb * 512:(nb + 1) * 512], writes=[w])
            for cl in range(4):
                c = nb * 4 + cl
                for kk in range(8):
                    P.op("pe", lambda e, kk=kk, w=w, c=c, cl=cl: e.matmul(ps[:, 2 * c:2 * c + 2], w[:, kk, cl * 128:(cl + 1) * 128], sT[:, :, kk],
                                                                     start=(kk == 0), stop=(kk == 7)),
                         reads=[sT, w], writes=[ps])
        P.op("dve", lambda e: e.tensor_tensor(mT[:], ps[:].rearrange("p (c r) -> p c r", r=2), bcol[:].unsqueeze(2).to_broadcast([128, 48, 2]), ALU.add),
             reads=[ps, bcol], writes=[mT])
        for r in range(2):
            pr = P.psum([48, 128], F32, "mod_pr%d" % r)
            sr = P.sbuf([48, 128], F32, "mod_sr%d" % r)
            mr = P.sbuf([128, 48], F32, "mod_mr%d" % r)
            P.op("dve", lambda e, r=r, mr=mr: e.tensor_copy(mr[:], mT[:, :, r]), reads=[mT], writes=[mr])
            P.op("pe", lambda e, r=r, pr=pr, mr=mr: e.transpose(pr[:], mr[:], k.ident_f[:]), reads=[mr, k.ident_f], writes=[pr])
            P.op("act", lambda e, pr=pr, sr=sr: e.copy(sr[:], pr[:]), reads=[pr], writes=[sr])
            P.dma("sp", modv.ap()[r].rearrange("(c p) -> c p", p=128), sr[:], reads=[sr], writes=[modv])


def phase_norm(k, x, gain, mT, which, hT_sb, hT32_d=None, t_lo=0):
    P = k.P
    with P.scope():
        gr = P.sbuf([8, 128], F32, "gr%d" % which)
        gps = P.psum([128, 8], F32, "gps%d" % which)
        P.dma("sp", gr[:], gain.rearrange("(k p) -> k p", p=128), writes=[gr])
        P.op("pe", lambda e: e.transpose(gps[:], gr[:], k.ident_f[0:8, 0:8]), reads=[gr, k.ident_f], writes=[gps])
        G = P.sbuf([128, 2, 8], F32, "G%d" % which)
        gcol = P.sbuf([128, 8], F32, "gcol%d" % which)
        P.op("dve", lambda e: e.tensor_copy(gcol[:], gps[:]), reads=[gps], writes=[gcol])
        sh_i, sc_i = 3 * which, 3 * which + 1
        for r in range(2):
            P.op("dve", lambda e, r=r: e.scalar_tensor_tensor(G[:, r, :], mT[:, sc_i * 8:sc_i * 8 + 8, r], 1.0, gcol[:], ALU.add, ALU.mult),
                 reads=[mT, gcol], writes=[G])
        xt = [P.sbuf([128, 1024], F32, "nx%d_%d" % (which, i)) for i in range(3)]
        junk = P.sbuf([128, 1024], F32, "njunk%d" % which)
        xs = [P.sbuf([128, 1024], F32, "nxs%d_%d" % (which, i)) for i in range(5)]
        ss = [P.sbuf([128, 2], F32, "nss%d_%d" % (which, i)) for i in range(3)]
        pts = [P.psum([128, 512], F32, "npt%d_%d" % (which, i)) for i in range(2)]
        h32 = [P.sbuf([128, 512], F32, "nh32_%d_%d" % (which, i)) for i in range(2)] if hT32_d is not None else None
        groups = [[0, 1]] + [list(range(2 + 4 * g, 6 + 4 * g)) for g in range(8)]
        ti = 0
        ng = 0
        for grp in groups:
            if grp[0] < t_lo:
                continue
            r = 0 if grp[0] < 2 else 1
            tiles = []
            for t in grp:
                xb = xt[ti % 3]; sb = ss[ti % 3]; xsb = xs[ti % 5]
                ti += 1
                P.dma("sp", xb[:], x.ap()[t * 128:(t + 1) * 128, :], writes=[xb])
                P.op("act", lambda e, xb=xb, sb=sb: e.activation(junk[:], xb[:], AF.Square, accum_out=sb[:, 0:1]),
                     reads=[xb], writes=[junk, sb])
                P.op("act", lambda e, sb=sb: e.activation(sb[:, 1:2], sb[:, 0:1], AF.Sqrt, bias=k.eps_col[:], scale=1.0 / D),
                     reads=[sb, k.eps_col], writes=[sb])
                P.op("dve", lambda e, sb=sb: e.reciprocal(sb[:, 1:2], sb[:, 1:2]), reads=[sb], writes=[sb])
                P.op("dve", lambda e, xb=xb, sb=sb, xsb=xsb: e.tensor_scalar(xsb[:], xb[:], sb[:, 1:2], None, ALU.mult),
                     reads=[xb, sb], writes=[xsb])
                tiles.append(xsb)
            n = len(grp) * 128
            tok0 = grp[0] * 128
            for kk in range(8):
                pt = pts[ng % 2]
                for j, xsb in enumerate(tiles):
                    P.op("pe", lambda e, pt=pt, xsb=xsb, kk=kk, j=j: e.transpose(pt[:, j * 128:(j + 1) * 128], xsb[:, kk * 128:(kk + 1) * 128], k.ident_f[:]),
                         reads=[xsb, k.ident_f], writes=[pt])
                bias_ap = mT[:, sh_i * 8 + kk, r:r + 1]
                P.op("act", lambda e, pt=pt, kk=kk, r=r, tok0=tok0, n=n, bias_ap=bias_ap: e.activation(
                    hT_sb[:, kk, tok0:tok0 + n], pt[:, 0:n], AF.Identity, bias=bias_ap, scale=G[:, r, kk:kk + 1]),
                    reads=[pt, mT, G], writes=[hT_sb])
                if hT32_d is not None:
                    hb = h32[ng % 2]
                    P.op("dve", lambda e, pt=pt, kk=kk, r=r, n=n, hb=hb, bias_ap=bias_ap: e.tensor_scalar(
                        hb[:, 0:n], pt[:, 0:n], G[:, r, kk:kk + 1], bias_ap, ALU.mult, ALU.add),
                        reads=[pt, mT, G], writes=[hb])
                    P.dma("sp", hT32_d.ap()[kk * 128:(kk + 1) * 128, tok0:tok0 + n], hb[:, 0:n], reads=[hb], writes=[hT32_d])
                ng += 1


def phase_proj(k, hT_sb, w, z, N, tag="pj"):
    P = k.P
    with P.scope():
        _phase_proj(k, hT_sb, w, z, N, tag)


def _phase_proj(k, hT_sb, w, z, N, tag):
    P = k.P
    wst = [P.sbuf([128, 8, 512], F32, "%s_st%d" % (tag, i)) for i in range(2)]
    wb = [P.sbuf([128, 8, 512], BF16, "%s_wb%d" % (tag, i)) for i in range(2)]
    pss = [P.psum([128, 512], F32, "%s_ps%d" % (tag, i)) for i in range(3)]
    ob = [P.sbuf([128, 512], F32, "%s_ob%d" % (tag, i)) for i in range(4)]
    wv = w.rearrange("(k p) n -> p k n", p=128)
    nblk = (N + 511) // 512
    it = 0
    for nb in range(nblk):
        c0 = nb * 512
        cw = min(512, N - c0)
        st, wbb = wst[nb % 2], wb[nb % 2]
        load_w_bf16(k, wbb, wbb[:, :, 0:cw], wv[:, :, c0:c0 + cw], st, st[:, :, 0:cw])
        for t in range(NT):
            ps = pss[it % 3]; o = ob[it % 4]
            it += 1
            for kk in range(8):
                P.op("pe", lambda e, ps=ps, kk=kk, t=t, wbb=wbb, cw=cw: e.matmul(
                    ps[:, 0:cw], hT_sb[:, kk, t * 128:(t + 1) * 128], wbb[:, kk, 0:cw], start=(kk == 0), stop=(kk == 7)),
                    reads=[hT_sb, wbb], writes=[ps])
            if it % 2:
                P.op("act", lambda e, ps=ps, o=o, cw=cw: e.copy(o[:, 0:cw], ps[:, 0:cw]), reads=[ps], writes=[o])
            else:
                P.op("dve", lambda e, ps=ps, o=o, cw=cw: e.tensor_copy(o[:, 0:cw], ps[:, 0:cw]), reads=[ps], writes=[o])
            P.dma("sp", z.ap()[t * 128:(t + 1) * 128, c0:c0 + cw], o[:, 0:cw], reads=[o], writes=[z])


ZOFF = dict(ret_q=0, ret_k=256, ret_v=512, ret_g=1024, win_q=1536, win_k=2048, win_v=2176,
            na_q=2304, na_k=2816, na_v=3328, gdn_q=3840, gdn_k=4352, gdn_v=4864, gdn_g=5376, gdn_a=5888, gdn_b=5896)
NEG = -30000.0


def rope_tables():
    t = np.arange(4096)
    row = (t // 64).astype(np.float32); col = (t % 64).astype(np.float32)
    inv = (10000.0 ** (-np.arange(16, dtype=np.float32) / 16)).astype(np.float32)
    ang = np.concatenate([row[:, None] * inv, col[:, None] * inv], axis=-1).astype(np.float32)
    return np.cos(ang).astype(np.float32), np.sin(ang).astype(np.float32)


def na_classes():
    cls = [(10, 10 + dl) for dl in range(-2, 3)]
    for p in (0, 1):
        cls += [(p, b) for b in range(4)]
    for p in (30, 31):
        cls += [(p, b) for b in range(28, 32)]
    return cls


def na_plan(p):
    if 2 <= p <= 29:
        return [(p + dl, dl + 2) for dl in range(-2, 3)]
    e = {0: 0, 1: 1, 30: 2, 31: 3}[p]
    b0 = 0 if p < 2 else 28
    return [(b0 + b, 5 + e * 4 + b) for b in range(4)]


def na_bias_gather(rpb):
    cls = na_classes()
    out = np.full((8, 128, len(cls), 128), NEG, np.float32)
    kc = np.arange(64)[:, None]; qc = np.arange(64)[None, :]
    cst = np.clip(qc - 8, 0, 48)
    colok = (kc >= cst) & (kc < cst + 16)
    coff = np.clip(kc - qc + 15, 0, 30)
    for ci, (p, blk) in enumerate(cls):
        for a in range(2):
            krow = 2 * blk + a
            for b in range(2):
                qrow = 2 * p + b
                rs = min(max(qrow - 4, 0), 56)
                if not (rs <= krow < rs + 8):
                    continue
                ro = krow - qrow + 7
                vals = rpb[:, ro][:, coff]
                blkv = np.where(colok[None], vals, NEG)
                out[:, a * 64:(a + 1) * 64, ci, b * 64:(b + 1) * 64] = blkv
    return out


def attn_prep_qk(k, z, col, gain_bc, rope, outT, tag, cs=None):
    P = k.P
    with P.scope():
        x = P.sbuf([128, NT, 64], F32, tag + "x")
        zv = z.ap()[:, col:col + 64].rearrange("(t p) c -> p t c", p=128)
        P.dma("sp", x[:, 0:17, :], zv[:, 0:17, :], writes=[x])
        P.dma("sp", x[:, 17:34, :], zv[:, 17:34, :], writes=[x])
        sq = P.sbuf([128, NT, 64], F32, tag + "sq")
        P.op("pool", lambda e: e.tensor_tensor(sq[:], x[:], x[:], ALU.mult), reads=[x], writes=[sq])
        ss = P.sbuf([128, NT], F32, tag + "ss")
        P.op("dve", lambda e: e.tensor_reduce(ss[:], sq[:], AX.X, ALU.add), reads=[sq], writes=[ss])
        P.op("act", lambda e: e.activation(ss[:], ss[:], AF.Sqrt, bias=k.eps_col[:], scale=1.0 / 64), reads=[ss, k.eps_col], writes=[ss])
        P.op("dve", lambda e: e.reciprocal(ss[:], ss[:]), reads=[ss], writes=[ss])
        P.op("dve", lambda e: e.tensor_tensor(x[:], x[:], ss[:].unsqueeze(2).to_broadcast([128, NT, 64]), ALU.mult), reads=[x, ss], writes=[x])
        P.op("pool", lambda e: e.tensor_tensor(x[:], x[:], gain_bc[:].unsqueeze(1).to_broadcast([128, NT, 64]), ALU.mult), reads=[x, gain_bc], writes=[x])
        if rope:
            rope_apply(k, x, 1, tag)
        transpose_to(k, lambda t: x[:, t, :], 64, outT, tag, src_bufs=[x])


def rope_apply(k, x, H, tag):
    P = k.P
    with P.scope():
        _rope_apply(k, x, H, tag)


def _rope_apply(k, x, H, tag):
    P = k.P
    xl = x[:, 2:NT, :].rearrange("p t (h c) -> p t h c", c=64)
    x1 = xl[:, :, :, 0:32]; x2 = xl[:, :, :, 32:64]
    shp = [128, 32, H, 32]
    a = P.sbuf(shp, F32, tag + "ra"); b = P.sbuf(shp, F32, tag + "rb")
    cosb = k.cos[:].unsqueeze(2).to_broadcast(shp); sinb = k.sin[:].unsqueeze(2).to_broadcast(shp)
    P.op("dve", lambda e: e.tensor_tensor(a[:], x1, sinb, ALU.mult), reads=[x, k.sin], writes=[a])
    P.op("pool", lambda e: e.tensor_tensor(b[:], x2, sinb, ALU.mult), reads=[x, k.sin], writes=[b])
    P.op("dve", lambda e: e.tensor_tensor(x1, x1, cosb, ALU.mult), reads=[x, k.cos, a, b], writes=[x])
    P.op("dve", lambda e: e.tensor_tensor(x2, x2, cosb, ALU.mult), reads=[x, k.cos], writes=[x])
    P.op("dve", lambda e: e.tensor_tensor(x1, x1, b[:], ALU.subtract), reads=[x, b], writes=[x])
    P.op("dve", lambda e: e.tensor_tensor(x2, x2, a[:], ALU.add), reads=[x, a], writes=[x])


def transpose_to(k, src_fn, C, outT, tag, deps=None, t_list=None, src_bufs=None, out_off=0):
    P = k.P
    with P.scope():
        pss = [P.psum([128, 512], F32, tag + "tp%d" % i) for i in range(2)]
        tl = list(range(NT)) if t_list is None else t_list
        for g0 in range(0, len(tl), 4):
            grp = tl[g0:g0 + 4]
            ps = pss[(g0 // 4) % 2]
            for j, t in enumerate(grp):
                ap, bufs = src_fn(t), (src_bufs or [])
                P.op("pe", lambda e, ps=ps, j=j, ap=ap: e.transpose(ps[0:C, j * 128:(j + 1) * 128], ap, k.ident_f[:]),
                     reads=list(bufs) + [k.ident_f] + (deps or []), writes=[ps])
            n = len(grp) * 128
            o = outT[0:C, out_off + grp[0] * 128: out_off + grp[0] * 128 + n]
            if (g0 // 4) % 2:
                P.op("act", lambda e, ps=ps, o=o, n=n: e.copy(o, ps[0:C, 0:n]), reads=[ps], writes=[outT])
            else:
                P.op("dve", lambda e, ps=ps, o=o, n=n: e.tensor_copy(o, ps[0:C, 0:n]), reads=[ps], writes=[outT])


def attn_prep_v(k, z, col, Vaug, tag):
    P = k.P
    with P.scope():
        v = P.sbuf([128, NT, 64], F32, tag + "v")
        zv = z.ap()[:, col:col + 64].rearrange("(t p) c -> p t c", p=128)
        P.dma("sp", v[:, 0:17, :], zv[:, 0:17, :], writes=[v])
        P.dma("sp", v[:, 17:34, :], zv[:, 17:34, :], writes=[v])
        P.op("pool", lambda e: e.tensor_copy(Vaug[:, :, 0:64], v[:]), reads=[v], writes=[Vaug])
        P.op("pool", lambda e: e.memset(Vaug[:, :, 64:65], 1.0), writes=[Vaug])


def attn_core(k, qT, kT, Vaug, plan, sink_ap, sink_buf, yh, tag):
    P = k.P
    with P.scope():
        pss = [[P.psum([128, 512], F32, "%ss%d_%d" % (tag, i, j)) for j in range(2)] for i in range(2)]
        pos = [P.psum([128, 65], F32, "%so%d" % (tag, i)) for i in range(2)]
        pts = [P.sbuf([128, 8 * 128], BF16, "%spt%d" % (tag, i)) for i in range(2)]
        dens = [P.sbuf([128, 1], F32, "%sden%d" % (tag, i)) for i in range(2)]
        def stage_a(qi):
            qt, blocks = plan[qi]
            sset = pss[qi % 2]
            for bi, (kt, m, mb) in enumerate(blocks):
                ps = sset[bi // 4]
                P.op("pe", lambda e, ps=ps, bi=bi, kt=kt, qt=qt: e.matmul(ps[:, (bi % 4) * 128:(bi % 4 + 1) * 128], kT[:, kt * 128:(kt + 1) * 128],
                                                                      qT[:, qt * 128:(qt + 1) * 128], start=True, stop=True),
                     reads=[kT, qT], writes=[ps])

        def stage_b(qi):
            qt, blocks = plan[qi]
            sset = pss[qi % 2]; pt = pts[qi % 2]; po = pos[qi % 2]; den = dens[qi % 2]
            nb = len(blocks)
            for bk in range((nb + 3) // 4):
                n = min(4, nb - bk * 4) * 128
                P.op("act", lambda e, pt=pt, bk=bk, n=n, sset=sset: e.activation(pt[:, bk * 512:bk * 512 + n], sset[bk][:, 0:n], AF.Exp, scale=0.125),
                     reads=[sset[bk]], writes=[pt])
            for bi, (kt, m, mb) in enumerate(blocks):
                if m is not None:
                    eng = "pool" if bi % 2 else "dve"
                    P.op(eng, lambda e, pt=pt, bi=bi, m=m: e.tensor_tensor(pt[:, bi * 128:(bi + 1) * 128], pt[:, bi * 128:(bi + 1) * 128], m, ALU.mult),
                         reads=[pt, mb], writes=[pt])
            for bi, (kt, m, mb) in enumerate(blocks):
                P.op("pe", lambda e, po=po, pt=pt, bi=bi, kt=kt, nb=nb: e.matmul(po[:], pt[:, bi * 128:(bi + 1) * 128], Vaug[:, kt, :],
                                                                             start=(bi == 0), stop=(bi == nb - 1)),
                     reads=[pt, Vaug], writes=[po])
            if sink_ap is not None:
                P.op("dve", lambda e, den=den, po=po: e.tensor_scalar(den[:], po[:, 64:65], sink_ap, None, ALU.add), reads=[po, sink_buf], writes=[den])
                P.op("dve", lambda e, den=den: e.reciprocal(den[:], den[:]), reads=[den], writes=[den])
            else:
                P.op("dve", lambda e, den=den, po=po: e.reciprocal(den[:], po[:, 64:65]), reads=[po], writes=[den])
            P.op("act", lambda e, den=den, po=po, qt=qt: e.activation(yh[:, qt, :], po[:, 0:64], AF.Copy, scale=den[:, 0:1]), reads=[po, den], writes=[yh])

        stage_a(0)
        for qi in range(len(plan)):
            if qi + 1 < len(plan):
                stage_a(qi + 1)
            stage_b(qi)


def store_head(k, yh, ybr_ap, col, t_lo=0):
    P = k.P
    C = yh.t.shape[2]
    yv = ybr_ap[:, col:col + C].rearrange("(t p) c -> p t c", p=128)
    P.dma("sp", yv[:, t_lo:17, :], yh[:, t_lo:17, :], reads=[yh], writes=[k.ybr])
    P.dma("sp", yv[:, 17:34, :], yh[:, 17:34, :], reads=[yh], writes=[k.ybr])


def bc_load(k, vec_ap, n, name):
    P = k.P
    t = P.sbuf([128, n], F32, name)
    P.dma("sp", t[:], vec_ap.partition_broadcast(128), writes=[t])
    return t


def phase_window(k, z, qn, kn, sink, ctx_out):
    P = k.P
    with P.scope():
        qg = bc_load(k, qn, 64, "wqg"); kg = bc_load(k, kn, 64, "wkg")
        sk = bc_load(k, sink, 8, "wsk")
        P.op("act", lambda e: e.activation(sk[:], sk[:], AF.Exp), reads=[sk], writes=[sk])
        qT = P.sbuf([64, T], BF16, "wqT"); kT = P.sbuf([64, T], BF16, "wkT")
        Vaug = P.sbuf([128, NT, 65], BF16, "wV")
        yh = P.sbuf([128, NT, 64], F32, "wyh")
        plan = []
        if ctx_out:
            plan += [(qt, [(0, None, None), (1, None, None)]) for qt in range(2)]
        for n in range(32):
            bl = []
            if n > 0:
                bl.append((2 + n - 1, k.maskPrev[:], k.maskPrev))
            bl.append((2 + n, None, None))
            if n < 31:
                bl.append((2 + n + 1, k.maskNext[:], k.maskNext))
            bl += [(0, None, None), (1, None, None)]
            plan.append((2 + n, bl))
        for h in range(8):
            g = h // 4
            if h % 4 == 0:
                attn_prep_qk(k, z, ZOFF["win_k"] + g * 64, kg, True, kT, "wk")
                attn_prep_v(k, z, ZOFF["win_v"] + g * 64, Vaug, "wv")
            attn_prep_qk(k, z, ZOFF["win_q"] + h * 64, qg, True, qT, "wq")
            attn_core(k, qT, kT, Vaug, plan, sk[:, h:h + 1], sk, yh, "wa")
            store_head(k, yh, k.ybr.ap()[1], h * 64, 0 if ctx_out else 2)


def phase_na(k, z, qn, kn, nab, ctx_out):
    P = k.P
    with P.scope():
        qg = bc_load(k, qn, 64, "nqg"); kg = bc_load(k, kn, 64, "nkg")
        qT = P.sbuf([64, T], BF16, "nqT"); kT = P.sbuf([64, T], BF16, "nkT")
        Vaug = P.sbuf([128, NT, 65], BF16, "nV")
        yh = P.sbuf([128, NT, 64], F32, "nyh")
        bst = P.sbuf([128, 21, 128], F32, "nbst")
        eb = P.sbuf([128, 21, 128], BF16, "neb")
        for h in range(8):
            P.dma("sp", bst[:], nab[h], writes=[bst])
            P.op("act", lambda e: e.activation(eb[:], bst[:], AF.Exp), reads=[bst], writes=[eb])
            plan = []
            if ctx_out:
                plan += [(qt, [(0, None, None), (1, None, None)]) for qt in range(2)]
            for p in range(32):
                bl = [(2 + blk, eb[:, ci, :], eb) for blk, ci in na_plan(p)]
                bl += [(0, None, None), (1, None, None)]
                plan.append((2 + p, bl))
            attn_prep_qk(k, z, ZOFF["na_k"] + h * 64, kg, False, kT, "nk")
            attn_prep_v(k, z, ZOFF["na_v"] + h * 64, Vaug, "nv")
            attn_prep_qk(k, z, ZOFF["na_q"] + h * 64, qg, False, qT, "nq")
            attn_core(k, qT, kT, Vaug, plan, None, None, yh, "na")
            store_head(k, yh, k.ybr.ap()[2], h * 64, 0 if ctx_out else 2)


def ret_consts(c):
    j = np.arange(128)[:, None].astype(np.float32); i = np.arange(128)[None, :].astype(np.float32)
    c["dpos"] = np.stack([np.maximum(i - j, 0), np.maximum(j - i, 0)], 1).astype(np.float32)
    c["dmsk"] = np.stack([(i >= j), (j >= i)], 1).astype(np.float32)
    c["rowidx"] = np.stack([np.broadcast_to(i + 1, (128, 128)), np.broadcast_to(128 - i, (128, 128))], 1).astype(np.float32)
    c["colidx"] = np.concatenate([127 - j, j], 1).astype(np.float32)


def phase_ret(k, z, decay, gn, ctx_out):
    P = k.P
    with P.scope():
        dpos = P.sbuf([128, 2, 128], F32, "r_dpos"); dmsk = P.sbuf([128, 2, 128], F32, "r_dmsk")
        rowidx = P.sbuf([128, 2, 128], F32, "r_rowidx"); colidx = P.sbuf([128, 2], F32, "r_colidx")
        for t_, n_ in ((dpos, "dpos"), (dmsk, "dmsk"), (rowidx, "rowidx"), (colidx, "colidx")):
            P.dma("sp", t_[:], k.cd[n_].ap(), writes=[t_])
        lg = bc_load(k, decay, 8, "r_lg")
        P.op("act", lambda e: e.activation(lg[:], lg[:], AF.Exp, scale=-1.0), reads=[lg], writes=[lg])
        P.op("act", lambda e: e.activation(lg[:], lg[:], AF.Ln, bias=k.one_col[:]), reads=[lg, k.one_col], writes=[lg])
        P.op("dve", lambda e: e.tensor_scalar(lg[:], lg[:], -1.0, None, ALU.mult), reads=[lg], writes=[lg])
        dm = P.sbuf([128, 8, 128], F32, "r_dm"); qdT = P.sbuf([128, 8, 128], F32, "r_qdT")
        kdec = P.sbuf([128, 8], F32, "r_kdec"); cdec = P.sbuf([128, 8], F32, "r_cdec")
        for dr in range(2):
            for h in range(4):
                c = dr * 4 + h
                P.op("act", lambda e, dr=dr, c=c: e.activation(dm[:, c, :], dpos[:, dr, :], AF.Exp, scale=lg[:, c:c + 1]), reads=[dpos, lg], writes=[dm])
                P.op("dve", lambda e, dr=dr, c=c: e.tensor_tensor(dm[:, c, :], dm[:, c, :], dmsk[:, dr, :], ALU.mult), reads=[dm, dmsk], writes=[dm])
                P.op("act", lambda e, dr=dr, c=c: e.activation(qdT[:, c, :], rowidx[:, dr, :], AF.Exp, scale=lg[:, c:c + 1]), reads=[rowidx, lg], writes=[qdT])
                P.op("act", lambda e, dr=dr, c=c: e.activation(kdec[:, c:c + 1], colidx[:, dr:dr + 1], AF.Exp, scale=lg[:, c:c + 1]), reads=[colidx, lg], writes=[kdec])
        P.op("act", lambda e: e.activation(cdec[:], lg[:], AF.Exp, scale=128.0), reads=[lg], writes=[cdec])
        gnb = bc_load(k, gn, 512, "r_gn")
        q = P.sbuf([128, NT, 64], F32, "r_q"); kk_ = P.sbuf([128, NT, 64], F32, "r_k")
        v = P.sbuf([128, NT, 128], F32, "r_v"); gt = P.sbuf([128, NT, 128], F32, "r_g")
        qT = P.sbuf([64, T], F32, "r_qT"); kT = P.sbuf([64, T], F32, "r_kT")
        qTd = P.sbuf([64, T], F32, "r_qTd"); kd = P.sbuf([128, NT, 64], F32, "r_kd")
        of = P.sbuf([128, NT, 128], F32, "r_of"); ot = P.sbuf([128, NT, 128], F32, "r_ot")
        S = [P.sbuf([64, 128], F32, "r_S%d" % i) for i in range(2)]
        pss = [P.psum([128, 128], F32, "r_pss%d" % i) for i in range(2)]
        pso = [P.psum([128, 128], F32, "r_pso%d" % i) for i in range(2)]
        pkv = [P.psum([64, 128], F32, "r_pkv%d" % i) for i in range(2)]
        sm = [P.sbuf([128, 128], F32, "r_sm%d" % i) for i in range(2)]
        st = P.sbuf([128, NT, 4], F32, "r_st")

        def ld(dst, col, w):
            zv = z.ap()[:, col:col + w].rearrange("(t p) c -> p t c", p=128)
            P.dma("sp", dst[:, 0:17, :], zv[:, 0:17, :], writes=[dst])
            P.dma("sp", dst[:, 17:34, :], zv[:, 17:34, :], writes=[dst])

        for h in range(4):
            ld(q, ZOFF["ret_q"] + h * 64, 64); ld(kk_, ZOFF["ret_k"] + h * 64, 64)
            ld(v, ZOFF["ret_v"] + h * 128, 128); ld(gt, ZOFF["ret_g"] + h * 128, 128)
            rope_apply(k, q, 1, "r_rq"); rope_apply(k, kk_, 1, "r_rk")
            P.op("pool", lambda e: e.tensor_scalar(kk_[:], kk_[:], 0.125, None, ALU.mult), reads=[kk_], writes=[kk_])
            transpose_to(k, lambda t: q[:, t, :], 64, qT, "r_tq", src_bufs=[q])
            transpose_to(k, lambda t: kk_[:, t, :], 64, kT, "r_tk", src_bufs=[kk_])
            it = 0
            for dr in range(2):
                c = dr * 4 + h
                P.op("dve", lambda e, c=c: e.tensor_tensor(qTd[:].rearrange("p (t i) -> p t i", i=128), qT[:].rearrange("p (t i) -> p t i", i=128),
                                                           qdT[0:64, c, :].unsqueeze(1).to_broadcast([64, NT, 128]), ALU.mult), reads=[qT, qdT], writes=[qTd])
                P.op("pool", lambda e, c=c: e.tensor_scalar(kd[:], kk_[:], kdec[:, c:c + 1], None, ALU.mult), reads=[kk_, kdec], writes=[kd])
                order = list(range(NT)) if dr == 0 else [1, 0] + list(range(NT - 1, 1, -1))
                Scur = None

                def stage_a(idx, c=c, order=order, it0=it):
                    n = order[idx]
                    ps, smb = pss[(it0 + idx) % 2], sm[(it0 + idx) % 2]
                    sl = slice(n * 128, (n + 1) * 128)
                    P.op("pe", lambda e, ps=ps, sl=sl: e.matmul(ps[:], kT[:, sl], qT[:, sl], start=True, stop=True), reads=[kT, qT], writes=[ps])
                    P.op("dve", lambda e, ps=ps, smb=smb, c=c: e.tensor_tensor(smb[:], ps[:], dm[:, c, :], ALU.mult), reads=[ps, dm], writes=[smb])

                stage_a(0)
                for idx, n in enumerate(order):
                    if idx + 1 < len(order):
                        stage_a(idx + 1)
                    ps, po, pk, smb = pss[it % 2], pso[it % 2], pkv[it % 2], sm[it % 2]
                    Snew = S[it % 2]
                    it += 1
                    sl = slice(n * 128, (n + 1) * 128)
                    P.op("pe", lambda e, po=po, smb=smb, n=n, last=(Scur is None): e.matmul(po[:], smb[:], v[:, n, :], start=True, stop=last), reads=[smb, v], writes=[po])
                    if Scur is not None:
                        P.op("pe", lambda e, po=po, sl=sl, Scur=Scur: e.matmul(po[:], qTd[:, sl], Scur[:], start=False, stop=True), reads=[qTd, Scur], writes=[po])
                    P.op("pe", lambda e, pk=pk, n=n: e.matmul(pk[:], kd[:, n, :], v[:, n, :], start=True, stop=True), reads=[kd, v], writes=[pk])
                    if Scur is None:
                        P.op("act", lambda e, pk=pk, Snew=Snew: e.copy(Snew[:], pk[:]), reads=[pk], writes=[Snew])
                    else:
                        P.op("dve", lambda e, pk=pk, Snew=Snew, Scur=Scur, c=c: e.scalar_tensor_tensor(Snew[:], Scur[:], cdec[0:64, c:c + 1], pk[:], ALU.mult, ALU.add),
                             reads=[pk, Scur, cdec], writes=[Snew])
                    Scur = Snew
                    if dr == 0:
                        P.op("act", lambda e, po=po, n=n: e.copy(of[:, n, :], po[:]), reads=[po], writes=[of])
                    else:
                        P.op("dve", lambda e, po=po, n=n: e.tensor_tensor(ot[:, n, :], po[:], of[:, n, :], ALU.add), reads=[po, of], writes=[ot])
            s1 = st[:, :, 0]; s2 = st[:, :, 1]; mu = st[:, :, 2]; rs = st[:, :, 3]
            bshape = [128, NT, 128]
            P.op("dve", lambda e: e.tensor_reduce(s1, ot[:], AX.X, ALU.add), reads=[ot], writes=[st])
            P.op("pool", lambda e: e.tensor_tensor(of[:], ot[:], ot[:], ALU.mult), reads=[ot], writes=[of])
            P.op("dve", lambda e: e.tensor_reduce(s2, of[:], AX.X, ALU.add), reads=[of], writes=[st])
            P.op("dve", lambda e: e.tensor_scalar(mu, s1, 1.0 / 128, None, ALU.mult), reads=[st], writes=[st])
            P.op("dve", lambda e: e.tensor_tensor(s1, mu, mu, ALU.mult), reads=[st], writes=[st])
            P.op("dve", lambda e: e.scalar_tensor_tensor(rs, s2, 1.0 / 128, s1, ALU.mult, ALU.subtract), reads=[st], writes=[st])
            P.op("act", lambda e: e.activation(rs, rs, AF.Sqrt, bias=k.eps_col[:]), reads=[st, k.eps_col], writes=[st])
            P.op("dve", lambda e: e.reciprocal(rs, rs), reads=[st], writes=[st])
            P.op("dve", lambda e: e.tensor_tensor(ot[:], ot[:], mu.unsqueeze(2).to_broadcast(bshape), ALU.subtract), reads=[ot, st], writes=[ot])
            P.op("dve", lambda e: e.tensor_tensor(ot[:], ot[:], rs.unsqueeze(2).to_broadcast(bshape), ALU.mult), reads=[ot, st], writes=[ot])
            P.op("pool", lambda e, h=h: e.tensor_tensor(ot[:], ot[:], gnb[:, h * 128:(h + 1) * 128].unsqueeze(1).to_broadcast(bshape), ALU.mult), reads=[ot, gnb], writes=[ot])
            P.op("act", lambda e: e.activation(gt[:], gt[:], AF.Silu), reads=[gt], writes=[gt])
            P.op("dve", lambda e: e.tensor_tensor(ot[:], ot[:], gt[:], ALU.mult), reads=[ot, gt], writes=[ot])
            store_head(k, ot, k.ybr.ap()[0], h * 128, 0 if ctx_out else 2)


TGROUPS = [[0, 1]] + [list(range(2 + 4 * g, 6 + 4 * g)) for g in range(8)]


def load_big_w(k, dst, dview_fn, src_fn, nblk, st):
    P = k.P
    for i in range(nblk):
        s = st[i % len(st)]
        sap = src_fn(i)
        shp = sap.shape
        sv = s[:, 0:shp[1], 0:shp[2]]
        P.dma("sp", sv, sap, writes=[s])
        P.op("pool", lambda e, i=i, sv=sv: e.tensor_copy(dview_fn(i), sv), reads=[s], writes=[dst])


def phase_merge(k, hT_d, x, xmid, w_merge, w_branch, w_out, modv, ctx_out):
    P = k.P
    with P.scope():
        wm = P.sbuf([128, 8, 4096], BF16, "m_wm"); wb = P.sbuf([128, 4, 4, 1024], BF16, "m_wb"); wo = P.sbuf([128, 8, 1024], BF16, "m_wo")
        with P.scope():
            st = [P.sbuf([128, 8, 512], F32, "m_st%d" % i) for i in range(2)]
            wmv = w_merge.rearrange("(k p) n -> p k n", p=128)
            load_big_w(k, wm, lambda i: wm[:, :, i * 512:(i + 1) * 512], lambda i: wmv[:, :, i * 512:(i + 1) * 512], 8, st)
            wbv = w_branch.rearrange("b (k p) n -> p b k n", p=128)
            load_big_w(k, wb, lambda i: wb[:, i // 2, :, (i % 2) * 512:(i % 2 + 1) * 512], lambda i: wbv[:, i // 2, :, (i % 2) * 512:(i % 2 + 1) * 512], 8, st)
            wov = w_out.rearrange("(k p) n -> p k n", p=128)
            load_big_w(k, wo, lambda i: wo[:, :, i * 512:(i + 1) * 512], lambda i: wov[:, :, i * 512:(i + 1) * 512], 2, st)
        g1bc = P.sbuf([128, 2, 1024], F32, "m_g1")
        for r in range(2):
            P.dma("sp", g1bc[:, r, :], modv.ap()[r, 2048:3072].partition_broadcast(128), writes=[g1bc])
        hT = [P.sbuf([128, 8, 512], BF16, "m_hT%d" % i) for i in range(2)]
        yin = [P.sbuf([128, 512], F32, "m_yin%d" % i) for i in range(3)]
        yT = P.sbuf([128, 4, 4, 512], BF16, "m_yT")
        accT = P.sbuf([128, 8, 512], BF16, "m_accT")
        acc = P.sbuf([128, 512], F32, "m_acc"); tmp = P.sbuf([128, 512], F32, "m_tmp"); sg = [P.sbuf([128, 512], F32, "m_sg%d" % i) for i in range(2)]
        xt = [P.sbuf([128, 1024], F32, "m_xt%d" % i) for i in range(2)]
        xo = [P.sbuf([128, 1024], F32, "m_xo%d" % i) for i in range(2)]
        pg = [P.psum([128, 512], F32, "m_pg%d" % i) for i in range(2)]
        pb = [P.psum([128, 512], F32, "m_pb%d" % i) for i in range(2)]
        po = [P.psum([128, 1024], F32, "m_po%d" % i) for i in range(2)]
        yi = 0; it = 0; oi = 0
        for gi, grp in enumerate(TGROUPS):
            if not ctx_out and grp[0] < 2:
                continue
            r = 0 if grp[0] < 2 else 1
            n = len(grp) * 128; tok0 = grp[0] * 128
            hb = hT[gi % 2]
            P.dma("sp", hb[:, :, 0:n], hT_d.ap().rearrange("(k p) t -> p k t", p=128)[:, :, tok0:tok0 + n], writes=[hb])
            for i in range(4):
                for j, t in enumerate(grp):
                    yb = yin[yi % 3]; yi += 1
                    P.dma("sp", yb[:], k.ybr.ap()[i, t * 128:(t + 1) * 128, :], reads=[k.ybr], writes=[yb])
                    pt = po[yi % 2]
                    for kk in range(4):
                        P.op("pe", lambda e, pt=pt, yb=yb, kk=kk: e.transpose(pt[:, kk * 128:(kk + 1) * 128], yb[:, kk * 128:(kk + 1) * 128], k.ident_f[:]),
                             reads=[yb, k.ident_f], writes=[pt])
                    eng = "act" if yi % 2 else "dve"
                    if eng == "act":
                        P.op("act", lambda e, pt=pt, i=i, j=j: e.copy(yT[:, i, :, j * 128:(j + 1) * 128], pt[:, 0:512].rearrange("p (k t) -> p k t", t=128)), reads=[pt], writes=[yT])
                    else:
                        P.op("dve", lambda e, pt=pt, i=i, j=j: e.tensor_copy(yT[:, i, :, j * 128:(j + 1) * 128], pt[:, 0:512].rearrange("p (k t) -> p k t", t=128)), reads=[pt], writes=[yT])
            for fc in range(8):
                for i in range(4):
                    g_, b_, s_ = pg[it % 2], pb[it % 2], sg[it % 2]; it += 1
                    for kk in range(8):
                        P.op("pe", lambda e, g_=g_, kk=kk, i=i, fc=fc, hb=hb, n=n: e.matmul(g_[:, 0:n], wm[:, kk, i * 1024 + fc * 128:i * 1024 + (fc + 1) * 128], hb[:, kk, 0:n],
                                                                                         start=(kk == 0), stop=(kk == 7)), reads=[wm, hb], writes=[g_])
                    for kk in range(4):
                        P.op("pe", lambda e, b_=b_, kk=kk, i=i, fc=fc, n=n: e.matmul(b_[:, 0:n], wb[:, i, kk, fc * 128:(fc + 1) * 128], yT[:, i, kk, 0:n],
                                                                                 start=(kk == 0), stop=(kk == 3)), reads=[wb, yT], writes=[b_])
                    P.op("act", lambda e, g_=g_, s_=s_, n=n: e.activation(s_[:, 0:n], g_[:, 0:n], AF.Sigmoid), reads=[g_], writes=[s_])
                    if i == 0:
                        P.op("dve", lambda e, s_=s_, b_=b_, n=n: e.tensor_tensor(acc[:, 0:n], s_[:, 0:n], b_[:, 0:n], ALU.mult), reads=[s_, b_], writes=[acc])
                    else:
                        P.op("dve", lambda e, s_=s_, b_=b_, n=n: e.tensor_tensor(tmp[:, 0:n], s_[:, 0:n], b_[:, 0:n], ALU.mult), reads=[s_, b_], writes=[tmp])
                        if i < 3:
                            P.op("pool", lambda e, n=n: e.tensor_tensor(acc[:, 0:n], acc[:, 0:n], tmp[:, 0:n], ALU.add), reads=[acc, tmp], writes=[acc])
                        else:
                            P.op("pool", lambda e, n=n, fc=fc: e.tensor_tensor(accT[:, fc, 0:n], acc[:, 0:n], tmp[:, 0:n], ALU.add), reads=[acc, tmp], writes=[accT])
            for j, t in enumerate(grp):
                o_ = po[oi % 2]; xb = xt[oi % 2]; xob = xo[oi % 2]; oi += 1
                P.dma("sp", xb[:], x.ap()[t * 128:(t + 1) * 128, :], reads=[x], writes=[xb])
                for half in range(2):
                    for fc in range(8):
                        P.op("pe", lambda e, o_=o_, half=half, fc=fc, j=j: e.matmul(o_[:, half * 512:(half + 1) * 512], accT[:, fc, j * 128:(j + 1) * 128], wo[:, fc, half * 512:(half + 1) * 512],
                                                                                start=(fc == 0), stop=(fc == 7)), reads=[accT, wo], writes=[o_])
                P.op("dve", lambda e, o_=o_, xob=xob, r=r: e.tensor_tensor(xob[:], o_[:], g1bc[:, r, :], ALU.mult), reads=[o_, g1bc], writes=[xob])
                P.op("pool", lambda e, xob=xob, xb=xb: e.tensor_tensor(xob[:], xob[:], xb[:], ALU.add), reads=[xob, xb], writes=[xob])
                P.dma("sp", xmid.ap()[t * 128:(t + 1) * 128, :], xob[:], reads=[xob], writes=[xmid])


def phase_router(k, h32_d, w_router, rbias, wgt, t_lo):
    P = k.P
    with P.scope():
        wr = P.sbuf([128, 8, 32], F32, "rt_w")
        P.dma("sp", wr[:], w_router.rearrange("(k p) n -> p k n", p=128), writes=[wr])
        rb = bc_load(k, rbias, 32, "rt_b")
        hs = [P.sbuf([128, 8, 128], F32, "rt_h%d" % i) for i in range(2)]
        ps = [P.psum([128, 32], F32, "rt_ps%d" % i) for i in range(2)]
        sc = P.sbuf([128, 32], F32, "rt_sc"); sel = P.sbuf([128, 32], F32, "rt_sel")
        w8 = P.sbuf([128, 10, 8], F32, "rt_w8"); msk = P.sbuf([128, 32], F32, "rt_msk"); c1 = P.sbuf([128, 2], F32, "rt_c1")
        hv = h32_d.ap().rearrange("(k p) t -> p k t", p=128)
        for t in range(t_lo, NT):
            hb = hs[t % 2]; p_ = ps[t % 2]
            P.dma("sp", hb[:], hv[:, :, t * 128:(t + 1) * 128], reads=[h32_d], writes=[hb])
            for kk in range(8):
                P.op("pe", lambda e, p_=p_, hb=hb, kk=kk: e.matmul(p_[:], hb[:, kk, :], wr[:, kk, :], start=(kk == 0), stop=(kk == 7)), reads=[hb, wr], writes=[p_])
            P.op("act", lambda e, p_=p_: e.activation(sc[:], p_[:], AF.Sigmoid), reads=[p_], writes=[sc])
            P.op("dve", lambda e: e.tensor_tensor(sel[:], sc[:], rb[:], ALU.add), reads=[sc, rb], writes=[sel])
            s4 = sel[:].rearrange("p (g e) -> p g e", e=4)
            a, b, c, d_ = s4[:, :, 0], s4[:, :, 1], s4[:, :, 2], s4[:, :, 3]
            W = lambda i: w8[:, i, :]
            seq = [(W(0), a, b, ALU.max), (W(1), a, b, ALU.min), (W(2), c, d_, ALU.max), (W(3), c, d_, ALU.min),
                   (W(4), W(0), W(2), ALU.max), (W(5), W(0), W(2), ALU.min), (W(6), W(1), W(3), ALU.max),
                   (W(7), W(5), W(6), ALU.max), (W(8), W(4), W(7), ALU.add)]
            for o, i0, i1, op in seq:
                P.op("dve", lambda e, o=o, i0=i0, i1=i1, op=op: e.tensor_tensor(o, i0, i1, op), reads=[sel, w8], writes=[w8])
            P.op("dve", lambda e: e.tensor_reduce(c1[:, 0:1], W(8), AX.X, ALU.max), reads=[w8], writes=[c1])
            P.op("dve", lambda e: e.tensor_scalar(W(9), W(8), c1[:, 0:1], None, ALU.is_ge), reads=[w8, c1], writes=[w8])
            m4 = msk[:].rearrange("p (g e) -> p g e", e=4)
            P.op("dve", lambda e: e.tensor_tensor(m4, s4, W(7).unsqueeze(2).to_broadcast([128, 8, 4]), ALU.is_ge), reads=[sel, w8], writes=[msk])
            P.op("dve", lambda e: e.tensor_tensor(m4, m4, W(9).unsqueeze(2).to_broadcast([128, 8, 4]), ALU.mult), reads=[msk, w8], writes=[msk])
            P.op("dve", lambda e: e.tensor_tensor(msk[:], msk[:], sc[:], ALU.mult), reads=[msk, sc], writes=[msk])
            P.op("dve", lambda e: e.tensor_reduce(c1[:, 1:2], msk[:], AX.X, ALU.add), reads=[msk], writes=[c1])
            P.op("dve", lambda e: e.reciprocal(c1[:, 1:2], c1[:, 1:2]), reads=[c1], writes=[c1])
            P.op("dve", lambda e, t=t: e.tensor_scalar(wgt[:, t, :], msk[:], c1[:, 1:2], None, ALU.mult), reads=[msk, c1], writes=[wgt])


def phase_moe(k, h2T_d, wgt, xmid, xout, weg, weu, wed, modv, t_lo, out_lat=None, nexp=32):
    P = k.P
    parts = [list(range(0, 12)), list(range(12, 23)), list(range(23, 34))]
    with P.scope():
        g2bc = P.sbuf([128, 2, 1024], F32, "e_g2")
        for r in range(2):
            P.dma("sp", g2bc[:, r, :], modv.ap()[r, 5120:6144].partition_broadcast(128), writes=[g2bc])
        acc = P.sbuf([128, 12, 1024], F32, "e_acc")
        hT = P.sbuf([128, 8, 12 * 128], BF16, "e_hT")
        stg = [P.sbuf([128, 8, 512], F32, "e_stg%d" % i) for i in range(2)]
        wg = [P.sbuf([128, 8, 512], BF16, "e_wg%d" % i) for i in range(2)]
        wu = [P.sbuf([128, 8, 512], BF16, "e_wu%d" % i) for i in range(2)]
        wd = [P.sbuf([128, 4, 1024], BF16, "e_wd%d" % i) for i in range(2)]
        hid = [P.sbuf([128, 4, 512], BF16, "e_hid%d" % i) for i in range(2)]
        sl = [P.sbuf([128, 512], F32, "e_sl%d" % i) for i in range(2)]
        pg = [P.psum([128, 512], F32, "e_pg%d" % i) for i in range(2)]
        pu = [P.psum([128, 512], F32, "e_pu%d" % i) for i in range(2)]
        po = [P.psum([128, 1024], F32, "e_po%d" % i) for i in range(2)]
        xt = [P.sbuf([128, 1024], F32, "e_xt%d" % i) for i in range(2)]
        hv = h2T_d.ap().rearrange("(k p) t -> p k t", p=128)
        si = 0; ci = 0; oi = 0; gi = 0
        for part in parts:
            tiles = [t for t in part if t >= t_lo]
            if not tiles:
                continue
            nt = len(tiles); tok0 = tiles[0] * 128; ntok = nt * 128
            P.dma("sp", hT[:, :, 0:ntok], hv[:, :, tok0:tok0 + ntok], reads=[h2T_d], writes=[hT])
            for e_ in range(nexp):
                wgb, wub, wdb = wg[e_ % 2], wu[e_ % 2], wd[e_ % 2]
                for dst, src in ((wgb, weg[e_].rearrange("(k p) n -> p k n", p=128)), (wub, weu[e_].rearrange("(k p) n -> p k n", p=128))):
                    s = stg[si % 2]; si += 1
                    P.dma("sp", s[:], src, writes=[s])
                    P.op("pool", lambda e, dst=dst, s=s: e.tensor_copy(dst[:], s[:]), reads=[s], writes=[dst])
                s = stg[si % 2]; si += 1
                sv = s[:].rearrange("p k n -> p (k n)").rearrange("p (k n) -> p k n", n=1024)
                P.dma("sp", sv, wed[e_].rearrange("(k p) n -> p k n", p=128), writes=[s])
                P.op("pool", lambda e, wdb=wdb, sv=sv: e.tensor_copy(wdb[:], sv), reads=[s], writes=[wdb])
                for g0 in range(0, nt, 4):
                    gt_ = tiles[g0:g0 + 4]; n = len(gt_) * 128; off = g0 * 128
                    hb = hid[gi % 2]; gi += 1
                    for c in range(4):
                        g_, u_, s_ = pg[ci % 2], pu[ci % 2], sl[ci % 2]; ci += 1
                        for kk in range(8):
                            P.op("pe", lambda e, g_=g_, kk=kk, c=c, wgb=wgb, off=off, n=n: e.matmul(g_[:, 0:n], wgb[:, kk, c * 128:(c + 1) * 128], hT[:, kk, off:off + n],
                                                                                                 start=(kk == 0), stop=(kk == 7)), reads=[wgb, hT], writes=[g_])
                        for kk in range(8):
                            P.op("pe", lambda e, u_=u_, kk=kk, c=c, wub=wub, off=off, n=n: e.matmul(u_[:, 0:n], wub[:, kk, c * 128:(c + 1) * 128], hT[:, kk, off:off + n],
                                                                                                 start=(kk == 0), stop=(kk == 7)), reads=[wub, hT], writes=[u_])
                        P.op("act", lambda e, g_=g_, s_=s_, n=n: e.activation(s_[:, 0:n], g_[:, 0:n], AF.Silu), reads=[g_], writes=[s_])
                        P.op("dve", lambda e, s_=s_, u_=u_, hb=hb, c=c, n=n: e.tensor_tensor(hb[:, c, 0:n], s_[:, 0:n], u_[:, 0:n], ALU.mult), reads=[s_, u_], writes=[hb])
                    for j, t in enumerate(gt_):
                        o_ = po[oi % 2]; oi += 1
                        for half in range(2):
                            for c in range(4):
                                P.op("pe", lambda e, o_=o_, half=half, c=c, j=j, hb=hb, wdb=wdb: e.matmul(o_[:, half * 512:(half + 1) * 512], hb[:, c, j * 128:(j + 1) * 128],
                                                                                                       wdb[:, c, half * 512:(half + 1) * 512], start=(c == 0), stop=(c == 3)),
                                     reads=[hb, wdb], writes=[o_])
                        ai = g0 + j
                        if e_ == 0:
                            P.op("dve", lambda e, o_=o_, ai=ai, t=t: e.tensor_scalar(acc[:, ai, :], o_[:], wgt[:, t, 0:1], None, ALU.mult), reads=[o_, wgt], writes=[acc])
                        else:
                            P.op("dve", lambda e, o_=o_, ai=ai, t=t, e_=e_: e.scalar_tensor_tensor(acc[:, ai, :], o_[:], wgt[:, t, e_:e_ + 1], acc[:, ai, :], ALU.mult, ALU.add),
                                 reads=[o_, wgt, acc], writes=[acc])
            for ai, t in enumerate(tiles):
                r = 0 if t < 2 else 1
                xb = xt[ai % 2]
                P.dma("sp", xb[:], xmid.ap()[t * 128:(t + 1) * 128, :], reads=[xmid], writes=[xb])
                P.op("pool", lambda e, ai=ai, r=r: e.tensor_tensor(acc[:, ai, :], acc[:, ai, :], g2bc[:, r, :], ALU.mult), reads=[acc, g2bc], writes=[acc])
                P.op("pool", lambda e, ai=ai, xb=xb: e.tensor_tensor(xb[:], xb[:], acc[:, ai, :], ALU.add), reads=[acc, xb], writes=[xb])
                if out_lat is not None:
                    if t >= 2:
                        P.dma("sp", out_lat.ap()[(t - 2) * 128:(t - 1) * 128, :], xb[:], reads=[xb], writes=[out_lat])
                else:
                    P.dma("sp", xout.ap()[t * 128:(t + 1) * 128, :], xb[:], reads=[xb], writes=[xout])


def gdn_consts(c):
    t = np.arange(128)[:, None]; i = np.arange(128)[None, :]
    sb = (t // 64) == (i // 64)
    c["g_tri"] = np.stack([(t <= i) & sb, (t >= i) & sb], 1).astype(np.float32)
    c["g_triS"] = np.stack([(t > i) & sb, (t < i) & sb], 1).astype(np.float32)
    c["g_mS"] = np.stack([(i > t) & sb, (i < t) & sb], 1).astype(np.float32)
    c["g_blk"] = np.concatenate([sb.astype(np.float32)[:, None, :],
                                 np.broadcast_to((t < 64), (128, 128)).astype(np.float32)[:, None, :],
                                 np.broadcast_to((t >= 64), (128, 128)).astype(np.float32)[:, None, :]], 1)
    c["zeros"] = np.zeros((1, 128), np.float32)


def phase_gdn(k, z, conv_w, a_log, dt_bias, gnorm, ctx_out):
    P = k.P
    with P.scope():
        tri = P.sbuf([128, 2, 128], F32, "g_tri"); triS = P.sbuf([128, 2, 128], F32, "g_triS")
        mS = P.sbuf([128, 2, 128], F32, "g_mS"); blk = P.sbuf([128, 3, 128], F32, "g_blk")
        for t_, n_ in ((tri, "g_tri"), (triS, "g_triS"), (mS, "g_mS"), (blk, "g_blk")):
            P.dma("sp", t_[:], k.cd[n_].ap(), writes=[t_])
        gnb = bc_load(k, gnorm, 128, "g_gn")
        gall = P.sbuf([128, NT, 8], F32, "g_gall"); ball = P.sbuf([128, NT, 8], F32, "g_ball")
        with P.scope():
            ab = P.sbuf([128, NT, 16], F32, "g_ab")
            zv = z.ap()[:, 5888:5904].rearrange("(t p) c -> p t c", p=128)
            P.dma("sp", ab[:, 0:17, :], zv[:, 0:17, :], writes=[ab]); P.dma("sp", ab[:, 17:34, :], zv[:, 17:34, :], writes=[ab])
            al = bc_load(k, a_log, 8, "g_al"); db = bc_load(k, dt_bias, 8, "g_db")
            P.op("act", lambda e: e.activation(al[:], al[:], AF.Exp), reads=[al], writes=[al])
            P.op("dve", lambda e: e.tensor_scalar(al[:], al[:], -1.0, None, ALU.mult), reads=[al], writes=[al])
            P.op("dve", lambda e: e.tensor_tensor(gall[:], ab[:, :, 0:8], db[:].unsqueeze(1).to_broadcast([128, NT, 8]), ALU.add), reads=[ab, db], writes=[gall])
            P.op("act", lambda e: e.activation(gall[:], gall[:], AF.Exp), reads=[gall], writes=[gall])
            P.op("act", lambda e: e.activation(gall[:], gall[:], AF.Ln, bias=k.one_col[:]), reads=[gall, k.one_col], writes=[gall])
            P.op("dve", lambda e: e.tensor_tensor(gall[:], gall[:], al[:].unsqueeze(1).to_broadcast([128, NT, 8]), ALU.mult), reads=[gall, al], writes=[gall])
            P.op("act", lambda e: e.activation(ball[:], ab[:, :, 8:16], AF.Sigmoid), reads=[ab], writes=[ball])
        q = P.sbuf([128, NT, 128], F32, "g_q"); kk_ = P.sbuf([128, NT, 128], F32, "g_k"); v = P.sbuf([128, NT, 128], F32, "g_v")
        kT = P.sbuf([128, T], F32, "g_kT"); qT = P.sbuf([128, T], F32, "g_qT")
        od = [P.sbuf([128, NT, 128], F32, "g_od%d" % i) for i in range(2)]
        zview = z.ap().rearrange("(t p) c -> p t c", p=128)
        for h in range(4):
            with P.scope():
                xp = P.sbuf([128, NT, 128], F32, "g_xp"); xn = P.sbuf([128, NT, 128], F32, "g_xn"); wcb = P.sbuf([128, 3, 128], F32, "g_wc")
                ss = P.sbuf([128, NT], F32, "g_ss")
                for name, dst in (("gdn_q", q), ("gdn_k", kk_), ("gdn_v", v)):
                    col = ZOFF[name] + h * 128
                    zc = zview[:, :, col:col + 128]
                    for lo, hi in ((0, 17), (17, 34)):
                        P.dma("sp", dst[:, lo:hi, :], zc[:, lo:hi, :], writes=[dst])
                        P.dma("sp", xp[1:128, lo:hi, :], zc[0:127, lo:hi, :], writes=[xp])
                        P.dma("sp", xn[0:127, lo:hi, :], zc[1:128, lo:hi, :], writes=[xn])
                    P.dma("sp", xp[0:1, 1:34, :], zc[127:128, 0:33, :], writes=[xp])
                    P.dma("sp", xn[127:128, 0:33, :], zc[0:1, 1:34, :], writes=[xn])
                    for tt in (0, 2):
                        P.dma("sp", xp[0:1, tt, :], k.cd["zeros"].ap(), writes=[xp])
                    for tt in (1, 33):
                        P.dma("sp", xn[127:128, tt, :], k.cd["zeros"].ap(), writes=[xn])
                    for j in range(3):
                        P.dma("sp", wcb[:, j, :], conv_w[j, col - ZOFF["gdn_q"]:col - ZOFF["gdn_q"] + 128].partition_broadcast(128), writes=[wcb])
                    bs = [128, NT, 128]
                    P.op("dve", lambda e, dst=dst: e.tensor_tensor(dst[:], dst[:], wcb[:, 1, :].unsqueeze(1).to_broadcast(bs), ALU.mult), reads=[dst, wcb], writes=[dst])
                    P.op("pool", lambda e: e.tensor_tensor(xp[:], xp[:], wcb[:, 0, :].unsqueeze(1).to_broadcast(bs), ALU.mult), reads=[xp, wcb], writes=[xp])
                    P.op("dve", lambda e: e.tensor_tensor(xn[:], xn[:], wcb[:, 2, :].unsqueeze(1).to_broadcast(bs), ALU.mult), reads=[xn, wcb], writes=[xn])
                    P.op("pool", lambda e, dst=dst: e.tensor_tensor(dst[:], dst[:], xp[:], ALU.add), reads=[dst, xp], writes=[dst])
                    P.op("dve", lambda e, dst=dst: e.tensor_tensor(dst[:], dst[:], xn[:], ALU.add), reads=[dst, xn], writes=[dst])
                    P.op("act", lambda e, dst=dst: e.activation(dst[:], dst[:], AF.Silu), reads=[dst], writes=[dst])
                    if name != "gdn_v":
                        P.op("pool", lambda e, dst=dst: e.tensor_tensor(xp[:], dst[:], dst[:], ALU.mult), reads=[dst], writes=[xp])
                        P.op("dve", lambda e: e.tensor_reduce(ss[:], xp[:], AX.X, ALU.add), reads=[xp], writes=[ss])
                        P.op("act", lambda e: e.activation(ss[:], ss[:], AF.Sqrt, bias=k.eps_col[:]), reads=[ss, k.eps_col], writes=[ss])
                        P.op("dve", lambda e: e.reciprocal(ss[:], ss[:]), reads=[ss], writes=[ss])
                        if name == "gdn_q":
                            P.op("dve", lambda e: e.tensor_scalar(ss[:], ss[:], 128.0 ** -0.5, None, ALU.mult), reads=[ss], writes=[ss])
                        P.op("dve", lambda e, dst=dst: e.tensor_tensor(dst[:], dst[:], ss[:].unsqueeze(2).to_broadcast(bs), ALU.mult), reads=[dst, ss], writes=[dst])
            transpose_to(k, lambda t: q[:, t, :], 128, qT, "g_tq", src_bufs=[q])
            transpose_to(k, lambda t: kk_[:, t, :], 128, kT, "g_tk", src_bufs=[kk_])
            with P.scope():
                psO = [P.psum([128, 512], F32, "g_psO%d" % i) for i in range(2)]
                psW = [P.psum([128, 512], F32, "g_psW%d" % i) for i in range(2)]
                psM = [P.psum([128, 512], F32, "g_psM%d" % i) for i in range(4)]
                gens = [gdn_dir(k, dr, h, q, kk_, v, qT, kT, gall, ball, tri, triS, mS, blk, od[dr], psO[dr], psW[dr], psM[2 * dr:2 * dr + 2]) for dr in range(2)]
                alive = [True, True]
                while any(alive):
                    for dr in range(2):
                        if alive[dr]:
                            try:
                                next(gens[dr])
                            except StopIteration:
                                alive[dr] = False
            with P.scope():
                gt = P.sbuf([128, NT, 128], F32, "g_gate"); ss = P.sbuf([128, NT], F32, "g_fss"); sq = P.sbuf([128, NT, 128], F32, "g_fsq")
                zc = zview[:, :, ZOFF["gdn_g"] + h * 128:ZOFF["gdn_g"] + (h + 1) * 128]
                P.dma("sp", gt[:, 0:17, :], zc[:, 0:17, :], writes=[gt]); P.dma("sp", gt[:, 17:34, :], zc[:, 17:34, :], writes=[gt])
                o = od[0]; bs = [128, NT, 128]
                P.op("dve", lambda e: e.tensor_tensor(o[:], o[:], od[1][:], ALU.add), reads=[o, od[1]], writes=[o])
                P.op("pool", lambda e: e.tensor_tensor(sq[:], o[:], o[:], ALU.mult), reads=[o], writes=[sq])
                P.op("dve", lambda e: e.tensor_reduce(ss[:], sq[:], AX.X, ALU.add), reads=[sq], writes=[ss])
                P.op("act", lambda e: e.activation(ss[:], ss[:], AF.Sqrt, bias=k.eps_col[:], scale=1.0 / 128), reads=[ss, k.eps_col], writes=[ss])
                P.op("dve", lambda e: e.reciprocal(ss[:], ss[:]), reads=[ss], writes=[ss])
                P.op("dve", lambda e: e.tensor_tensor(o[:], o[:], ss[:].unsqueeze(2).to_broadcast(bs), ALU.mult), reads=[o, ss], writes=[o])
                P.op("pool", lambda e: e.tensor_tensor(o[:], o[:], gnb[:].unsqueeze(1).to_broadcast(bs), ALU.mult), reads=[o, gnb], writes=[o])
                P.op("act", lambda e: e.activation(gt[:], gt[:], AF.Silu), reads=[gt], writes=[gt])
                P.op("dve", lambda e: e.tensor_tensor(o[:], o[:], gt[:], ALU.mult), reads=[o, gt], writes=[o])
                store_head(k, o, k.ybr.ap()[3], h * 128, 0 if ctx_out else 2)


def gdn_dir(k, dr, h, q, kk_, v, qT, kT, gall, ball, tri, triS, mS, blk, od, psO, psW, psM):
    P = k.P
    c = dr * 4 + h
    tg = "g%d_" % dr
    A = lambda nm, shape=(128, 128), n=2: [P.sbuf(list(shape), F32, tg + nm + str(i)) for i in range(n)]
    gcs = P.sbuf([128, 4, NT], F32, tg + "gcs")
    cdb = P.sbuf([128, 2, NT], F32, tg + "cdb")
    gc = gall[:, :, c]; bt = ball[:, :, c]
    ident = k.ident_f
    pm = psM[0]
    P.op("pe", lambda e: e.matmul(pm[:, 0:NT], tri[:, dr, :], gc, start=True, stop=True), reads=[tri, gall], writes=[pm]); yield
    P.op("pe", lambda e: e.matmul(pm[:, 64:64 + NT], blk[:, 0, :], gc, start=True, stop=True), reads=[blk, gall], writes=[pm]); yield
    P.op("pe", lambda e: e.matmul(pm[:, 128:128 + NT], blk[:, 1, :], gc, start=True, stop=True), reads=[blk, gall], writes=[pm]); yield
    P.op("pe", lambda e: e.matmul(pm[:, 192:192 + NT], blk[:, 2, :], gc, start=True, stop=True), reads=[blk, gall], writes=[pm]); yield
    P.op("dve", lambda e: e.tensor_copy(gcs[:, 0, :], pm[:, 0:NT]), reads=[pm], writes=[gcs]); yield
    P.op("act", lambda e: e.activation(gcs[:, 1, :], pm[:, 0:NT], AF.Exp), reads=[pm], writes=[gcs]); yield
    P.op("dve", lambda e: e.tensor_tensor(gcs[:, 2, :], pm[:, 64:64 + NT], gcs[:, 0, :], ALU.subtract), reads=[pm, gcs], writes=[gcs]); yield
    P.op("act", lambda e: e.activation(gcs[:, 2, :], gcs[:, 2, :], AF.Exp), reads=[gcs], writes=[gcs]); yield
    P.op("act", lambda e: e.activation(cdb[:, 0, :], pm[:, 128:128 + NT], AF.Exp), reads=[pm], writes=[cdb]); yield
    P.op("act", lambda e: e.activation(cdb[:, 1, :], pm[:, 192:192 + NT], AF.Exp), reads=[pm], writes=[cdb]); yield
    kb, kbg, vb, kend, qd, gtri = A("kb"), A("kbg"), A("vb"), A("kend"), A("qd"), A("gtri")
    EDs, EDi, kbT, AT, aqkT = A("EDs"), A("EDi"), A("kbT"), A("AT"), A("aqkT")
    Ap, Bp, Pm = A("Ap", n=3), A("Bp", n=3), A("Pm", n=3)
    u, wT, vnew = A("u"), A("wT"), A("vnew")
    qdTm = [P.sbuf([128, 2, 128], F32, tg + "qdTm%d" % i) for i in range(2)]
    for b_ in qdTm:
        P.op("pool", lambda e, b_=b_: e.memset(b_[:], 0.0), writes=[b_]); yield
    S = [P.sbuf([128, 128], F32, tg + "S%d" % i) for i in range(2)]
    P.op("pool", lambda e: e.memset(S[0][:], 0.0), writes=[S[0]]); yield
    Scur = S[0]; si = 0
    order = list(range(NT)) if dr == 0 else [1, 0] + list(range(NT - 1, 1, -1))
    slot = [0]

    def ms():
        s_ = slot[0]; slot[0] += 1
        bank = psM[(s_ // 4) % 2]; o_ = (s_ % 4) * 128
        return bank, bank[:, o_:o_ + 128]

    for it, n in enumerate(order):
        r2 = it % 2
        sl = slice(n * 128, (n + 1) * 128)
        kt, qt, vt = kk_[:, n, :], q[:, n, :], v[:, n, :]
        bcol = ball[:, n, c:c + 1]; gcol = gall[:, n, c:c + 1]
        eg = gcs[:, 1, n:n + 1]; ek = gcs[:, 2, n:n + 1]
        kb_, kbg_, vb_, kend_, qd_, gtri_ = kb[r2], kbg[r2], vb[r2], kend[r2], qd[r2], gtri[r2]
        P.op("dve", lambda e: e.tensor_scalar(kb_[:], kt, bcol, None, ALU.mult), reads=[kk_, ball], writes=[kb_]); yield
        P.op("pool", lambda e: e.tensor_scalar(kbg_[:], kb_[:], eg, None, ALU.mult), reads=[kb_, gcs], writes=[kbg_]); yield
        P.op("pool", lambda e: e.tensor_scalar(vb_[:], vt, bcol, None, ALU.mult), reads=[v, ball], writes=[vb_]); yield
        P.op("pool", lambda e: e.tensor_scalar(kend_[:], kt, ek, None, ALU.mult), reads=[kk_, gcs], writes=[kend_]); yield
        P.op("dve", lambda e: e.tensor_scalar(qd_[:], qt, eg, None, ALU.mult), reads=[q, gcs], writes=[qd_]); yield
        P.op("dve", lambda e: e.tensor_scalar(gtri_[:], tri[:, dr, :], gcol, None, ALU.mult), reads=[tri, gall], writes=[gtri_]); yield
        bk, p_ = ms()
        P.op("pe", lambda e: e.matmul(p_, triS[:, dr, :], gtri_[:], start=True, stop=True), reads=[triS, gtri_], writes=[bk]); yield
        EDs_, EDi_ = EDs[r2], EDi[r2]
        P.op("act", lambda e: e.activation(EDs_[:], p_, AF.Exp), reads=[bk], writes=[EDs_]); yield
        P.op("pool", lambda e: e.tensor_tensor(EDs_[:], EDs_[:], mS[:, dr, :], ALU.mult), reads=[EDs_, mS], writes=[EDs_]); yield
        P.op("pool", lambda e: e.tensor_tensor(EDi_[:], EDs_[:], ident[:], ALU.add), reads=[EDs_, ident], writes=[EDi_]); yield
        bk, p_ = ms(); kbT_ = kbT[r2]
        P.op("pe", lambda e: e.transpose(p_, kb_[:], ident[:]), reads=[kb_, ident], writes=[bk]); yield
        P.op("act", lambda e: e.copy(kbT_[:], p_), reads=[bk], writes=[kbT_]); yield
        bk, p_ = ms(); AT_ = AT[r2]
        P.op("pe", lambda e: e.matmul(p_, kT[:, sl], kbT_[:], start=True, stop=True), reads=[kT, kbT_], writes=[bk]); yield
        P.op("dve", lambda e: e.tensor_tensor(AT_[:], p_, EDs_[:], ALU.mult), reads=[bk, EDs_], writes=[AT_]); yield
        bk, p_ = ms(); aqkT_ = aqkT[r2]
        P.op("pe", lambda e: e.matmul(p_, kT[:, sl], qT[:, sl], start=True, stop=True), reads=[kT, qT], writes=[bk]); yield
        P.op("dve", lambda e: e.tensor_tensor(aqkT_[:], p_, EDi_[:], ALU.mult), reads=[bk, EDi_], writes=[aqkT_]); yield
        bk, p_ = ms(); pi = 0
        Ac, Bc, Pc = Ap[0], AT_, Pm[0]
        P.op("pe", lambda e: e.transpose(p_, AT_[:], ident[:]), reads=[AT_, ident], writes=[bk]); yield
        P.op("act", lambda e: e.copy(Ac[:], p_), reads=[bk], writes=[Ac]); yield
        P.op("dve", lambda e: e.tensor_tensor(Pc[:], ident[:], AT_[:], ALU.subtract), reads=[ident, AT_], writes=[Pc]); yield
        for lv in range(1, 6):
            An, Bn, Pn = Ap[lv % 3], Bp[lv % 3], Pm[lv % 3]
            bk, p_ = ms()
            P.op("pe", lambda e, p_=p_, Ac=Ac, Bc=Bc: e.matmul(p_, Bc[:], Ac[:], start=True, stop=True), reads=[Ac, Bc], writes=[bk]); yield
            P.op("act", lambda e, p_=p_, An=An: e.copy(An[:], p_), reads=[bk], writes=[An]); yield
            if lv < 5:
                bk2, p2 = ms()
                P.op("pe", lambda e, p2=p2, Ac=Ac, Bc=Bc: e.matmul(p2, Ac[:], Bc[:], start=True, stop=True), reads=[Ac, Bc], writes=[bk2]); yield
                P.op("dve", lambda e, p2=p2, Bn=Bn: e.tensor_copy(Bn[:], p2), reads=[bk2], writes=[Bn]); yield
            bk3, p3 = ms()
            P.op("pe", lambda e, p3=p3, Pc=Pc: e.matmul(p3, ident[:], Pc[:], start=True, stop=False), reads=[ident, Pc], writes=[bk3]); yield
            P.op("pe", lambda e, p3=p3, Pc=Pc, An=An: e.matmul(p3, An[:], Pc[:], start=False, stop=True), reads=[An, Pc], writes=[bk3]); yield
            P.op("dve", lambda e, p3=p3, Pn=Pn: e.tensor_copy(Pn[:], p3), reads=[bk3], writes=[Pn]); yield
            Ac, Bc, Pc = An, Bn, Pn
        MT = Pc
        u_, wT_, vnew_, qdTm_ = u[r2], wT[r2], vnew[r2], qdTm[r2]
        bk, p_ = ms()
        P.op("pe", lambda e: e.matmul(p_, MT[:], vb_[:], start=True, stop=True), reads=[MT, vb_], writes=[bk]); yield
        P.op("act", lambda e: e.copy(u_[:], p_), reads=[bk], writes=[u_]); yield
        bk, p_ = ms()
        P.op("pe", lambda e: e.matmul(p_, kbg_[:], MT[:], start=True, stop=True), reads=[MT, kbg_], writes=[bk]); yield
        P.op("dve", lambda e: e.tensor_copy(wT_[:], p_), reads=[bk], writes=[wT_]); yield
        bk, p_ = ms()
        P.op("pe", lambda e: e.transpose(p_, qd_[:], ident[:]), reads=[qd_, ident], writes=[bk]); yield
        P.op("act", lambda e: e.copy(qdTm_[:, 0, 0:64], p_[:, 0:64]), reads=[bk], writes=[qdTm_]); yield
        P.op("dve", lambda e: e.tensor_copy(qdTm_[:, 1, 64:128], p_[:, 64:128]), reads=[bk], writes=[qdTm_]); yield
        pO = psO[:, 0:128]
        for bi, b in enumerate((0, 1) if dr == 0 else (1, 0)):
            rb = slice(b * 64, (b + 1) * 64)
            pW = psW[:, 0:128]; pKV = psW[:, 128:256]
            Sn = S[(si + 1) % 2]
            P.op("pe", lambda e, Scur=Scur: e.matmul(pW, wT_[:], Scur[:], start=True, stop=True), reads=[wT_, Scur], writes=[psW]); yield
            P.op("dve", lambda e, rb=rb: e.tensor_tensor(vnew_[rb, :], u_[rb, :], pW[rb, :], ALU.subtract), reads=[u_, psW], writes=[vnew_]); yield
            P.op("pe", lambda e, b=b, bi=bi, Scur=Scur: e.matmul(pO, qdTm_[:, b, :], Scur[:], start=(bi == 0), stop=False), reads=[qdTm_, Scur], writes=[psO]); yield
            P.op("pe", lambda e, rb=rb, bi=bi: e.matmul(pO, aqkT_[rb, :], vnew_[rb, :], start=False, stop=(bi == 1)), reads=[aqkT_, vnew_], writes=[psO]); yield
            P.op("pe", lambda e, rb=rb: e.matmul(pKV, kend_[rb, :], vnew_[rb, :], start=True, stop=True), reads=[kend_, vnew_], writes=[psW]); yield
            P.op("dve", lambda e, b=b, Scur=Scur, Sn=Sn: e.scalar_tensor_tensor(Sn[:], Scur[:], cdb[:, b, n:n + 1], pKV, ALU.mult, ALU.add),
                 reads=[Scur, cdb, psW], writes=[Sn]); yield
            Scur = Sn; si += 1
        P.op("act", lambda e: e.copy(od[:, n, :], pO), reads=[psO], writes=[od]); yield


from concourse.bass_utils import run_bass_kernel_spmd

DEPTH = 2
W_NAMES = ["w_mod", "b_mod", "norm1", "norm2", "w_in", "ret_decay", "ret_gn", "win_qnorm", "win_knorm", "win_sink",
           "na_qnorm", "na_knorm", "nab", "gdn_conv", "gdn_a_log", "gdn_dt_bias", "gdn_norm", "w_branch", "w_merge", "w_out",
           "w_router", "router_bias", "w_e_gate", "w_e_up", "w_e_down"]
W_SHAPES = dict(w_mod=[2, D, 6144], b_mod=[2, 6144], norm1=[2, D], norm2=[2, D], w_in=[2, D, DIN], ret_decay=[2, 8], ret_gn=[2, 512],
                win_qnorm=[2, 64], win_knorm=[2, 64], win_sink=[2, 8], na_qnorm=[2, 64], na_knorm=[2, 64], nab=[2, 8, 128, 21, 128],
                gdn_conv=[2, 3, 1536], gdn_a_log=[2, 8], gdn_dt_bias=[2, 8], gdn_norm=[2, 128], w_branch=[2, 4, 512, D],
                w_merge=[2, D, 4096], w_out=[2, D, D], w_router=[D, 32], router_bias=[32], w_e_gate=[2, 32, D, 512],
                w_e_up=[2, 32, D, 512], w_e_down=[2, 32, 512, D])


LAYERED = [n for n in W_NAMES if n not in ("w_router", "router_bias")]


def build_program(fused=True):
    nc = bass.Bass("TRN2", target_bir_lowering=False)
    P = Prog(nc)
    k = K(P)
    nl = DEPTH if fused else 1
    cd = {n: P.dram("c_" + n, list(a.shape), F32, kind="ExternalInput") for n, a in mk_consts().items()}
    xin = P.dram("xin", [T, D], F32, kind="ExternalInput")
    cc = P.dram("cc", [2, D], F32, kind="ExternalInput")
    W = {n: P.dram(n, (W_SHAPES[n] if (fused or n not in LAYERED) else W_SHAPES[n][1:]), F32, kind="ExternalInput") for n in W_NAMES}
    if fused:
        out = P.dram("out", [4096, D], F32, kind="ExternalOutput")
        x1 = P.dram("x1", [T, D], F32)
    else:
        out = None
        x1 = P.dram("xo", [T, D], F32, kind="ExternalOutput")
    modv = [P.dram("modv%d" % l, [2, 6144], F32) for l in range(nl)]
    hT_d = P.dram("hT_d", [D, T], BF16)
    z = P.dram("z", [T, DIN], F32)
    k.ybr = P.dram("ybr", [4, T, 512], F32)
    xmid = P.dram("xmid", [T, D], F32)
    h2T_d = P.dram("h2T_d", [D, T], BF16)
    h32_d = P.dram("h32_d", [D, T], F32)
    load_consts(k, cd)
    mT = [P.sbuf([128, 48, 2], F32, "mT%d" % l) for l in range(nl)]

    def wsub(n, l):
        return _Sub(W[n], l) if (fused and n in LAYERED) else W[n]

    for l in range(nl):
        phase_mod(k, cc, wsub("w_mod", l), wsub("b_mod", l), modv[l], mT[l])
    xs = [xin, x1]
    for l in range(nl):
        ctx_out = (l < DEPTH - 1) if fused else True
        x = xs[l]
        w = lambda n: wsub(n, l).ap()
        with P.scope():
            hsb = P.sbuf([128, 8, T], BF16, "hsb")
            phase_norm(k, x, w("norm1"), mT[l], 0, hsb)
            for kk in range(8):
                P.dma("sp", hT_d.ap()[kk * 128:(kk + 1) * 128, :], hsb[:, kk, :], reads=[hsb], writes=[hT_d])
            phase_proj(k, hsb, w("w_in"), z, DIN)
        phase_ret(k, z, w("ret_decay"), w("ret_gn"), ctx_out)
        phase_window(k, z, w("win_qnorm"), w("win_knorm"), w("win_sink"), ctx_out)
        phase_na(k, z, w("na_qnorm"), w("na_knorm"), w("nab"), ctx_out)
        phase_gdn(k, z, w("gdn_conv"), w("gdn_a_log"), w("gdn_dt_bias"), w("gdn_norm"), ctx_out)
        phase_merge(k, hT_d, x, xmid, w("w_merge"), w("w_branch"), w("w_out"), modv[l], ctx_out)
        t_lo = 0 if ctx_out else 2
        with P.scope():
            h2 = P.sbuf([128, 8, T], BF16, "h2sb")
            phase_norm(k, xmid, w("norm2"), mT[l], 1, h2, hT32_d=h32_d, t_lo=t_lo)
            for kk in range(8):
                P.dma("sp", h2T_d.ap()[kk * 128:(kk + 1) * 128, t_lo * 128:], h2[:, kk, t_lo * 128:], reads=[h2], writes=[h2T_d])
        with P.scope():
            wgt = P.sbuf([128, NT, 32], F32, "wgt")
            phase_router(k, h32_d, W["w_router"].ap(), W["router_bias"].ap(), wgt, t_lo)
            phase_moe(k, h2T_d, wgt, xmid, x1, w("w_e_gate"), w("w_e_up"), w("w_e_down"), modv[l], t_lo,
                      out_lat=(out if (fused and l == DEPTH - 1) else None))
    P.wait_all("sp", [out] if fused else [x1])
    P.emit()
    return nc, P


class _Sub:
    def __init__(self, parent, l):
        self.p = parent; self.l = l
        self.w = parent.w; self.r = parent.r

    def ap(self):
        return self.p.ap()[self.l]


def _sub(buf, l):
    return _Sub(buf, l)


_CACHE = {}
FUSED = True


def host_inputs(inputs, layer=None, x_prev=None):
    f = lambda a: np.ascontiguousarray(np.asarray(a, dtype=np.float32))
    shared = {"c_" + n: a for n, a in mk_consts().items()}
    for n in W_NAMES:
        if n == "nab":
            full = np.stack([na_bias_gather(f(inputs["na_rpb"][l])) for l in range(DEPTH)])
        else:
            full = f(inputs[n]).reshape(W_SHAPES[n])
        shared[n] = full if (layer is None or n not in LAYERED) else np.ascontiguousarray(full[layer])
    maps = []
    for b in range(8):
        m = dict(shared)
        if x_prev is None:
            m["xin"] = np.concatenate([f(inputs["ctx"][b]), f(inputs["x"][b])], axis=0)
        else:
            m["xin"] = x_prev[b]
        m["cc"] = np.stack([f(inputs["c_ctx"]), f(inputs["c"][b])])
        maps.append(m)
    return maps


def kernel(**inputs):
    if "nc" not in _CACHE:
        _CACHE["nc"] = build_program(FUSED)[0]
    nc = _CACHE["nc"]
    cores = list(range(8))
    if FUSED:
        res = run_bass_kernel_spmd(nc, host_inputs(inputs), core_ids=cores)
        return np.stack([np.asarray(r["out"], dtype=np.float32) for r in res.results], axis=0)
    xp = None
    for l in range(DEPTH):
        res = run_bass_kernel_spmd(nc, host_inputs(inputs, layer=l, x_prev=xp), core_ids=cores)
        xp = [np.asarray(r["xo"], dtype=np.float32) for r in res.results]
    return np.stack([x[LC:] for x in xp], axis=0)
```

```python
from contextlib import ExitStack
import numpy as np
import concourse.bass as bass
import concourse.mybir as mybir

F32 = mybir.dt.float32
BF16 = mybir.dt.bfloat16
I32 = mybir.dt.int32
AF = mybir.ActivationFunctionType
ALU = mybir.AluOpType
AX = mybir.AxisListType

ENGS = ("pe", "dve", "act", "pool", "sp")
N_DSEM = 8
SEM_WRAP = 30000


class Buf:
    def __init__(self, t, name=""):
        self.t = t
        self.name = name
        self.w = {}
        self.r = {}

    v = None
    is_psum = False

    def __getitem__(self, idx):
        if self.v is not None:
            return self.v[idx]
        return self.t[idx]

    def ap(self):
        return self.t.ap() if hasattr(self.t, "ap") else self.t[:]


class Prog:
    def __init__(self, nc, same_engine_sync=True, direct=True):
        self.nc = nc
        self.es = ExitStack()
        self.ops = {e: [] for e in ENGS}
        self.cnt = {e: 0 for e in ENGS}
        self.seen = {e: {} for e in ENGS}
        self.sems = {}
        self.same = same_engine_sync
        self.dma_n = {e: 0 for e in ENGS}
        self.uid = 0
        self.stacks = [self.es]
        self.scope_bufs = [[]]
        self.free_deps = {}
        self.direct = direct
        self.nops = {}
        self.engobj = {"pe": nc.tensor, "dve": nc.vector, "act": nc.scalar, "pool": nc.gpsimd, "sp": nc.sync}

    def scope(self):
        prog = self

        class _S:
            def __enter__(s):
                st = ExitStack()
                prog.stacks.append(st)
                prog.scope_bufs.append([])
                return s

            def __exit__(s, *a):
                st = prog.stacks.pop()
                for b in prog.scope_bufs.pop():
                    for dd in (b.w, b.r):
                        for kk, v in dd.items():
                            prog.free_deps[kk] = max(prog.free_deps.get(kk, 0), v)
                st.close()
                return False
        return _S()

    def sem(self, key):
        if key not in self.sems:
            self.sems[key] = self.es.enter_context(self.nc.semaphore("s_%s_%s_%d" % key))
        return self.sems[key]

    def sbuf(self, shape, dt, name=None):
        self.uid += 1
        name = "%s_%d" % (name or "sb", self.uid)
        t = self.stacks[-1].enter_context(self.nc.sbuf_tensor(name, list(shape), dt))
        b = Buf(t, name)
        b.w = dict(self.free_deps)
        self.scope_bufs[-1].append(b)
        return b

    def psum(self, shape, dt=F32, name=None):
        self.uid += 1
        name = "%s_%d" % (name or "ps", self.uid)
        p, n = shape
        nb = (n * 4 + 2047) // 2048
        t = self.stacks[-1].enter_context(self.nc.psum_tensor(name, [128, nb * 512], F32))
        b = Buf(t, name)
        b.v = t[0:p, 0:n]
        b.is_psum = True
        b.w = dict(self.free_deps)
        self.scope_bufs[-1].append(b)
        return b

    def dram(self, name, shape, dt, kind="Internal"):
        t = self.nc.dram_tensor(name, list(shape), dt, kind=kind)
        return Buf(t, name)

    def _waits(self, eng, reads, writes):
        need = {}
        for b in reads:
            for k, v in b.w.items():
                need[k] = max(need.get(k, 0), v)
        for b in writes:
            for k, v in b.w.items():
                need[k] = max(need.get(k, 0), v)
            for k, v in b.r.items():
                need[k] = max(need.get(k, 0), v)
        out = []
        seen = self.seen[eng]
        for k, v in need.items():
            if k[0] == eng and k[1] != "d":
                if eng == "pe" or not self.same:
                    continue
            if seen.get(k, 0) >= v:
                continue
            seen[k] = v
            out.append((k, v))
        return out

    def op(self, eng, fn, reads=(), writes=()):
        pr = [b for b in reads if b.is_psum]
        if pr:
            writes = list(writes) + pr
        waits = self._waits(eng, reads, writes)
        self.cnt[eng] += 1
        n = self.cnt[eng]
        key = (eng, "c", (n - 1) // SEM_WRAP)
        val = (n - 1) % SEM_WRAP + 1
        self._put(eng, waits, fn, key, 1)
        for b in reads:
            b.r[key] = max(b.r.get(key, 0), val)
        for b in writes:
            b.w[key] = max(b.w.get(key, 0), val)

    def dma(self, q, out_ap, in_ap, reads=(), writes=(), **kw):
        waits = self._waits(q, reads, writes)
        n = self.dma_n[q]
        self.dma_n[q] += 1
        key = (q, "d", n % N_DSEM)
        val = 16 * (n // N_DSEM + 1)
        if val > 16 and self.seen[q].get(key, 0) < val - 16:
            self.seen[q][key] = val - 16
            waits.append((key, val - 16))

        def fn(e, out_ap=out_ap, in_ap=in_ap, kw=kw):
            return e.dma_start(out=out_ap, in_=in_ap, **kw)

        self._put(q, waits, fn, key, 16)
        for b in reads:
            b.r[key] = max(b.r.get(key, 0), val)
        for b in writes:
            b.w[key] = max(b.w.get(key, 0), val)

    def wait_all(self, eng, bufs):
        need = {}
        for b in bufs:
            for k, v in b.w.items():
                need[k] = max(need.get(k, 0), v)
        self._put(eng, list(need.items()), None, None, 0)

    def _put(self, eng, waits, fn, key, inc):
        if not self.direct:
            self.ops[eng].append((waits, fn, key, inc))
            return
        self.nops[eng] = self.nops.get(eng, 0) + 1
        e = self.engobj[eng]
        for k, v in waits:
            e.wait_ge(self.sem(k), v)
        if fn is not None:
            fn(e).then_inc(self.sem(key), inc)

    def emit(self):
        nc = self.nc
        if self.direct:
            self.es.close()
            return
        for e in ENGS:
            for waits, fn, key, inc in self.ops[e]:
                for k, v in waits:
                    self.sem(k)
                if key is not None:
                    self.sem(key)
        with nc.Block() as block:
            def run(engname):
                def body(eng):
                    for waits, fn, key, inc in self.ops[engname]:
                        for k, v in waits:
                            eng.wait_ge(self.sems[k], v)
                        if fn is not None:
                            fn(eng).then_inc(self.sems[key], inc)
                return body
            if self.ops["sp"]:
                block.sync(run("sp"))
            if self.ops["pe"]:
                block.tensor(run("pe"))
            if self.ops["dve"]:
                block.vector(run("dve"))
            if self.ops["act"]:
                block.scalar(run("act"))
            if self.ops["pool"]:
                block.gpsimd(run("pool"))
        self.es.close()


D = 1024
T = 4352
NT = T // 128
LC = 256
DIN = 5904
EPS = 1e-6


class K:
    def __init__(self, P):
        self.P = P
        self.rr = 0

    def evac_engine(self):
        self.rr += 1
        return "act" if self.rr % 2 else "dve"


def mk_consts():
    c = {}
    c["ident_f"] = np.eye(128, dtype=np.float32)
    cos, sin = rope_tables()
    c["cos"] = cos; c["sin"] = sin
    j = np.arange(128)[:, None]; i = np.arange(128)[None, :]
    c["maskPrev"] = (i <= j).astype(np.float32)
    c["maskNext"] = (j <= i).astype(np.float32)
    ret_consts(c)
    gdn_consts(c)
    return c


def load_consts(k, cd):
    P = k.P
    k.ident_f = P.sbuf([128, 128], F32, "ident_f")
    P.dma("sp", k.ident_f[:], cd["ident_f"].ap(), writes=[k.ident_f])
    k.ident_b = P.sbuf([128, 128], BF16, "ident_b")
    P.op("dve", lambda e: e.tensor_copy(k.ident_b[:], k.ident_f[:]), reads=[k.ident_f], writes=[k.ident_b])
    k.cos = P.sbuf([128, 32, 32], F32, "cos"); k.sin = P.sbuf([128, 32, 32], F32, "sin")
    P.dma("sp", k.cos[:], cd["cos"].ap().rearrange("(t p) f -> p t f", p=128), writes=[k.cos])
    P.dma("sp", k.sin[:], cd["sin"].ap().rearrange("(t p) f -> p t f", p=128), writes=[k.sin])
    k.maskPrev = P.sbuf([128, 128], BF16, "maskPrev"); k.maskNext = P.sbuf([128, 128], BF16, "maskNext")
    with P.scope():
        mst = P.sbuf([128, 2, 128], F32, "mst")
        P.dma("sp", mst[:, 0, :], cd["maskPrev"].ap(), writes=[mst])
        P.dma("sp", mst[:, 1, :], cd["maskNext"].ap(), writes=[mst])
        P.op("dve", lambda e: e.tensor_copy(k.maskPrev[:], mst[:, 0, :]), reads=[mst], writes=[k.maskPrev])
        P.op("dve", lambda e: e.tensor_copy(k.maskNext[:], mst[:, 1, :]), reads=[mst], writes=[k.maskNext])
    k.cd = cd
    k.one_col = P.sbuf([128, 1], F32, "one_col")
    P.op("dve", lambda e: e.memset(k.one_col[:], 1.0), writes=[k.one_col])
    k.eps_col = P.sbuf([128, 1], F32, "eps_col")
    P.op("dve", lambda e: e.memset(k.eps_col[:], EPS), writes=[k.eps_col])


def load_w_bf16(k, dst, dst_ap, src_ap, stage, stage_ap):
    P = k.P
    P.dma("sp", stage_ap, src_ap, writes=[stage])
    P.op("pool", lambda e: e.tensor_copy(dst_ap, stage_ap), reads=[stage], writes=[dst])


def cols_from_rows(k, dst_ap, rows_ap, R, tag):
    P = k.P
    with P.scope():
        st = P.sbuf([R, 128], F32, tag + "_rows")
        ps = P.psum([128, R], F32, tag + "_ps")
        P.dma("sp", st[:], rows_ap, writes=[st])
        P.op("pe", lambda e: e.transpose(ps[:], st[:], k.ident_f[0:R, 0:R]), reads=[st, k.ident_f], writes=[ps])
        return st, ps


def phase_mod(k, cc, w_mod, b_mod, modv, mT):
    P = k.P
    with P.scope():
        ccr = P.sbuf([16, 128], F32, "ccr")
        bcr = P.sbuf([48, 128], F32, "bcr")
        pst = P.psum([128, 64], F32, "mod_pst")
        P.dma("sp", ccr[:], cc.ap().rearrange("r (k p) -> (r k) p", p=128), writes=[ccr])
        P.dma("sp", bcr[:], b_mod.ap().rearrange("(c p) -> c p", p=128), writes=[bcr])
        P.op("pe", lambda e: e.transpose(pst[:, 0:16], ccr[:], k.ident_f[0:16, 0:16]), reads=[ccr, k.ident_f], writes=[pst])
        P.op("pe", lambda e: e.transpose(pst[:, 16:64], bcr[:], k.ident_f[0:48, 0:48]), reads=[bcr, k.ident_f], writes=[pst])
        sT = P.sbuf([128, 2, 8], F32, "sccT")
        bcol = P.sbuf([128, 48], F32, "bcol")
        P.op("act", lambda e: e.activation(sT[:].rearrange("p r k -> p (r k)"), pst[:, 0:16], AF.Silu), reads=[pst], writes=[sT])
        P.op("dve", lambda e: e.tensor_copy(bcol[:], pst[:, 16:64]), reads=[pst], writes=[bcol])
        wst = [P.sbuf([128, 8, 512], F32, "wmod_st%d" % i) for i in range(2)]
        ps = P.psum([128, 96], F32, "ps_mod")
        wv = w_mod.ap().rearrange("(k p) n -> p k n", p=128)
        for nb in range(12):
            w = wst[nb % 2]
            P.dma("sp", w[:], wv[:, :, nb * 512:(nb + 1) * 512], writes=[w])
            for cl in range(4):
                c = nb * 4 + cl
                for kk in range(8):
                    P.op("pe", lambda e, kk=kk, w=w, c=c, cl=cl: e.matmul(ps[:, 2 * c:2 * c + 2], w[:, kk, cl * 128:(cl + 1) * 128], sT[:, :, kk],
                                                                     start=(kk == 0), stop=(kk == 7)),
                         reads=[sT, w], writes=[ps])
        P.op("dve", lambda e: e.tensor_tensor(mT[:], ps[:].rearrange("p (c r) -> p c r", r=2), bcol[:].unsqueeze(2).to_broadcast([128, 48, 2]), ALU.add),
             reads=[ps, bcol], writes=[mT])
        for r in range(2):
            pr = P.psum([48, 128], F32, "mod_pr%d" % r)
            sr = P.sbuf([48, 128], F32, "mod_sr%d" % r)
            mr = P.sbuf([128, 48], F32, "mod_mr%d" % r)
            P.op("dve", lambda e, r=r, mr=mr: e.tensor_copy(mr[:], mT[:, :, r]), reads=[mT], writes=[mr])
            P.op("pe", lambda e, r=r, pr=pr, mr=mr: e.transpose(pr[:], mr[:], k.ident_f[:]), reads=[mr, k.ident_f], writes=[pr])
            P.op("act", lambda e, pr=pr, sr=sr: e.copy(sr[:], pr[:]), reads=[pr], writes=[sr])
            P.dma("sp", modv.ap()[r].rearrange("(c p) -> c p", p=128), sr[:], reads=[sr], writes=[modv])


def phase_norm(k, x, gain, mT, which, hT_sb, hT32_d=None, t_lo=0):
    P = k.P
    with P.scope():
        gr = P.sbuf([8, 128], F32, "gr%d" % which)
        gps = P.psum([128, 8], F32, "gps%d" % which)
        P.dma("sp", gr[:], gain.rearrange("(k p) -> k p", p=128), writes=[gr])
        P.op("pe", lambda e: e.transpose(gps[:], gr[:], k.ident_f[0:8, 0:8]), reads=[gr, k.ident_f], writes=[gps])
        G = P.sbuf([128, 2, 8], F32, "G%d" % which)
        gcol = P.sbuf([128, 8], F32, "gcol%d" % which)
        P.op("dve", lambda e: e.tensor_copy(gcol[:], gps[:]), reads=[gps], writes=[gcol])
        sh_i, sc_i = 3 * which, 3 * which + 1
        for r in range(2):
            P.op("dve", lambda e, r=r: e.scalar_tensor_tensor(G[:, r, :], mT[:, sc_i * 8:sc_i * 8 + 8, r], 1.0, gcol[:], ALU.add, ALU.mult),
                 reads=[mT, gcol], writes=[G])
        xt = [P.sbuf([128, 1024], F32, "nx%d_%d" % (which, i)) for i in range(3)]
        junk = P.sbuf([128, 1024], F32, "njunk%d" % which)
        xs = [P.sbuf([128, 1024], F32, "nxs%d_%d" % (which, i)) for i in range(5)]
        ss = [P.sbuf([128, 2], F32, "nss%d_%d" % (which, i)) for i in range(3)]
        pts = [P.psum([128, 512], F32, "npt%d_%d" % (which, i)) for i in range(2)]
        h32 = [P.sbuf([128, 512], F32, "nh32_%d_%d" % (which, i)) for i in range(2)] if hT32_d is not None else None
        groups = [[0, 1]] + [list(range(2 + 4 * g, 6 + 4 * g)) for g in range(8)]
        ti = 0
        ng = 0
        for grp in groups:
            if grp[0] < t_lo:
                continue
            r = 0 if grp[0] < 2 else 1
            tiles = []
            for t in grp:
                xb = xt[ti % 3]; sb = ss[ti % 3]; xsb = xs[ti % 5]
                ti += 1
                P.dma("sp", xb[:], x.ap()[t * 128:(t + 1) * 128, :], writes=[xb])
                P.op("act", lambda e, xb=xb, sb=sb: e.activation(junk[:], xb[:], AF.Square, accum_out=sb[:, 0:1]),
                     reads=[xb], writes=[junk, sb])
                P.op("act", lambda e, sb=sb: e.activation(sb[:, 1:2], sb[:, 0:1], AF.Sqrt, bias=k.eps_col[:], scale=1.0 / D),
                     reads=[sb, k.eps_col], writes=[sb])
                P.op("dve", lambda e, sb=sb: e.reciprocal(sb[:, 1:2], sb[:, 1:2]), reads=[sb], writes=[sb])
                P.op("dve", lambda e, xb=xb, sb=sb, xsb=xsb: e.tensor_scalar(xsb[:], xb[:], sb[:, 1:2], None, ALU.mult),
                     reads=[xb, sb], writes=[xsb])
                tiles.append(xsb)
            n = len(grp) * 128
            tok0 = grp[0] * 128
            for kk in range(8):
                pt = pts[ng % 2]
                for j, xsb in enumerate(tiles):
                    P.op("pe", lambda e, pt=pt, xsb=xsb, kk=kk, j=j: e.transpose(pt[:, j * 128:(j + 1) * 128], xsb[:, kk * 128:(kk + 1) * 128], k.ident_f[:]),
                         reads=[xsb, k.ident_f], writes=[pt])
                bias_ap = mT[:, sh_i * 8 + kk, r:r + 1]
                P.op("act", lambda e, pt=pt, kk=kk, r=r, tok0=tok0, n=n, bias_ap=bias_ap: e.activation(
                    hT_sb[:, kk, tok0:tok0 + n], pt[:, 0:n], AF.Identity, bias=bias_ap, scale=G[:, r, kk:kk + 1]),
                    reads=[pt, mT, G], writes=[hT_sb])
                if hT32_d is not None:
                    hb = h32[ng % 2]
                    P.op("dve", lambda e, pt=pt, kk=kk, r=r, n=n, hb=hb, bias_ap=bias_ap: e.tensor_scalar(
                        hb[:, 0:n], pt[:, 0:n], G[:, r, kk:kk + 1], bias_ap, ALU.mult, ALU.add),
                        reads=[pt, mT, G], writes=[hb])
                    P.dma("sp", hT32_d.ap()[kk * 128:(kk + 1) * 128, tok0:tok0 + n], hb[:, 0:n], reads=[hb], writes=[hT32_d])
                ng += 1


def phase_proj(k, hT_sb, w, z, N, tag="pj"):
    P = k.P
    with P.scope():
        _phase_proj(k, hT_sb, w, z, N, tag)


def _phase_proj(k, hT_sb, w, z, N, tag):
    P = k.P
    wst = [P.sbuf([128, 8, 512], F32, "%s_st%d" % (tag, i)) for i in range(2)]
    wb = [P.sbuf([128, 8, 512], BF16, "%s_wb%d" % (tag, i)) for i in range(2)]
    pss = [P.psum([128, 512], F32, "%s_ps%d" % (tag, i)) for i in range(3)]
    ob = [P.sbuf([128, 512], F32, "%s_ob%d" % (tag, i)) for i in range(4)]
    wv = w.rearrange("(k p) n -> p k n", p=128)
    nblk = (N + 511) // 512
    it = 0
    for nb in range(nblk):
        c0 = nb * 512
        cw = min(512, N - c0)
        st, wbb = wst[nb % 2], wb[nb % 2]
        load_w_bf16(k, wbb, wbb[:, :, 0:cw], wv[:, :, c0:c0 + cw], st, st[:, :, 0:cw])
        for t in range(NT):
            ps = pss[it % 3]; o = ob[it % 4]
            it += 1
            for kk in range(8):
                P.op("pe", lambda e, ps=ps, kk=kk, t=t, wbb=wbb, cw=cw: e.matmul(
                    ps[:, 0:cw], hT_sb[:, kk, t * 128:(t + 1) * 128], wbb[:, kk, 0:cw], start=(kk == 0), stop=(kk == 7)),
                    reads=[hT_sb, wbb], writes=[ps])
            if it % 2:
                P.op("act", lambda e, ps=ps, o=o, cw=cw: e.copy(o[:, 0:cw], ps[:, 0:cw]), reads=[ps], writes=[o])
            else:
                P.op("dve", lambda e, ps=ps, o=o, cw=cw: e.tensor_copy(o[:, 0:cw], ps[:, 0:cw]), reads=[ps], writes=[o])
            P.dma("sp", z.ap()[t * 128:(t + 1) * 128, c0:c0 + cw], o[:, 0:cw], reads=[o], writes=[z])


ZOFF = dict(ret_q=0, ret_k=256, ret_v=512, ret_g=1024, win_q=1536, win_k=2048, win_v=2176,
            na_q=2304, na_k=2816, na_v=3328, gdn_q=3840, gdn_k=4352, gdn_v=4864, gdn_g=5376, gdn_a=5888, gdn_b=5896)
NEG = -30000.0


def rope_tables():
    t = np.arange(4096)
    row = (t // 64).astype(np.float32); col = (t % 64).astype(np.float32)
    inv = (10000.0 ** (-np.arange(16, dtype=np.float32) / 16)).astype(np.float32)
    ang = np.concatenate([row[:, None] * inv, col[:, None] * inv], axis=-1).astype(np.float32)
    return np.cos(ang).astype(np.float32), np.sin(ang).astype(np.float32)


def na_classes():
    cls = [(10, 10 + dl) for dl in range(-2, 3)]
    for p in (0, 1):
        cls += [(p, b) for b in range(4)]
    for p in (30, 31):
        cls += [(p, b) for b in range(28, 32)]
    return cls


def na_plan(p):
    if 2 <= p <= 29:
        return [(p + dl, dl + 2) for dl in range(-2, 3)]
    e = {0: 0, 1: 1, 30: 2, 31: 3}[p]
    b0 = 0 if p < 2 else 28
    return [(b0 + b, 5 + e * 4 + b) for b in range(4)]


def na_bias_gather(rpb):
    cls = na_classes()
    out = np.full((8, 128, len(cls), 128), NEG, np.float32)
    kc = np.arange(64)[:, None]; qc = np.arange(64)[None, :]
    cst = np.clip(qc - 8, 0, 48)
    colok = (kc >= cst) & (kc < cst + 16)
    coff = np.clip(kc - qc + 15, 0, 30)
    for ci, (p, blk) in enumerate(cls):
        for a in range(2):
            krow = 2 * blk + a
            for b in range(2):
                qrow = 2 * p + b
                rs = min(max(qrow - 4, 0), 56)
                if not (rs <= krow < rs + 8):
                    continue
                ro = krow - qrow + 7
                vals = rpb[:, ro][:, coff]
                blkv = np.where(colok[None], vals, NEG)
                out[:, a * 64:(a + 1) * 64, ci, b * 64:(b + 1) * 64] = blkv
    return out


def attn_prep_qk(k, z, col, gain_bc, rope, outT, tag, cs=None):
    P = k.P
    with P.scope():
        x = P.sbuf([128, NT, 64], F32, tag + "x")
        zv = z.ap()[:, col:col + 64].rearrange("(t p) c -> p t c", p=128)
        P.dma("sp", x[:, 0:17, :], zv[:, 0:17, :], writes=[x])
        P.dma("sp", x[:, 17:34, :], zv[:, 17:34, :], writes=[x])
        sq = P.sbuf([128, NT, 64], F32, tag + "sq")
        P.op("pool", lambda e: e.tensor_tensor(sq[:], x[:], x[:], ALU.mult), reads=[x], writes=[sq])
        ss = P.sbuf([128, NT], F32, tag + "ss")
        P.op("dve", lambda e: e.tensor_reduce(ss[:], sq[:], AX.X, ALU.add), reads=[sq], writes=[ss])
        P.op("act", lambda e: e.activation(ss[:], ss[:], AF.Sqrt, bias=k.eps_col[:], scale=1.0 / 64), reads=[ss, k.eps_col], writes=[ss])
        P.op("dve", lambda e: e.reciprocal(ss[:], ss[:]), reads=[ss], writes=[ss])
        P.op("dve", lambda e: e.tensor_tensor(x[:], x[:], ss[:].unsqueeze(2).to_broadcast([128, NT, 64]), ALU.mult), reads=[x, ss], writes=[x])
        P.op("pool", lambda e: e.tensor_tensor(x[:], x[:], gain_bc[:].unsqueeze(1).to_broadcast([128, NT, 64]), ALU.mult), reads=[x, gain_bc], writes=[x])
        if rope:
            rope_apply(k, x, 1, tag)
        transpose_to(k, lambda t: x[:, t, :], 64, outT, tag, src_bufs=[x])


def rope_apply(k, x, H, tag):
    P = k.P
    with P.scope():
        _rope_apply(k, x, H, tag)


def _rope_apply(k, x, H, tag):
    P = k.P
    xl = x[:, 2:NT, :].rearrange("p t (h c) -> p t h c", c=64)
    x1 = xl[:, :, :, 0:32]; x2 = xl[:, :, :, 32:64]
    shp = [128, 32, H, 32]
    a = P.sbuf(shp, F32, tag + "ra"); b = P.sbuf(shp, F32, tag + "rb")
    cosb = k.cos[:].unsqueeze(2).to_broadcast(shp); sinb = k.sin[:].unsqueeze(2).to_broadcast(shp)
    P.op("dve", lambda e: e.tensor_tensor(a[:], x1, sinb, ALU.mult), reads=[x, k.sin], writes=[a])
    P.op("pool", lambda e: e.tensor_tensor(b[:], x2, sinb, ALU.mult), reads=[x, k.sin], writes=[b])
    P.op("dve", lambda e: e.tensor_tensor(x1, x1, cosb, ALU.mult), reads=[x, k.cos, a, b], writes=[x])
    P.op("dve", lambda e: e.tensor_tensor(x2, x2, cosb, ALU.mult), reads=[x, k.cos], writes=[x])
    P.op("dve", lambda e: e.tensor_tensor(x1, x1, b[:], ALU.subtract), reads=[x, b], writes=[x])
    P.op("dve", lambda e: e.tensor_tensor(x2, x2, a[:], ALU.add), reads=[x, a], writes=[x])


def transpose_to(k, src_fn, C, outT, tag, deps=None, t_list=None, src_bufs=None, out_off=0):
    P = k.P
    with P.scope():
        pss = [P.psum([128, 512], F32, tag + "tp%d" % i) for i in range(2)]
        tl = list(range(NT)) if t_list is None else t_list
        for g0 in range(0, len(tl), 4):
            grp = tl[g0:g0 + 4]
            ps = pss[(g0 // 4) % 2]
            for j, t in enumerate(grp):
                ap, bufs = src_fn(t), (src_bufs or [])
                P.op("pe", lambda e, ps=ps, j=j, ap=ap: e.transpose(ps[0:C, j * 128:(j + 1) * 128], ap, k.ident_f[:]),
                     reads=list(bufs) + [k.ident_f] + (deps or []), writes=[ps])
            n = len(grp) * 128
            o = outT[0:C, out_off + grp[0] * 128: out_off + grp[0] * 128 + n]
            if (g0 // 4) % 2:
                P.op("act", lambda e, ps=ps, o=o, n=n: e.copy(o, ps[0:C, 0:n]), reads=[ps], writes=[outT])
            else:
                P.op("dve", lambda e, ps=ps, o=o, n=n: e.tensor_copy(o, ps[0:C, 0:n]), reads=[ps], writes=[outT])


def attn_prep_v(k, z, col, Vaug, tag):
    P = k.P
    with P.scope():
        v = P.sbuf([128, NT, 64], F32, tag + "v")
        zv = z.ap()[:, col:col + 64].rearrange("(t p) c -> p t c", p=128)
        P.dma("sp", v[:, 0:17, :], zv[:, 0:17, :], writes=[v])
        P.dma("sp", v[:, 17:34, :], zv[:, 17:34, :], writes=[v])
        P.op("pool", lambda e: e.tensor_copy(Vaug[:, :, 0:64], v[:]), reads=[v], writes=[Vaug])
        P.op("pool", lambda e: e.memset(Vaug[:, :, 64:65], 1.0), writes=[Vaug])


def attn_core(k, qT, kT, Vaug, plan, sink_ap, sink_buf, yh, tag):
    P = k.P
    with P.scope():
        pss = [[P.psum([128, 512], F32, "%ss%d_%d" % (tag, i, j)) for j in range(2)] for i in range(2)]
        pos = [P.psum([128, 65], F32, "%so%d" % (tag, i)) for i in range(2)]
        pts = [P.sbuf([128, 8 * 128], BF16, "%spt%d" % (tag, i)) for i in range(2)]
        dens = [P.sbuf([128, 1], F32, "%sden%d" % (tag, i)) for i in range(2)]
        def st1(qi):
            qt, blocks = plan[qi]
            sset = pss[qi % 2]
            for bi, (kt, m, mb) in enumerate(blocks):
                ps = sset[bi // 4]
                P.op("pe", lambda e, ps=ps, bi=bi, kt=kt, qt=qt: e.matmul(ps[:, (bi % 4) * 128:(bi % 4 + 1) * 128], kT[:, kt * 128:(kt + 1) * 128],
                                                                      qT[:, qt * 128:(qt + 1) * 128], start=True, stop=True),
                     reads=[kT, qT], writes=[ps])

        def st2(qi):
            qt, blocks = plan[qi]
            sset = pss[qi % 2]; pt = pts[qi % 2]
            nb = len(blocks)
            for bk in range((nb + 3) // 4):
                n = min(4, nb - bk * 4) * 128
                P.op("act", lambda e, pt=pt, bk=bk, n=n, sset=sset: e.activation(pt[:, bk * 512:bk * 512 + n], sset[bk][:, 0:n], AF.Exp, scale=0.125),
                     reads=[sset[bk]], writes=[pt])
            for bi, (kt, m, mb) in enumerate(blocks):
                if m is not None:
                    eng = "pool" if bi % 2 else "dve"
                    P.op(eng, lambda e, pt=pt, bi=bi, m=m: e.tensor_tensor(pt[:, bi * 128:(bi + 1) * 128], pt[:, bi * 128:(bi + 1) * 128], m, ALU.mult),
                         reads=[pt, mb], writes=[pt])

        def st3(qi):
            qt, blocks = plan[qi]
            pt = pts[qi % 2]; po = pos[qi % 2]
            nb = len(blocks)
            for bi, (kt, m, mb) in enumerate(blocks):
                P.op("pe", lambda e, po=po, pt=pt, bi=bi, kt=kt, nb=nb: e.matmul(po[:], pt[:, bi * 128:(bi + 1) * 128], Vaug[:, kt, :],
                                                                             start=(bi == 0), stop=(bi == nb - 1)),
                     reads=[pt, Vaug], writes=[po])

        def st4(qi):
            qt, blocks = plan[qi]
            po = pos[qi % 2]; den = dens[qi % 2]
            if sink_ap is not None:
                P.op("dve", lambda e, den=den, po=po: e.tensor_scalar(den[:], po[:, 64:65], sink_ap, None, ALU.add), reads=[po, sink_buf], writes=[den])
                P.op("dve", lambda e, den=den: e.reciprocal(den[:], den[:]), reads=[den], writes=[den])
            else:
                P.op("dve", lambda e, den=den, po=po: e.reciprocal(den[:], po[:, 64:65]), reads=[po], writes=[den])
            P.op("act", lambda e, den=den, po=po, qt=qt: e.activation(yh[:, qt, :], po[:, 0:64], AF.Copy, scale=den[:, 0:1]), reads=[po, den], writes=[yh])

        nq = len(plan)
        for step in range(nq + 3):
            if step < nq:
                st1(step)
            if 0 <= step - 1 < nq:
                st2(step - 1)
            if 0 <= step - 2 < nq:
                st3(step - 2)
            if 0 <= step - 3 < nq:
                st4(step - 3)


def store_head(k, yh, ybr_ap, col, t_lo=0):
    P = k.P
    C = yh.t.shape[2]
    yv = ybr_ap[:, col:col + C].rearrange("(t p) c -> p t c", p=128)
    P.dma("sp", yv[:, t_lo:17, :], yh[:, t_lo:17, :], reads=[yh], writes=[k.ybr])
    P.dma("sp", yv[:, 17:34, :], yh[:, 17:34, :], reads=[yh], writes=[k.ybr])


def bc_load(k, vec_ap, n, name):
    P = k.P
    t = P.sbuf([128, n], F32, name)
    P.dma("sp", t[:], vec_ap.partition_broadcast(128), writes=[t])
    return t


def phase_window(k, z, qn, kn, sink, ctx_out):
    P = k.P
    with P.scope():
        qg = bc_load(k, qn, 64, "wqg"); kg = bc_load(k, kn, 64, "wkg")
        sk = bc_load(k, sink, 8, "wsk")
        P.op("act", lambda e: e.activation(sk[:], sk[:], AF.Exp), reads=[sk], writes=[sk])
        qT = P.sbuf([64, T], BF16, "wqT"); kT = P.sbuf([64, T], BF16, "wkT")
        Vaug = P.sbuf([128, NT, 65], BF16, "wV")
        yh = P.sbuf([128, NT, 64], F32, "wyh")
        plan = []
        if ctx_out:
            plan += [(qt, [(0, None, None), (1, None, None)]) for qt in range(2)]
        for n in range(32):
            bl = []
            if n > 0:
                bl.append((2 + n - 1, k.maskPrev[:], k.maskPrev))
            bl.append((2 + n, None, None))
            if n < 31:
                bl.append((2 + n + 1, k.maskNext[:], k.maskNext))
            bl += [(0, None, None), (1, None, None)]
            plan.append((2 + n, bl))
        for h in range(8):
            g = h // 4
            if h % 4 == 0:
                attn_prep_qk(k, z, ZOFF["win_k"] + g * 64, kg, True, kT, "wk")
                attn_prep_v(k, z, ZOFF["win_v"] + g * 64, Vaug, "wv")
            attn_prep_qk(k, z, ZOFF["win_q"] + h * 64, qg, True, qT, "wq")
            attn_core(k, qT, kT, Vaug, plan, sk[:, h:h + 1], sk, yh, "wa")
            store_head(k, yh, k.ybr.ap()[1], h * 64, 0 if ctx_out else 2)


def phase_na(k, z, qn, kn, nab, ctx_out):
    P = k.P
    with P.scope():
        qg = bc_load(k, qn, 64, "nqg"); kg = bc_load(k, kn, 64, "nkg")
        qT = P.sbuf([64, T], BF16, "nqT"); kT = P.sbuf([64, T], BF16, "nkT")
        Vaug = P.sbuf([128, NT, 65], BF16, "nV")
        yh = P.sbuf([128, NT, 64], F32, "nyh")
        bst = P.sbuf([128, 21, 128], F32, "nbst")
        eb = P.sbuf([128, 21, 128], BF16, "neb")
        for h in range(8):
            P.dma("sp", bst[:], nab[h], writes=[bst])
            P.op("act", lambda e: e.activation(eb[:], bst[:], AF.Exp), reads=[bst], writes=[eb])
            plan = []
            if ctx_out:
                plan += [(qt, [(0, None, None), (1, None, None)]) for qt in range(2)]
            for p in range(32):
                bl = [(2 + blk, eb[:, ci, :], eb) for blk, ci in na_plan(p)]
                bl += [(0, None, None), (1, None, None)]
                plan.append((2 + p, bl))
            attn_prep_qk(k, z, ZOFF["na_k"] + h * 64, kg, False, kT, "nk")
            attn_prep_v(k, z, ZOFF["na_v"] + h * 64, Vaug, "nv")
            attn_prep_qk(k, z, ZOFF["na_q"] + h * 64, qg, False, qT, "nq")
            attn_core(k, qT, kT, Vaug, plan, None, None, yh, "na")
            store_head(k, yh, k.ybr.ap()[2], h * 64, 0 if ctx_out else 2)


def ret_consts(c):
    j = np.arange(128)[:, None].astype(np.float32); i = np.arange(128)[None, :].astype(np.float32)
    c["dpos"] = np.stack([np.maximum(i - j, 0), np.maximum(j - i, 0)], 1).astype(np.float32)
    c["dmsk"] = np.stack([(i >= j), (j >= i)], 1).astype(np.float32)
    c["rowidx"] = np.stack([np.broadcast_to(i + 1, (128, 128)), np.broadcast_to(128 - i, (128, 128))], 1).astype(np.float32)
    c["colidx"] = np.concatenate([127 - j, j], 1).astype(np.float32)


def phase_ret(k, z, decay, gn, ctx_out):
    P = k.P
    with P.scope():
        dpos = P.sbuf([128, 2, 128], F32, "r_dpos"); dmsk = P.sbuf([128, 2, 128], F32, "r_dmsk")
        rowidx = P.sbuf([128, 2, 128], F32, "r_rowidx"); colidx = P.sbuf([128, 2], F32, "r_colidx")
        for t_, n_ in ((dpos, "dpos"), (dmsk, "dmsk"), (rowidx, "rowidx"), (colidx, "colidx")):
            P.dma("sp", t_[:], k.cd[n_].ap(), writes=[t_])
        lg = bc_load(k, decay, 8, "r_lg")
        P.op("act", lambda e: e.activation(lg[:], lg[:], AF.Exp, scale=-1.0), reads=[lg], writes=[lg])
        P.op("act", lambda e: e.activation(lg[:], lg[:], AF.Ln, bias=k.one_col[:]), reads=[lg, k.one_col], writes=[lg])
        P.op("dve", lambda e: e.tensor_scalar(lg[:], lg[:], -1.0, None, ALU.mult), reads=[lg], writes=[lg])
        dm = P.sbuf([128, 8, 128], F32, "r_dm"); qdT = P.sbuf([128, 8, 128], F32, "r_qdT")
        kdec = P.sbuf([128, 8], F32, "r_kdec"); cdec = P.sbuf([128, 8], F32, "r_cdec")
        for dr in range(2):
            for h in range(4):
                c = dr * 4 + h
                P.op("act", lambda e, dr=dr, c=c: e.activation(dm[:, c, :], dpos[:, dr, :], AF.Exp, scale=lg[:, c:c + 1]), reads=[dpos, lg], writes=[dm])
                P.op("dve", lambda e, dr=dr, c=c: e.tensor_tensor(dm[:, c, :], dm[:, c, :], dmsk[:, dr, :], ALU.mult), reads=[dm, dmsk], writes=[dm])
                P.op("act", lambda e, dr=dr, c=c: e.activation(qdT[:, c, :], rowidx[:, dr, :], AF.Exp, scale=lg[:, c:c + 1]), reads=[rowidx, lg], writes=[qdT])
                P.op("act", lambda e, dr=dr, c=c: e.activation(kdec[:, c:c + 1], colidx[:, dr:dr + 1], AF.Exp, scale=lg[:, c:c + 1]), reads=[colidx, lg], writes=[kdec])
        P.op("act", lambda e: e.activation(cdec[:], lg[:], AF.Exp, scale=128.0), reads=[lg], writes=[cdec])
        gnb = bc_load(k, gn, 512, "r_gn")
        q = P.sbuf([128, NT, 64], F32, "r_q"); kk_ = P.sbuf([128, NT, 64], F32, "r_k")
        v = P.sbuf([128, NT, 128], F32, "r_v"); gt = P.sbuf([128, NT, 128], F32, "r_g")
        qT = P.sbuf([64, T], F32, "r_qT"); kT = P.sbuf([64, T], F32, "r_kT")
        qTd = P.sbuf([64, T], F32, "r_qTd"); kd = P.sbuf([128, NT, 64], F32, "r_kd")
        of = P.sbuf([128, NT, 128], F32, "r_of"); ot = P.sbuf([128, NT, 128], F32, "r_ot")
        S = [P.sbuf([64, 128], F32, "r_S%d" % i) for i in range(2)]
        pss = [P.psum([128, 128], F32, "r_pss%d" % i) for i in range(2)]
        pso = [P.psum([128, 128], F32, "r_pso%d" % i) for i in range(2)]
        pkv = [P.psum([64, 128], F32, "r_pkv%d" % i) for i in range(2)]
        sm = [P.sbuf([128, 128], F32, "r_sm%d" % i) for i in range(2)]
        st = P.sbuf([128, NT, 4], F32, "r_st")

        def ld(dst, col, w):
            zv = z.ap()[:, col:col + w].rearrange("(t p) c -> p t c", p=128)
            P.dma("sp", dst[:, 0:17, :], zv[:, 0:17, :], writes=[dst])
            P.dma("sp", dst[:, 17:34, :], zv[:, 17:34, :], writes=[dst])

        for h in range(4):
            ld(q, ZOFF["ret_q"] + h * 64, 64); ld(kk_, ZOFF["ret_k"] + h * 64, 64)
            ld(v, ZOFF["ret_v"] + h * 128, 128); ld(gt, ZOFF["ret_g"] + h * 128, 128)
            rope_apply(k, q, 1, "r_rq"); rope_apply(k, kk_, 1, "r_rk")
            P.op("pool", lambda e: e.tensor_scalar(kk_[:], kk_[:], 0.125, None, ALU.mult), reads=[kk_], writes=[kk_])
            transpose_to(k, lambda t: q[:, t, :], 64, qT, "r_tq", src_bufs=[q])
            transpose_to(k, lambda t: kk_[:, t, :], 64, kT, "r_tk", src_bufs=[kk_])
            it = 0
            for dr in range(2):
                c = dr * 4 + h
                P.op("dve", lambda e, c=c: e.tensor_tensor(qTd[:].rearrange("p (t i) -> p t i", i=128), qT[:].rearrange("p (t i) -> p t i", i=128),
                                                           qdT[0:64, c, :].unsqueeze(1).to_broadcast([64, NT, 128]), ALU.mult), reads=[qT, qdT], writes=[qTd])
                P.op("pool", lambda e, c=c: e.tensor_scalar(kd[:], kk_[:], kdec[:, c:c + 1], None, ALU.mult), reads=[kk_, kdec], writes=[kd])
                order = list(range(NT)) if dr == 0 else [1, 0] + list(range(NT - 1, 1, -1))
                Scur = None

                def stage_a(idx, c=c, order=order, it0=it):
                    n = order[idx]
                    ps, smb = pss[(it0 + idx) % 2], sm[(it0 + idx) % 2]
                    sl = slice(n * 128, (n + 1) * 128)
                    P.op("pe", lambda e, ps=ps, sl=sl: e.matmul(ps[:], kT[:, sl], qT[:, sl], start=True, stop=True), reads=[kT, qT], writes=[ps])
                    P.op("dve", lambda e, ps=ps, smb=smb, c=c: e.tensor_tensor(smb[:], ps[:], dm[:, c, :], ALU.mult), reads=[ps, dm], writes=[smb])

                stage_a(0)
                for idx, n in enumerate(order):
                    if idx + 1 < len(order):
                        stage_a(idx + 1)
                    ps, po, pk, smb = pss[it % 2], pso[it % 2], pkv[it % 2], sm[it % 2]
                    Snew = S[it % 2]
                    it += 1
                    sl = slice(n * 128, (n + 1) * 128)
                    P.op("pe", lambda e, po=po, smb=smb, n=n, last=(Scur is None): e.matmul(po[:], smb[:], v[:, n, :], start=True, stop=last), reads=[smb, v], writes=[po])
                    if Scur is not None:
                        P.op("pe", lambda e, po=po, sl=sl, Scur=Scur: e.matmul(po[:], qTd[:, sl], Scur[:], start=False, stop=True), reads=[qTd, Scur], writes=[po])
                    P.op("pe", lambda e, pk=pk, n=n: e.matmul(pk[:], kd[:, n, :], v[:, n, :], start=True, stop=True), reads=[kd, v], writes=[pk])
                    if Scur is None:
                        P.op("act", lambda e, pk=pk, Snew=Snew: e.copy(Snew[:], pk[:]), reads=[pk], writes=[Snew])
                    else:
                        P.op("dve", lambda e, pk=pk, Snew=Snew, Scur=Scur, c=c: e.scalar_tensor_tensor(Snew[:], Scur[:], cdec[0:64, c:c + 1], pk[:], ALU.mult, ALU.add),
                             reads=[pk, Scur, cdec], writes=[Snew])
                    Scur = Snew
                    if dr == 0:
                        P.op("act", lambda e, po=po, n=n: e.copy(of[:, n, :], po[:]), reads=[po], writes=[of])
                    else:
                        P.op("dve", lambda e, po=po, n=n: e.tensor_tensor(ot[:, n, :], po[:], of[:, n, :], ALU.add), reads=[po, of], writes=[ot])
            s1 = st[:, :, 0]; s2 = st[:, :, 1]; mu = st[:, :, 2]; rs = st[:, :, 3]
            bshape = [128, NT, 128]
            P.op("dve", lambda e: e.tensor_reduce(s1, ot[:], AX.X, ALU.add), reads=[ot], writes=[st])
            P.op("pool", lambda e: e.tensor_tensor(of[:], ot[:], ot[:], ALU.mult), reads=[ot], writes=[of])
            P.op("dve", lambda e: e.tensor_reduce(s2, of[:], AX.X, ALU.add), reads=[of], writes=[st])
            P.op("dve", lambda e: e.tensor_scalar(mu, s1, 1.0 / 128, None, ALU.mult), reads=[st], writes=[st])
            P.op("dve", lambda e: e.tensor_tensor(s1, mu, mu, ALU.mult), reads=[st], writes=[st])
            P.op("dve", lambda e: e.scalar_tensor_tensor(rs, s2, 1.0 / 128, s1, ALU.mult, ALU.subtract), reads=[st], writes=[st])
            P.op("act", lambda e: e.activation(rs, rs, AF.Sqrt, bias=k.eps_col[:]), reads=[st, k.eps_col], writes=[st])
            P.op("dve", lambda e: e.reciprocal(rs, rs), reads=[st], writes=[st])
            P.op("dve", lambda e: e.tensor_tensor(ot[:], ot[:], mu.unsqueeze(2).to_broadcast(bshape), ALU.subtract), reads=[ot, st], writes=[ot])
            P.op("dve", lambda e: e.tensor_tensor(ot[:], ot[:], rs.unsqueeze(2).to_broadcast(bshape), ALU.mult), reads=[ot, st], writes=[ot])
            P.op("pool", lambda e, h=h: e.tensor_tensor(ot[:], ot[:], gnb[:, h * 128:(h + 1) * 128].unsqueeze(1).to_broadcast(bshape), ALU.mult), reads=[ot, gnb], writes=[ot])
            P.op("act", lambda e: e.activation(gt[:], gt[:], AF.Silu), reads=[gt], writes=[gt])
            P.op("dve", lambda e: e.tensor_tensor(ot[:], ot[:], gt[:], ALU.mult), reads=[ot, gt], writes=[ot])
            store_head(k, ot, k.ybr.ap()[0], h * 128, 0 if ctx_out else 2)


TGROUPS = [[0, 1]] + [list(range(2 + 4 * g, 6 + 4 * g)) for g in range(8)]


def load_big_w(k, dst, dview_fn, src_fn, nblk, st):
    P = k.P
    for i in range(nblk):
        s = st[i % len(st)]
        sap = src_fn(i)
        shp = sap.shape
        sv = s[:, 0:shp[1], 0:shp[2]]
        P.dma("sp", sv, sap, writes=[s])
        P.op("pool", lambda e, i=i, sv=sv: e.tensor_copy(dview_fn(i), sv), reads=[s], writes=[dst])


def phase_merge(k, hT_d, x, xmid, w_merge, w_branch, w_out, modv, ctx_out):
    P = k.P
    with P.scope():
        wm = P.sbuf([128, 8, 4096], BF16, "m_wm"); wb = P.sbuf([128, 4, 4, 1024], BF16, "m_wb"); wo = P.sbuf([128, 8, 1024], BF16, "m_wo")
        with P.scope():
            st = [P.sbuf([128, 8, 512], F32, "m_st%d" % i) for i in range(2)]
            wmv = w_merge.rearrange("(k p) n -> p k n", p=128)
            load_big_w(k, wm, lambda i: wm[:, :, i * 512:(i + 1) * 512], lambda i: wmv[:, :, i * 512:(i + 1) * 512], 8, st)
            wbv = w_branch.rearrange("b (k p) n -> p b k n", p=128)
            load_big_w(k, wb, lambda i: wb[:, i // 2, :, (i % 2) * 512:(i % 2 + 1) * 512], lambda i: wbv[:, i // 2, :, (i % 2) * 512:(i % 2 + 1) * 512], 8, st)
            wov = w_out.rearrange("(k p) n -> p k n", p=128)
            load_big_w(k, wo, lambda i: wo[:, :, i * 512:(i + 1) * 512], lambda i: wov[:, :, i * 512:(i + 1) * 512], 2, st)
        g1bc = P.sbuf([128, 2, 1024], F32, "m_g1")
        for r in range(2):
            P.dma("sp", g1bc[:, r, :], modv.ap()[r, 2048:3072].partition_broadcast(128), writes=[g1bc])
        hT = [P.sbuf([128, 8, 512], BF16, "m_hT%d" % i) for i in range(2)]
        yin = [P.sbuf([128, 512], F32, "m_yin%d" % i) for i in range(3)]
        yT = P.sbuf([128, 4, 4, 512], BF16, "m_yT")
        accT = P.sbuf([128, 8, 512], BF16, "m_accT")
        acc = P.sbuf([128, 512], F32, "m_acc"); tmp = P.sbuf([128, 512], F32, "m_tmp"); sg = [P.sbuf([128, 512], F32, "m_sg%d" % i) for i in range(2)]
        xt = [P.sbuf([128, 1024], F32, "m_xt%d" % i) for i in range(2)]
        xo = [P.sbuf([128, 1024], F32, "m_xo%d" % i) for i in range(2)]
        pg = [P.psum([128, 512], F32, "m_pg%d" % i) for i in range(2)]
        pb = [P.psum([128, 512], F32, "m_pb%d" % i) for i in range(2)]
        po = [P.psum([128, 1024], F32, "m_po%d" % i) for i in range(2)]
        yi = 0; it = 0; oi = 0
        for gi, grp in enumerate(TGROUPS):
            if not ctx_out and grp[0] < 2:
                continue
            r = 0 if grp[0] < 2 else 1
            n = len(grp) * 128; tok0 = grp[0] * 128
            hb = hT[gi % 2]
            P.dma("sp", hb[:, :, 0:n], hT_d.ap().rearrange("(k p) t -> p k t", p=128)[:, :, tok0:tok0 + n], writes=[hb])
            for i in range(4):
                for j, t in enumerate(grp):
                    yb = yin[yi % 3]; yi += 1
                    P.dma("sp", yb[:], k.ybr.ap()[i, t * 128:(t + 1) * 128, :], reads=[k.ybr], writes=[yb])
                    pt = po[yi % 2]
                    for kk in range(4):
                        P.op("pe", lambda e, pt=pt, yb=yb, kk=kk: e.transpose(pt[:, kk * 128:(kk + 1) * 128], yb[:, kk * 128:(kk + 1) * 128], k.ident_f[:]),
                             reads=[yb, k.ident_f], writes=[pt])
                    eng = "act" if yi % 2 else "dve"
                    if eng == "act":
                        P.op("act", lambda e, pt=pt, i=i, j=j: e.copy(yT[:, i, :, j * 128:(j + 1) * 128], pt[:, 0:512].rearrange("p (k t) -> p k t", t=128)), reads=[pt], writes=[yT])
                    else:
                        P.op("dve", lambda e, pt=pt, i=i, j=j: e.tensor_copy(yT[:, i, :, j * 128:(j + 1) * 128], pt[:, 0:512].rearrange("p (k t) -> p k t", t=128)), reads=[pt], writes=[yT])
            for fc in range(8):
                for i in range(4):
                    g_, b_, s_ = pg[it % 2], pb[it % 2], sg[it % 2]; it += 1
                    for kk in range(8):
                        P.op("pe", lambda e, g_=g_, kk=kk, i=i, fc=fc, hb=hb, n=n: e.matmul(g_[:, 0:n], wm[:, kk, i * 1024 + fc * 128:i * 1024 + (fc + 1) * 128], hb[:, kk, 0:n],
                                                                                         start=(kk == 0), stop=(kk == 7)), reads=[wm, hb], writes=[g_])
                    for kk in range(4):
                        P.op("pe", lambda e, b_=b_, kk=kk, i=i, fc=fc, n=n: e.matmul(b_[:, 0:n], wb[:, i, kk, fc * 128:(fc + 1) * 128], yT[:, i, kk, 0:n],
                                                                                 start=(kk == 0), stop=(kk == 3)), reads=[wb, yT], writes=[b_])
                    P.op("act", lambda e, g_=g_, s_=s_, n=n: e.activation(s_[:, 0:n], g_[:, 0:n], AF.Sigmoid), reads=[g_], writes=[s_])
                    if i == 0:
                        P.op("dve", lambda e, s_=s_, b_=b_, n=n: e.tensor_tensor(acc[:, 0:n], s_[:, 0:n], b_[:, 0:n], ALU.mult), reads=[s_, b_], writes=[acc])
                    else:
                        P.op("dve", lambda e, s_=s_, b_=b_, n=n: e.tensor_tensor(tmp[:, 0:n], s_[:, 0:n], b_[:, 0:n], ALU.mult), reads=[s_, b_], writes=[tmp])
                        if i < 3:
                            P.op("pool", lambda e, n=n: e.tensor_tensor(acc[:, 0:n], acc[:, 0:n], tmp[:, 0:n], ALU.add), reads=[acc, tmp], writes=[acc])
                        else:
                            P.op("pool", lambda e, n=n, fc=fc: e.tensor_tensor(accT[:, fc, 0:n], acc[:, 0:n], tmp[:, 0:n], ALU.add), reads=[acc, tmp], writes=[accT])
            for j, t in enumerate(grp):
                o_ = po[oi % 2]; xb = xt[oi % 2]; xob = xo[oi % 2]; oi += 1
                P.dma("sp", xb[:], x.ap()[t * 128:(t + 1) * 128, :], reads=[x], writes=[xb])
                for half in range(2):
                    for fc in range(8):
                        P.op("pe", lambda e, o_=o_, half=half, fc=fc, j=j: e.matmul(o_[:, half * 512:(half + 1) * 512], accT[:, fc, j * 128:(j + 1) * 128], wo[:, fc, half * 512:(half + 1) * 512],
                                                                                start=(fc == 0), stop=(fc == 7)), reads=[accT, wo], writes=[o_])
                P.op("dve", lambda e, o_=o_, xob=xob, r=r: e.tensor_tensor(xob[:], o_[:], g1bc[:, r, :], ALU.mult), reads=[o_, g1bc], writes=[xob])
                P.op("pool", lambda e, xob=xob, xb=xb: e.tensor_tensor(xob[:], xob[:], xb[:], ALU.add), reads=[xob, xb], writes=[xob])
                P.dma("sp", xmid.ap()[t * 128:(t + 1) * 128, :], xob[:], reads=[xob], writes=[xmid])


def phase_router(k, h32_d, w_router, rbias, wgt, t_lo):
    P = k.P
    with P.scope():
        wr = P.sbuf([128, 8, 32], F32, "rt_w")
        P.dma("sp", wr[:], w_router.rearrange("(k p) n -> p k n", p=128), writes=[wr])
        rb = bc_load(k, rbias, 32, "rt_b")
        hs = [P.sbuf([128, 8, 128], F32, "rt_h%d" % i) for i in range(2)]
        ps = [P.psum([128, 32], F32, "rt_ps%d" % i) for i in range(2)]
        sc = P.sbuf([128, 32], F32, "rt_sc"); sel = P.sbuf([128, 32], F32, "rt_sel")
        w8 = P.sbuf([128, 10, 8], F32, "rt_w8"); msk = P.sbuf([128, 32], F32, "rt_msk"); c1 = P.sbuf([128, 2], F32, "rt_c1")
        hv = h32_d.ap().rearrange("(k p) t -> p k t", p=128)
        for t in range(t_lo, NT):
            hb = hs[t % 2]; p_ = ps[t % 2]
            P.dma("sp", hb[:], hv[:, :, t * 128:(t + 1) * 128], reads=[h32_d], writes=[hb])
            for kk in range(8):
                P.op("pe", lambda e, p_=p_, hb=hb, kk=kk: e.matmul(p_[:], hb[:, kk, :], wr[:, kk, :], start=(kk == 0), stop=(kk == 7)), reads=[hb, wr], writes=[p_])
            P.op("act", lambda e, p_=p_: e.activation(sc[:], p_[:], AF.Sigmoid), reads=[p_], writes=[sc])
            P.op("dve", lambda e: e.tensor_tensor(sel[:], sc[:], rb[:], ALU.add), reads=[sc, rb], writes=[sel])
            s4 = sel[:].rearrange("p (g e) -> p g e", e=4)
            a, b, c, d_ = s4[:, :, 0], s4[:, :, 1], s4[:, :, 2], s4[:, :, 3]
            W = lambda i: w8[:, i, :]
            seq = [(W(0), a, b, ALU.max), (W(1), a, b, ALU.min), (W(2), c, d_, ALU.max), (W(3), c, d_, ALU.min),
                   (W(4), W(0), W(2), ALU.max), (W(5), W(0), W(2), ALU.min), (W(6), W(1), W(3), ALU.max),
                   (W(7), W(5), W(6), ALU.max), (W(8), W(4), W(7), ALU.add)]
            for o, i0, i1, op in seq:
                P.op("dve", lambda e, o=o, i0=i0, i1=i1, op=op: e.tensor_tensor(o, i0, i1, op), reads=[sel, w8], writes=[w8])
            P.op("dve", lambda e: e.tensor_reduce(c1[:, 0:1], W(8), AX.X, ALU.max), reads=[w8], writes=[c1])
            P.op("dve", lambda e: e.tensor_scalar(W(9), W(8), c1[:, 0:1], None, ALU.is_ge), reads=[w8, c1], writes=[w8])
            m4 = msk[:].rearrange("p (g e) -> p g e", e=4)
            P.op("dve", lambda e: e.tensor_tensor(m4, s4, W(7).unsqueeze(2).to_broadcast([128, 8, 4]), ALU.is_ge), reads=[sel, w8], writes=[msk])
            P.op("dve", lambda e: e.tensor_tensor(m4, m4, W(9).unsqueeze(2).to_broadcast([128, 8, 4]), ALU.mult), reads=[msk, w8], writes=[msk])
            P.op("dve", lambda e: e.tensor_tensor(msk[:], msk[:], sc[:], ALU.mult), reads=[msk, sc], writes=[msk])
            P.op("dve", lambda e: e.tensor_reduce(c1[:, 1:2], msk[:], AX.X, ALU.add), reads=[msk], writes=[c1])
            P.op("dve", lambda e: e.reciprocal(c1[:, 1:2], c1[:, 1:2]), reads=[c1], writes=[c1])
            P.op("dve", lambda e, t=t: e.tensor_scalar(wgt[:, t, :], msk[:], c1[:, 1:2], None, ALU.mult), reads=[msk, c1], writes=[wgt])


def phase_moe(k, h2T_d, wgt, xmid, xout, weg, weu, wed, modv, t_lo, out_lat=None, nexp=32):
    P = k.P
    parts = [list(range(0, 12)), list(range(12, 23)), list(range(23, 34))]
    with P.scope():
        g2bc = P.sbuf([128, 2, 1024], F32, "e_g2")
        for r in range(2):
            P.dma("sp", g2bc[:, r, :], modv.ap()[r, 5120:6144].partition_broadcast(128), writes=[g2bc])
        acc = P.sbuf([128, 12, 1024], F32, "e_acc")
        hT = P.sbuf([128, 8, 12 * 128], BF16, "e_hT")
        stg = [P.sbuf([128, 8, 512], F32, "e_stg%d" % i) for i in range(2)]
        wg = [P.sbuf([128, 8, 512], BF16, "e_wg%d" % i) for i in range(2)]
        wu = [P.sbuf([128, 8, 512], BF16, "e_wu%d" % i) for i in range(2)]
        wd = [P.sbuf([128, 4, 1024], BF16, "e_wd%d" % i) for i in range(2)]
        hid = [P.sbuf([128, 4, 512], BF16, "e_hid%d" % i) for i in range(2)]
        sl = [P.sbuf([128, 512], F32, "e_sl%d" % i) for i in range(2)]
        pg = [P.psum([128, 512], F32, "e_pg%d" % i) for i in range(2)]
        pu = [P.psum([128, 512], F32, "e_pu%d" % i) for i in range(2)]
        po = [P.psum([128, 1024], F32, "e_po%d" % i) for i in range(2)]
        xt = [P.sbuf([128, 1024], F32, "e_xt%d" % i) for i in range(2)]
        hv = h2T_d.ap().rearrange("(k p) t -> p k t", p=128)
        si = 0; ci = 0; oi = 0; gi = 0
        for part in parts:
            tiles = [t for t in part if t >= t_lo]
            if not tiles:
                continue
            nt = len(tiles); tok0 = tiles[0] * 128; ntok = nt * 128
            P.dma("sp", hT[:, :, 0:ntok], hv[:, :, tok0:tok0 + ntok], reads=[h2T_d], writes=[hT])
            for e_ in range(nexp):
                wgb, wub, wdb = wg[e_ % 2], wu[e_ % 2], wd[e_ % 2]
                for dst, src in ((wgb, weg[e_].rearrange("(k p) n -> p k n", p=128)), (wub, weu[e_].rearrange("(k p) n -> p k n", p=128))):
                    s = stg[si % 2]; si += 1
                    P.dma("sp", s[:], src, writes=[s])
                    P.op("pool", lambda e, dst=dst, s=s: e.tensor_copy(dst[:], s[:]), reads=[s], writes=[dst])
                s = stg[si % 2]; si += 1
                sv = s[:].rearrange("p k n -> p (k n)").rearrange("p (k n) -> p k n", n=1024)
                P.dma("sp", sv, wed[e_].rearrange("(k p) n -> p k n", p=128), writes=[s])
                P.op("pool", lambda e, wdb=wdb, sv=sv: e.tensor_copy(wdb[:], sv), reads=[s], writes=[wdb])
                for g0 in range(0, nt, 4):
                    gt_ = tiles[g0:g0 + 4]; n = len(gt_) * 128; off = g0 * 128
                    hb = hid[gi % 2]; gi += 1
                    for c in range(4):
                        g_, u_, s_ = pg[ci % 2], pu[ci % 2], sl[ci % 2]; ci += 1
                        for kk in range(8):
                            P.op("pe", lambda e, g_=g_, kk=kk, c=c, wgb=wgb, off=off, n=n: e.matmul(g_[:, 0:n], wgb[:, kk, c * 128:(c + 1) * 128], hT[:, kk, off:off + n],
                                                                                                 start=(kk == 0), stop=(kk == 7)), reads=[wgb, hT], writes=[g_])
                        for kk in range(8):
                            P.op("pe", lambda e, u_=u_, kk=kk, c=c, wub=wub, off=off, n=n: e.matmul(u_[:, 0:n], wub[:, kk, c * 128:(c + 1) * 128], hT[:, kk, off:off + n],
                                                                                                 start=(kk == 0), stop=(kk == 7)), reads=[wub, hT], writes=[u_])
                        P.op("act", lambda e, g_=g_, s_=s_, n=n: e.activation(s_[:, 0:n], g_[:, 0:n], AF.Silu), reads=[g_], writes=[s_])
                        P.op("dve", lambda e, s_=s_, u_=u_, hb=hb, c=c, n=n: e.tensor_tensor(hb[:, c, 0:n], s_[:, 0:n], u_[:, 0:n], ALU.mult), reads=[s_, u_], writes=[hb])
                    for j, t in enumerate(gt_):
                        o_ = po[oi % 2]; oi += 1
                        for half in range(2):
                            for c in range(4):
                                P.op("pe", lambda e, o_=o_, half=half, c=c, j=j, hb=hb, wdb=wdb: e.matmul(o_[:, half * 512:(half + 1) * 512], hb[:, c, j * 128:(j + 1) * 128],
                                                                                                       wdb[:, c, half * 512:(half + 1) * 512], start=(c == 0), stop=(c == 3)),
                                     reads=[hb, wdb], writes=[o_])
                        ai = g0 + j
                        if e_ == 0:
                            P.op("dve", lambda e, o_=o_, ai=ai, t=t: e.tensor_scalar(acc[:, ai, :], o_[:], wgt[:, t, 0:1], None, ALU.mult), reads=[o_, wgt], writes=[acc])
                        else:
                            P.op("dve", lambda e, o_=o_, ai=ai, t=t, e_=e_: e.scalar_tensor_tensor(acc[:, ai, :], o_[:], wgt[:, t, e_:e_ + 1], acc[:, ai, :], ALU.mult, ALU.add),
                                 reads=[o_, wgt, acc], writes=[acc])
            for ai, t in enumerate(tiles):
                r = 0 if t < 2 else 1
                xb = xt[ai % 2]
                P.dma("sp", xb[:], xmid.ap()[t * 128:(t + 1) * 128, :], reads=[xmid], writes=[xb])
                P.op("pool", lambda e, ai=ai, r=r: e.tensor_tensor(acc[:, ai, :], acc[:, ai, :], g2bc[:, r, :], ALU.mult), reads=[acc, g2bc], writes=[acc])
                P.op("pool", lambda e, ai=ai, xb=xb: e.tensor_tensor(xb[:], xb[:], acc[:, ai, :], ALU.add), reads=[acc, xb], writes=[xb])
                if out_lat is not None:
                    if t >= 2:
                        P.dma("sp", out_lat.ap()[(t - 2) * 128:(t - 1) * 128, :], xb[:], reads=[xb], writes=[out_lat])
                else:
                    P.dma("sp", xout.ap()[t * 128:(t + 1) * 128, :], xb[:], reads=[xb], writes=[xout])


def gdn_consts(c):
    t = np.arange(128)[:, None]; i = np.arange(128)[None, :]
    sb = (t // 64) == (i // 64)
    c["g_tri"] = np.stack([(t <= i) & sb, (t >= i) & sb], 1).astype(np.float32)
    c["g_triS"] = np.stack([(t > i) & sb, (t < i) & sb], 1).astype(np.float32)
    c["g_mS"] = np.stack([(i > t) & sb, (i < t) & sb], 1).astype(np.float32)
    c["g_blk"] = np.concatenate([sb.astype(np.float32)[:, None, :],
                                 np.broadcast_to((t < 64), (128, 128)).astype(np.float32)[:, None, :],
                                 np.broadcast_to((t >= 64), (128, 128)).astype(np.float32)[:, None, :]], 1)
    c["zeros"] = np.zeros((1, 128), np.float32)


def phase_gdn(k, z, conv_w, a_log, dt_bias, gnorm, ctx_out):
    P = k.P
    with P.scope():
        tri = P.sbuf([128, 2, 128], F32, "g_tri"); triS = P.sbuf([128, 2, 128], F32, "g_triS")
        mS = P.sbuf([128, 2, 128], F32, "g_mS"); blk = P.sbuf([128, 3, 128], F32, "g_blk")
        for t_, n_ in ((tri, "g_tri"), (triS, "g_triS"), (mS, "g_mS"), (blk, "g_blk")):
            P.dma("sp", t_[:], k.cd[n_].ap(), writes=[t_])
        gnb = bc_load(k, gnorm, 128, "g_gn")
        gall = P.sbuf([128, NT, 8], F32, "g_gall"); ball = P.sbuf([128, NT, 8], F32, "g_ball")
        with P.scope():
            ab = P.sbuf([128, NT, 16], F32, "g_ab")
            zv = z.ap()[:, 5888:5904].rearrange("(t p) c -> p t c", p=128)
            P.dma("sp", ab[:, 0:17, :], zv[:, 0:17, :], writes=[ab]); P.dma("sp", ab[:, 17:34, :], zv[:, 17:34, :], writes=[ab])
            al = bc_load(k, a_log, 8, "g_al"); db = bc_load(k, dt_bias, 8, "g_db")
            P.op("act", lambda e: e.activation(al[:], al[:], AF.Exp), reads=[al], writes=[al])
            P.op("dve", lambda e: e.tensor_scalar(al[:], al[:], -1.0, None, ALU.mult), reads=[al], writes=[al])
            P.op("dve", lambda e: e.tensor_tensor(gall[:], ab[:, :, 0:8], db[:].unsqueeze(1).to_broadcast([128, NT, 8]), ALU.add), reads=[ab, db], writes=[gall])
            P.op("act", lambda e: e.activation(gall[:], gall[:], AF.Exp), reads=[gall], writes=[gall])
            P.op("act", lambda e: e.activation(gall[:], gall[:], AF.Ln, bias=k.one_col[:]), reads=[gall, k.one_col], writes=[gall])
            P.op("dve", lambda e: e.tensor_tensor(gall[:], gall[:], al[:].unsqueeze(1).to_broadcast([128, NT, 8]), ALU.mult), reads=[gall, al], writes=[gall])
            P.op("act", lambda e: e.activation(ball[:], ab[:, :, 8:16], AF.Sigmoid), reads=[ab], writes=[ball])
        q = P.sbuf([128, NT, 128], F32, "g_q"); kk_ = P.sbuf([128, NT, 128], F32, "g_k"); v = P.sbuf([128, NT, 128], F32, "g_v")
        kT = P.sbuf([128, T], F32, "g_kT"); qT = P.sbuf([128, T], F32, "g_qT")
        od = [P.sbuf([128, NT, 128], F32, "g_od%d" % i) for i in range(2)]
        zview = z.ap().rearrange("(t p) c -> p t c", p=128)
        for h in range(4):
            with P.scope():
                xp = P.sbuf([128, NT, 128], F32, "g_xp"); xn = P.sbuf([128, NT, 128], F32, "g_xn"); wcb = P.sbuf([128, 3, 128], F32, "g_wc")
                ss = P.sbuf([128, NT], F32, "g_ss")
                for name, dst in (("gdn_q", q), ("gdn_k", kk_), ("gdn_v", v)):
                    col = ZOFF[name] + h * 128
                    zc = zview[:, :, col:col + 128]
                    for lo, hi in ((0, 17), (17, 34)):
                        P.dma("sp", dst[:, lo:hi, :], zc[:, lo:hi, :], writes=[dst])
                        P.dma("sp", xp[1:128, lo:hi, :], zc[0:127, lo:hi, :], writes=[xp])
                        P.dma("sp", xn[0:127, lo:hi, :], zc[1:128, lo:hi, :], writes=[xn])
                    P.dma("sp", xp[0:1, 1:34, :], zc[127:128, 0:33, :], writes=[xp])
                    P.dma("sp", xn[127:128, 0:33, :], zc[0:1, 1:34, :], writes=[xn])
                    for tt in (0, 2):
                        P.dma("sp", xp[0:1, tt, :], k.cd["zeros"].ap(), writes=[xp])
                    for tt in (1, 33):
                        P.dma("sp", xn[127:128, tt, :], k.cd["zeros"].ap(), writes=[xn])
                    for j in range(3):
                        P.dma("sp", wcb[:, j, :], conv_w[j, col - ZOFF["gdn_q"]:col - ZOFF["gdn_q"] + 128].partition_broadcast(128), writes=[wcb])
                    bs = [128, NT, 128]
                    P.op("dve", lambda e, dst=dst: e.tensor_tensor(dst[:], dst[:], wcb[:, 1, :].unsqueeze(1).to_broadcast(bs), ALU.mult), reads=[dst, wcb], writes=[dst])
                    P.op("pool", lambda e: e.tensor_tensor(xp[:], xp[:], wcb[:, 0, :].unsqueeze(1).to_broadcast(bs), ALU.mult), reads=[xp, wcb], writes=[xp])
                    P.op("dve", lambda e: e.tensor_tensor(xn[:], xn[:], wcb[:, 2, :].unsqueeze(1).to_broadcast(bs), ALU.mult), reads=[xn, wcb], writes=[xn])
                    P.op("pool", lambda e, dst=dst: e.tensor_tensor(dst[:], dst[:], xp[:], ALU.add), reads=[dst, xp], writes=[dst])
                    P.op("dve", lambda e, dst=dst: e.tensor_tensor(dst[:], dst[:], xn[:], ALU.add), reads=[dst, xn], writes=[dst])
                    P.op("act", lambda e, dst=dst: e.activation(dst[:], dst[:], AF.Silu), reads=[dst], writes=[dst])
                    if name != "gdn_v":
                        P.op("pool", lambda e, dst=dst: e.tensor_tensor(xp[:], dst[:], dst[:], ALU.mult), reads=[dst], writes=[xp])
                        P.op("dve", lambda e: e.tensor_reduce(ss[:], xp[:], AX.X, ALU.add), reads=[xp], writes=[ss])
                        P.op("act", lambda e: e.activation(ss[:], ss[:], AF.Sqrt, bias=k.eps_col[:]), reads=[ss, k.eps_col], writes=[ss])
                        P.op("dve", lambda e: e.reciprocal(ss[:], ss[:]), reads=[ss], writes=[ss])
                        if name == "gdn_q":
                            P.op("dve", lambda e: e.tensor_scalar(ss[:], ss[:], 128.0 ** -0.5, None, ALU.mult), reads=[ss], writes=[ss])
                        P.op("dve", lambda e, dst=dst: e.tensor_tensor(dst[:], dst[:], ss[:].unsqueeze(2).to_broadcast(bs), ALU.mult), reads=[dst, ss], writes=[dst])
            transpose_to(k, lambda t: q[:, t, :], 128, qT, "g_tq", src_bufs=[q])
            transpose_to(k, lambda t: kk_[:, t, :], 128, kT, "g_tk", src_bufs=[kk_])
            with P.scope():
                psO = [P.psum([128, 512], F32, "g_psO%d" % i) for i in range(2)]
                psW = [P.psum([128, 512], F32, "g_psW%d" % i) for i in range(2)]
                psM = [P.psum([128, 512], F32, "g_psM%d" % i) for i in range(4)]
                dirs = [gdn_dir(k, dr, h, q, kk_, v, qT, kT, gall, ball, tri, triS, mS, blk, od[dr], psO[dr], psW[dr], psM[2 * dr:2 * dr + 2]) for dr in range(2)]

                def run_all(gens):
                    alive = list(gens)
                    while alive:
                        for g_ in list(alive):
                            try:
                                next(g_)
                            except StopIteration:
                                alive.remove(g_)

                run_all([dirs[dr][0](0, dirs[dr][2][0]) for dr in range(2)])
                for i_ in range(NT):
                    gens = []
                    for dr in range(2):
                        prep_, scan_, order_ = dirs[dr]
                        gens.append(scan_(i_, order_[i_]))
                        if i_ + 1 < NT:
                            gens.append(prep_(i_ + 1, order_[i_ + 1]))
                    run_all(gens)
            with P.scope():
                gt = P.sbuf([128, NT, 128], F32, "g_gate"); ss = P.sbuf([128, NT], F32, "g_fss"); sq = P.sbuf([128, NT, 128], F32, "g_fsq")
                zc = zview[:, :, ZOFF["gdn_g"] + h * 128:ZOFF["gdn_g"] + (h + 1) * 128]
                P.dma("sp", gt[:, 0:17, :], zc[:, 0:17, :], writes=[gt]); P.dma("sp", gt[:, 17:34, :], zc[:, 17:34, :], writes=[gt])
                o = od[0]; bs = [128, NT, 128]
                P.op("dve", lambda e: e.tensor_tensor(o[:], o[:], od[1][:], ALU.add), reads=[o, od[1]], writes=[o])
                P.op("pool", lambda e: e.tensor_tensor(sq[:], o[:], o[:], ALU.mult), reads=[o], writes=[sq])
                P.op("dve", lambda e: e.tensor_reduce(ss[:], sq[:], AX.X, ALU.add), reads=[sq], writes=[ss])
                P.op("act", lambda e: e.activation(ss[:], ss[:], AF.Sqrt, bias=k.eps_col[:], scale=1.0 / 128), reads=[ss, k.eps_col], writes=[ss])
                P.op("dve", lambda e: e.reciprocal(ss[:], ss[:]), reads=[ss], writes=[ss])
                P.op("dve", lambda e: e.tensor_tensor(o[:], o[:], ss[:].unsqueeze(2).to_broadcast(bs), ALU.mult), reads=[o, ss], writes=[o])
                P.op("pool", lambda e: e.tensor_tensor(o[:], o[:], gnb[:].unsqueeze(1).to_broadcast(bs), ALU.mult), reads=[o, gnb], writes=[o])
                P.op("act", lambda e: e.activation(gt[:], gt[:], AF.Silu), reads=[gt], writes=[gt])
                P.op("dve", lambda e: e.tensor_tensor(o[:], o[:], gt[:], ALU.mult), reads=[o, gt], writes=[o])
                store_head(k, o, k.ybr.ap()[3], h * 128, 0 if ctx_out else 2)


def gdn_dir(k, dr, h, q, kk_, v, qT, kT, gall, ball, tri, triS, mS, blk, od, psO, psW, psM):
    P = k.P
    c = dr * 4 + h
    tg = "g%d_" % dr
    A = lambda nm, shape=(128, 128), n=2: [P.sbuf(list(shape), F32, tg + nm + str(i)) for i in range(n)]
    gcs = P.sbuf([128, 4, NT], F32, tg + "gcs")
    cdb = P.sbuf([128, 2, NT], F32, tg + "cdb")
    gc = gall[:, :, c]; bt = ball[:, :, c]
    ident = k.ident_f
    pm = psM[0]
    P.op("pe", lambda e: e.matmul(pm[:, 0:NT], tri[:, dr, :], gc, start=True, stop=True), reads=[tri, gall], writes=[pm])
    P.op("pe", lambda e: e.matmul(pm[:, 64:64 + NT], blk[:, 0, :], gc, start=True, stop=True), reads=[blk, gall], writes=[pm])
    P.op("pe", lambda e: e.matmul(pm[:, 128:128 + NT], blk[:, 1, :], gc, start=True, stop=True), reads=[blk, gall], writes=[pm])
    P.op("pe", lambda e: e.matmul(pm[:, 192:192 + NT], blk[:, 2, :], gc, start=True, stop=True), reads=[blk, gall], writes=[pm])
    P.op("dve", lambda e: e.tensor_copy(gcs[:, 0, :], pm[:, 0:NT]), reads=[pm], writes=[gcs])
    P.op("act", lambda e: e.activation(gcs[:, 1, :], pm[:, 0:NT], AF.Exp), reads=[pm], writes=[gcs])
    P.op("dve", lambda e: e.tensor_tensor(gcs[:, 2, :], pm[:, 64:64 + NT], gcs[:, 0, :], ALU.subtract), reads=[pm, gcs], writes=[gcs])
    P.op("act", lambda e: e.activation(gcs[:, 2, :], gcs[:, 2, :], AF.Exp), reads=[gcs], writes=[gcs])
    P.op("act", lambda e: e.activation(cdb[:, 0, :], pm[:, 128:128 + NT], AF.Exp), reads=[pm], writes=[cdb])
    P.op("act", lambda e: e.activation(cdb[:, 1, :], pm[:, 192:192 + NT], AF.Exp), reads=[pm], writes=[cdb])
    kb, kbg, vb, kend, qd, gtri = A("kb"), A("kbg"), A("vb"), A("kend"), A("qd"), A("gtri")
    EDs, EDi, kbT, AT, aqkT = A("EDs"), A("EDi"), A("kbT"), A("AT"), A("aqkT")
    Ap, Bp, Pm = A("Ap", n=3), A("Bp", n=3), A("Pm", n=3)
    u, wT, vnew = A("u"), A("wT"), A("vnew")
    qdTm = [P.sbuf([128, 2, 128], F32, tg + "qdTm%d" % i) for i in range(2)]
    for b_ in qdTm:
        P.op("pool", lambda e, b_=b_: e.memset(b_[:], 0.0), writes=[b_])
    S = [P.sbuf([128, 128], F32, tg + "S%d" % i) for i in range(2)]
    P.op("pool", lambda e: e.memset(S[0][:], 0.0), writes=[S[0]])
    stt = {"S": S[0], "si": 0}
    order = list(range(NT)) if dr == 0 else [1, 0] + list(range(NT - 1, 1, -1))
    slot = [0]

    def ms():
        s_ = slot[0]; slot[0] += 1
        bank = psM[(s_ // 4) % 2]; o_ = (s_ % 4) * 128
        return bank, bank[:, o_:o_ + 128]

    def prep(it, n):
        r2 = it % 2
        sl = slice(n * 128, (n + 1) * 128)
        kt, qt, vt = kk_[:, n, :], q[:, n, :], v[:, n, :]
        bcol = ball[:, n, c:c + 1]; gcol = gall[:, n, c:c + 1]
        eg = gcs[:, 1, n:n + 1]; ek = gcs[:, 2, n:n + 1]
        kb_, kbg_, vb_, kend_, qd_, gtri_ = kb[r2], kbg[r2], vb[r2], kend[r2], qd[r2], gtri[r2]
        P.op("dve", lambda e: e.tensor_scalar(kb_[:], kt, bcol, None, ALU.mult), reads=[kk_, ball], writes=[kb_]); yield
        P.op("pool", lambda e: e.tensor_scalar(kbg_[:], kb_[:], eg, None, ALU.mult), reads=[kb_, gcs], writes=[kbg_]); yield
        P.op("pool", lambda e: e.tensor_scalar(vb_[:], vt, bcol, None, ALU.mult), reads=[v, ball], writes=[vb_]); yield
        P.op("pool", lambda e: e.tensor_scalar(kend_[:], kt, ek, None, ALU.mult), reads=[kk_, gcs], writes=[kend_]); yield
        P.op("dve", lambda e: e.tensor_scalar(qd_[:], qt, eg, None, ALU.mult), reads=[q, gcs], writes=[qd_]); yield
        P.op("dve", lambda e: e.tensor_scalar(gtri_[:], tri[:, dr, :], gcol, None, ALU.mult), reads=[tri, gall], writes=[gtri_]); yield
        bk, p_ = ms()
        P.op("pe", lambda e: e.matmul(p_, triS[:, dr, :], gtri_[:], start=True, stop=True), reads=[triS, gtri_], writes=[bk]); yield
        EDs_, EDi_ = EDs[r2], EDi[r2]
        P.op("act", lambda e: e.activation(EDs_[:], p_, AF.Exp), reads=[bk], writes=[EDs_]); yield
        P.op("pool", lambda e: e.tensor_tensor(EDs_[:], EDs_[:], mS[:, dr, :], ALU.mult), reads=[EDs_, mS], writes=[EDs_]); yield
        P.op("pool", lambda e: e.tensor_tensor(EDi_[:], EDs_[:], ident[:], ALU.add), reads=[EDs_, ident], writes=[EDi_]); yield
        bk, p_ = ms(); kbT_ = kbT[r2]
        P.op("pe", lambda e: e.transpose(p_, kb_[:], ident[:]), reads=[kb_, ident], writes=[bk]); yield
        P.op("act", lambda e: e.copy(kbT_[:], p_), reads=[bk], writes=[kbT_]); yield
        bk, p_ = ms(); AT_ = AT[r2]
        P.op("pe", lambda e: e.matmul(p_, kT[:, sl], kbT_[:], start=True, stop=True), reads=[kT, kbT_], writes=[bk]); yield
        P.op("dve", lambda e: e.tensor_tensor(AT_[:], p_, EDs_[:], ALU.mult), reads=[bk, EDs_], writes=[AT_]); yield
        bk, p_ = ms(); aqkT_ = aqkT[r2]
        P.op("pe", lambda e: e.matmul(p_, kT[:, sl], qT[:, sl], start=True, stop=True), reads=[kT, qT], writes=[bk]); yield
        P.op("dve", lambda e: e.tensor_tensor(aqkT_[:], p_, EDi_[:], ALU.mult), reads=[bk, EDi_], writes=[aqkT_]); yield
        bk, p_ = ms(); pi = 0
        Ac, Bc, Pc = Ap[0], AT_, Pm[0]
        P.op("pe", lambda e: e.transpose(p_, AT_[:], ident[:]), reads=[AT_, ident], writes=[bk]); yield
        P.op("act", lambda e: e.copy(Ac[:], p_), reads=[bk], writes=[Ac]); yield
        P.op("dve", lambda e: e.tensor_tensor(Pc[:], ident[:], AT_[:], ALU.subtract), reads=[ident, AT_], writes=[Pc]); yield
        for lv in range(1, 6):
            An, Bn, Pn = Ap[lv % 3], Bp[lv % 3], Pm[lv % 3]
            bk, p_ = ms()
            P.op("pe", lambda e, p_=p_, Ac=Ac, Bc=Bc: e.matmul(p_, Bc[:], Ac[:], start=True, stop=True), reads=[Ac, Bc], writes=[bk]); yield
            P.op("act", lambda e, p_=p_, An=An: e.copy(An[:], p_), reads=[bk], writes=[An]); yield
            if lv < 5:
                bk2, p2 = ms()
                P.op("pe", lambda e, p2=p2, Ac=Ac, Bc=Bc: e.matmul(p2, Ac[:], Bc[:], start=True, stop=True), reads=[Ac, Bc], writes=[bk2]); yield
                P.op("dve", lambda e, p2=p2, Bn=Bn: e.tensor_copy(Bn[:], p2), reads=[bk2], writes=[Bn]); yield
            bk3, p3 = ms()
            P.op("pe", lambda e, p3=p3, Pc=Pc: e.matmul(p3, ident[:], Pc[:], start=True, stop=False), reads=[ident, Pc], writes=[bk3]); yield
            P.op("pe", lambda e, p3=p3, Pc=Pc, An=An: e.matmul(p3, An[:], Pc[:], start=False, stop=True), reads=[An, Pc], writes=[bk3]); yield
            P.op("dve", lambda e, p3=p3, Pn=Pn: e.tensor_copy(Pn[:], p3), reads=[bk3], writes=[Pn]); yield
            Ac, Bc, Pc = An, Bn, Pn
        MT = Pc
        u_, wT_, vnew_, qdTm_ = u[r2], wT[r2], vnew[r2], qdTm[r2]
        bk, p_ = ms()
        P.op("pe", lambda e: e.matmul(p_, MT[:], vb_[:], start=True, stop=True), reads=[MT, vb_], writes=[bk]); yield
        P.op("act", lambda e: e.copy(u_[:], p_), reads=[bk], writes=[u_]); yield
        bk, p_ = ms()
        P.op("pe", lambda e: e.matmul(p_, kbg_[:], MT[:], start=True, stop=True), reads=[MT, kbg_], writes=[bk]); yield
        P.op("dve", lambda e: e.tensor_copy(wT_[:], p_), reads=[bk], writes=[wT_]); yield
        bk, p_ = ms()
        P.op("pe", lambda e: e.transpose(p_, qd_[:], ident[:]), reads=[qd_, ident], writes=[bk]); yield
        P.op("act", lambda e: e.copy(qdTm_[:, 0, 0:64], p_[:, 0:64]), reads=[bk], writes=[qdTm_]); yield
        P.op("dve", lambda e: e.tensor_copy(qdTm_[:, 1, 64:128], p_[:, 64:128]), reads=[bk], writes=[qdTm_]); yield

    def scan(it, n):
        r2 = it % 2
        kend_, aqkT_, u_, wT_, vnew_, qdTm_ = kend[r2], aqkT[r2], u[r2], wT[r2], vnew[r2], qdTm[r2]
        Scur = stt["S"]; si = stt["si"]
        pO = psO[:, 0:128]
        for bi, b in enumerate((0, 1) if dr == 0 else (1, 0)):
            rb = slice(b * 64, (b + 1) * 64)
            pW = psW[:, 0:128]; pKV = psW[:, 128:256]
            Sn = S[(si + 1) % 2]
            P.op("pe", lambda e, Scur=Scur: e.matmul(pW, wT_[:], Scur[:], start=True, stop=True), reads=[wT_, Scur], writes=[psW]); yield
            P.op("dve", lambda e, rb=rb: e.tensor_tensor(vnew_[rb, :], u_[rb, :], pW[rb, :], ALU.subtract), reads=[u_, psW], writes=[vnew_]); yield
            P.op("pe", lambda e, b=b, bi=bi, Scur=Scur: e.matmul(pO, qdTm_[:, b, :], Scur[:], start=(bi == 0), stop=False), reads=[qdTm_, Scur], writes=[psO]); yield
            P.op("pe", lambda e, rb=rb, bi=bi: e.matmul(pO, aqkT_[rb, :], vnew_[rb, :], start=False, stop=(bi == 1)), reads=[aqkT_, vnew_], writes=[psO]); yield
            P.op("pe", lambda e, rb=rb: e.matmul(pKV, kend_[rb, :], vnew_[rb, :], start=True, stop=True), reads=[kend_, vnew_], writes=[psW]); yield
            P.op("dve", lambda e, b=b, Scur=Scur, Sn=Sn: e.scalar_tensor_tensor(Sn[:], Scur[:], cdb[:, b, n:n + 1], pKV, ALU.mult, ALU.add),
                 reads=[Scur, cdb, psW], writes=[Sn]); yield
            Scur = Sn; si += 1
        P.op("act", lambda e: e.copy(od[:, n, :], pO), reads=[psO], writes=[od]); yield
        stt["S"] = Scur; stt["si"] = si

    return prep, scan, order


from concourse.bass_utils import run_bass_kernel_spmd

DEPTH = 2
W_NAMES = ["w_mod", "b_mod", "norm1", "norm2", "w_in", "ret_decay", "ret_gn", "win_qnorm", "win_knorm", "win_sink",
           "na_qnorm", "na_knorm", "nab", "gdn_conv", "gdn_a_log", "gdn_dt_bias", "gdn_norm", "w_branch", "w_merge", "w_out",
           "w_router", "router_bias", "w_e_gate", "w_e_up", "w_e_down"]
W_SHAPES = dict(w_mod=[2, D, 6144], b_mod=[2, 6144], norm1=[2, D], norm2=[2, D], w_in=[2, D, DIN], ret_decay=[2, 8], ret_gn=[2, 512],
                win_qnorm=[2, 64], win_knorm=[2, 64], win_sink=[2, 8], na_qnorm=[2, 64], na_knorm=[2, 64], nab=[2, 8, 128, 21, 128],
                gdn_conv=[2, 3, 1536], gdn_a_log=[2, 8], gdn_dt_bias=[2, 8], gdn_norm=[2, 128], w_branch=[2, 4, 512, D],
                w_merge=[2, D, 4096], w_out=[2, D, D], w_router=[D, 32], router_bias=[32], w_e_gate=[2, 32, D, 512],
                w_e_up=[2, 32, D, 512], w_e_down=[2, 32, 512, D])


LAYERED = [n for n in W_NAMES if n not in ("w_router", "router_bias")]


def build_program(fused=True):
    nc = bass.Bass("TRN2", target_bir_lowering=False)
    P = Prog(nc)
    k = K(P)
    nl = DEPTH if fused else 1
    cd = {n: P.dram("c_" + n, list(a.shape), F32, kind="ExternalInput") for n, a in mk_consts().items()}
    xin = P.dram("xin", [T, D], F32, kind="ExternalInput")
    cc = P.dram("cc", [2, D], F32, kind="ExternalInput")
    W = {n: P.dram(n, (W_SHAPES[n] if (fused or n not in LAYERED) else W_SHAPES[n][1:]), F32, kind="ExternalInput") for n in W_NAMES}
    if fused:
        out = P.dram("out", [4096, D], F32, kind="ExternalOutput")
        x1 = P.dram("x1", [T, D], F32)
    else:
        out = None
        x1 = P.dram("xo", [T, D], F32, kind="ExternalOutput")
    modv = [P.dram("modv%d" % l, [2, 6144], F32) for l in range(nl)]
    hT_d = P.dram("hT_d", [D, T], BF16)
    z = P.dram("z", [T, DIN], F32)
    k.ybr = P.dram("ybr", [4, T, 512], F32)
    xmid = P.dram("xmid", [T, D], F32)
    h2T_d = P.dram("h2T_d", [D, T], BF16)
    h32_d = P.dram("h32_d", [D, T], F32)
    load_consts(k, cd)
    mT = [P.sbuf([128, 48, 2], F32, "mT%d" % l) for l in range(nl)]

    def wsub(n, l):
        return _Sub(W[n], l) if (fused and n in LAYERED) else W[n]

    for l in range(nl):
        phase_mod(k, cc, wsub("w_mod", l), wsub("b_mod", l), modv[l], mT[l])
    xs = [xin, x1]
    for l in range(nl):
        ctx_out = (l < DEPTH - 1) if fused else True
        x = xs[l]
        w = lambda n: wsub(n, l).ap()
        with P.scope():
            hsb = P.sbuf([128, 8, T], BF16, "hsb")
            phase_norm(k, x, w("norm1"), mT[l], 0, hsb)
            for kk in range(8):
                P.dma("sp", hT_d.ap()[kk * 128:(kk + 1) * 128, :], hsb[:, kk, :], reads=[hsb], writes=[hT_d])
            phase_proj(k, hsb, w("w_in"), z, DIN)
        phase_ret(k, z, w("ret_decay"), w("ret_gn"), ctx_out)
        phase_window(k, z, w("win_qnorm"), w("win_knorm"), w("win_sink"), ctx_out)
        phase_na(k, z, w("na_qnorm"), w("na_knorm"), w("nab"), ctx_out)
        phase_gdn(k, z, w("gdn_conv"), w("gdn_a_log"), w("gdn_dt_bias"), w("gdn_norm"), ctx_out)
        phase_merge(k, hT_d, x, xmid, w("w_merge"), w("w_branch"), w("w_out"), modv[l], ctx_out)
        t_lo = 0 if ctx_out else 2
        with P.scope():
            h2 = P.sbuf([128, 8, T], BF16, "h2sb")
            phase_norm(k, xmid, w("norm2"), mT[l], 1, h2, hT32_d=h32_d, t_lo=t_lo)
            for kk in range(8):
                P.dma("sp", h2T_d.ap()[kk * 128:(kk + 1) * 128, t_lo * 128:], h2[:, kk, t_lo * 128:], reads=[h2], writes=[h2T_d])
        with P.scope():
            wgt = P.sbuf([128, NT, 32], F32, "wgt")
            phase_router(k, h32_d, W["w_router"].ap(), W["router_bias"].ap(), wgt, t_lo)
            phase_moe(k, h2T_d, wgt, xmid, x1, w("w_e_gate"), w("w_e_up"), w("w_e_down"), modv[l], t_lo,
                      out_lat=(out if (fused and l == DEPTH - 1) else None))
    P.wait_all("sp", [out] if fused else [x1])
    P.emit()
    return nc, P


class _Sub:
    def __init__(self, parent, l):
        self.p = parent; self.l = l
        self.w = parent.w; self.r = parent.r

    def ap(self):
        return self.p.ap()[self.l]


def _sub(buf, l):
    return _Sub(buf, l)


_CACHE = {}
FUSED = True


def host_inputs(inputs, layer=None, x_prev=None):
    f = lambda a: np.ascontiguousarray(np.asarray(a, dtype=np.float32))
    shared = {"c_" + n: a for n, a in mk_consts().items()}
    for n in W_NAMES:
        if n == "nab":
            full = np.stack([na_bias_gather(f(inputs["na_rpb"][l])) for l in range(DEPTH)])
        else:
            full = f(inputs[n]).reshape(W_SHAPES[n])
        shared[n] = full if (layer is None or n not in LAYERED) else np.ascontiguousarray(full[layer])
    maps = []
    for b in range(8):
        m = dict(shared)
        if x_prev is None:
            m["xin"] = np.concatenate([f(inputs["ctx"][b]), f(inputs["x"][b])], axis=0)
        else:
            m["xin"] = x_prev[b]
        m["cc"] = np.stack([f(inputs["c_ctx"]), f(inputs["c"][b])])
        maps.append(m)
    return maps


def kernel(**inputs):
    if "nc" not in _CACHE:
        _CACHE["nc"] = build_program(FUSED)[0]
    nc = _CACHE["nc"]
    cores = list(range(8))
    if FUSED:
        res = run_bass_kernel_spmd(nc, host_inputs(inputs), core_ids=cores)
        return np.stack([np.asarray(r["out"], dtype=np.float32) for r in res.results], axis=0)
    xp = None
    for l in range(DEPTH):
        res = run_bass_kernel_spmd(nc, host_inputs(inputs, layer=l, x_prev=xp), core_ids=cores)
        xp = [np.asarray(r["xo"], dtype=np.float32) for r in res.results]
    return np.stack([x[LC:] for x in xp], axis=0)
```

```python
from contextlib import ExitStack
import numpy as np
import concourse.bass as bass
import concourse.mybir as mybir

F32 = mybir.dt.float32
BF16 = mybir.dt.bfloat16
I32 = mybir.dt.int32
AF = mybir.ActivationFunctionType
ALU = mybir.AluOpType
AX = mybir.AxisListType

ENGS = ("pe", "dve", "act", "pool", "sp")
N_DSEM = 8
SEM_WRAP = 30000


class Buf:
    def __init__(self, t, name=""):
        self.t = t
        self.name = name
        self.w = {}
        self.r = {}

    v = None
    is_psum = False

    def __getitem__(self, idx):
        if self.v is not None:
            return self.v[idx]
        return self.t[idx]

    def ap(self):
        return self.t.ap() if hasattr(self.t, "ap") else self.t[:]


class Prog:
    def __init__(self, nc, same_engine_sync=True, direct=True):
        self.nc = nc
        self.es = ExitStack()
        self.ops = {e: [] for e in ENGS}
        self.cnt = {e: 0 for e in ENGS}
        self.seen = {e: {} for e in ENGS}
        self.sems = {}
        self.same = same_engine_sync
        self.dma_n = {e: 0 for e in ENGS}
        self.uid = 0
        self.stacks = [self.es]
        self.scope_bufs = [[]]
        self.free_deps = {}
        self.direct = direct
        self.nops = {}
        self.engobj = {"pe": nc.tensor, "dve": nc.vector, "act": nc.scalar, "pool": nc.gpsimd, "sp": nc.sync}

    def scope(self):
        prog = self

        class _S:
            def __enter__(s):
                st = ExitStack()
                prog.stacks.append(st)
                prog.scope_bufs.append([])
                return s

            def __exit__(s, *a):
                st = prog.stacks.pop()
                for b in prog.scope_bufs.pop():
                    for dd in (b.w, b.r):
                        for kk, v in dd.items():
                            prog.free_deps[kk] = max(prog.free_deps.get(kk, 0), v)
                st.close()
                return False
        return _S()

    def sem(self, key):
        if key not in self.sems:
            self.sems[key] = self.es.enter_context(self.nc.semaphore("s_%s_%s_%d" % key))
        return self.sems[key]

    def sbuf(self, shape, dt, name=None):
        self.uid += 1
        name = "%s_%d" % (name or "sb", self.uid)
        t = self.stacks[-1].enter_context(self.nc.sbuf_tensor(name, list(shape), dt))
        b = Buf(t, name)
        b.w = dict(self.free_deps)
        self.scope_bufs[-1].append(b)
        return b

    def psum(self, shape, dt=F32, name=None):
        self.uid += 1
        name = "%s_%d" % (name or "ps", self.uid)
        p, n = shape
        nb = (n * 4 + 2047) // 2048
        t = self.stacks[-1].enter_context(self.nc.psum_tensor(name, [128, nb * 512], F32))
        b = Buf(t, name)
        b.v = t[0:p, 0:n]
        b.is_psum = True
        b.w = dict(self.free_deps)
        self.scope_bufs[-1].append(b)
        return b

    def dram(self, name, shape, dt, kind="Internal"):
        t = self.nc.dram_tensor(name, list(shape), dt, kind=kind)
        return Buf(t, name)

    def _waits(self, eng, reads, writes):
        need = {}
        for b in reads:
            for k, v in b.w.items():
                need[k] = max(need.get(k, 0), v)
        for b in writes:
            for k, v in b.w.items():
                need[k] = max(need.get(k, 0), v)
            for k, v in b.r.items():
                need[k] = max(need.get(k, 0), v)
        out = []
        seen = self.seen[eng]
        for k, v in need.items():
            if k[0] == eng and k[1] != "d":
                if eng == "pe" or not self.same:
                    continue
            if seen.get(k, 0) >= v:
                continue
            seen[k] = v
            out.append((k, v))
        return out

    def op(self, eng, fn, reads=(), writes=()):
        pr = [b for b in reads if b.is_psum]
        if pr:
            writes = list(writes) + pr
        waits = self._waits(eng, reads, writes)
        self.cnt[eng] += 1
        n = self.cnt[eng]
        key = (eng, "c", (n - 1) // SEM_WRAP)
        val = (n - 1) % SEM_WRAP + 1
        self._put(eng, waits, fn, key, 1)
        for b in reads:
            b.r[key] = max(b.r.get(key, 0), val)
        for b in writes:
            b.w[key] = max(b.w.get(key, 0), val)

    def dma(self, q, out_ap, in_ap, reads=(), writes=(), **kw):
        waits = self._waits(q, reads, writes)
        n = self.dma_n[q]
        self.dma_n[q] += 1
        key = (q, "d", n % N_DSEM)
        val = 16 * (n // N_DSEM + 1)
        if val > 16 and self.seen[q].get(key, 0) < val - 16:
            self.seen[q][key] = val - 16
            waits.append((key, val - 16))

        def fn(e, out_ap=out_ap, in_ap=in_ap, kw=kw):
            return e.dma_start(out=out_ap, in_=in_ap, **kw)

        self._put(q, waits, fn, key, 16)
        for b in reads:
            b.r[key] = max(b.r.get(key, 0), val)
        for b in writes:
            b.w[key] = max(b.w.get(key, 0), val)

    def wait_all(self, eng, bufs):
        need = {}
        for b in bufs:
            for k, v in b.w.items():
                need[k] = max(need.get(k, 0), v)
        self._put(eng, list(need.items()), None, None, 0)

    def _put(self, eng, waits, fn, key, inc):
        if not self.direct:
            self.ops[eng].append((waits, fn, key, inc))
            return
        self.nops[eng] = self.nops.get(eng, 0) + 1
        e = self.engobj[eng]
        for k, v in waits:
            e.wait_ge(self.sem(k), v)
        if fn is not None:
            fn(e).then_inc(self.sem(key), inc)

    def emit(self):
        nc = self.nc
        if self.direct:
            self.es.close()
            return
        for e in ENGS:
            for waits, fn, key, inc in self.ops[e]:
                for k, v in waits:
                    self.sem(k)
                if key is not None:
                    self.sem(key)
        with nc.Block() as block:
            def run(engname):
                def body(eng):
                    for waits, fn, key, inc in self.ops[engname]:
                        for k, v in waits:
                            eng.wait_ge(self.sems[k], v)
                        if fn is not None:
                            fn(eng).then_inc(self.sems[key], inc)
                return body
            if self.ops["sp"]:
                block.sync(run("sp"))
            if self.ops["pe"]:
                block.tensor(run("pe"))
            if self.ops["dve"]:
                block.vector(run("dve"))
            if self.ops["act"]:
                block.scalar(run("act"))
            if self.ops["pool"]:
                block.gpsimd(run("pool"))
        self.es.close()


D = 1024
T = 4352
NT = T // 128
LC = 256
DIN = 5904
EPS = 1e-6


class K:
    def __init__(self, P):
        self.P = P
        self.rr = 0

    def evac_engine(self):
        self.rr += 1
        return "act" if self.rr % 2 else "dve"


def mk_consts():
    c = {}
    c["ident_f"] = np.eye(128, dtype=np.float32)
    cos, sin = rope_tables()
    c["cos"] = cos; c["sin"] = sin
    j = np.arange(128)[:, None]; i = np.arange(128)[None, :]
    c["maskPrev"] = (i <= j).astype(np.float32)
    c["maskNext"] = (j <= i).astype(np.float32)
    ret_consts(c)
    gdn_consts(c)
    return c


def load_consts(k, cd):
    P = k.P
    k.ident_f = P.sbuf([128, 128], F32, "ident_f")
    P.dma("sp", k.ident_f[:], cd["ident_f"].ap(), writes=[k.ident_f])
    k.ident_b = P.sbuf([128, 128], BF16, "ident_b")
    P.op("dve", lambda e: e.tensor_copy(k.ident_b[:], k.ident_f[:]), reads=[k.ident_f], writes=[k.ident_b])
    k.cos = P.sbuf([128, 32, 32], F32, "cos"); k.sin = P.sbuf([128, 32, 32], F32, "sin")
    P.dma("sp", k.cos[:], cd["cos"].ap().rearrange("(t p) f -> p t f", p=128), writes=[k.cos])
    P.dma("sp", k.sin[:], cd["sin"].ap().rearrange("(t p) f -> p t f", p=128), writes=[k.sin])
    k.maskPrev = P.sbuf([128, 128], BF16, "maskPrev"); k.maskNext = P.sbuf([128, 128], BF16, "maskNext")
    with P.scope():
        mst = P.sbuf([128, 2, 128], F32, "mst")
        P.dma("sp", mst[:, 0, :], cd["maskPrev"].ap(), writes=[mst])
        P.dma("sp", mst[:, 1, :], cd["maskNext"].ap(), writes=[mst])
        P.op("dve", lambda e: e.tensor_copy(k.maskPrev[:], mst[:, 0, :]), reads=[mst], writes=[k.maskPrev])
        P.op("dve", lambda e: e.tensor_copy(k.maskNext[:], mst[:, 1, :]), reads=[mst], writes=[k.maskNext])
    k.cd = cd
    k.one_col = P.sbuf([128, 1], F32, "one_col")
    P.op("dve", lambda e: e.memset(k.one_col[:], 1.0), writes=[k.one_col])
    k.eps_col = P.sbuf([128, 1], F32, "eps_col")
    P.op("dve", lambda e: e.memset(k.eps_col[:], EPS), writes=[k.eps_col])


def load_w_bf16(k, dst, dst_ap, src_ap, stage, stage_ap):
    P = k.P
    P.dma("sp", stage_ap, src_ap, writes=[stage])
    P.op("pool", lambda e: e.tensor_copy(dst_ap, stage_ap), reads=[stage], writes=[dst])


def cols_from_rows(k, dst_ap, rows_ap, R, tag):
    P = k.P
    with P.scope():
        st = P.sbuf([R, 128], F32, tag + "_rows")
        ps = P.psum([128, R], F32, tag + "_ps")
        P.dma("sp", st[:], rows_ap, writes=[st])
        P.op("pe", lambda e: e.transpose(ps[:], st[:], k.ident_f[0:R, 0:R]), reads=[st, k.ident_f], writes=[ps])
        return st, ps


def phase_mod(k, cc, w_mod, b_mod, modv, mT):
    P = k.P
    with P.scope():
        ccr = P.sbuf([16, 128], F32, "ccr")
        bcr = P.sbuf([48, 128], F32, "bcr")
        pst = P.psum([128, 64], F32, "mod_pst")
        P.dma("sp", ccr[:], cc.ap().rearrange("r (k p) -> (r k) p", p=128), writes=[ccr])
        P.dma("sp", bcr[:], b_mod.ap().rearrange("(c p) -> c p", p=128), writes=[bcr])
        P.op("pe", lambda e: e.transpose(pst[:, 0:16], ccr[:], k.ident_f[0:16, 0:16]), reads=[ccr, k.ident_f], writes=[pst])
        P.op("pe", lambda e: e.transpose(pst[:, 16:64], bcr[:], k.ident_f[0:48, 0:48]), reads=[bcr, k.ident_f], writes=[pst])
        sT = P.sbuf([128, 2, 8], F32, "sccT")
        bcol = P.sbuf([128, 48], F32, "bcol")
        P.op("act", lambda e: e.activation(sT[:].rearrange("p r k -> p (r k)"), pst[:, 0:16], AF.Silu), reads=[pst], writes=[sT])
        P.op("dve", lambda e: e.tensor_copy(bcol[:], pst[:, 16:64]), reads=[pst], writes=[bcol])
        wst = [P.sbuf([128, 8, 512], F32, "wmod_st%d" % i) for i in range(2)]
        ps = P.psum([128, 96], F32, "ps_mod")
        wv = w_mod.ap().rearrange("(k p) n -> p k n", p=128)
        for nb in range(12):
            w = wst[nb % 2]
            P.dma("sp", w[:], wv[:, :, nb * 512:(nb + 1) * 512], writes=[w])
            for cl in range(4):
                c = nb * 4 + cl
                for kk in range(8):
                    P.op("pe", lambda e, kk=kk, w=w, c=c, cl=cl: e.matmul(ps[:, 2 * c:2 * c + 2], w[:, kk, cl * 128:(cl + 1) * 128], sT[:, :, kk],
                                                                     start=(kk == 0), stop=(kk == 7)),
                         reads=[sT, w], writes=[ps])
        P.op("dve", lambda e: e.tensor_tensor(mT[:], ps[:].rearrange("p (c r) -> p c r", r=2), bcol[:].unsqueeze(2).to_broadcast([128, 48, 2]), ALU.add),
             reads=[ps, bcol], writes=[mT])
        for r in range(2):
            pr = P.psum([48, 128], F32, "mod_pr%d" % r)
            sr = P.sbuf([48, 128], F32, "mod_sr%d" % r)
            mr = P.sbuf([128, 48], F32, "mod_mr%d" % r)
            P.op("dve", lambda e, r=r, mr=mr: e.tensor_copy(mr[:], mT[:, :, r]), reads=[mT], writes=[mr])
            P.op("pe", lambda e, r=r, pr=pr, mr=mr: e.transpose(pr[:], mr[:], k.ident_f[:]), reads=[mr, k.ident_f], writes=[pr])
            P.op("act", lambda e, pr=pr, sr=sr: e.copy(sr[:], pr[:]), reads=[pr], writes=[sr])
            P.dma("sp", modv.ap()[r].rearrange("(c p) -> c p", p=128), sr[:], reads=[sr], writes=[modv])


def phase_norm(k, x, gain, mT, which, hT_sb, hT32_d=None, t_lo=0):
    P = k.P
    with P.scope():
        gr = P.sbuf([8, 128], F32, "gr%d" % which)
        gps = P.psum([128, 8], F32, "gps%d" % which)
        P.dma("sp", gr[:], gain.rearrange("(k p) -> k p", p=128), writes=[gr])
        P.op("pe", lambda e: e.transpose(gps[:], gr[:], k.ident_f[0:8, 0:8]), reads=[gr, k.ident_f], writes=[gps])
        G = P.sbuf([128, 2, 8], F32, "G%d" % which)
        gcol = P.sbuf([128, 8], F32, "gcol%d" % which)
        P.op("dve", lambda e: e.tensor_copy(gcol[:], gps[:]), reads=[gps], writes=[gcol])
        sh_i, sc_i = 3 * which, 3 * which + 1
        for r in range(2):
            P.op("dve", lambda e, r=r: e.scalar_tensor_tensor(G[:, r, :], mT[:, sc_i * 8:sc_i * 8 + 8, r], 1.0, gcol[:], ALU.add, ALU.mult),
                 reads=[mT, gcol], writes=[G])
        xt = [P.sbuf([128, 1024], F32, "nx%d_%d" % (which, i)) for i in range(3)]
        junk = P.sbuf([128, 1024], F32, "njunk%d" % which)
        xs = [P.sbuf([128, 1024], F32, "nxs%d_%d" % (which, i)) for i in range(5)]
        ss = [P.sbuf([128, 2], F32, "nss%d_%d" % (which, i)) for i in range(3)]
        pts = [P.psum([128, 512], F32, "npt%d_%d" % (which, i)) for i in range(2)]
        h32 = [P.sbuf([128, 512], F32, "nh32_%d_%d" % (which, i)) for i in range(2)] if hT32_d is not None else None
        groups = [[0, 1]] + [list(range(2 + 4 * g, 6 + 4 * g)) for g in range(8)]
        ti = 0
        ng = 0
        for grp in groups:
            if grp[0] < t_lo:
                continue
            r = 0 if grp[0] < 2 else 1
            tiles = []
            for t in grp:
                xb = xt[ti % 3]; sb = ss[ti % 3]; xsb = xs[ti % 5]
                ti += 1
                P.dma("sp", xb[:], x.ap()[t * 128:(t + 1) * 128, :], writes=[xb])
                P.op("act", lambda e, xb=xb, sb=sb: e.activation(junk[:], xb[:], AF.Square, accum_out=sb[:, 0:1]),
                     reads=[xb], writes=[junk, sb])
                P.op("act", lambda e, sb=sb: e.activation(sb[:, 1:2], sb[:, 0:1], AF.Sqrt, bias=k.eps_col[:], scale=1.0 / D),
                     reads=[sb, k.eps_col], writes=[sb])
                P.op("dve", lambda e, sb=sb: e.reciprocal(sb[:, 1:2], sb[:, 1:2]), reads=[sb], writes=[sb])
                P.op("dve", lambda e, xb=xb, sb=sb, xsb=xsb: e.tensor_scalar(xsb[:], xb[:], sb[:, 1:2], None, ALU.mult),
                     reads=[xb, sb], writes=[xsb])
                tiles.append(xsb)
            n = len(grp) * 128
            tok0 = grp[0] * 128
            for kk in range(8):
                pt = pts[ng % 2]
                for j, xsb in enumerate(tiles):
                    P.op("pe", lambda e, pt=pt, xsb=xsb, kk=kk, j=j: e.transpose(pt[:, j * 128:(j + 1) * 128], xsb[:, kk * 128:(kk + 1) * 128], k.ident_f[:]),
                         reads=[xsb, k.ident_f], writes=[pt])
                bias_ap = mT[:, sh_i * 8 + kk, r:r + 1]
                P.op("act", lambda e, pt=pt, kk=kk, r=r, tok0=tok0, n=n, bias_ap=bias_ap: e.activation(
                    hT_sb[:, kk, tok0:tok0 + n], pt[:, 0:n], AF.Identity, bias=bias_ap, scale=G[:, r, kk:kk + 1]),
                    reads=[pt, mT, G], writes=[hT_sb])
                if hT32_d is not None:
                    hb = h32[ng % 2]
                    P.op("dve", lambda e, pt=pt, kk=kk, r=r, n=n, hb=hb, bias_ap=bias_ap: e.tensor_scalar(
                        hb[:, 0:n], pt[:, 0:n], G[:, r, kk:kk + 1], bias_ap, ALU.mult, ALU.add),
                        reads=[pt, mT, G], writes=[hb])
                    P.dma("sp", hT32_d.ap()[kk * 128:(kk + 1) * 128, tok0:tok0 + n], hb[:, 0:n], reads=[hb], writes=[hT32_d])
                ng += 1


def phase_proj(k, hT_sb, w, z, N, tag="pj"):
    P = k.P
    with P.scope():
        _phase_proj(k, hT_sb, w, z, N, tag)


def _phase_proj(k, hT_sb, w, z, N, tag):
    P = k.P
    wst = [P.sbuf([128, 8, 512], F32, "%s_st%d" % (tag, i)) for i in range(2)]
    wb = [P.sbuf([128, 8, 512], BF16, "%s_wb%d" % (tag, i)) for i in range(2)]
    pss = [P.psum([128, 512], F32, "%s_ps%d" % (tag, i)) for i in range(3)]
    ob = [P.sbuf([128, 512], F32, "%s_ob%d" % (tag, i)) for i in range(4)]
    wv = w.rearrange("(k p) n -> p k n", p=128)
    nblk = (N + 511) // 512
    it = 0
    for nb in range(nblk):
        c0 = nb * 512
        cw = min(512, N - c0)
        st, wbb = wst[nb % 2], wb[nb % 2]
        load_w_bf16(k, wbb, wbb[:, :, 0:cw], wv[:, :, c0:c0 + cw], st, st[:, :, 0:cw])
        for t in range(NT):
            ps = pss[it % 3]; o = ob[it % 4]
            it += 1
            for kk in range(8):
                P.op("pe", lambda e, ps=ps, kk=kk, t=t, wbb=wbb, cw=cw: e.matmul(
                    ps[:, 0:cw], hT_sb[:, kk, t * 128:(t + 1) * 128], wbb[:, kk, 0:cw], start=(kk == 0), stop=(kk == 7)),
                    reads=[hT_sb, wbb], writes=[ps])
            if it % 2:
                P.op("act", lambda e, ps=ps, o=o, cw=cw: e.copy(o[:, 0:cw], ps[:, 0:cw]), reads=[ps], writes=[o])
            else:
                P.op("dve", lambda e, ps=ps, o=o, cw=cw: e.tensor_copy(o[:, 0:cw], ps[:, 0:cw]), reads=[ps], writes=[o])
            P.dma("sp", z.ap()[t * 128:(t + 1) * 128, c0:c0 + cw], o[:, 0:cw], reads=[o], writes=[z])


ZOFF = dict(ret_q=0, ret_k=256, ret_v=512, ret_g=1024, win_q=1536, win_k=2048, win_v=2176,
            na_q=2304, na_k=2816, na_v=3328, gdn_q=3840, gdn_k=4352, gdn_v=4864, gdn_g=5376, gdn_a=5888, gdn_b=5896)
NEG = -30000.0


def rope_tables():
    t = np.arange(4096)
    row = (t // 64).astype(np.float32); col = (t % 64).astype(np.float32)
    inv = (10000.0 ** (-np.arange(16, dtype=np.float32) / 16)).astype(np.float32)
    ang = np.concatenate([row[:, None] * inv, col[:, None] * inv], axis=-1).astype(np.float32)
    return np.cos(ang).astype(np.float32), np.sin(ang).astype(np.float32)


def na_classes():
    cls = [(10, 10 + dl) for dl in range(-2, 3)]
    for p in (0, 1):
        cls += [(p, b) for b in range(4)]
    for p in (30, 31):
        cls += [(p, b) for b in range(28, 32)]
    return cls


def na_plan(p):
    if 2 <= p <= 29:
        return [(p + dl, dl + 2) for dl in range(-2, 3)]
    e = {0: 0, 1: 1, 30: 2, 31: 3}[p]
    b0 = 0 if p < 2 else 28
    return [(b0 + b, 5 + e * 4 + b) for b in range(4)]


def na_bias_gather(rpb):
    cls = na_classes()
    out = np.full((8, 128, len(cls), 128), NEG, np.float32)
    kc = np.arange(64)[:, None]; qc = np.arange(64)[None, :]
    cst = np.clip(qc - 8, 0, 48)
    colok = (kc >= cst) & (kc < cst + 16)
    coff = np.clip(kc - qc + 15, 0, 30)
    for ci, (p, blk) in enumerate(cls):
        for a in range(2):
            krow = 2 * blk + a
            for b in range(2):
                qrow = 2 * p + b
                rs = min(max(qrow - 4, 0), 56)
                if not (rs <= krow < rs + 8):
                    continue
                ro = krow - qrow + 7
                vals = rpb[:, ro][:, coff]
                blkv = np.where(colok[None], vals, NEG)
                out[:, a * 64:(a + 1) * 64, ci, b * 64:(b + 1) * 64] = blkv
    return out


def attn_prep_qk(k, z, col, gain_bc, rope, outT, tag, cs=None):
    P = k.P
    with P.scope():
        x = P.sbuf([128, NT, 64], F32, tag + "x")
        zv = z.ap()[:, col:col + 64].rearrange("(t p) c -> p t c", p=128)
        P.dma("sp", x[:, 0:17, :], zv[:, 0:17, :], writes=[x])
        P.dma("sp", x[:, 17:34, :], zv[:, 17:34, :], writes=[x])
        sq = P.sbuf([128, NT, 64], F32, tag + "sq")
        P.op("pool", lambda e: e.tensor_tensor(sq[:], x[:], x[:], ALU.mult), reads=[x], writes=[sq])
        ss = P.sbuf([128, NT], F32, tag + "ss")
        P.op("dve", lambda e: e.tensor_reduce(ss[:], sq[:], AX.X, ALU.add), reads=[sq], writes=[ss])
        P.op("act", lambda e: e.activation(ss[:], ss[:], AF.Sqrt, bias=k.eps_col[:], scale=1.0 / 64), reads=[ss, k.eps_col], writes=[ss])
        P.op("dve", lambda e: e.reciprocal(ss[:], ss[:]), reads=[ss], writes=[ss])
        P.op("dve", lambda e: e.tensor_tensor(x[:], x[:], ss[:].unsqueeze(2).to_broadcast([128, NT, 64]), ALU.mult), reads=[x, ss], writes=[x])
        P.op("pool", lambda e: e.tensor_tensor(x[:], x[:], gain_bc[:].unsqueeze(1).to_broadcast([128, NT, 64]), ALU.mult), reads=[x, gain_bc], writes=[x])
        if rope:
            rope_apply(k, x, 1, tag)
        transpose_to(k, lambda t: x[:, t, :], 64, outT, tag, src_bufs=[x])


def rope_apply(k, x, H, tag):
    P = k.P
    with P.scope():
        _rope_apply(k, x, H, tag)


def _rope_apply(k, x, H, tag):
    P = k.P
    xl = x[:, 2:NT, :].rearrange("p t (h c) -> p t h c", c=64)
    x1 = xl[:, :, :, 0:32]; x2 = xl[:, :, :, 32:64]
    shp = [128, 32, H, 32]
    a = P.sbuf(shp, F32, tag + "ra"); b = P.sbuf(shp, F32, tag + "rb")
    cosb = k.cos[:].unsqueeze(2).to_broadcast(shp); sinb = k.sin[:].unsqueeze(2).to_broadcast(shp)
    P.op("dve", lambda e: e.tensor_tensor(a[:], x1, sinb, ALU.mult), reads=[x, k.sin], writes=[a])
    P.op("pool", lambda e: e.tensor_tensor(b[:], x2, sinb, ALU.mult), reads=[x, k.sin], writes=[b])
    P.op("dve", lambda e: e.tensor_tensor(x1, x1, cosb, ALU.mult), reads=[x, k.cos, a, b], writes=[x])
    P.op("dve", lambda e: e.tensor_tensor(x2, x2, cosb, ALU.mult), reads=[x, k.cos], writes=[x])
    P.op("dve", lambda e: e.tensor_tensor(x1, x1, b[:], ALU.subtract), reads=[x, b], writes=[x])
    P.op("dve", lambda e: e.tensor_tensor(x2, x2, a[:], ALU.add), reads=[x, a], writes=[x])


def transpose_to(k, src_fn, C, outT, tag, deps=None, t_list=None, src_bufs=None, out_off=0):
    P = k.P
    with P.scope():
        pss = [P.psum([128, 512], F32, tag + "tp%d" % i) for i in range(2)]
        tl = list(range(NT)) if t_list is None else t_list
        for g0 in range(0, len(tl), 4):
            grp = tl[g0:g0 + 4]
            ps = pss[(g0 // 4) % 2]
            for j, t in enumerate(grp):
                ap, bufs = src_fn(t), (src_bufs or [])
                P.op("pe", lambda e, ps=ps, j=j, ap=ap: e.transpose(ps[0:C, j * 128:(j + 1) * 128], ap, k.ident_f[:]),
                     reads=list(bufs) + [k.ident_f] + (deps or []), writes=[ps])
            n = len(grp) * 128
            o = outT[0:C, out_off + grp[0] * 128: out_off + grp[0] * 128 + n]
            if (g0 // 4) % 2:
                P.op("act", lambda e, ps=ps, o=o, n=n: e.copy(o, ps[0:C, 0:n]), reads=[ps], writes=[outT])
            else:
                P.op("dve", lambda e, ps=ps, o=o, n=n: e.tensor_copy(o, ps[0:C, 0:n]), reads=[ps], writes=[outT])


def attn_prep_v(k, z, col, Vaug, tag):
    P = k.P
    with P.scope():
        v = P.sbuf([128, NT, 64], F32, tag + "v")
        zv = z.ap()[:, col:col + 64].rearrange("(t p) c -> p t c", p=128)
        P.dma("sp", v[:, 0:17, :], zv[:, 0:17, :], writes=[v])
        P.dma("sp", v[:, 17:34, :], zv[:, 17:34, :], writes=[v])
        P.op("pool", lambda e: e.tensor_copy(Vaug[:, :, 0:64], v[:]), reads=[v], writes=[Vaug])
        P.op("pool", lambda e: e.memset(Vaug[:, :, 64:65], 1.0), writes=[Vaug])


def attn_core(k, qT, kT, Vaug, plan, sink_ap, sink_buf, yh, tag):
    P = k.P
    with P.scope():
        pss = [[P.psum([128, 512], F32, "%ss%d_%d" % (tag, i, j)) for j in range(2)] for i in range(2)]
        pos = [P.psum([128, 65], F32, "%so%d" % (tag, i)) for i in range(2)]
        pts = [P.sbuf([128, 8 * 128], BF16, "%spt%d" % (tag, i)) for i in range(2)]
        dens = [P.sbuf([128, 1], F32, "%sden%d" % (tag, i)) for i in range(2)]
        def st1(qi):
            qt, blocks = plan[qi]
            sset = pss[qi % 2]
            for bi, (kt, m, mb) in enumerate(blocks):
                ps = sset[bi // 4]
                P.op("pe", lambda e, ps=ps, bi=bi, kt=kt, qt=qt: e.matmul(ps[:, (bi % 4) * 128:(bi % 4 + 1) * 128], kT[:, kt * 128:(kt + 1) * 128],
                                                                      qT[:, qt * 128:(qt + 1) * 128], start=True, stop=True),
                     reads=[kT, qT], writes=[ps])

        def st2(qi):
            qt, blocks = plan[qi]
            sset = pss[qi % 2]; pt = pts[qi % 2]
            nb = len(blocks)
            for bk in range((nb + 3) // 4):
                n = min(4, nb - bk * 4) * 128
                P.op("act", lambda e, pt=pt, bk=bk, n=n, sset=sset: e.activation(pt[:, bk * 512:bk * 512 + n], sset[bk][:, 0:n], AF.Exp, scale=0.125),
                     reads=[sset[bk]], writes=[pt])
            for bi, (kt, m, mb) in enumerate(blocks):
                if m is not None:
                    eng = "pool" if bi % 2 else "dve"
                    P.op(eng, lambda e, pt=pt, bi=bi, m=m: e.tensor_tensor(pt[:, bi * 128:(bi + 1) * 128], pt[:, bi * 128:(bi + 1) * 128], m, ALU.mult),
                         reads=[pt, mb], writes=[pt])

        def st3(qi):
            qt, blocks = plan[qi]
            pt = pts[qi % 2]; po = pos[qi % 2]
            nb = len(blocks)
            for bi, (kt, m, mb) in enumerate(blocks):
                P.op("pe", lambda e, po=po, pt=pt, bi=bi, kt=kt, nb=nb: e.matmul(po[:], pt[:, bi * 128:(bi + 1) * 128], Vaug[:, kt, :],
                                                                             start=(bi == 0), stop=(bi == nb - 1)),
                     reads=[pt, Vaug], writes=[po])

        def st4(qi):
            qt, blocks = plan[qi]
            po = pos[qi % 2]; den = dens[qi % 2]
            if sink_ap is not None:
                P.op("dve", lambda e, den=den, po=po: e.tensor_scalar(den[:], po[:, 64:65], sink_ap, None, ALU.add), reads=[po, sink_buf], writes=[den])
                P.op("dve", lambda e, den=den: e.reciprocal(den[:], den[:]), reads=[den], writes=[den])
            else:
                P.op("dve", lambda e, den=den, po=po: e.reciprocal(den[:], po[:, 64:65]), reads=[po], writes=[den])
            P.op("act", lambda e, den=den, po=po, qt=qt: e.activation(yh[:, qt, :], po[:, 0:64], AF.Copy, scale=den[:, 0:1]), reads=[po, den], writes=[yh])

        nq = len(plan)
        for step in range(nq + 3):
            if step < nq:
                st1(step)
            if 0 <= step - 1 < nq:
                st2(step - 1)
            if 0 <= step - 2 < nq:
                st3(step - 2)
            if 0 <= step - 3 < nq:
                st4(step - 3)


def store_head(k, yh, ybr_ap, col, t_lo=0):
    P = k.P
    C = yh.t.shape[2]
    yv = ybr_ap[:, col:col + C].rearrange("(t p) c -> p t c", p=128)
    P.dma("sp", yv[:, t_lo:17, :], yh[:, t_lo:17, :], reads=[yh], writes=[k.ybr])
    P.dma("sp", yv[:, 17:34, :], yh[:, 17:34, :], reads=[yh], writes=[k.ybr])


def bc_load(k, vec_ap, n, name):
    P = k.P
    t = P.sbuf([128, n], F32, name)
    P.dma("sp", t[:], vec_ap.partition_broadcast(128), writes=[t])
    return t


def phase_window(k, z, qn, kn, sink, ctx_out):
    P = k.P
    with P.scope():
        qg = bc_load(k, qn, 64, "wqg"); kg = bc_load(k, kn, 64, "wkg")
        sk = bc_load(k, sink, 8, "wsk")
        P.op("act", lambda e: e.activation(sk[:], sk[:], AF.Exp), reads=[sk], writes=[sk])
        qT = P.sbuf([64, T], BF16, "wqT"); kT = P.sbuf([64, T], BF16, "wkT")
        Vaug = P.sbuf([128, NT, 65], BF16, "wV")
        yh = P.sbuf([128, NT, 64], F32, "wyh")
        plan = []
        if ctx_out:
            plan += [(qt, [(0, None, None), (1, None, None)]) for qt in range(2)]
        for n in range(32):
            bl = []
            if n > 0:
                bl.append((2 + n - 1, k.maskPrev[:], k.maskPrev))
            bl.append((2 + n, None, None))
            if n < 31:
                bl.append((2 + n + 1, k.maskNext[:], k.maskNext))
            bl += [(0, None, None), (1, None, None)]
            plan.append((2 + n, bl))
        for h in range(8):
            g = h // 4
            if h % 4 == 0:
                attn_prep_qk(k, z, ZOFF["win_k"] + g * 64, kg, True, kT, "wk")
                attn_prep_v(k, z, ZOFF["win_v"] + g * 64, Vaug, "wv")
            attn_prep_qk(k, z, ZOFF["win_q"] + h * 64, qg, True, qT, "wq")
            attn_core(k, qT, kT, Vaug, plan, sk[:, h:h + 1], sk, yh, "wa")
            store_head(k, yh, k.ybr.ap()[1], h * 64, 0 if ctx_out else 2)


def phase_na(k, z, qn, kn, nab, ctx_out):
    P = k.P
    with P.scope():
        qg = bc_load(k, qn, 64, "nqg"); kg = bc_load(k, kn, 64, "nkg")
        qT = P.sbuf([64, T], BF16, "nqT"); kT = P.sbuf([64, T], BF16, "nkT")
        Vaug = P.sbuf([128, NT, 65], BF16, "nV")
        yh = P.sbuf([128, NT, 64], F32, "nyh")
        bst = P.sbuf([128, 21, 128], F32, "nbst")
        eb = P.sbuf([128, 21, 128], BF16, "neb")
        for h in range(8):
            P.dma("sp", bst[:], nab[h], writes=[bst])
            P.op("act", lambda e: e.activation(eb[:], bst[:], AF.Exp), reads=[bst], writes=[eb])
            plan = []
            if ctx_out:
                plan += [(qt, [(0, None, None), (1, None, None)]) for qt in range(2)]
            for p in range(32):
                bl = [(2 + blk, eb[:, ci, :], eb) for blk, ci in na_plan(p)]
                bl += [(0, None, None), (1, None, None)]
                plan.append((2 + p, bl))
            attn_prep_qk(k, z, ZOFF["na_k"] + h * 64, kg, False, kT, "nk")
            attn_prep_v(k, z, ZOFF["na_v"] + h * 64, Vaug, "nv")
            attn_prep_qk(k, z, ZOFF["na_q"] + h * 64, qg, False, qT, "nq")
            attn_core(k, qT, kT, Vaug, plan, None, None, yh, "na")
            store_head(k, yh, k.ybr.ap()[2], h * 64, 0 if ctx_out else 2)


def ret_consts(c):
    j = np.arange(128)[:, None].astype(np.float32); i = np.arange(128)[None, :].astype(np.float32)
    c["dpos"] = np.stack([np.maximum(i - j, 0), np.maximum(j - i, 0)], 1).astype(np.float32)
    c["dmsk"] = np.stack([(i >= j), (j >= i)], 1).astype(np.float32)
    c["rowidx"] = np.stack([np.broadcast_to(i + 1, (128, 128)), np.broadcast_to(128 - i, (128, 128))], 1).astype(np.float32)
    c["colidx"] = np.concatenate([127 - j, j], 1).astype(np.float32)


def phase_ret(k, z, decay, gn, ctx_out):
    P = k.P
    with P.scope():
        dpos = P.sbuf([128, 2, 128], F32, "r_dpos"); dmsk = P.sbuf([128, 2, 128], F32, "r_dmsk")
        rowidx = P.sbuf([128, 2, 128], F32, "r_rowidx"); colidx = P.sbuf([128, 2], F32, "r_colidx")
        for t_, n_ in ((dpos, "dpos"), (dmsk, "dmsk"), (rowidx, "rowidx"), (colidx, "colidx")):
            P.dma("sp", t_[:], k.cd[n_].ap(), writes=[t_])
        lg = bc_load(k, decay, 8, "r_lg")
        P.op("act", lambda e: e.activation(lg[:], lg[:], AF.Exp, scale=-1.0), reads=[lg], writes=[lg])
        P.op("act", lambda e: e.activation(lg[:], lg[:], AF.Ln, bias=k.one_col[:]), reads=[lg, k.one_col], writes=[lg])
        P.op("dve", lambda e: e.tensor_scalar(lg[:], lg[:], -1.0, None, ALU.mult), reads=[lg], writes=[lg])
        dm = P.sbuf([128, 8, 128], F32, "r_dm"); qdT = P.sbuf([128, 8, 128], F32, "r_qdT")
        kdec = P.sbuf([128, 8], F32, "r_kdec"); cdec = P.sbuf([128, 8], F32, "r_cdec")
        for dr in range(2):
            for h in range(4):
                c = dr * 4 + h
                P.op("act", lambda e, dr=dr, c=c: e.activation(dm[:, c, :], dpos[:, dr, :], AF.Exp, scale=lg[:, c:c + 1]), reads=[dpos, lg], writes=[dm])
                P.op("dve", lambda e, dr=dr, c=c: e.tensor_tensor(dm[:, c, :], dm[:, c, :], dmsk[:, dr, :], ALU.mult), reads=[dm, dmsk], writes=[dm])
                P.op("act", lambda e, dr=dr, c=c: e.activation(qdT[:, c, :], rowidx[:, dr, :], AF.Exp, scale=lg[:, c:c + 1]), reads=[rowidx, lg], writes=[qdT])
                P.op("act", lambda e, dr=dr, c=c: e.activation(kdec[:, c:c + 1], colidx[:, dr:dr + 1], AF.Exp, scale=lg[:, c:c + 1]), reads=[colidx, lg], writes=[kdec])
        P.op("act", lambda e: e.activation(cdec[:], lg[:], AF.Exp, scale=128.0), reads=[lg], writes=[cdec])
        gnb = bc_load(k, gn, 512, "r_gn")
        q = P.sbuf([128, NT, 64], F32, "r_q"); kk_ = P.sbuf([128, NT, 64], F32, "r_k")
        v = P.sbuf([128, NT, 128], F32, "r_v"); gt = P.sbuf([128, NT, 128], F32, "r_g")
        qT = P.sbuf([64, T], F32, "r_qT"); kT = P.sbuf([64, T], F32, "r_kT")
        qTd = P.sbuf([64, T], F32, "r_qTd"); kd = P.sbuf([128, NT, 64], F32, "r_kd")
        of = P.sbuf([128, NT, 128], F32, "r_of"); ot = P.sbuf([128, NT, 128], F32, "r_ot")
        S = [P.sbuf([64, 128], F32, "r_S%d" % i) for i in range(2)]
        pss = [P.psum([128, 128], F32, "r_pss%d" % i) for i in range(2)]
        pso = [P.psum([128, 128], F32, "r_pso%d" % i) for i in range(2)]
        pkv = [P.psum([64, 128], F32, "r_pkv%d" % i) for i in range(2)]
        sm = [P.sbuf([128, 128], F32, "r_sm%d" % i) for i in range(2)]
        st = P.sbuf([128, NT, 4], F32, "r_st")

        def ld(dst, col, w):
            zv = z.ap()[:, col:col + w].rearrange("(t p) c -> p t c", p=128)
            P.dma("sp", dst[:, 0:17, :], zv[:, 0:17, :], writes=[dst])
            P.dma("sp", dst[:, 17:34, :], zv[:, 17:34, :], writes=[dst])

        for h in range(4):
            ld(q, ZOFF["ret_q"] + h * 64, 64); ld(kk_, ZOFF["ret_k"] + h * 64, 64)
            ld(v, ZOFF["ret_v"] + h * 128, 128); ld(gt, ZOFF["ret_g"] + h * 128, 128)
            rope_apply(k, q, 1, "r_rq"); rope_apply(k, kk_, 1, "r_rk")
            P.op("pool", lambda e: e.tensor_scalar(kk_[:], kk_[:], 0.125, None, ALU.mult), reads=[kk_], writes=[kk_])
            transpose_to(k, lambda t: q[:, t, :], 64, qT, "r_tq", src_bufs=[q])
            transpose_to(k, lambda t: kk_[:, t, :], 64, kT, "r_tk", src_bufs=[kk_])
            it = 0
            for dr in range(2):
                c = dr * 4 + h
                P.op("dve", lambda e, c=c: e.tensor_tensor(qTd[:].rearrange("p (t i) -> p t i", i=128), qT[:].rearrange("p (t i) -> p t i", i=128),
                                                           qdT[0:64, c, :].unsqueeze(1).to_broadcast([64, NT, 128]), ALU.mult), reads=[qT, qdT], writes=[qTd])
                P.op("pool", lambda e, c=c: e.tensor_scalar(kd[:], kk_[:], kdec[:, c:c + 1], None, ALU.mult), reads=[kk_, kdec], writes=[kd])
                order = list(range(NT)) if dr == 0 else [1, 0] + list(range(NT - 1, 1, -1))
                Scur = None

                def stage_a(idx, c=c, order=order, it0=it):
                    n = order[idx]
                    ps, smb = pss[(it0 + idx) % 2], sm[(it0 + idx) % 2]
                    sl = slice(n * 128, (n + 1) * 128)
                    P.op("pe", lambda e, ps=ps, sl=sl: e.matmul(ps[:], kT[:, sl], qT[:, sl], start=True, stop=True), reads=[kT, qT], writes=[ps])
                    P.op("dve", lambda e, ps=ps, smb=smb, c=c: e.tensor_tensor(smb[:], ps[:], dm[:, c, :], ALU.mult), reads=[ps, dm], writes=[smb])

                stage_a(0)
                for idx, n in enumerate(order):
                    if idx + 1 < len(order):
                        stage_a(idx + 1)
                    ps, po, pk, smb = pss[it % 2], pso[it % 2], pkv[it % 2], sm[it % 2]
                    Snew = S[it % 2]
                    it += 1
                    sl = slice(n * 128, (n + 1) * 128)
                    P.op("pe", lambda e, po=po, smb=smb, n=n, last=(Scur is None): e.matmul(po[:], smb[:], v[:, n, :], start=True, stop=last), reads=[smb, v], writes=[po])
                    if Scur is not None:
                        P.op("pe", lambda e, po=po, sl=sl, Scur=Scur: e.matmul(po[:], qTd[:, sl], Scur[:], start=False, stop=True), reads=[qTd, Scur], writes=[po])
                    P.op("pe", lambda e, pk=pk, n=n: e.matmul(pk[:], kd[:, n, :], v[:, n, :], start=True, stop=True), reads=[kd, v], writes=[pk])
                    if Scur is None:
                        P.op("act", lambda e, pk=pk, Snew=Snew: e.copy(Snew[:], pk[:]), reads=[pk], writes=[Snew])
                    else:
                        P.op("dve", lambda e, pk=pk, Snew=Snew, Scur=Scur, c=c: e.scalar_tensor_tensor(Snew[:], Scur[:], cdec[0:64, c:c + 1], pk[:], ALU.mult, ALU.add),
                             reads=[pk, Scur, cdec], writes=[Snew])
                    Scur = Snew
                    if dr == 0:
                        P.op("act", lambda e, po=po, n=n: e.copy(of[:, n, :], po[:]), reads=[po], writes=[of])
                    else:
                        P.op("dve", lambda e, po=po, n=n: e.tensor_tensor(ot[:, n, :], po[:], of[:, n, :], ALU.add), reads=[po, of], writes=[ot])
            s1 = st[:, :, 0]; s2 = st[:, :, 1]; mu = st[:, :, 2]; rs = st[:, :, 3]
            bshape = [128, NT, 128]
            P.op("dve", lambda e: e.tensor_reduce(s1, ot[:], AX.X, ALU.add), reads=[ot], writes=[st])
            P.op("pool", lambda e: e.tensor_tensor(of[:], ot[:], ot[:], ALU.mult), reads=[ot], writes=[of])
            P.op("dve", lambda e: e.tensor_reduce(s2, of[:], AX.X, ALU.add), reads=[of], writes=[st])
            P.op("dve", lambda e: e.tensor_scalar(mu, s1, 1.0 / 128, None, ALU.mult), reads=[st], writes=[st])
            P.op("dve", lambda e: e.tensor_tensor(s1, mu, mu, ALU.mult), reads=[st], writes=[st])
            P.op("dve", lambda e: e.scalar_tensor_tensor(rs, s2, 1.0 / 128, s1, ALU.mult, ALU.subtract), reads=[st], writes=[st])
            P.op("act", lambda e: e.activation(rs, rs, AF.Sqrt, bias=k.eps_col[:]), reads=[st, k.eps_col], writes=[st])
            P.op("dve", lambda e: e.reciprocal(rs, rs), reads=[st], writes=[st])
            P.op("dve", lambda e: e.tensor_tensor(ot[:], ot[:], mu.unsqueeze(2).to_broadcast(bshape), ALU.subtract), reads=[ot, st], writes=[ot])
            P.op("dve", lambda e: e.tensor_tensor(ot[:], ot[:], rs.unsqueeze(2).to_broadcast(bshape), ALU.mult), reads=[ot, st], writes=[ot])
            P.op("pool", lambda e, h=h: e.tensor_tensor(ot[:], ot[:], gnb[:, h * 128:(h + 1) * 128].unsqueeze(1).to_broadcast(bshape), ALU.mult), reads=[ot, gnb], writes=[ot])
            P.op("act", lambda e: e.activation(gt[:], gt[:], AF.Silu), reads=[gt], writes=[gt])
            P.op("dve", lambda e: e.tensor_tensor(ot[:], ot[:], gt[:], ALU.mult), reads=[ot, gt], writes=[ot])
            store_head(k, ot, k.ybr.ap()[0], h * 128, 0 if ctx_out else 2)


TGROUPS = [[0, 1]] + [list(range(2 + 4 * g, 6 + 4 * g)) for g in range(8)]


def load_big_w(k, dst, dview_fn, src_fn, nblk, st):
    P = k.P
    for i in range(nblk):
        s = st[i % len(st)]
        sap = src_fn(i)
        shp = sap.shape
        sv = s[:, 0:shp[1], 0:shp[2]]
        P.dma("sp", sv, sap, writes=[s])
        P.op("pool", lambda e, i=i, sv=sv: e.tensor_copy(dview_fn(i), sv), reads=[s], writes=[dst])


def phase_merge(k, hT_d, x, xmid, w_merge, w_branch, w_out, modv, ctx_out):
    P = k.P
    with P.scope():
        wm = P.sbuf([128, 8, 4096], BF16, "m_wm"); wb = P.sbuf([128, 4, 4, 1024], BF16, "m_wb"); wo = P.sbuf([128, 8, 1024], BF16, "m_wo")
        with P.scope():
            st = [P.sbuf([128, 8, 512], F32, "m_st%d" % i) for i in range(2)]
            wmv = w_merge.rearrange("(k p) n -> p k n", p=128)
            load_big_w(k, wm, lambda i: wm[:, :, i * 512:(i + 1) * 512], lambda i: wmv[:, :, i * 512:(i + 1) * 512], 8, st)
            wbv = w_branch.rearrange("b (k p) n -> p b k n", p=128)
            load_big_w(k, wb, lambda i: wb[:, i // 2, :, (i % 2) * 512:(i % 2 + 1) * 512], lambda i: wbv[:, i // 2, :, (i % 2) * 512:(i % 2 + 1) * 512], 8, st)
            wov = w_out.rearrange("(k p) n -> p k n", p=128)
            load_big_w(k, wo, lambda i: wo[:, :, i * 512:(i + 1) * 512], lambda i: wov[:, :, i * 512:(i + 1) * 512], 2, st)
        g1bc = P.sbuf([128, 2, 1024], F32, "m_g1")
        for r in range(2):
            P.dma("sp", g1bc[:, r, :], modv.ap()[r, 2048:3072].partition_broadcast(128), writes=[g1bc])
        hT = [P.sbuf([128, 8, 512], BF16, "m_hT%d" % i) for i in range(2)]
        yin = [P.sbuf([128, 512], F32, "m_yin%d" % i) for i in range(3)]
        yT = P.sbuf([128, 4, 4, 512], BF16, "m_yT")
        accT = P.sbuf([128, 8, 512], BF16, "m_accT")
        acc = P.sbuf([128, 512], F32, "m_acc"); tmp = P.sbuf([128, 512], F32, "m_tmp"); sg = [P.sbuf([128, 512], F32, "m_sg%d" % i) for i in range(2)]
        xt = [P.sbuf([128, 1024], F32, "m_xt%d" % i) for i in range(2)]
        xo = [P.sbuf([128, 1024], F32, "m_xo%d" % i) for i in range(2)]
        pg = [P.psum([128, 512], F32, "m_pg%d" % i) for i in range(2)]
        pb = [P.psum([128, 512], F32, "m_pb%d" % i) for i in range(2)]
        po = [P.psum([128, 1024], F32, "m_po%d" % i) for i in range(2)]
        yi = 0; it = 0; oi = 0
        for gi, grp in enumerate(TGROUPS):
            if not ctx_out and grp[0] < 2:
                continue
            r = 0 if grp[0] < 2 else 1
            n = len(grp) * 128; tok0 = grp[0] * 128
            hb = hT[gi % 2]
            P.dma("sp", hb[:, :, 0:n], hT_d.ap().rearrange("(k p) t -> p k t", p=128)[:, :, tok0:tok0 + n], writes=[hb])
            for i in range(4):
                for j, t in enumerate(grp):
                    yb = yin[yi % 3]; yi += 1
                    P.dma("sp", yb[:], k.ybr.ap()[i, t * 128:(t + 1) * 128, :], reads=[k.ybr], writes=[yb])
                    pt = po[yi % 2]
                    for kk in range(4):
                        P.op("pe", lambda e, pt=pt, yb=yb, kk=kk: e.transpose(pt[:, kk * 128:(kk + 1) * 128], yb[:, kk * 128:(kk + 1) * 128], k.ident_f[:]),
                             reads=[yb, k.ident_f], writes=[pt])
                    eng = "act" if yi % 2 else "dve"
                    if eng == "act":
                        P.op("act", lambda e, pt=pt, i=i, j=j: e.copy(yT[:, i, :, j * 128:(j + 1) * 128], pt[:, 0:512].rearrange("p (k t) -> p k t", t=128)), reads=[pt], writes=[yT])
                    else:
                        P.op("dve", lambda e, pt=pt, i=i, j=j: e.tensor_copy(yT[:, i, :, j * 128:(j + 1) * 128], pt[:, 0:512].rearrange("p (k t) -> p k t", t=128)), reads=[pt], writes=[yT])
            for fc in range(8):
                for i in range(4):
                    g_, b_, s_ = pg[it % 2], pb[it % 2], sg[it % 2]; it += 1
                    for kk in range(8):
                        P.op("pe", lambda e, g_=g_, kk=kk, i=i, fc=fc, hb=hb, n=n: e.matmul(g_[:, 0:n], wm[:, kk, i * 1024 + fc * 128:i * 1024 + (fc + 1) * 128], hb[:, kk, 0:n],
                                                                                         start=(kk == 0), stop=(kk == 7)), reads=[wm, hb], writes=[g_])
                    for kk in range(4):
                        P.op("pe", lambda e, b_=b_, kk=kk, i=i, fc=fc, n=n: e.matmul(b_[:, 0:n], wb[:, i, kk, fc * 128:(fc + 1) * 128], yT[:, i, kk, 0:n],
                                                                                 start=(kk == 0), stop=(kk == 3)), reads=[wb, yT], writes=[b_])
                    P.op("act", lambda e, g_=g_, s_=s_, n=n: e.activation(s_[:, 0:n], g_[:, 0:n], AF.Sigmoid), reads=[g_], writes=[s_])
                    if i == 0:
                        P.op("dve", lambda e, s_=s_, b_=b_, n=n: e.tensor_tensor(acc[:, 0:n], s_[:, 0:n], b_[:, 0:n], ALU.mult), reads=[s_, b_], writes=[acc])
                    else:
                        P.op("dve", lambda e, s_=s_, b_=b_, n=n: e.tensor_tensor(tmp[:, 0:n], s_[:, 0:n], b_[:, 0:n], ALU.mult), reads=[s_, b_], writes=[tmp])
                        if i < 3:
                            P.op("pool", lambda e, n=n: e.tensor_tensor(acc[:, 0:n], acc[:, 0:n], tmp[:, 0:n], ALU.add), reads=[acc, tmp], writes=[acc])
                        else:
                            P.op("pool", lambda e, n=n, fc=fc: e.tensor_tensor(accT[:, fc, 0:n], acc[:, 0:n], tmp[:, 0:n], ALU.add), reads=[acc, tmp], writes=[accT])
            for j, t in enumerate(grp):
                o_ = po[oi % 2]; xb = xt[oi % 2]; xob = xo[oi % 2]; oi += 1
                P.dma("sp", xb[:], x.ap()[t * 128:(t + 1) * 128, :], reads=[x], writes=[xb])
                for half in range(2):
                    for fc in range(8):
                        P.op("pe", lambda e, o_=o_, half=half, fc=fc, j=j: e.matmul(o_[:, half * 512:(half + 1) * 512], accT[:, fc, j * 128:(j + 1) * 128], wo[:, fc, half * 512:(half + 1) * 512],
                                                                                start=(fc == 0), stop=(fc == 7)), reads=[accT, wo], writes=[o_])
                P.op("dve", lambda e, o_=o_, xob=xob, r=r: e.tensor_tensor(xob[:], o_[:], g1bc[:, r, :], ALU.mult), reads=[o_, g1bc], writes=[xob])
                P.op("pool", lambda e, xob=xob, xb=xb: e.tensor_tensor(xob[:], xob[:], xb[:], ALU.add), reads=[xob, xb], writes=[xob])
                P.dma("sp", xmid.ap()[t * 128:(t + 1) * 128, :], xob[:], reads=[xob], writes=[xmid])


def phase_router(k, h32_d, w_router, rbias, wgt, t_lo):
    P = k.P
    with P.scope():
        wr = P.sbuf([128, 8, 32], F32, "rt_w")
        P.dma("sp", wr[:], w_router.rearrange("(k p) n -> p k n", p=128), writes=[wr])
        rb = bc_load(k, rbias, 32, "rt_b")
        hs = [P.sbuf([128, 8, 128], F32, "rt_h%d" % i) for i in range(2)]
        ps = [P.psum([128, 32], F32, "rt_ps%d" % i) for i in range(2)]
        sc = P.sbuf([128, 32], F32, "rt_sc"); sel = P.sbuf([128, 32], F32, "rt_sel")
        w8 = P.sbuf([128, 10, 8], F32, "rt_w8"); msk = P.sbuf([128, 32], F32, "rt_msk"); c1 = P.sbuf([128, 2], F32, "rt_c1")
        hv = h32_d.ap().rearrange("(k p) t -> p k t", p=128)
        for t in range(t_lo, NT):
            hb = hs[t % 2]; p_ = ps[t % 2]
            P.dma("sp", hb[:], hv[:, :, t * 128:(t + 1) * 128], reads=[h32_d], writes=[hb])
            for kk in range(8):
                P.op("pe", lambda e, p_=p_, hb=hb, kk=kk: e.matmul(p_[:], hb[:, kk, :], wr[:, kk, :], start=(kk == 0), stop=(kk == 7)), reads=[hb, wr], writes=[p_])
            P.op("act", lambda e, p_=p_: e.activation(sc[:], p_[:], AF.Sigmoid), reads=[p_], writes=[sc])
            P.op("dve", lambda e: e.tensor_tensor(sel[:], sc[:], rb[:], ALU.add), reads=[sc, rb], writes=[sel])
            s4 = sel[:].rearrange("p (g e) -> p g e", e=4)
            a, b, c, d_ = s4[:, :, 0], s4[:, :, 1], s4[:, :, 2], s4[:, :, 3]
            W = lambda i: w8[:, i, :]
            seq = [(W(0), a, b, ALU.max), (W(1), a, b, ALU.min), (W(2), c, d_, ALU.max), (W(3), c, d_, ALU.min),
                   (W(4), W(0), W(2), ALU.max), (W(5), W(0), W(2), ALU.min), (W(6), W(1), W(3), ALU.max),
                   (W(7), W(5), W(6), ALU.max), (W(8), W(4), W(7), ALU.add)]
            for o, i0, i1, op in seq:
                P.op("dve", lambda e, o=o, i0=i0, i1=i1, op=op: e.tensor_tensor(o, i0, i1, op), reads=[sel, w8], writes=[w8])
            P.op("dve", lambda e: e.tensor_reduce(c1[:, 0:1], W(8), AX.X, ALU.max), reads=[w8], writes=[c1])
            P.op("dve", lambda e: e.tensor_scalar(W(9), W(8), c1[:, 0:1], None, ALU.is_ge), reads=[w8, c1], writes=[w8])
            m4 = msk[:].rearrange("p (g e) -> p g e", e=4)
            P.op("dve", lambda e: e.tensor_tensor(m4, s4, W(7).unsqueeze(2).to_broadcast([128, 8, 4]), ALU.is_ge), reads=[sel, w8], writes=[msk])
            P.op("dve", lambda e: e.tensor_tensor(m4, m4, W(9).unsqueeze(2).to_broadcast([128, 8, 4]), ALU.mult), reads=[msk, w8], writes=[msk])
            P.op("dve", lambda e: e.tensor_tensor(msk[:], msk[:], sc[:], ALU.mult), reads=[msk, sc], writes=[msk])
            P.op("dve", lambda e: e.tensor_reduce(c1[:, 1:2], msk[:], AX.X, ALU.add), reads=[msk], writes=[c1])
            P.op("dve", lambda e: e.reciprocal(c1[:, 1:2], c1[:, 1:2]), reads=[c1], writes=[c1])
            P.op("dve", lambda e, t=t: e.tensor_scalar(wgt[:, t, :], msk[:], c1[:, 1:2], None, ALU.mult), reads=[msk, c1], writes=[wgt])


def phase_moe(k, h2T_d, wgt, xmid, xout, weg, weu, wed, modv, t_lo, out_lat=None, nexp=32):
    P = k.P
    parts = [list(range(0, 12)), list(range(12, 23)), list(range(23, 34))]
    with P.scope():
        g2bc = P.sbuf([128, 2, 1024], F32, "e_g2")
        for r in range(2):
            P.dma("sp", g2bc[:, r, :], modv.ap()[r, 5120:6144].partition_broadcast(128), writes=[g2bc])
        acc = P.sbuf([128, 12, 1024], F32, "e_acc")
        hT = P.sbuf([128, 8, 12 * 128], BF16, "e_hT")
        stg = [P.sbuf([128, 8, 512], F32, "e_stg%d" % i) for i in range(2)]
        wg = [P.sbuf([128, 8, 512], BF16, "e_wg%d" % i) for i in range(2)]
        wu = [P.sbuf([128, 8, 512], BF16, "e_wu%d" % i) for i in range(2)]
        wd = [P.sbuf([128, 4, 1024], BF16, "e_wd%d" % i) for i in range(2)]
        hid = [P.sbuf([128, 4, 512], BF16, "e_hid%d" % i) for i in range(2)]
        sl = [P.sbuf([128, 512], F32, "e_sl%d" % i) for i in range(2)]
        pg = [P.psum([128, 512], F32, "e_pg%d" % i) for i in range(2)]
        pu = [P.psum([128, 512], F32, "e_pu%d" % i) for i in range(2)]
        po = [P.psum([128, 1024], F32, "e_po%d" % i) for i in range(2)]
        xt = [P.sbuf([128, 1024], F32, "e_xt%d" % i) for i in range(2)]
        hv = h2T_d.ap().rearrange("(k p) t -> p k t", p=128)
        si = 0; ci = 0; oi = 0; gi = 0
        for part in parts:
            tiles = [t for t in part if t >= t_lo]
            if not tiles:
                continue
            nt = len(tiles); tok0 = tiles[0] * 128; ntok = nt * 128
            P.dma("sp", hT[:, :, 0:ntok], hv[:, :, tok0:tok0 + ntok], reads=[h2T_d], writes=[hT])
            for e_ in range(nexp):
                wgb, wub, wdb = wg[e_ % 2], wu[e_ % 2], wd[e_ % 2]
                for dst, src in ((wgb, weg[e_].rearrange("(k p) n -> p k n", p=128)), (wub, weu[e_].rearrange("(k p) n -> p k n", p=128))):
                    s = stg[si % 2]; si += 1
                    P.dma("sp", s[:], src, writes=[s])
                    P.op("pool", lambda e, dst=dst, s=s: e.tensor_copy(dst[:], s[:]), reads=[s], writes=[dst])
                s = stg[si % 2]; si += 1
                sv = s[:].rearrange("p k n -> p (k n)").rearrange("p (k n) -> p k n", n=1024)
                P.dma("sp", sv, wed[e_].rearrange("(k p) n -> p k n", p=128), writes=[s])
                P.op("pool", lambda e, wdb=wdb, sv=sv: e.tensor_copy(wdb[:], sv), reads=[s], writes=[wdb])
                for g0 in range(0, nt, 4):
                    gt_ = tiles[g0:g0 + 4]; n = len(gt_) * 128; off = g0 * 128
                    hb = hid[gi % 2]; gi += 1
                    for c in range(4):
                        g_, u_, s_ = pg[ci % 2], pu[ci % 2], sl[ci % 2]; ci += 1
                        for kk in range(8):
                            P.op("pe", lambda e, g_=g_, kk=kk, c=c, wgb=wgb, off=off, n=n: e.matmul(g_[:, 0:n], wgb[:, kk, c * 128:(c + 1) * 128], hT[:, kk, off:off + n],
                                                                                                 start=(kk == 0), stop=(kk == 7)), reads=[wgb, hT], writes=[g_])
                        for kk in range(8):
                            P.op("pe", lambda e, u_=u_, kk=kk, c=c, wub=wub, off=off, n=n: e.matmul(u_[:, 0:n], wub[:, kk, c * 128:(c + 1) * 128], hT[:, kk, off:off + n],
                                                                                                 start=(kk == 0), stop=(kk == 7)), reads=[wub, hT], writes=[u_])
                        P.op("act", lambda e, g_=g_, s_=s_, n=n: e.activation(s_[:, 0:n], g_[:, 0:n], AF.Silu), reads=[g_], writes=[s_])
                        P.op("dve", lambda e, s_=s_, u_=u_, hb=hb, c=c, n=n: e.tensor_tensor(hb[:, c, 0:n], s_[:, 0:n], u_[:, 0:n], ALU.mult), reads=[s_, u_], writes=[hb])
                    for j, t in enumerate(gt_):
                        o_ = po[oi % 2]; oi += 1
                        for half in range(2):
                            for c in range(4):
                                P.op("pe", lambda e, o_=o_, half=half, c=c, j=j, hb=hb, wdb=wdb: e.matmul(o_[:, half * 512:(half + 1) * 512], hb[:, c, j * 128:(j + 1) * 128],
                                                                                                       wdb[:, c, half * 512:(half + 1) * 512], start=(c == 0), stop=(c == 3)),
                                     reads=[hb, wdb], writes=[o_])
                        ai = g0 + j
                        if e_ == 0:
                            P.op("dve", lambda e, o_=o_, ai=ai, t=t: e.tensor_scalar(acc[:, ai, :], o_[:], wgt[:, t, 0:1], None, ALU.mult), reads=[o_, wgt], writes=[acc])
                        else:
                            P.op("dve", lambda e, o_=o_, ai=ai, t=t, e_=e_: e.scalar_tensor_tensor(acc[:, ai, :], o_[:], wgt[:, t, e_:e_ + 1], acc[:, ai, :], ALU.mult, ALU.add),
                                 reads=[o_, wgt, acc], writes=[acc])
            for ai, t in enumerate(tiles):
                r = 0 if t < 2 else 1
                xb = xt[ai % 2]
                P.dma("sp", xb[:], xmid.ap()[t * 128:(t + 1) * 128, :], reads=[xmid], writes=[xb])
                P.op("pool", lambda e, ai=ai, r=r: e.tensor_tensor(acc[:, ai, :], acc[:, ai, :], g2bc[:, r, :], ALU.mult), reads=[acc, g2bc], writes=[acc])
                P.op("pool", lambda e, ai=ai, xb=xb: e.tensor_tensor(xb[:], xb[:], acc[:, ai, :], ALU.add), reads=[acc, xb], writes=[xb])
                if out_lat is not None:
                    if t >= 2:
                        P.dma("sp", out_lat.ap()[(t - 2) * 128:(t - 1) * 128, :], xb[:], reads=[xb], writes=[out_lat])
                else:
                    P.dma("sp", xout.ap()[t * 128:(t + 1) * 128, :], xb[:], reads=[xb], writes=[xout])


def gdn_consts(c):
    t = np.arange(128)[:, None]; i = np.arange(128)[None, :]
    sb = (t // 64) == (i // 64)
    c["g_tri"] = np.stack([(t <= i) & sb, (t >= i) & sb], 1).astype(np.float32)
    c["g_triS"] = np.stack([(t > i) & sb, (t < i) & sb], 1).astype(np.float32)
    c["g_mS"] = np.stack([(i > t) & sb, (i < t) & sb], 1).astype(np.float32)
    c["g_blk"] = np.concatenate([sb.astype(np.float32)[:, None, :],
                                 np.broadcast_to((t < 64), (128, 128)).astype(np.float32)[:, None, :],
                                 np.broadcast_to((t >= 64), (128, 128)).astype(np.float32)[:, None, :]], 1)
    c["zeros"] = np.zeros((1, 128), np.float32)


def phase_gdn(k, z, conv_w, a_log, dt_bias, gnorm, ctx_out):
    P = k.P
    with P.scope():
        tri = P.sbuf([128, 2, 128], F32, "g_tri"); triS = P.sbuf([128, 2, 128], F32, "g_triS")
        mS = P.sbuf([128, 2, 128], F32, "g_mS"); blk = P.sbuf([128, 3, 128], F32, "g_blk")
        for t_, n_ in ((tri, "g_tri"), (triS, "g_triS"), (mS, "g_mS"), (blk, "g_blk")):
            P.dma("sp", t_[:], k.cd[n_].ap(), writes=[t_])
        gnb = bc_load(k, gnorm, 128, "g_gn")
        gall = P.sbuf([128, NT, 8], F32, "g_gall"); ball = P.sbuf([128, NT, 8], F32, "g_ball")
        with P.scope():
            ab = P.sbuf([128, NT, 16], F32, "g_ab")
            zv = z.ap()[:, 5888:5904].rearrange("(t p) c -> p t c", p=128)
            P.dma("sp", ab[:, 0:17, :], zv[:, 0:17, :], writes=[ab]); P.dma("sp", ab[:, 17:34, :], zv[:, 17:34, :], writes=[ab])
            al = bc_load(k, a_log, 8, "g_al"); db = bc_load(k, dt_bias, 8, "g_db")
            P.op("act", lambda e: e.activation(al[:], al[:], AF.Exp), reads=[al], writes=[al])
            P.op("dve", lambda e: e.tensor_scalar(al[:], al[:], -1.0, None, ALU.mult), reads=[al], writes=[al])
            P.op("dve", lambda e: e.tensor_tensor(gall[:], ab[:, :, 0:8], db[:].unsqueeze(1).to_broadcast([128, NT, 8]), ALU.add), reads=[ab, db], writes=[gall])
            P.op("act", lambda e: e.activation(gall[:], gall[:], AF.Exp), reads=[gall], writes=[gall])
            P.op("act", lambda e: e.activation(gall[:], gall[:], AF.Ln, bias=k.one_col[:]), reads=[gall, k.one_col], writes=[gall])
            P.op("dve", lambda e: e.tensor_tensor(gall[:], gall[:], al[:].unsqueeze(1).to_broadcast([128, NT, 8]), ALU.mult), reads=[gall, al], writes=[gall])
            P.op("act", lambda e: e.activation(ball[:], ab[:, :, 8:16], AF.Sigmoid), reads=[ab], writes=[ball])
        q = P.sbuf([128, NT, 128], F32, "g_q"); kk_ = P.sbuf([128, NT, 128], F32, "g_k"); v = P.sbuf([128, NT, 128], F32, "g_v")
        kT = P.sbuf([128, T], F32, "g_kT"); qT = P.sbuf([128, T], F32, "g_qT")
        od = [P.sbuf([128, NT, 128], F32, "g_od%d" % i) for i in range(2)]
        zview = z.ap().rearrange("(t p) c -> p t c", p=128)
        for h in range(4):
            with P.scope():
                xp = P.sbuf([128, NT, 128], F32, "g_xp"); xn = P.sbuf([128, NT, 128], F32, "g_xn"); wcb = P.sbuf([128, 3, 128], F32, "g_wc")
                ss = P.sbuf([128, NT], F32, "g_ss")
                for name, dst in (("gdn_q", q), ("gdn_k", kk_), ("gdn_v", v)):
                    col = ZOFF[name] + h * 128
                    zc = zview[:, :, col:col + 128]
                    for lo, hi in ((0, 17), (17, 34)):
                        P.dma("sp", dst[:, lo:hi, :], zc[:, lo:hi, :], writes=[dst])
                        P.dma("sp", xp[1:128, lo:hi, :], zc[0:127, lo:hi, :], writes=[xp])
                        P.dma("sp", xn[0:127, lo:hi, :], zc[1:128, lo:hi, :], writes=[xn])
                    P.dma("sp", xp[0:1, 1:34, :], zc[127:128, 0:33, :], writes=[xp])
                    P.dma("sp", xn[127:128, 0:33, :], zc[0:1, 1:34, :], writes=[xn])
                    for tt in (0, 2):
                        P.dma("sp", xp[0:1, tt, :], k.cd["zeros"].ap(), writes=[xp])
                    for tt in (1, 33):
                        P.dma("sp", xn[127:128, tt, :], k.cd["zeros"].ap(), writes=[xn])
                    for j in range(3):
                        P.dma("sp", wcb[:, j, :], conv_w[j, col - ZOFF["gdn_q"]:col - ZOFF["gdn_q"] + 128].partition_broadcast(128), writes=[wcb])
                    bs = [128, NT, 128]
                    P.op("dve", lambda e, dst=dst: e.tensor_tensor(dst[:], dst[:], wcb[:, 1, :].unsqueeze(1).to_broadcast(bs), ALU.mult), reads=[dst, wcb], writes=[dst])
                    P.op("pool", lambda e: e.tensor_tensor(xp[:], xp[:], wcb[:, 0, :].unsqueeze(1).to_broadcast(bs), ALU.mult), reads=[xp, wcb], writes=[xp])
                    P.op("dve", lambda e: e.tensor_tensor(xn[:], xn[:], wcb[:, 2, :].unsqueeze(1).to_broadcast(bs), ALU.mult), reads=[xn, wcb], writes=[xn])
                    P.op("pool", lambda e, dst=dst: e.tensor_tensor(dst[:], dst[:], xp[:], ALU.add), reads=[dst, xp], writes=[dst])
                    P.op("dve", lambda e, dst=dst: e.tensor_tensor(dst[:], dst[:], xn[:], ALU.add), reads=[dst, xn], writes=[dst])
                    P.op("act", lambda e, dst=dst: e.activation(dst[:], dst[:], AF.Silu), reads=[dst], writes=[dst])
                    if name != "gdn_v":
                        P.op("pool", lambda e, dst=dst: e.tensor_tensor(xp[:], dst[:], dst[:], ALU.mult), reads=[dst], writes=[xp])
                        P.op("dve", lambda e: e.tensor_reduce(ss[:], xp[:], AX.X, ALU.add), reads=[xp], writes=[ss])
                        P.op("act", lambda e: e.activation(ss[:], ss[:], AF.Sqrt, bias=k.eps_col[:]), reads=[ss, k.eps_col], writes=[ss])
                        P.op("dve", lambda e: e.reciprocal(ss[:], ss[:]), reads=[ss], writes=[ss])
                        if name == "gdn_q":
                            P.op("dve", lambda e: e.tensor_scalar(ss[:], ss[:], 128.0 ** -0.5, None, ALU.mult), reads=[ss], writes=[ss])
                        P.op("dve", lambda e, dst=dst: e.tensor_tensor(dst[:], dst[:], ss[:].unsqueeze(2).to_broadcast(bs), ALU.mult), reads=[dst, ss], writes=[dst])
            transpose_to(k, lambda t: q[:, t, :], 128, qT, "g_tq", src_bufs=[q])
            transpose_to(k, lambda t: kk_[:, t, :], 128, kT, "g_tk", src_bufs=[kk_])
            with P.scope():
                psO = [P.psum([128, 512], F32, "g_psO%d" % i) for i in range(2)]
                psW = [P.psum([128, 512], F32, "g_psW%d" % i) for i in range(2)]
                psM = [P.psum([128, 512], F32, "g_psM%d" % i) for i in range(4)]
                dirs = [gdn_dir(k, dr, h, q, kk_, v, qT, kT, gall, ball, tri, triS, mS, blk, od[dr], psO[dr], psW[dr], psM[2 * dr:2 * dr + 2]) for dr in range(2)]

                def run_all(gens):
                    alive = list(gens)
                    while alive:
                        for g_ in list(alive):
                            try:
                                next(g_)
                            except StopIteration:
                                alive.remove(g_)

                O_ = [dirs[dr][3] for dr in range(2)]
                run_all([dirs[dr][0](0, O_[dr][0]) for dr in range(2)])
                run_all([dirs[dr][1](0, O_[dr][0]) for dr in range(2)] + [dirs[dr][0](1, O_[dr][1]) for dr in range(2)])
                for i_ in range(NT):
                    gens = []
                    for dr in range(2):
                        p1, p2, sc, order_ = dirs[dr]
                        gens.append(sc(i_, order_[i_]))
                        if i_ + 1 < NT:
                            gens.append(p2(i_ + 1, order_[i_ + 1]))
                        if i_ + 2 < NT:
                            gens.append(p1(i_ + 2, order_[i_ + 2]))
                    run_all(gens)
            with P.scope():
                gt = P.sbuf([128, NT, 128], F32, "g_gate"); ss = P.sbuf([128, NT], F32, "g_fss"); sq = P.sbuf([128, NT, 128], F32, "g_fsq")
                zc = zview[:, :, ZOFF["gdn_g"] + h * 128:ZOFF["gdn_g"] + (h + 1) * 128]
                P.dma("sp", gt[:, 0:17, :], zc[:, 0:17, :], writes=[gt]); P.dma("sp", gt[:, 17:34, :], zc[:, 17:34, :], writes=[gt])
                o = od[0]; bs = [128, NT, 128]
                P.op("dve", lambda e: e.tensor_tensor(o[:], o[:], od[1][:], ALU.add), reads=[o, od[1]], writes=[o])
                P.op("pool", lambda e: e.tensor_tensor(sq[:], o[:], o[:], ALU.mult), reads=[o], writes=[sq])
                P.op("dve", lambda e: e.tensor_reduce(ss[:], sq[:], AX.X, ALU.add), reads=[sq], writes=[ss])
                P.op("act", lambda e: e.activation(ss[:], ss[:], AF.Sqrt, bias=k.eps_col[:], scale=1.0 / 128), reads=[ss, k.eps_col], writes=[ss])
                P.op("dve", lambda e: e.reciprocal(ss[:], ss[:]), reads=[ss], writes=[ss])
                P.op("dve", lambda e: e.tensor_tensor(o[:], o[:], ss[:].unsqueeze(2).to_broadcast(bs), ALU.mult), reads=[o, ss], writes=[o])
                P.op("pool", lambda e: e.tensor_tensor(o[:], o[:], gnb[:].unsqueeze(1).to_broadcast(bs), ALU.mult), reads=[o, gnb], writes=[o])
                P.op("act", lambda e: e.activation(gt[:], gt[:], AF.Silu), reads=[gt], writes=[gt])
                P.op("dve", lambda e: e.tensor_tensor(o[:], o[:], gt[:], ALU.mult), reads=[o, gt], writes=[o])
                store_head(k, o, k.ybr.ap()[3], h * 128, 0 if ctx_out else 2)


def gdn_dir(k, dr, h, q, kk_, v, qT, kT, gall, ball, tri, triS, mS, blk, od, psO, psW, psM):
    P = k.P
    c = dr * 4 + h
    tg = "g%d_" % dr
    A = lambda nm, shape=(128, 128), n=3: [P.sbuf(list(shape), F32, tg + nm + str(i)) for i in range(n)]
    gcs = P.sbuf([128, 4, NT], F32, tg + "gcs")
    cdb = P.sbuf([128, 2, NT], F32, tg + "cdb")
    gc = gall[:, :, c]; bt = ball[:, :, c]
    ident = k.ident_f
    pm = psM[0]
    P.op("pe", lambda e: e.matmul(pm[:, 0:NT], tri[:, dr, :], gc, start=True, stop=True), reads=[tri, gall], writes=[pm])
    P.op("pe", lambda e: e.matmul(pm[:, 64:64 + NT], blk[:, 0, :], gc, start=True, stop=True), reads=[blk, gall], writes=[pm])
    P.op("pe", lambda e: e.matmul(pm[:, 128:128 + NT], blk[:, 1, :], gc, start=True, stop=True), reads=[blk, gall], writes=[pm])
    P.op("pe", lambda e: e.matmul(pm[:, 192:192 + NT], blk[:, 2, :], gc, start=True, stop=True), reads=[blk, gall], writes=[pm])
    P.op("dve", lambda e: e.tensor_copy(gcs[:, 0, :], pm[:, 0:NT]), reads=[pm], writes=[gcs])
    P.op("act", lambda e: e.activation(gcs[:, 1, :], pm[:, 0:NT], AF.Exp), reads=[pm], writes=[gcs])
    P.op("dve", lambda e: e.tensor_tensor(gcs[:, 2, :], pm[:, 64:64 + NT], gcs[:, 0, :], ALU.subtract), reads=[pm, gcs], writes=[gcs])
    P.op("act", lambda e: e.activation(gcs[:, 2, :], gcs[:, 2, :], AF.Exp), reads=[gcs], writes=[gcs])
    P.op("act", lambda e: e.activation(cdb[:, 0, :], pm[:, 128:128 + NT], AF.Exp), reads=[pm], writes=[cdb])
    P.op("act", lambda e: e.activation(cdb[:, 1, :], pm[:, 192:192 + NT], AF.Exp), reads=[pm], writes=[cdb])
    kb, kbg, vb, kend, qd, gtri = A("kb"), A("kbg"), A("vb"), A("kend"), A("qd"), A("gtri")
    EDs, EDi, kbT, AT, aqkT = A("EDs"), A("EDi"), A("kbT"), A("AT"), A("aqkT")
    Ap, Bp, Pm = A("Ap", n=3), A("Bp", n=3), A("Pm", n=3)
    u, wT, vnew = A("u"), A("wT"), A("vnew")
    qdTm = [P.sbuf([128, 2, 128], F32, tg + "qdTm%d" % i) for i in range(3)]
    for b_ in qdTm:
        P.op("pool", lambda e, b_=b_: e.memset(b_[:], 0.0), writes=[b_])
    S = [P.sbuf([128, 128], F32, tg + "S%d" % i) for i in range(2)]
    P.op("pool", lambda e: e.memset(S[0][:], 0.0), writes=[S[0]])
    stt = {"S": S[0], "si": 0}
    order = list(range(NT)) if dr == 0 else [1, 0] + list(range(NT - 1, 1, -1))
    slot = [0]

    slots = [0, 0]

    def ms(w):
        s_ = slots[w]; slots[w] += 1
        bank = psM[w]; o_ = (s_ % 4) * 128
        return bank, bank[:, o_:o_ + 128]

    def prep(it, n):
        r2 = it % 3
        sl = slice(n * 128, (n + 1) * 128)
        kt, qt, vt = kk_[:, n, :], q[:, n, :], v[:, n, :]
        bcol = ball[:, n, c:c + 1]; gcol = gall[:, n, c:c + 1]
        eg = gcs[:, 1, n:n + 1]; ek = gcs[:, 2, n:n + 1]
        kb_, kbg_, vb_, kend_, qd_, gtri_ = kb[r2], kbg[r2], vb[r2], kend[r2], qd[r2], gtri[r2]
        P.op("dve", lambda e: e.tensor_scalar(kb_[:], kt, bcol, None, ALU.mult), reads=[kk_, ball], writes=[kb_]); yield
        P.op("pool", lambda e: e.tensor_scalar(kbg_[:], kb_[:], eg, None, ALU.mult), reads=[kb_, gcs], writes=[kbg_]); yield
        P.op("pool", lambda e: e.tensor_scalar(vb_[:], vt, bcol, None, ALU.mult), reads=[v, ball], writes=[vb_]); yield
        P.op("pool", lambda e: e.tensor_scalar(kend_[:], kt, ek, None, ALU.mult), reads=[kk_, gcs], writes=[kend_]); yield
        P.op("dve", lambda e: e.tensor_scalar(qd_[:], qt, eg, None, ALU.mult), reads=[q, gcs], writes=[qd_]); yield
        P.op("dve", lambda e: e.tensor_scalar(gtri_[:], tri[:, dr, :], gcol, None, ALU.mult), reads=[tri, gall], writes=[gtri_]); yield
        bk, p_ = ms(0)
        P.op("pe", lambda e: e.matmul(p_, triS[:, dr, :], gtri_[:], start=True, stop=True), reads=[triS, gtri_], writes=[bk]); yield
        EDs_, EDi_ = EDs[r2], EDi[r2]
        P.op("act", lambda e: e.activation(EDs_[:], p_, AF.Exp), reads=[bk], writes=[EDs_]); yield
        P.op("pool", lambda e: e.tensor_tensor(EDs_[:], EDs_[:], mS[:, dr, :], ALU.mult), reads=[EDs_, mS], writes=[EDs_]); yield
        P.op("pool", lambda e: e.tensor_tensor(EDi_[:], EDs_[:], ident[:], ALU.add), reads=[EDs_, ident], writes=[EDi_]); yield
        bk, p_ = ms(0); kbT_ = kbT[r2]
        P.op("pe", lambda e: e.transpose(p_, kb_[:], ident[:]), reads=[kb_, ident], writes=[bk]); yield
        P.op("act", lambda e: e.copy(kbT_[:], p_), reads=[bk], writes=[kbT_]); yield
        bk, p_ = ms(0); AT_ = AT[r2]
        P.op("pe", lambda e: e.matmul(p_, kT[:, sl], kbT_[:], start=True, stop=True), reads=[kT, kbT_], writes=[bk]); yield
        P.op("dve", lambda e: e.tensor_tensor(AT_[:], p_, EDs_[:], ALU.mult), reads=[bk, EDs_], writes=[AT_]); yield
        bk, p_ = ms(0); aqkT_ = aqkT[r2]
        P.op("pe", lambda e: e.matmul(p_, kT[:, sl], qT[:, sl], start=True, stop=True), reads=[kT, qT], writes=[bk]); yield
        P.op("dve", lambda e: e.tensor_tensor(aqkT_[:], p_, EDi_[:], ALU.mult), reads=[bk, EDi_], writes=[aqkT_]); yield

    def prep2(it, n):
        r2 = it % 3
        sl = slice(n * 128, (n + 1) * 128)
        kbg_, vb_, qd_ = kbg[r2], vb[r2], qd[r2]
        AT_ = AT[r2]
        bk, p_ = ms(1); pi = 0
        Ac, Bc, Pc = Ap[0], AT_, Pm[0]
        P.op("pe", lambda e: e.transpose(p_, AT_[:], ident[:]), reads=[AT_, ident], writes=[bk]); yield
        P.op("act", lambda e: e.copy(Ac[:], p_), reads=[bk], writes=[Ac]); yield
        P.op("dve", lambda e: e.tensor_tensor(Pc[:], ident[:], AT_[:], ALU.subtract), reads=[ident, AT_], writes=[Pc]); yield
        for lv in range(1, 6):
            An, Bn, Pn = Ap[lv % 3], Bp[lv % 3], Pm[lv % 3]
            bk, p_ = ms(1)
            P.op("pe", lambda e, p_=p_, Ac=Ac, Bc=Bc: e.matmul(p_, Bc[:], Ac[:], start=True, stop=True), reads=[Ac, Bc], writes=[bk]); yield
            P.op("act", lambda e, p_=p_, An=An: e.copy(An[:], p_), reads=[bk], writes=[An]); yield
            if lv < 5:
                bk2, p2 = ms(1)
                P.op("pe", lambda e, p2=p2, Ac=Ac, Bc=Bc: e.matmul(p2, Ac[:], Bc[:], start=True, stop=True), reads=[Ac, Bc], writes=[bk2]); yield
                P.op("dve", lambda e, p2=p2, Bn=Bn: e.tensor_copy(Bn[:], p2), reads=[bk2], writes=[Bn]); yield
            bk3, p3 = ms(1)
            P.op("pe", lambda e, p3=p3, Pc=Pc: e.matmul(p3, ident[:], Pc[:], start=True, stop=False), reads=[ident, Pc], writes=[bk3]); yield
            P.op("pe", lambda e, p3=p3, Pc=Pc, An=An: e.matmul(p3, An[:], Pc[:], start=False, stop=True), reads=[An, Pc], writes=[bk3]); yield
            P.op("dve", lambda e, p3=p3, Pn=Pn: e.tensor_copy(Pn[:], p3), reads=[bk3], writes=[Pn]); yield
            Ac, Bc, Pc = An, Bn, Pn
        MT = Pc
        u_, wT_, vnew_, qdTm_ = u[r2], wT[r2], vnew[r2], qdTm[r2]
        bk, p_ = ms(1)
        P.op("pe", lambda e: e.matmul(p_, MT[:], vb_[:], start=True, stop=True), reads=[MT, vb_], writes=[bk]); yield
        P.op("act", lambda e: e.copy(u_[:], p_), reads=[bk], writes=[u_]); yield
        bk, p_ = ms(1)
        P.op("pe", lambda e: e.matmul(p_, kbg_[:], MT[:], start=True, stop=True), reads=[MT, kbg_], writes=[bk]); yield
        P.op("dve", lambda e: e.tensor_copy(wT_[:], p_), reads=[bk], writes=[wT_]); yield
        bk, p_ = ms(1)
        P.op("pe", lambda e: e.transpose(p_, qd_[:], ident[:]), reads=[qd_, ident], writes=[bk]); yield
        P.op("act", lambda e: e.copy(qdTm_[:, 0, 0:64], p_[:, 0:64]), reads=[bk], writes=[qdTm_]); yield
        P.op("dve", lambda e: e.tensor_copy(qdTm_[:, 1, 64:128], p_[:, 64:128]), reads=[bk], writes=[qdTm_]); yield

    def scan(it, n):
        r2 = it % 3
        kend_, aqkT_, u_, wT_, vnew_, qdTm_ = kend[r2], aqkT[r2], u[r2], wT[r2], vnew[r2], qdTm[r2]
        Scur = stt["S"]; si = stt["si"]
        pO = psO[:, 0:128]
        for bi, b in enumerate((0, 1) if dr == 0 else (1, 0)):
            rb = slice(b * 64, (b + 1) * 64)
            pW = psW[:, 0:128]; pKV = psW[:, 128:256]
            Sn = S[(si + 1) % 2]
            P.op("pe", lambda e, Scur=Scur: e.matmul(pW, wT_[:], Scur[:], start=True, stop=True), reads=[wT_, Scur], writes=[psW]); yield
            P.op("dve", lambda e, rb=rb: e.tensor_tensor(vnew_[rb, :], u_[rb, :], pW[rb, :], ALU.subtract), reads=[u_, psW], writes=[vnew_]); yield
            P.op("pe", lambda e, b=b, bi=bi, Scur=Scur: e.matmul(pO, qdTm_[:, b, :], Scur[:], start=(bi == 0), stop=False), reads=[qdTm_, Scur], writes=[psO]); yield
            P.op("pe", lambda e, rb=rb, bi=bi: e.matmul(pO, aqkT_[rb, :], vnew_[rb, :], start=False, stop=(bi == 1)), reads=[aqkT_, vnew_], writes=[psO]); yield
            P.op("pe", lambda e, rb=rb: e.matmul(pKV, kend_[rb, :], vnew_[rb, :], start=True, stop=True), reads=[kend_, vnew_], writes=[psW]); yield
            P.op("dve", lambda e, b=b, Scur=Scur, Sn=Sn: e.scalar_tensor_tensor(Sn[:], Scur[:], cdb[:, b, n:n + 1], pKV, ALU.mult, ALU.add),
                 reads=[Scur, cdb, psW], writes=[Sn]); yield
            Scur = Sn; si += 1
        P.op("act", lambda e: e.copy(od[:, n, :], pO), reads=[psO], writes=[od]); yield
        stt["S"] = Scur; stt["si"] = si

    return prep, prep2, scan, order


from concourse.bass_utils import run_bass_kernel_spmd

DEPTH = 2
W_NAMES = ["w_mod", "b_mod", "norm1", "norm2", "w_in", "ret_decay", "ret_gn", "win_qnorm", "win_knorm", "win_sink",
           "na_qnorm", "na_knorm", "nab", "gdn_conv", "gdn_a_log", "gdn_dt_bias", "gdn_norm", "w_branch", "w_merge", "w_out",
           "w_router", "router_bias", "w_e_gate", "w_e_up", "w_e_down"]
W_SHAPES = dict(w_mod=[2, D, 6144], b_mod=[2, 6144], norm1=[2, D], norm2=[2, D], w_in=[2, D, DIN], ret_decay=[2, 8], ret_gn=[2, 512],
                win_qnorm=[2, 64], win_knorm=[2, 64], win_sink=[2, 8], na_qnorm=[2, 64], na_knorm=[2, 64], nab=[2, 8, 128, 21, 128],
                gdn_conv=[2, 3, 1536], gdn_a_log=[2, 8], gdn_dt_bias=[2, 8], gdn_norm=[2, 128], w_branch=[2, 4, 512, D],
                w_merge=[2, D, 4096], w_out=[2, D, D], w_router=[D, 32], router_bias=[32], w_e_gate=[2, 32, D, 512],
                w_e_up=[2, 32, D, 512], w_e_down=[2, 32, 512, D])


LAYERED = [n for n in W_NAMES if n not in ("w_router", "router_bias")]


def build_program(fused=True):
    nc = bass.Bass("TRN2", target_bir_lowering=False)
    P = Prog(nc)
    k = K(P)
    nl = DEPTH if fused else 1
    cd = {n: P.dram("c_" + n, list(a.shape), F32, kind="ExternalInput") for n, a in mk_consts().items()}
    xin = P.dram("xin", [T, D], F32, kind="ExternalInput")
    cc = P.dram("cc", [2, D], F32, kind="ExternalInput")
    W = {n: P.dram(n, (W_SHAPES[n] if (fused or n not in LAYERED) else W_SHAPES[n][1:]), F32, kind="ExternalInput") for n in W_NAMES}
    if fused:
        out = P.dram("out", [4096, D], F32, kind="ExternalOutput")
        x1 = P.dram("x1", [T, D], F32)
    else:
        out = None
        x1 = P.dram("xo", [T, D], F32, kind="ExternalOutput")
    modv = [P.dram("modv%d" % l, [2, 6144], F32) for l in range(nl)]
    hT_d = P.dram("hT_d", [D, T], BF16)
    z = P.dram("z", [T, DIN], F32)
    k.ybr = P.dram("ybr", [4, T, 512], F32)
    xmid = P.dram("xmid", [T, D], F32)
    h2T_d = P.dram("h2T_d", [D, T], BF16)
    h32_d = P.dram("h32_d", [D, T], F32)
    load_consts(k, cd)
    mT = [P.sbuf([128, 48, 2], F32, "mT%d" % l) for l in range(nl)]

    def wsub(n, l):
        return _Sub(W[n], l) if (fused and n in LAYERED) else W[n]

    for l in range(nl):
        phase_mod(k, cc, wsub("w_mod", l), wsub("b_mod", l), modv[l], mT[l])
    xs = [xin, x1]
    for l in range(nl):
        ctx_out = (l < DEPTH - 1) if fused else True
        x = xs[l]
        w = lambda n: wsub(n, l).ap()
        with P.scope():
            hsb = P.sbuf([128, 8, T], BF16, "hsb")
            phase_norm(k, x, w("norm1"), mT[l], 0, hsb)
            for kk in range(8):
                P.dma("sp", hT_d.ap()[kk * 128:(kk + 1) * 128, :], hsb[:, kk, :], reads=[hsb], writes=[hT_d])
            phase_proj(k, hsb, w("w_in"), z, DIN)
        phase_ret(k, z, w("ret_decay"), w("ret_gn"), ctx_out)
        phase_window(k, z, w("win_qnorm"), w("win_knorm"), w("win_sink"), ctx_out)
        phase_na(k, z, w("na_qnorm"), w("na_knorm"), w("nab"), ctx_out)
        phase_gdn(k, z, w("gdn_conv"), w("gdn_a_log"), w("gdn_dt_bias"), w("gdn_norm"), ctx_out)
        phase_merge(k, hT_d, x, xmid, w("w_merge"), w("w_branch"), w("w_out"), modv[l], ctx_out)
        t_lo = 0 if ctx_out else 2
        with P.scope():
            h2 = P.sbuf([128, 8, T], BF16, "h2sb")
            phase_norm(k, xmid, w("norm2"), mT[l], 1, h2, hT32_d=h32_d, t_lo=t_lo)
            for kk in range(8):
                P.dma("sp", h2T_d.ap()[kk * 128:(kk + 1) * 128, t_lo * 128:], h2[:, kk, t_lo * 128:], reads=[h2], writes=[h2T_d])
        with P.scope():
            wgt = P.sbuf([128, NT, 32], F32, "wgt")
            phase_router(k, h32_d, W["w_router"].ap(), W["router_bias"].ap(), wgt, t_lo)
            phase_moe(k, h2T_d, wgt, xmid, x1, w("w_e_gate"), w("w_e_up"), w("w_e_down"), modv[l], t_lo,
                      out_lat=(out if (fused and l == DEPTH - 1) else None))
    P.wait_all("sp", [out] if fused else [x1])
    P.emit()
    return nc, P


class _Sub:
    def __init__(self, parent, l):
        self.p = parent; self.l = l
        self.w = parent.w; self.r = parent.r

    def ap(self):
        return self.p.ap()[self.l]


def _sub(buf, l):
    return _Sub(buf, l)


_CACHE = {}
FUSED = True


def host_inputs(inputs, layer=None, x_prev=None):
    f = lambda a: np.ascontiguousarray(np.asarray(a, dtype=np.float32))
    shared = {"c_" + n: a for n, a in mk_consts().items()}
    for n in W_NAMES:
        if n == "nab":
            full = np.stack([na_bias_gather(f(inputs["na_rpb"][l])) for l in range(DEPTH)])
        else:
            full = f(inputs[n]).reshape(W_SHAPES[n])
        shared[n] = full if (layer is None or n not in LAYERED) else np.ascontiguousarray(full[layer])
    maps = []
    for b in range(8):
        m = dict(shared)
        if x_prev is None:
            m["xin"] = np.concatenate([f(inputs["ctx"][b]), f(inputs["x"][b])], axis=0)
        else:
            m["xin"] = x_prev[b]
        m["cc"] = np.stack([f(inputs["c_ctx"]), f(inputs["c"][b])])
        maps.append(m)
    return maps


def kernel(**inputs):
    if "nc" not in _CACHE:
        _CACHE["nc"] = build_program(FUSED)[0]
    nc = _CACHE["nc"]
    cores = list(range(8))
    if FUSED:
        res = run_bass_kernel_spmd(nc, host_inputs(inputs), core_ids=cores)
        return np.stack([np.asarray(r["out"], dtype=np.float32) for r in res.results], axis=0)
    xp = None
    for l in range(DEPTH):
        res = run_bass_kernel_spmd(nc, host_inputs(inputs, layer=l, x_prev=xp), core_ids=cores)
        xp = [np.asarray(r["xo"], dtype=np.float32) for r in res.results]
    return np.stack([x[LC:] for x in xp], axis=0)
```
